# Optimizing a Trainium2 kernel written in Bass

```python
import math
import jax, jax.numpy as jnp
from jax import lax
import numpy as np

D_MODEL = 1024
BATCH = 2
SEQ = 16384
DEPTH = 2

N_EVEN = (DEPTH + 1) // 2
N_ODD = DEPTH // 2

ATTN_HEADS = 16
HEAD_DIM = 64
ATTN_DIM = ATTN_HEADS * HEAD_DIM
DILATED_PATTERNS = ((128, 1), (512, 4), (2048, 16))
ROPE_THETA = 500000.0
ROPE_DIM = HEAD_DIM // 4

SSD_HEADS = 16
SSD_HEAD_DIM = 64
SSD_DIM = SSD_HEADS * SSD_HEAD_DIM
SSD_GROUPS = 2
SSD_STATE = 128
SSD_CONV = 5
SSD_CHUNK = 128
SSD_XBC = SSD_DIM + 2 * SSD_GROUPS * SSD_STATE

MIX_DIM_EVEN = ATTN_DIM + SSD_DIM
EVEN_SPLIT = (ATTN_DIM, ATTN_DIM, ATTN_DIM, SSD_DIM, SSD_XBC, SSD_HEADS, SSD_HEADS)
IN_DIM_EVEN = sum(EVEN_SPLIT)

CONV_DIM = D_MODEL
SHORT_CONV = 3

N_EXPERTS = 16
CAPACITY_FACTOR = 2
D_FF_EXPERT = 2 * D_MODEL

NORM_EPS = 1e-6

kernel_name = "hybrid_dilated_attn_ssd_shortconv_ecmoe"


def rms_norm(x, g):
    xf = x.astype(jnp.float32)
    y = xf * lax.rsqrt(jnp.mean(xf * xf, axis=-1, keepdims=True) + NORM_EPS)
    return (y * g.astype(jnp.float32)).astype(x.dtype)


def partial_rope(t, positions):
    half = ROPE_DIM // 2
    inv_freq = ROPE_THETA ** (-jnp.arange(half, dtype=jnp.float32) * 2.0 / ROPE_DIM)
    ang = positions[:, None] * inv_freq[None, :]
    cos = jnp.cos(ang)[:, None, :]
    sin = jnp.sin(ang)[:, None, :]
    tf = t.astype(jnp.float32)
    x1, x2, rest = tf[..., :half], tf[..., half:ROPE_DIM], tf[..., ROPE_DIM:]
    return jnp.concatenate([x1 * cos - x2 * sin, x2 * cos + x1 * sin, rest], axis=-1).astype(t.dtype)


def window_branch(q, k, v, dilation, half):
    b, s, h, e = q.shape
    n = s // dilation
    nb = -(-n // half)
    n_pad = nb * half

    def to_blocks(t):
        t = t.reshape(b, n, dilation, h, e).transpose(0, 2, 3, 1, 4)
        t = jnp.pad(t, ((0, 0), (0, 0), (0, 0), (0, n_pad - n), (0, 0)))
        return t.reshape(b, dilation, h, nb, half, e)

    def neighbours(t):
        tp = jnp.pad(t, ((0, 0), (0, 0), (0, 0), (1, 1), (0, 0), (0, 0)))
        return jnp.concatenate([tp[:, :, :, :-2], tp[:, :, :, 1:-1], tp[:, :, :, 2:]], axis=4)

    qb = to_blocks(q)
    kc = neighbours(to_blocks(k))
    vc = neighbours(to_blocks(v))
    blk = jnp.arange(nb)[:, None] * half
    qpos = blk + jnp.arange(half)[None, :]
    kpos = blk + jnp.arange(-half, 2 * half)[None, :]
    mask = ((jnp.abs(qpos[:, :, None] - kpos[:, None, :]) <= half)
            & (kpos >= 0)[:, None, :] & (kpos < n)[:, None, :])
    scores = jnp.einsum("bdhnqe,bdhnke->bdhnqk", qb, kc).astype(jnp.float32) * (e ** -0.5)
    scores = jnp.where(mask, scores, -jnp.inf)
    m = jnp.max(scores, axis=-1, keepdims=True)
    p = jnp.exp(scores - m)
    l = jnp.sum(p, axis=-1, keepdims=True)
    o = jnp.einsum("bdhnqk,bdhnke->bdhnqe", p, vc.astype(jnp.float32)) / l
    lse = m + jnp.log(l)

    def back(t):
        t = t.reshape(b, dilation, h, n_pad, -1)[:, :, :, :n]
        return t.transpose(0, 3, 1, 2, 4).reshape(b, s, h, -1)

    return back(o), back(lse)[..., 0]


def dilated_attention(q, k, v):
    outs, lses = zip(*[window_branch(q, k, v, d, w // (2 * d)) for (w, d) in DILATED_PATTERNS])
    wts = jax.nn.softmax(jnp.stack(lses, axis=0), axis=0)
    return jnp.sum(jnp.stack(outs, axis=0) * wts[..., None], axis=0)


def dwconv_centred(x, w):
    ksz = w.shape[0]
    pad = ksz // 2
    s = x.shape[1]
    xp = jnp.pad(x, ((0, 0), (pad, pad), (0, 0)))
    y = xp[:, 0:s] * w[0]
    for j in range(1, ksz):
        y = y + xp[:, j:j + s] * w[j]
    return y


def segsum(a):
    cs = jnp.cumsum(a, axis=-1)
    t = a.shape[-1]
    diff = cs[..., :, None] - cs[..., None, :]
    return jnp.where(jnp.tril(jnp.ones((t, t), dtype=bool)), diff, -jnp.inf)


def ssd_scan(x, dt, a_neg, bm, cm):
    b, l, h, p = x.shape
    g, n = bm.shape[2], bm.shape[3]
    r = h // g
    c = l // SSD_CHUNK
    xdt = (x * dt[..., None]).reshape(b, c, SSD_CHUNK, g, r, p)
    a = (dt * a_neg).reshape(b, c, SSD_CHUNK, g, r).transpose(0, 3, 4, 1, 2)
    bm = bm.reshape(b, c, SSD_CHUNK, g, n)
    cm = cm.reshape(b, c, SSD_CHUNK, g, n)
    a_cs = jnp.cumsum(a, axis=-1)
    decay_in = jnp.exp(segsum(a))
    y_diag = jnp.einsum("bclgn,bcsgn,bgrcls,bcsgrp->bclgrp", cm, bm, decay_in, xdt)
    decay_to_end = jnp.exp(a_cs[..., -1:] - a_cs)
    chunk_states = jnp.einsum("bclgn,bgrcl,bclgrp->bcgrpn", bm, decay_to_end, xdt)
    chunk_decay = jnp.exp(a_cs[..., -1])

    def step(state, inp):
        st, dec = inp
        return state * dec[..., None, None] + st, state

    _, prev = lax.scan(step, jnp.zeros((b, g, r, p, n), x.dtype),
                       (chunk_states.transpose(1, 0, 2, 3, 4, 5), chunk_decay.transpose(3, 0, 1, 2)))
    prev = prev.transpose(1, 0, 2, 3, 4, 5)
    y_off = jnp.einsum("bclgn,bcgrpn,bgrcl->bclgrp", cm, prev, jnp.exp(a_cs))
    return (y_diag + y_off).reshape(b, l, h, p)


def bidirectional_ssd(xs, bm, cm, dt_f, dt_b, a_log_f, a_log_b, dt_bias_f, dt_bias_b, d_skip):
    f32 = jnp.float32
    step_f = jax.nn.softplus(dt_f + dt_bias_f.astype(f32))
    step_b = jax.nn.softplus(dt_b + dt_bias_b.astype(f32))
    y_f = ssd_scan(xs, step_f, -jnp.exp(a_log_f.astype(f32)), bm, cm)
    flip = lambda t: jnp.flip(t, axis=1)
    y_b = flip(ssd_scan(flip(xs), flip(step_b), -jnp.exp(a_log_b.astype(f32)), flip(bm), flip(cm)))
    return y_f + y_b + xs * d_skip.astype(f32)[:, None]


def even_mixer(h, w_in, q_norm, k_norm, conv_w, conv_b, a_log_f, a_log_b,
               dt_bias_f, dt_bias_b, d_skip, out_norm, w_out, positions):
    b, s, _ = h.shape
    f32 = jnp.float32
    idx = np.cumsum(EVEN_SPLIT)[:-1].tolist()
    q, k, v, z, xbc, dt_f, dt_b = jnp.split(h @ w_in, idx, axis=-1)
    heads = lambda t: t.reshape(b, s, ATTN_HEADS, HEAD_DIM)
    q = partial_rope(rms_norm(heads(q), q_norm), positions)
    k = partial_rope(rms_norm(heads(k), k_norm), positions)
    attn = dilated_attention(q, k, heads(v)).reshape(b, s, ATTN_DIM).astype(h.dtype)
    xbc = jax.nn.silu(dwconv_centred(xbc, conv_w) + conv_b)
    xs, bm, cm = jnp.split(xbc, [SSD_DIM, SSD_DIM + SSD_GROUPS * SSD_STATE], axis=-1)
    y = bidirectional_ssd(
        xs.astype(f32).reshape(b, s, SSD_HEADS, SSD_HEAD_DIM),
        bm.astype(f32).reshape(b, s, SSD_GROUPS, SSD_STATE),
        cm.astype(f32).reshape(b, s, SSD_GROUPS, SSD_STATE),
        dt_f.astype(f32), dt_b.astype(f32), a_log_f, a_log_b, dt_bias_f, dt_bias_b, d_skip)
    y = rms_norm(y.reshape(b, s, SSD_DIM) * jax.nn.silu(z.astype(f32)), out_norm).astype(h.dtype)
    return jnp.concatenate([attn, y], axis=-1) @ w_out


def odd_mixer(h, w_in, conv_w, w_out):
    gate_b, gate_c, u = jnp.split(h @ w_in, 3, axis=-1)
    return (gate_b * dwconv_centred(gate_c * u, conv_w)) @ w_out


def expert_choice_moe(h, router_w, w_gate, w_up, w_down):
    b, s, _ = h.shape
    cap = CAPACITY_FACTOR * s // N_EXPERTS
    affinity = jax.nn.softmax((h @ router_w).astype(jnp.float32), axis=-1)
    gate, idx = lax.top_k(jnp.swapaxes(affinity, 1, 2), cap)
    bidx = jnp.arange(b)[:, None, None]
    xe = h[bidx, idx]
    hid = jax.nn.silu(jnp.einsum("becd,edf->becf", xe, w_gate)) * jnp.einsum("becd,edf->becf", xe, w_up)
    ye = jnp.einsum("becf,efd->becd", hid, w_down) * gate[..., None].astype(h.dtype)
    return jnp.zeros_like(h).at[bidx, idx].add(ye)


def setup_inputs(seed: int = 0) -> dict:
    key = jax.random.key(seed)
    ks = jax.random.split(key, 24)
    f32 = jnp.float32
    nrm = lambda k, shape, fan_in: jax.random.normal(k, shape, f32) * (fan_in ** -0.5)
    gain = lambda k, shape: 1.0 + 0.05 * jax.random.normal(k, shape, f32)

    def dt_bias(k, shape):
        dt = jnp.exp(jax.random.uniform(k, shape, f32, minval=math.log(1e-3), maxval=math.log(1e-1)))
        return dt + jnp.log(-jnp.expm1(-dt))

    return {
        "x": jax.random.normal(ks[0], (BATCH, SEQ, D_MODEL), f32),
        "attn_norm": gain(ks[1], (N_EVEN, D_MODEL)),
        "w_in_even": nrm(ks[2], (N_EVEN, D_MODEL, IN_DIM_EVEN), D_MODEL),
        "q_norm": gain(ks[3], (N_EVEN, HEAD_DIM)),
        "k_norm": gain(ks[4], (N_EVEN, HEAD_DIM)),
        "ssd_conv_w": nrm(ks[5], (N_EVEN, SSD_CONV, SSD_XBC), SSD_CONV),
        "ssd_conv_b": 0.02 * jax.random.normal(ks[6], (N_EVEN, SSD_XBC), f32),
        "ssd_a_log_fwd": jnp.log(jax.random.uniform(ks[7], (N_EVEN, SSD_HEADS), f32, minval=1.0, maxval=16.0)),
        "ssd_a_log_bwd": jnp.log(jax.random.uniform(ks[8], (N_EVEN, SSD_HEADS), f32, minval=1.0, maxval=16.0)),
        "ssd_dt_bias_fwd": dt_bias(ks[9], (N_EVEN, SSD_HEADS)),
        "ssd_dt_bias_bwd": dt_bias(ks[10], (N_EVEN, SSD_HEADS)),
        "ssd_d": gain(ks[11], (N_EVEN, SSD_HEADS)),
        "ssd_out_norm": gain(ks[12], (N_EVEN, SSD_DIM)),
        "w_out_even": nrm(ks[13], (N_EVEN, MIX_DIM_EVEN, D_MODEL), MIX_DIM_EVEN),
        "conv_norm": gain(ks[14], (N_ODD, D_MODEL)),
        "conv_w_in": nrm(ks[15], (N_ODD, D_MODEL, 3 * CONV_DIM), D_MODEL),
        "conv_w": nrm(ks[16], (N_ODD, SHORT_CONV, CONV_DIM), SHORT_CONV),
        "conv_w_out": nrm(ks[17], (N_ODD, CONV_DIM, D_MODEL), CONV_DIM),
        "ffn_norm": gain(ks[18], (DEPTH, D_MODEL)),
        "router_w": nrm(ks[19], (DEPTH, D_MODEL, N_EXPERTS), D_MODEL),
        "expert_w_gate": nrm(ks[20], (DEPTH, N_EXPERTS, D_MODEL, D_FF_EXPERT), D_MODEL),
        "expert_w_up": nrm(ks[21], (DEPTH, N_EXPERTS, D_MODEL, D_FF_EXPERT), D_MODEL),
        "expert_w_down": nrm(ks[22], (DEPTH, N_EXPERTS, D_FF_EXPERT, D_MODEL), D_FF_EXPERT),
    }


def reference(x, attn_norm, w_in_even, q_norm, k_norm, ssd_conv_w, ssd_conv_b,
              ssd_a_log_fwd, ssd_a_log_bwd, ssd_dt_bias_fwd, ssd_dt_bias_bwd, ssd_d,
              ssd_out_norm, w_out_even, conv_norm, conv_w_in, conv_w, conv_w_out,
              ffn_norm, router_w, expert_w_gate, expert_w_up, expert_w_down):
    positions = jnp.arange(x.shape[1], dtype=jnp.float32)
    for layer in range(DEPTH):
        i = layer // 2
        if layer % 2 == 0:
            x = x + even_mixer(rms_norm(x, attn_norm[i]), w_in_even[i], q_norm[i], k_norm[i],
                               ssd_conv_w[i], ssd_conv_b[i], ssd_a_log_fwd[i], ssd_a_log_bwd[i],
                               ssd_dt_bias_fwd[i], ssd_dt_bias_bwd[i], ssd_d[i], ssd_out_norm[i],
                               w_out_even[i], positions)
        else:
            x = x + odd_mixer(rms_norm(x, conv_norm[i]), conv_w_in[i], conv_w[i], conv_w_out[i])
        x = x + expert_choice_moe(rms_norm(x, ffn_norm[layer]), router_w[layer],
                                  expert_w_gate[layer], expert_w_up[layer], expert_w_down[layer])
    return x
```

```python
import contextlib
import numpy as np
import concourse.bass as bass
import concourse.mybir as mybir

F32 = mybir.dt.float32
BF16 = mybir.dt.bfloat16
I32 = mybir.dt.int32
U32 = mybir.dt.uint32
AF = mybir.ActivationFunctionType
ALU = mybir.AluOpType
AX = mybir.AxisListType

ENGS = ("tensor", "vector", "scalar", "gpsimd", "sync")


class Buf:
    __slots__ = ("name", "w", "r", "dsem", "dcount")

    def __init__(self, name):
        self.name = name
        self.w = {}
        self.r = {}
        self.dsem = None
        self.dcount = 0


class Prog:
    def __init__(self, nc, stack):
        self.nc = nc
        self.stack = stack
        self.esem = {e: stack.enter_context(nc.semaphore("es_" + e)) for e in ENGS}
        self.base = {e: 0 for e in ENGS}
        self.dsems = {}
        self.dvals = {}
        self.dfree = []
        self.phase = 0
        self.reset_phase()
        self.same_engine_sync = True
        self.no_self = set()

    def reset_phase(self):
        self.ops = {e: [] for e in ENGS}
        self.seen = {e: {} for e in ENGS}
        self.needed = {e: set() for e in ENGS}

    def _collect(self, eng, reads, writes, partial):
        waits = {}
        def add(d):
            for k, v in d.items():
                if waits.get(k, -1) < v:
                    waits[k] = v
        for b in reads:
            add(b.w)
        for b in writes:
            add(b.w); add(b.r)
        for b in partial:
            add(b.r)
        out = []
        seen = self.seen[eng]
        for k, v in waits.items():
            if k[2] != self.phase:
                continue
            if k[0] == "E" and k[1] == eng and (not self.same_engine_sync or eng in self.no_self):
                continue
            if seen.get(k, -1) >= v:
                continue
            seen[k] = v
            if k[0] == "E":
                self.needed[k[1]].add(v)
            out.append((k, v))
        return out

    def _commit(self, tok, reads, writes, partial):
        k, v = tok
        for b in reads:
            if b.r.get(k, -1) < v:
                b.r[k] = v
        for b in writes:
            b.w = {k: v}
            b.r = {}
        for b in partial:
            if b.w.get(k, -1) < v:
                b.w[k] = v

    def _serial_waits(self, eng, waits):
        import os
        if not os.environ.get("FW_SERIAL"):
            return waits
        if os.environ["FW_SERIAL"] != "1" and eng not in os.environ["FW_SERIAL"].split(","):
            return waits
        have = {k for k, _ in waits}
        for e2 in ENGS:
            if e2 == eng:
                continue
            ee = [o["idx"] for o in self.ops[e2] if o["kind"] == "E"]
            if not ee:
                continue
            k = ("E", e2, self.phase)
            if self.seen[eng].get(k, -1) < ee[-1]:
                self.seen[eng][k] = ee[-1]
                self.needed[e2].add(ee[-1])
                waits.append((k, ee[-1]))
        return waits

    def op(self, eng, emit, reads=(), writes=(), partial=()):
        waits = self._collect(eng, reads, writes, partial)
        waits = self._serial_waits(eng, waits)
        idx = len(self.ops[eng]) + 1
        self.ops[eng].append(dict(waits=waits, emit=emit, kind="E", idx=idx))
        self._commit((("E", eng, self.phase), idx), reads, writes, partial)

    def dma(self, eng, emit, key, reads=(), writes=(), partial=(), inc=16):
        waits = self._collect(eng, reads, writes, partial)
        if key not in self.dsems:
            if self.dfree:
                self.dsems[key], self.dvals[key] = self.dfree.pop(0)
            else:
                self.dsems[key] = self.stack.enter_context(self.nc.semaphore("ds%d_%s" % (self.phase, key)))
                self.dvals[key] = 0
        self.dvals[key] += inc
        v = self.dvals[key]
        idx = len(self.ops[eng]) + 1
        self.ops[eng].append(dict(waits=waits, emit=emit, kind="D", key=key, inc=inc, idx=idx))
        self._commit((("D", key, self.phase), v), reads, writes, partial)

    def barrier(self):
        last = {}
        for e in ENGS:
            ee = [o["idx"] for o in self.ops[e] if o["kind"] == "E"]
            last[e] = ee[-1] if ee else 0
        for e in ENGS:
            waits = []
            for e2 in ENGS:
                if e2 != e and last[e2] > 0:
                    k = ("E", e2, self.phase)
                    if self.seen[e].get(k, -1) < last[e2]:
                        self.seen[e][k] = last[e2]
                        self.needed[e2].add(last[e2])
                        waits.append((k, last[e2]))
            for key, v in self.dvals.items():
                k = ("D", key, self.phase)
                if self.seen[e].get(k, -1) < v:
                    self.seen[e][k] = v
                    waits.append((k, v))
            self.ops[e].append(dict(waits=waits, emit=None, kind="N", idx=len(self.ops[e]) + 1))

    def emit_phase(self):
        nc = self.nc
        val = {}
        for e in ENGS:
            c = self.base[e]
            m = {}
            for o in self.ops[e]:
                if o["kind"] == "E" and o["idx"] in self.needed[e]:
                    c += 1
                    m[o["idx"]] = c
            val[e] = m
        stats = {}

        def replay(e, eng):
            n = 0
            for o in self.ops[e]:
                for (k, v) in o["waits"]:
                    if k[0] == "E":
                        eng.wait_ge(self.esem[k[1]], val[k[1]][v])
                    else:
                        eng.wait_ge(self.dsems[k[1]], v)
                    n += 1
                if o["emit"] is None:
                    continue
                ins = o["emit"](eng)
                n += 1
                if o["kind"] == "E":
                    if o["idx"] in self.needed[e]:
                        ins.then_inc(self.esem[e], 1)
                else:
                    ins.then_inc(self.dsems[o["key"]], o["inc"])
            stats[e] = n

        with nc.Block() as block:
            @block.tensor
            def _(eng):
                replay("tensor", eng)

            @block.vector
            def _(eng):
                replay("vector", eng)

            @block.scalar
            def _(eng):
                replay("scalar", eng)

            @block.gpsimd
            def _(eng):
                replay("gpsimd", eng)

            @block.sync
            def _(eng):
                replay("sync", eng)
        for e in ENGS:
            if val[e]:
                self.base[e] = max(val[e].values())
        for key in list(self.dsems):
            self.dfree.append((self.dsems[key], self.dvals[key]))
        self.dsems = {}
        self.dvals = {}
        self.phase += 1
        self.reset_phase()
        return stats


S = 16384
DM = 1024
NCORE = 8
PAD = 1024
WIN = 512
NW = S // WIN
NCH = S // 128
NE = 16
CAP = 2048
FF = 2048
EPS = 1e-6
NEG = -30000.0
C_Q, C_K, C_V, C_XS, C_B, C_C, C_Z, C_DT, NC1 = 0, 256, 512, 768, 1024, 1152, 1280, 1536, 1544
GROUPS = [[0, 1, 2, 3], [4, 5, 6, 7]]


def sl(start, n, step):
    return slice(start, start + (n - 1) * step + 1, step)


class T:
    def __init__(self, t, name):
        self.t = t
        self.b = Buf(name)
        self.name = name

    def __getitem__(self, k):
        return self.t[k]


class KB:
    def __init__(self, nc, st):
        self.nc = nc
        self.st = st
        self.ph = None
        self.P = Prog(nc, st)
        self.P.no_self = {"tensor"}
        self.uid = 0

    def sb(self, name, shape, dt):
        self.uid += 1
        nm = "s%d_%s" % (self.uid, name)
        return T(self.ph.enter_context(self.nc.sbuf_tensor(nm, shape, dt)), name)

    def ps(self, name, shape, dt=F32):
        self.uid += 1
        nm = "p%d_%s" % (self.uid, name)
        return T(self.ph.enter_context(self.nc.psum_tensor(nm, shape, dt)), name)

    def pool(self, kind, name, n, shape, dt):
        f = self.sb if kind == "sb" else self.ps
        return Pool([f("%s%d" % (name, i), shape, dt) for i in range(n)])

    def dram(self, name, shape, dt, kind=None):
        if kind is None:
            h = self.nc.dram_tensor(name, shape, dt)
        else:
            h = self.nc.dram_tensor(name, shape, dt, kind=kind)
        return T(h.ap(), name)

    def mm(self, out, oap, lt, ltap, rt, rtap, start=True, stop=True):
        self.P.op("tensor", lambda e: e.matmul(oap, lhsT=ltap, rhs=rtap, start=start, stop=stop),
                  reads=[lt.b, rt.b], writes=[out.b])

    def tr(self, out, oap, inp, iap, ident, idap):
        self.P.op("tensor", lambda e: e.transpose(oap, iap, idap), reads=[inp.b, ident.b], writes=[out.b])

    def act(self, out, oap, inp, iap, func, bias=None, scale=None, accum=None, part=False):
        reads = [inp.b]
        kw = {}
        if bias is not None:
            reads.append(bias[0].b)
            kw["bias"] = bias[1]
        if scale is not None:
            if isinstance(scale, tuple):
                reads.append(scale[0].b)
                kw["scale"] = scale[1]
            else:
                kw["scale"] = scale
        writes = [out.b]
        if accum is not None:
            writes.append(accum[0].b)
            kw["accum_out"] = accum[1]
        self.P.op("scalar", lambda e: e.activation(out=oap, in_=iap, func=func, **kw), reads=reads,
                  writes=[] if part else writes, partial=writes if part else [])

    def tt(self, eng, out, oap, a, aap, b, bap, op, part=False):
        self.P.op(eng, lambda e: e.tensor_tensor(out=oap, in0=aap, in1=bap, op=op), reads=[a.b, b.b],
                  writes=[] if part else [out.b], partial=[out.b] if part else [])

    def ts(self, eng, out, oap, a, aap, s1, s2, op0, op1=None, accum=None, part=False):
        reads = [a.b]
        def cv(s):
            if isinstance(s, tuple):
                reads.append(s[0].b)
                return s[1]
            return s
        v1, v2 = cv(s1), cv(s2)
        kw = {}
        writes = [out.b]
        if accum is not None:
            writes.append(accum[0].b)
            kw["accum_out"] = accum[1]
        if op1 is not None:
            kw["op1"] = op1
        self.P.op(eng, lambda e: e.tensor_scalar(out=oap, in0=aap, scalar1=v1, scalar2=v2, op0=op0, **kw), reads=reads,
                  writes=[] if part else writes, partial=writes if part else [])

    def stt(self, out, oap, a, aap, scalar, b, bap, op0, op1, part=False):
        reads = [a.b, b.b]
        sv = scalar
        if isinstance(scalar, tuple):
            reads.append(scalar[0].b)
            sv = scalar[1]
        self.P.op("vector", lambda e: e.scalar_tensor_tensor(out=oap, in0=aap, scalar=sv, in1=bap, op0=op0, op1=op1),
                  reads=reads, writes=[] if part else [out.b], partial=[out.b] if part else [])

    def copy(self, eng, out, oap, a, aap, part=False):
        if eng == "scalar":
            f = lambda e: e.activation(out=oap, in_=aap, func=AF.Copy)
        else:
            f = lambda e: e.tensor_copy(out=oap, in_=aap)
        self.P.op(eng, f, reads=[a.b], writes=[] if part else [out.b], partial=[out.b] if part else [])

    def memset(self, eng, out, oap, val, part=False):
        self.P.op(eng, lambda e: e.memset(oap, val), writes=[] if part else [out.b], partial=[out.b] if part else [])

    def recip(self, out, oap, a, aap):
        self.P.op("vector", lambda e: e.reciprocal(out=oap, in_=aap), reads=[a.b], writes=[out.b])

    def dma(self, q, out, oap, inp, iap, key, part=False, extra_reads=(), **kw):
        self.P.dma(q, lambda e: e.dma_start(out=oap, in_=iap, **kw), key, reads=[inp.b] + [x.b for x in extra_reads],
                   writes=[] if part else [out.b], partial=[out.b] if part else [])

    def rstd_from(self, out, oap, inp, iap, n, tmp, tap, eps):
        self.act(tmp, tap, inp, iap, AF.Ln, bias=(eps, eps[:, 0:1]), scale=1.0 / n)
        self.act(out, oap, tmp, tap, AF.Exp, scale=-0.5)


class Pool:
    def __init__(self, tiles):
        self.tiles = tiles
        self.i = 0

    def next(self):
        t = self.tiles[self.i % len(self.tiles)]
        self.i += 1
        return t


def host_constants():
    import ml_dtypes
    bf = ml_dtypes.bfloat16
    c = {}
    c["ident_bf"] = np.eye(128, dtype=np.float32).astype(bf)
    c["ident_f"] = np.eye(128, dtype=np.float32)
    p = np.arange(128)[:, None]
    f = np.arange(128)[None, :]
    mA = np.where(f <= p, 0.0, NEG).astype(np.float32)
    mB = np.where(f >= p, 0.0, NEG).astype(np.float32)
    mA_first = mA.copy(); mA_first[:64, :] = NEG
    mB_last = mB.copy(); mB_last[64:, :] = NEG
    nm = np.zeros((128, 3, 4, 128), np.float32)
    for v, (a, b) in enumerate([(mA, mB), (mA_first, mB), (mA, mB_last)]):
        nm[:, v, 0] = a; nm[:, v, 1] = b; nm[:, v, 2] = a; nm[:, v, 3] = b
    c["negmask"] = nm.reshape(128, 3, 512).astype(bf)
    bo = np.zeros((128, 128), np.float32); bo[:64, :64] = 1; bo[64:, 64:] = 1
    c["blockones"] = bo.astype(bf)
    rm = np.zeros((128, 128), np.float32)
    for o in (0, 64):
        for i in range(8):
            rm[o + 8 + i, o + i] = -1.0
            rm[o + i, o + 8 + i] = 1.0
    c["rotm"] = rm.astype(bf)
    so = np.zeros((128, 2, 128), np.float32); so[:, 0, :64] = 1; so[:, 1, 64:] = 1
    c["sel_ones"] = so.astype(bf)
    c["ones_bf"] = np.ones((128, 128), np.float32).astype(bf)
    c["ones_f"] = np.ones((128, 128), np.float32)
    t = np.arange(128)[:, None]; l = np.arange(128)[None, :]
    c["tri"] = np.stack([(t <= l), (t >= l)], 1).astype(np.float32)
    mf = np.where(t <= l, 0.0, NEG).astype(np.float32); mb = np.where(t >= l, 0.0, NEG).astype(np.float32)
    c["smask"] = np.stack([np.tile(mf, (1, 4)), np.tile(mb, (1, 4))], 1).astype(np.float32)
    c["ustrict"] = (t < l).astype(np.float32)
    half = 8
    inv_freq = (np.float32(500000.0) ** (-np.arange(half, dtype=np.float32) * np.float32(2.0) / np.float32(16))).astype(np.float32)
    ang = (np.arange(S, dtype=np.float32)[None, :] * inv_freq[:, None]).astype(np.float32)
    cos = np.cos(ang.astype(np.float64)).astype(np.float32); sin = np.sin(ang.astype(np.float64)).astype(np.float32)
    cf = np.ones((128, S), np.float32); sf = np.zeros((128, S), np.float32)
    for o in (0, 64):
        cf[o:o + 8] = cos; cf[o + 8:o + 16] = cos
        sf[o:o + 8] = sin; sf[o + 8:o + 16] = sin
    c["cosf"] = cf; c["sinf"] = sf
    c["iota_tok"] = (np.arange(128)[:, None] * 128 + np.arange(128)[None, :]).astype(np.float32)
    tt_ = np.arange(128)[:, None] * 128 + np.arange(128)[None, :]
    c["iota_h2row"] = (((tt_ % 4096) // 512) * 2048 + (tt_ // 4096) * 512 + (tt_ % 512)).astype(np.float32)
    return c


CONST_DT = {"ident_bf": BF16, "negmask": BF16, "blockones": BF16, "rotm": BF16, "sel_ones": BF16, "ones_bf": BF16}
SMALL_CONSTS_UNUSED = ["ident_bf", "ident_f", "negmask", "blockones", "rotm", "sel_ones", "ones_bf", "ones_f", "tri", "smask",
                "ustrict", "iota_tok"]


def load_consts(kb, io, names):
    cs = {}
    for n in names:
        src = io[n]
        shape = list(src.t.shape)
        t = kb.sb("c_" + n, shape, CONST_DT.get(n, F32))
        kb.dma("sync", t, t[:], src, src.t, "c_" + n)
        cs[n] = t
    eps = kb.sb("c_eps", [128, 1], F32)
    kb.memset("vector", eps, eps[:], EPS)
    cs["eps"] = eps
    return cs


def load_w1(kb, io, w1b, c0, ncols, stage_pool, g_attn):
    w1 = io["w1"]
    for kc in range(8):
        stg = stage_pool.next()
        kb.dma("sync", stg, stg[:, 0:ncols], w1, w1.t[kc * 128:(kc + 1) * 128, c0:c0 + ncols], stg.name)
        kb.ts("vector", w1b, w1b[:, kc, 0:ncols], stg, stg[:, 0:ncols], (g_attn, g_attn[:, kc:kc + 1]), None, ALU.mult, part=True)


def window_front(kb, io, cs, w, xb_pool, sq_pool, rstd_pool, psA):
    xT = io["xT"]
    xb = xb_pool.next()
    xbd = io["xb_dram"]
    xbv = xbd.t.rearrange("(k p) t -> p k t", p=128)[:, :, w * WIN:(w + 1) * WIN]
    if io.get("_xb_ready"):
        kb.dma("sync", xb, xb[:], xbd, xbv, xb.name)
    else:
        kb.dma("gpsimd", xb, xb[:], xT, xT.t.rearrange("(k p) t -> p k t", p=128)[:, :, w * WIN:(w + 1) * WIN], xb.name)
        kb.dma("sync", xbd, xbv, xb, xb[:], xb.name, part=True)
    sq = sq_pool.next()
    kb.act(sq, sq[:], xb, xb[:], AF.Square)
    pss = psA.next()
    for kc in range(8):
        kb.mm(pss, pss[:], cs["ones_bf"], cs["ones_bf"][:], sq, sq[:, kc, :], start=(kc == 0), stop=(kc == 7))
    rstd = rstd_pool.next()
    kb.rstd_from(rstd, rstd[:, 0, :], pss, pss[:], DM, rstd, rstd[:, 1, :], cs["eps"])
    return xb, sq, rstd


def attn_inproj(kb, io, hp, qz, kT, vT):
    if True:
      with contextlib.ExitStack() as ph:
        kb.ph = ph
        cs = load_consts(kb, io, ["blockones", "rotm", "ones_bf"])
        g_attn = kb.sb("g_attn", [128, 8], F32)
        kb.dma("sync", g_attn, g_attn[:], io["g_attn"], io["g_attn"].t, "g_attn")
        gqk = kb.sb("gqk", [128, 2], F32)
        kb.dma("sync", gqk, gqk[:], io["gqk"], io["gqk"].t, "gqk")
        w1b = kb.sb("w1b", [128, 8, 384], BF16)
        stage = kb.pool("sb", "w1stg", 2, [128, 128], F32)
        for j, c0 in enumerate((C_Q + hp * 128, C_K + hp * 128, C_V + hp * 128)):
            w1 = io["w1"]
            for kc in range(8):
                stg = stage.next()
                kb.dma("sync", stg, stg[:], w1, w1.t[kc * 128:(kc + 1) * 128, c0:c0 + 128], stg.name)
                kb.ts("vector", w1b, w1b[:, kc, j * 128:(j + 1) * 128], stg, stg[:], (g_attn, g_attn[:, kc:kc + 1]), None,
                      ALU.mult, part=True)
        for t in (kT, vT):
            kb.memset("gpsimd", t, t[:, 0:PAD], 0.0, part=True)
            kb.memset("gpsimd", t, t[:, PAD + S:], 0.0, part=True)
        kb.memset("gpsimd", qz, qz[64:128, 0, :], 0.0, part=True)
        kb.memset("gpsimd", qz, qz[0:64, 1, :], 0.0, part=True)
        xb_pool = kb.pool("sb", "xb", 2, [128, 8, WIN], BF16)
        sq_pool = kb.pool("sb", "sq", 1, [128, 8, WIN], BF16)
        rstd_pool = kb.pool("sb", "rstd", 2, [128, 2, WIN], F32)
        cos_pool = kb.pool("sb", "cosw", 2, [128, 2, WIN], F32)
        raw_pool = kb.pool("sb", "raw", 2, [128, WIN], F32)
        sqh_pool = kb.pool("sb", "sqh", 2, [128, WIN], BF16)
        rsh_pool = kb.pool("sb", "rsh", 2, [128, 2, WIN], F32)
        qn_pool = kb.pool("sb", "qn", 2, [128, WIN], BF16)
        t12_pool = kb.pool("sb", "t12", 2, [128, 2, WIN], F32)
        psA = kb.pool("ps", "psA", 2, [128, WIN], F32)
        psB = kb.pool("ps", "psB", 2, [128, WIN], F32)
        psC = kb.pool("ps", "psC", 2, [128, WIN], F32)
        for w in range(NW):
            xb, sq, rstd = window_front(kb, io, cs, w, xb_pool, sq_pool, rstd_pool, psA)
            cw = cos_pool.next()
            kb.dma("sync", cw, cw[:, 0, :], io["cosf"], io["cosf"].t[:, w * WIN:(w + 1) * WIN], cw.name, part=True)
            kb.dma("sync", cw, cw[:, 1, :], io["sinf"], io["sinf"].t[:, w * WIN:(w + 1) * WIN], cw.name, part=True)
            for j, dst in enumerate((qz, kT, vT)):
                ps = psB.next()
                for kc in range(8):
                    kb.mm(ps, ps[:], w1b, w1b[:, kc, j * 128:(j + 1) * 128], xb, xb[:, kc, :], start=(kc == 0), stop=(kc == 7))
                off = 0 if j == 0 else PAD
                dap = None if j == 0 else dst[:, off + w * WIN: off + (w + 1) * WIN]
                if j == 2:
                    kb.tt("vector", dst, dap, ps, ps[:], rstd, rstd[:, 0, :], ALU.mult, part=True)
                    continue
                raw = raw_pool.next()
                kb.tt("vector", raw, raw[:], ps, ps[:], rstd, rstd[:, 0, :], ALU.mult)
                sqh = sqh_pool.next()
                kb.act(sqh, sqh[:], raw, raw[:], AF.Square)
                ps2 = psC.next()
                kb.mm(ps2, ps2[:], cs["blockones"], cs["blockones"][:], sqh, sqh[:])
                rsh = rsh_pool.next()
                kb.rstd_from(rsh, rsh[:, 0, :], ps2, ps2[:], 64, rsh, rsh[:, 1, :], cs["eps"])
                qn = qn_pool.next()
                kb.stt(qn, qn[:], raw, raw[:], (gqk, gqk[:, j:j + 1]), rsh, rsh[:, 0, :], ALU.mult, ALU.mult)
                ps3 = psC.next()
                kb.mm(ps3, ps3[:], cs["rotm"], cs["rotm"][:], qn, qn[:])
                t12 = t12_pool.next()
                kb.tt("vector", t12, t12[:, 0, :], qn, qn[:], cw, cw[:, 0, :], ALU.mult, part=True)
                kb.tt("vector", t12, t12[:, 1, :], ps3, ps3[:], cw, cw[:, 1, :], ALU.mult, part=True)
                if j == 0:
                    for h in range(2):
                        hs = slice(h * 64, (h + 1) * 64)
                        kb.tt("vector", qz, qz[hs, h, w * WIN:(w + 1) * WIN], t12, t12[hs, 0, :], t12, t12[hs, 1, :], ALU.add, part=True)
                else:
                    kb.tt("vector", dst, dap, t12, t12[:, 0, :], t12, t12[:, 1, :], ALU.add, part=True)
        if io.get("dbg_qk") is not None:
            kb.dma("sync", io["dbg_qk"], io["dbg_qk"].t[0, 0:64], qz, qz[0:64, 0, :], "dbgq", part=True)
            kb.dma("sync", io["dbg_qk"], io["dbg_qk"].t[0, 64:128], qz, qz[64:128, 1, :], "dbgq", part=True)
            kb.dma("sync", io["dbg_qk"], io["dbg_qk"].t[1], kT, kT[:, PAD:PAD + S], "dbgk", part=True)
            kb.dma("sync", io["dbg_qk"], io["dbg_qk"].t[2], vT, vT[:, PAD:PAD + S], "dbgv", part=True)
        kb.P.barrier()
        stats0 = kb.P.emit_phase()
        kb.ph = None
    return stats0


def attn_core(kb, io, hp, qz, kT, vT):
    import os
    if True:
      with contextlib.ExitStack() as ph:
        kb.ph = ph
        cs = load_consts(kb, io, ["ident_bf", "negmask", "sel_ones"])
        vpad_pool = kb.pool("sb", "vpad", int(os.environ.get("P1_NB", 3)), [128, 2, 2, 128], BF16)
        for t in vpad_pool.tiles:
            kb.memset("vector", t, t[:], 0.0)
        pt_pool = kb.pool("sb", "pt", int(os.environ.get("P1_NB", 3)), [128, 512], BF16)
        accden = kb.sb("accden", [128, 2, 2048], F32)
        rden = kb.sb("rden", [128, 2048], F32)
        attn_o = kb.pool("sb", "attn_o", 2, [128, 2048], BF16)
        psT = kb.pool("ps", "psT", int(os.environ.get("P1_NT", 2)), [128, 4, 128], F32)
        psS = kb.pool("ps", "psS", int(os.environ.get("P1_NS", 2)), [128, 512], F32)
        psO = kb.pool("ps", "psO", int(os.environ.get("P1_NO", 2)), [128, 4, 128], F32)
        mix = io["mixA_loc"]

        def stage1(u):
            d, rho, j, jj = u
            n = S // d
            nb = n // 128
            var = 1 if j == 0 else (2 if j == nb - 1 else 0)
            pT = psT.next()
            kcols = []
            for c in range(2):
                k0 = PAD + rho + d * (128 * j - 64 + 128 * c)
                kcols.append(k0)
                kb.mm(pT, pT[:, c, :], vT, vT[:, sl(k0, 128, d)], cs["ident_bf"], cs["ident_bf"][:])
            vp = vpad_pool.next()
            if MODE >= 1:
                kb.copy("vector", vp, vp[:, :, 0, 0:64], pT, pT[:, 0:2, 0:64], part=True)
            if MODE >= 2:
                kb.copy(os.environ.get("P1_CE", "vector"), vp, vp[:, :, 1, 64:128], pT, pT[:, 0:2, 64:128], part=True)
            sS = psS.next()
            kb.mm(sS, sS[:], cs["ident_bf"], cs["ident_bf"][:], cs["negmask"], cs["negmask"][:, var, :], start=True, stop=False)
            q0 = rho + d * 128 * j
            for h in range(2):
                for c in range(2):
                    sub = 2 * h + c
                    kb.mm(sS, sS[:, sub * 128:(sub + 1) * 128], kT, kT[:, sl(kcols[c], 128, d)],
                          qz, qz[:, h, sl(q0, 128, d)], start=False, stop=True)
            pt = pt_pool.next()
            if MODE >= 3:
                kb.act(pt, pt[:], sS, sS[:], AF.Exp, scale=0.125)
            return (u, vp, pt)

        def stage2(st, first_in_sb):
            u, vp, pt = st
            d, rho, j, jj = u
            if MODE < 4:
                return
            pO = psO.next()
            SUB = os.environ.get("P1_SUB", "")
            k = 0
            for h in range(2 if SUB != "den" else 0):
                for c in range(2):
                    sub = 2 * h + c
                    kb.mm(pO, pO[:, 0, :], vp, vp[:, c, h, :], pt, pt[:, sub * 128:(sub + 1) * 128], start=(k == 0), stop=(k == 3))
                    k += 1
            k = 0
            for h in range(2 if SUB != "pv" else 0):
                for c in range(2):
                    sub = 2 * h + c
                    kb.mm(pO, pO[:, 1, :], cs["sel_ones"], cs["sel_ones"][:, h, :], pt, pt[:, sub * 128:(sub + 1) * 128],
                          start=(k == 0), stop=(k == 3))
                    k += 1
            c0 = rho + d * 128 * jj
            dap = accden[:, :, sl(c0, 128, d)]
            if MODE < 5:
                return
            if first_in_sb:
                kb.copy("vector", accden, dap, pO, pO[:, 0:2, :])
            else:
                kb.tt("vector", accden, dap, accden, dap, pO, pO[:, 0:2, :], ALU.add)

        import os
        MODE = int(os.environ.get("P1_MODE", "9"))
        for sbk in range(int(os.environ.get("P1_NSB", S // 2048))):
            units = []
            for d in (1, 4, 16):
                per = 16 // d
                for rho in range(d):
                    for jj in range(per):
                        units.append((d, rho, sbk * per + jj, jj))
            pend = None
            units = units[:int(os.environ.get("P1_NU", len(units)))]
            for ui, u in enumerate(units):
                st1 = stage1(u)
                if os.environ.get("P1_NOPIPE"):
                    stage2(st1, u[0] == 1)
                    continue
                if pend is not None:
                    stage2(pend[0], pend[1])
                pend = (st1, u[0] == 1)
            if pend is not None:
                stage2(pend[0], pend[1])
            kb.recip(rden, rden[:], accden, accden[:, 1, :])
            ao = attn_o.next()
            kb.tt("gpsimd", ao, ao[:], accden, accden[:, 0, :], rden, rden[:], ALU.mult)
            kb.dma("sync", mix, mix.t.rearrange("(g c) t -> c g t", c=256)[hp * 128:(hp + 1) * 128, sbk * 4:(sbk + 1) * 4, :],
                   ao, ao[:].rearrange("p (g t) -> p g t", t=512), ao.name, part=True)
        kb.P.barrier()
        stats = kb.P.emit_phase()
        kb.ph = None
    return stats


def phase1_attn(kb, io, hp):
    import os
    with contextlib.ExitStack() as outer:
        kb.ph = outer
        qz = kb.sb("qz", [128, 2, S], BF16)
        kT = kb.sb("kT", [128, S + 2 * PAD], BF16)
        vT = kb.sb("vT", [128, S + 2 * PAD], BF16)
        st = attn_inproj(kb, io, hp, qz, kT, vT)
        if os.environ.get("P1_STOP"):
            return st
        st = attn_core(kb, io, hp, qz, kT, vT)
    return st


def phase1_ssd_in(kb, io):
    with contextlib.ExitStack() as ph:
        kb.ph = ph
        if "mixA_all" in io:
            collective(kb, "AllGather", ALU.bypass, io["mixA_loc"], io["mixA_all"], "ccA", 8)
        cs = load_consts(kb, io, ["ones_bf"])
        g_attn = kb.sb("g_attn", [128, 8], F32)
        kb.dma("sync", g_attn, g_attn[:], io["g_attn"], io["g_attn"].t, "g_attn")
        ncol = NC1 - C_XS
        w1b = kb.sb("w1b", [128, 8, ncol], BF16)
        stage = kb.pool("sb", "w1stg", 2, [128, ncol], F32)
        w1 = io["w1"]
        for kc in range(8):
            stg = stage.next()
            kb.dma("sync", stg, stg[:], w1, w1.t[kc * 128:(kc + 1) * 128, C_XS:NC1], stg.name)
            kb.ts("vector", w1b, w1b[:, kc, :], stg, stg[:], (g_attn, g_attn[:, kc:kc + 1]), None, ALU.mult, part=True)
        cw = kb.sb("convw", [128, 4, 6], F32)
        kb.dma("sync", cw, cw[:], io["convw"], io["convw"].t, "convw")
        dtc = kb.sb("dtc", [8, 2], F32)
        kb.dma("sync", dtc, dtc[:], io["dtc"], io["dtc"].t, "dtc")
        negA = kb.sb("negA", [8, 1], F32)
        kb.act(negA, negA[:], dtc, dtc[:, 1:2], AF.Exp)
        kb.ts("vector", negA, negA[:], negA, negA[:], -1.0, None, ALU.mult)
        one8 = kb.sb("one8", [8, 1], F32)
        kb.memset("vector", one8, one8[:], 1.0)
        xb_pool = kb.pool("sb", "xb", 2, [128, 8, WIN], BF16)
        sq_pool = kb.pool("sb", "sq", 1, [128, 8, WIN], BF16)
        rstd_pool = kb.pool("sb", "rstd", 2, [128, 2, WIN], F32)
        pre_pool = kb.pool("sb", "pre", 2, [128, 4, WIN], F32)
        cacc_pool = kb.pool("sb", "cacc", 2, [128, 4, WIN], F32)
        post_pool = kb.pool("sb", "post", 2, [128, 4, WIN], BF16)
        zs_pool = kb.pool("sb", "zsw", 2, [128, 2, WIN], BF16)
        dt_pool = kb.pool("sb", "dtw", 2, [8, 4, WIN], F32)
        psA = kb.pool("ps", "psA", 2, [128, WIN], F32)
        psB = kb.pool("ps", "psB", 3, [128, WIN], F32)
        xT = io["xT"]
        nwin = 33
        for w in range(nwin):
            t0 = 508 * w - 2 if w < nwin - 1 else S - 510
            lo, hi = max(t0, 0), min(t0 + WIN, S)
            xb = xb_pool.next()
            xbd = io["xb_dram"]
            if lo > t0 or hi < t0 + WIN:
                kb.memset("vector", xb, xb[:], 0.0)
                kb.dma("sync", xb, xb[:, :, lo - t0:hi - t0], xbd, xbd.t.rearrange("(k p) t -> p k t", p=128)[:, :, lo:hi], xb.name)
            else:
                kb.dma("sync", xb, xb[:], xbd, xbd.t.rearrange("(k p) t -> p k t", p=128)[:, :, lo:hi], xb.name)
            sq = sq_pool.next()
            kb.act(sq, sq[:], xb, xb[:], AF.Square)
            pss = psA.next()
            for kc in range(8):
                kb.mm(pss, pss[:], cs["ones_bf"], cs["ones_bf"][:], sq, sq[:, kc, :], start=(kc == 0), stop=(kc == 7))
            rstd = rstd_pool.next()
            kb.rstd_from(rstd, rstd[:, 0, :], pss, pss[:], DM, rstd, rstd[:, 1, :], cs["eps"])
            pre = pre_pool.next()
            for ci in range(4):
                ps = psB.next()
                for kc in range(8):
                    kb.mm(ps, ps[:], w1b, w1b[:, kc, ci * 128:(ci + 1) * 128], xb, xb[:, kc, :], start=(kc == 0), stop=(kc == 7))
                kb.tt("vector", pre, pre[:, ci, :], ps, ps[:], rstd, rstd[:, 0, :], ALU.mult, part=True)
            cacc = cacc_pool.next()
            post = post_pool.next()
            NV = WIN - 4
            for ci in range(4):
                kb.act(cacc, cacc[:, ci, 0:NV], pre, pre[:, ci, 0:NV], AF.Copy, scale=(cw, cw[:, ci, 0:1]), part=True)
                for j in range(1, 5):
                    kb.stt(cacc, cacc[:, ci, 0:NV], pre, pre[:, ci, j:j + NV], (cw, cw[:, ci, j:j + 1]), cacc, cacc[:, ci, 0:NV],
                           ALU.mult, ALU.add, part=True)
            for ci in range(4):
                kb.act(post, post[:, ci, 0:NV], cacc, cacc[:, ci, 0:NV], AF.Silu, bias=(cw, cw[:, ci, 5:6]), part=True)
            vlo, vhi = max(t0 + 2, 0), min(t0 + 2 + NV, S)
            o0 = vlo - (t0 + 2)
            kb.dma("sync", io["post"], io["post"].t.rearrange("(c p) t -> p c t", p=128)[:, :, vlo:vhi], post, post[:, :, o0:o0 + (vhi - vlo)],
                   post.name, part=True)
            zs = zs_pool.next()
            for zi in range(2):
                ps = psB.next()
                c0 = (C_Z - C_XS) + zi * 128
                for kc in range(8):
                    kb.mm(ps, ps[:], w1b, w1b[:, kc, c0:c0 + 128], xb, xb[:, kc, :], start=(kc == 0), stop=(kc == 7))
                kb.tt("vector", pre, pre[:, zi, :], ps, ps[:], rstd, rstd[:, 0, :], ALU.mult, part=True)
                kb.act(zs, zs[:, zi, :], pre, pre[:, zi, :], AF.Silu, part=True)
            kb.dma("sync", io["zs"], io["zs"].t.rearrange("(c p) t -> p c t", p=128)[:, :, vlo:vhi], zs, zs[:, :, o0 + 2:o0 + 2 + (vhi - vlo)],
                   zs.name, part=True)
            ps = psB.next()
            c0 = C_DT - C_XS
            for kc in range(8):
                kb.mm(ps, ps[0:8, :], w1b, w1b[:, kc, c0:c0 + 8], xb, xb[:, kc, :], start=(kc == 0), stop=(kc == 7))
            dtw = dt_pool.next()
            kb.tt("vector", dtw, dtw[:, 0, :], ps, ps[0:8, :], rstd, rstd[0:8, 0, :], ALU.mult, part=True)
            kb.act(dtw, dtw[:, 1, :], dtw, dtw[:, 0, :], AF.Exp, bias=(dtc, dtc[:, 0:1]), part=True)
            kb.act(dtw, dtw[:, 2, :], dtw, dtw[:, 1, :], AF.Ln, bias=(one8, one8[:, 0:1]), part=True)
            kb.ts("vector", dtw, dtw[:, 3, :], dtw, dtw[:, 2, :], (negA, negA[:, 0:1]), None, ALU.mult, part=True)
            kb.dma("sync", io["dta"], io["dta"].t[0:8, vlo:vhi], dtw, dtw[:, 2, o0 + 2:o0 + 2 + (vhi - vlo)], dtw.name, part=True)
            kb.dma("sync", io["dta"], io["dta"].t[8:16, vlo:vhi], dtw, dtw[:, 3, o0 + 2:o0 + 2 + (vhi - vlo)], dtw.name, part=True)
        kb.P.barrier()
        st = kb.P.emit_phase()
        kb.ph = None
    return st


def phase1_ssd_scan(kb, io, dirn, nchunks=NCH):
    with contextlib.ExitStack() as ph:
        kb.ph = ph
        cs = load_consts(kb, io, ["ident_bf", "ident_f", "ones_f", "tri", "smask"])
        Dbc = kb.sb("Dbc", [128, 256], F32)
        kb.dma("sync", Dbc, Dbc[:], io["Dbc"], io["Dbc"].t, "Dbc")
        H = kb.sb("H", [128, 256], F32)
        Hbf = kb.sb("Hbf", [128, 256], BF16)
        kb.memset("vector", H, H[:], 0.0)
        kb.memset("vector", Hbf, Hbf[:], 0.0)
        inT = kb.pool("sb", "inT", 2, [128, 4, 128], BF16)
        dtaT = kb.pool("sb", "dtaT", 2, [16, 128], F32)
        xsb = kb.pool("sb", "xsb", 2, [128, 384], BF16)
        dta = kb.pool("sb", "dta", 2, [128, 16], F32)
        abc = kb.pool("sb", "abc", 2, [128, 4, 128], F32)
        col = kb.pool("sb", "col", 2, [128, 40], F32)
        LT = kb.pool("sb", "LT", 2, [128, 4, 128], F32)
        MT = kb.pool("sb", "MT", 2, [128, 4, 128], BF16)
        Ysb = kb.pool("sb", "Ysb", 2, [128, 256], F32)
        yout = kb.pool("sb", "yout", 2, [128, 256], F32)
        xsw = kb.pool("sb", "xsw", 2, [128, 256], BF16)
        yprev = kb.pool("sb", "yprev", 2, [128, 256], F32)
        ysum = kb.pool("sb", "ysum", 2, [128, 2, 256], F32)
        ybf = kb.pool("sb", "ybf", 2, [128, 256], BF16)
        zsT = kb.pool("sb", "zsT", 2, [128, 2, 128], BF16)
        ygT = kb.pool("sb", "ygT", 2, [128, 2, 128], BF16)
        pX = kb.ps("pX", [128, 512], F32)
        pD = kb.ps("pD", [128, 512], F32)
        pCS = kb.ps("pCS", [128, 512], F32)
        pG = kb.ps("pG", [128, 512], F32)
        pY = kb.ps("pY", [128, 512], F32)
        pYo = kb.ps("pYo", [128, 512], F32)
        pST = kb.ps("pST", [128, 512], F32)
        pYT = kb.ps("pYT", [128, 512], F32)
        post_v = io["post"].t.rearrange("(c p) t -> p c t", p=128)
        zs_v = io["zs"].t.rearrange("(c p) t -> p c t", p=128)
        order = range(nchunks) if dirn == 0 else range(NCH - 1, NCH - 1 - nchunks, -1)
        def stageA(c):
            tk = slice(c * 128, (c + 1) * 128)
            it = inT.next()
            kb.dma("sync", it, it[:], io["post"], post_v[:, :, tk], it.name)
            dT = dtaT.next()
            kb.dma("sync", dT, dT[:], io["dta"], io["dta"].t[:, tk], dT.name)
            for i in range(3):
                kb.mm(pX, pX[:, i * 128:(i + 1) * 128], it, it[:, i, :], cs["ident_bf"], cs["ident_bf"][:])
            xs = xsb.next()
            kb.copy("vector", xs, xs[:], pX, pX[:, 0:384])
            kb.mm(pD, pD[:, 0:16], dT, dT[:, :], cs["ident_f"], cs["ident_f"][0:16, 0:16])
            dt = dta.next()
            kb.copy("vector", dt, dt[:], pD, pD[:, 0:16])
            dtc = dt[:, 4 * dirn:4 * dirn + 4]
            ac = dt[:, 8 + 4 * dirn:8 + 4 * dirn + 4]
            ab = abc.next()
            for h in range(4):
                kb.act(ab, ab[:, h, :], cs["ones_f"], cs["ones_f"][:], AF.Copy, scale=(dt, dt[:, 8 + 4 * dirn + h:8 + 4 * dirn + h + 1]), part=True)
            kb.mm(pCS, pCS[:], cs["ident_f"], cs["ident_f"][:], cs["smask"], cs["smask"][:, dirn, :], start=True, stop=False)
            for h in range(4):
                kb.mm(pCS, pCS[:, h * 128:(h + 1) * 128], ab, ab[:, h, :], cs["tri"], cs["tri"][:, dirn, :], start=False, stop=True)
            kb.mm(pD, pD[:, 16:20], cs["tri"], cs["tri"][:, dirn, :], dt, ac)
            kb.mm(pD, pD[:, 20:24], cs["ones_f"], cs["ones_f"][:], dt, ac)
            cl = col.next()
            kb.copy("vector", cl, cl[:, 0:8], pD, pD[:, 16:24], part=True)
            kb.ts("vector", cl, cl[:, 8:12], cl, cl[:, 0:4], -1.0, None, ALU.mult, part=True)
            kb.act(cl, cl[:, 12:16], cl, cl[:, 0:4], AF.Exp, part=True)
            kb.tt("vector", cl, cl[:, 16:20], cl, cl[:, 4:8], cl, cl[:, 0:4], ALU.subtract, part=True)
            kb.act(cl, cl[:, 20:24], cl, cl[:, 16:20], AF.Exp, part=True)
            kb.tt("vector", cl, cl[:, 24:28], cl, cl[:, 20:24], dt, dtc, ALU.mult, part=True)
            kb.act(cl, cl[:, 28:32], cl, cl[:, 4:8], AF.Exp, part=True)
            lt = LT.next()
            for h in range(4):
                kb.act(lt, lt[:, h, :], pCS, pCS[:, h * 128:(h + 1) * 128], AF.Exp, bias=(cl, cl[:, 8 + h:9 + h]), part=True)
            kb.mm(pG, pG[:, 0:128], it, it[:, 2, :], it, it[:, 3, :])
            mt = MT.next()
            for h in range(4):
                kb.stt(mt, mt[:, h, :], pG, pG[:, 0:128], (dt, dt[:, 4 * dirn + h:4 * dirn + h + 1]), lt, lt[:, h, :], ALU.mult, ALU.mult, part=True)
            return dict(c=c, tk=tk, it=it, xs=xs, dt=dt, cl=cl, mt=mt)

        def stageB(ctx):
            c, tk, it, xs, dt, cl, mt = ctx['c'], ctx['tk'], ctx['it'], ctx['xs'], ctx['dt'], ctx['cl'], ctx['mt']
            for h in range(4):
                kb.mm(pY, pY[:, h * 64:(h + 1) * 64], mt, mt[:, h, :], xs, xs[:, h * 64:(h + 1) * 64])
            kb.mm(pYo, pYo[:, 0:256], it, it[:, 3, :], Hbf, Hbf[:])
            ysb = Ysb.next()
            kb.copy("vector", ysb, ysb[:], pY, pY[:, 0:256])
            yo = yout.next()
            for h in range(4):
                hs = slice(h * 64, (h + 1) * 64)
                kb.stt(yo, yo[:, hs], pYo, pYo[:, hs], (cl, cl[:, 12 + h:13 + h]), ysb, ysb[:, hs], ALU.mult, ALU.add, part=True)
            xw = xsw.next()
            for h in range(4):
                hs = slice(h * 64, (h + 1) * 64)
                kb.act(xw, xw[:, hs], xs, xs[:, hs], AF.Copy, scale=(cl, cl[:, 24 + h:25 + h]), part=True)
            kb.mm(pST, pST[:, 0:256], xs, xs[:, 256:384], xw, xw[:])
            for h in range(4):
                hs = slice(h * 64, (h + 1) * 64)
                kb.stt(H, H[:, hs], H, H[:, hs], (cl, cl[:, 28 + h:29 + h]), pST, pST[:, hs], ALU.mult, ALU.add)
            kb.copy("scalar", Hbf, Hbf[:], H, H[:])
            if dirn == 0:
                kb.dma("sync", io["yf"], io["yf"].t[tk, :], yo, yo[:], yo.name, part=True)
            else:
                yp = yprev.next()
                kb.dma("sync", yp, yp[:], io["yf"], io["yf"].t[tk, :], yp.name)
                zt = zsT.next()
                kb.dma("sync", zt, zt[:], io["zs"], zs_v[:, :, tk], zt.name)
                ys = ysum.next()
                kb.tt("vector", ys, ys[:, 0, :], xs, xs[:, 0:256], Dbc, Dbc[:], ALU.mult, part=True)
                kb.tt("vector", ys, ys[:, 1, :], yo, yo[:], yp, yp[:], ALU.add, part=True)
                yb = ybf.next()
                kb.tt("vector", yb, yb[:], ys, ys[:, 0, :], ys, ys[:, 1, :], ALU.add)
                for i in range(2):
                    kb.mm(pYT, pYT[:, i * 128:(i + 1) * 128], yb, yb[:, i * 128:(i + 1) * 128], cs["ident_bf"], cs["ident_bf"][:])
                yg = ygT.next()
                kb.tt("vector", yg, yg[:], pYT, pYT[:, 0:256].rearrange("p (c t) -> p c t", c=2), zt, zt[:], ALU.mult)
                mv = io["mixS_loc"].t.rearrange("(g c) t -> c g t", c=256)
                for i in range(2):
                    kb.dma("sync", io["mixS_loc"], mv[i * 128:(i + 1) * 128, c // 4, (c % 4) * 128:(c % 4 + 1) * 128], yg, yg[:, i, :],
                           yg.name, part=True)
        pend = None
        for c in order:
            ctx = stageA(c)
            if pend is not None:
                stageB(pend)
            pend = ctx
        stageB(pend)
        kb.P.barrier()
        st = kb.P.emit_phase()
        kb.ph = None
    return st


def router_setup(kb, io, L):
    r = {}
    r["gbc"] = kb.sb("gffn_bc", [128, DM], F32)
    kb.dma("sync", r["gbc"], r["gbc"][:], io["g_ffn_bc%d" % L], io["g_ffn_bc%d" % L].t, "gffn_bc")
    gcol = kb.sb("gffn_col", [128, 8], F32)
    kb.dma("sync", gcol, gcol[:], io["g_ffn_col%d" % L], io["g_ffn_col%d" % L].t, "gffn_col")
    wr = kb.sb("wr", [128, 8, NE], F32)
    kb.dma("sync", wr, wr[:], io["wr%d" % L], io["wr%d" % L].t.rearrange("(k p) e -> p k e", p=128), "wr")
    r["wrg"] = kb.sb("wrg", [128, 8, NE], F32)
    for kc in range(8):
        kb.ts("vector", r["wrg"], r["wrg"][:, kc, :], wr, wr[:, kc, :], (gcol, gcol[:, kc:kc + 1]), None, ALU.mult, part=True)
    r["affT"] = kb.sb("affT", [NE, 4096], F32)
    r["sqj"] = kb.pool("sb", "sqj", 2, [128, DM], BF16)
    r["st"] = kb.pool("sb", "rst", 2, [128, 8], F32)
    r["h2"] = kb.pool("sb", "h2t", 2, [128, DM], BF16)
    r["xT"] = kb.pool("sb", "x1T", 2, [128, 8, 128], F32)
    r["lg"] = kb.pool("sb", "lg", 2, [128, 3, NE], F32)
    r["pXT"] = kb.pool("ps", "pXT", 2, [128, 4, 128], F32)
    r["pL"] = kb.ps("pL", [128, 512], F32)
    return r


def router_tile(kb, io, cs, r, x1, i):
    st = r["st"].next()
    sqj = r["sqj"].next()
    kb.act(sqj, sqj[:], x1, x1[:], AF.Square, accum=(st, st[:, 0:1]))
    kb.rstd_from(st, st[:, 2:3], st, st[:, 0:1], DM, st, st[:, 1:2], cs["eps"])
    h2 = r["h2"].next()
    kb.stt(h2, h2[:], x1, x1[:], (st, st[:, 2:3]), r["gbc"], r["gbc"][:], ALU.mult, ALU.mult)
    kb.dma("sync", io["h2_loc"], io["h2_loc"].t[i * 128:(i + 1) * 128, :], h2, h2[:], h2.name, part=True)
    xT = r["xT"].next()
    for half in range(2):
        pX = r["pXT"].next()
        for k4 in range(4):
            kc = half * 4 + k4
            kb.mm(pX, pX[:, k4, :], x1, x1[:, kc * 128:(kc + 1) * 128], cs["ident_f"], cs["ident_f"][:])
        kb.copy("vector", xT, xT[:, half * 4:(half + 1) * 4, :], pX, pX[:], part=True)
    pL = r["pL"]
    for kc in range(8):
        kb.mm(pL, pL[:, 0:NE], xT, xT[:, kc, :], r["wrg"], r["wrg"][:, kc, :], start=(kc == 0), stop=(kc == 7))
    lg = r["lg"].next()
    kb.ts("vector", lg, lg[:, 0, :], pL, pL[:, 0:NE], (st, st[:, 2:3]), None, ALU.mult, part=True)
    kb.P.op("vector", lambda e: e.tensor_reduce(out=st[:, 3:4], in_=lg[:, 0, :], axis=AX.X, op=ALU.max), reads=[lg.b], partial=[st.b])
    kb.ts("vector", st, st[:, 4:5], st, st[:, 3:4], -1.0, None, ALU.mult, part=True)
    kb.act(lg, lg[:, 1, :], lg, lg[:, 0, :], AF.Exp, bias=(st, st[:, 4:5]), accum=(st, st[:, 5:6]), part=True)
    kb.P.op("vector", lambda e: e.reciprocal(out=st[:, 6:7], in_=st[:, 5:6]), reads=[st.b], partial=[st.b])
    kb.ts("vector", lg, lg[:, 2, :], lg, lg[:, 1, :], (st, st[:, 6:7]), None, ALU.mult, part=True)
    kb.mm(pL, pL[0:NE, 128:256], lg, lg[:, 2, :], cs["ident_f"], cs["ident_f"][:])
    kb.copy("vector", r["affT"], r["affT"][:, i * 128:(i + 1) * 128], pL, pL[0:NE, 128:256], part=True)


def phase2(kb, io, ntiles=32):
    with contextlib.ExitStack() as ph:
        kb.ph = ph
        cs = load_consts(kb, io, ["ident_f", "ones_bf"])
        r = router_setup(kb, io, 0)
        gwo = kb.sb("gwo", [128, 16], F32)
        kb.dma("sync", gwo, gwo[:], io["g_wo"], io["g_wo"].t, "gwo")
        wob = kb.sb("wob", [128, 16, DM], BF16)
        stage = kb.pool("sb", "wostg", 2, [128, DM], F32)
        for ck in range(16):
            stg = stage.next()
            kb.dma("sync", stg, stg[:], io["wo"], io["wo"].t[ck * 128:(ck + 1) * 128, :], stg.name)
            kb.ts("vector", wob, wob[:, ck, :], stg, stg[:], (gwo, gwo[:, ck:ck + 1]), None, ALU.mult, part=True)
        midx = kb.sb("mixidx", [128, 8, 16], I32)
        kb.dma("sync", midx, midx[:], io["mixidx"], io["mixidx"].t, "mixidx")
        mixT = kb.pool("sb", "mixT", 2, [128, 16, WIN], BF16)
        ysq = kb.pool("sb", "ysq", 2, [128, 8, 128], BF16)
        xt = kb.pool("sb", "xt", 2, [128, DM], F32)
        x1p = kb.pool("sb", "x1", 2, [128, DM], F32)
        rsy = kb.pool("sb", "rsy", 2, [128, 4], F32)
        pA = kb.ps("pA", [128, 2, 512], F32)
        pS = kb.ps("pS", [128, 2, 512], F32)
        pq = kb.ps("pq", [128, 512], F32)
        ssd_ck = [q * 4 + j for q in range(4) for j in (2, 3)]
        att_ck = [q * 4 + j for q in range(4) for j in (0, 1)]
        for w in range((ntiles + 3) // 4):
            mt = mixT.next()
            for ck in range(16):
                src = io["mixA_all"] if ck % 4 < 2 else io["mixS_all"]
                kb.P.dma("gpsimd", (lambda e, mt=mt, ck=ck, w=w, src=src: e.indirect_dma_start(
                    out=mt[:, ck, :], out_offset=None, in_=src.t,
                    in_offset=bass.IndirectOffsetOnAxis(ap=midx[:, w, ck:ck + 1], axis=0))),
                    mt.name, reads=[src.b, midx.b], partial=[mt.b])
            for j in range(min(4, ntiles - 4 * w)):
                i = 4 * w + j
                tk = slice(j * 128, (j + 1) * 128)
                ys = ysq.next()
                for n, ck in enumerate(ssd_ck):
                    kb.act(ys, ys[:, n, :], mt, mt[:, ck, tk], AF.Square, part=True)
                for n in range(8):
                    kb.mm(pq, pq[:, 0:1], ys, ys[:, n, :], cs["ones_bf"], cs["ones_bf"][:, 0:1], start=(n == 0), stop=(n == 7))
                rs = rsy.next()
                kb.rstd_from(rs, rs[:, 1:2], pq, pq[:, 0:1], 1024, rs, rs[:, 0:1], cs["eps"])
                for half in range(2):
                    for n, ck in enumerate(att_ck):
                        kb.mm(pA, pA[:, half, :], mt, mt[:, ck, tk], wob, wob[:, ck, half * 512:(half + 1) * 512], start=(n == 0), stop=(n == 7))
                    for n, ck in enumerate(ssd_ck):
                        kb.mm(pS, pS[:, half, :], mt, mt[:, ck, tk], wob, wob[:, ck, half * 512:(half + 1) * 512], start=(n == 0), stop=(n == 7))
                x = xt.next()
                kb.dma("sync", x, x[:], io["x_tok"], io["x_tok"].t[i * 128:(i + 1) * 128, :], x.name)
                x1 = x1p.next()
                kb.stt(x1, x1[:], pS, pS[:].rearrange("p a b -> p (a b)"), (rs, rs[:, 1:2]), x, x[:], ALU.mult, ALU.add)
                kb.tt("vector", x1, x1[:], x1, x1[:], pA, pA[:].rearrange("p a b -> p (a b)"), ALU.add)
                kb.dma("sync", io["x1_loc"], io["x1_loc"].t[i * 128:(i + 1) * 128, :], x1, x1[:], x1.name, part=True)
                router_tile(kb, io, cs, r, x1, i)
        kb.dma("sync", io["aff_loc"], io["aff_loc"].t, r["affT"], r["affT"][:], "affT")
        kb.P.barrier()
        st = kb.P.emit_phase()
        kb.ph = None
    return st


def zero_ydense(kb, io):
    zt = kb.sb("zt", [128, 4, DM], F32)
    kb.memset("gpsimd", zt, zt[:], 0.0)
    yv = io["ydense"].t.rearrange("(a p) d -> p a d", p=128)
    for a in range(32):
        kb.dma("sync", io["ydense"], yv[:, a * 4:(a + 1) * 4, :], zt, zt[:], "zt", part=True)


def moe_topk(kb, io, cs, L):
    aidx = kb.sb("affidx", [128, 1], I32)
    kb.dma("sync", aidx, aidx[:], io["affidx"], io["affidx"].t, "affidx")
    arow = kb.sb("arow", [128, 4096], F32)
    kb.P.dma("gpsimd", lambda e: e.indirect_dma_start(out=arow[0:16, :], out_offset=None, in_=io["aff_all"].t,
             in_offset=bass.IndirectOffsetOnAxis(ap=aidx[0:16, 0:1], axis=0)), "arow", reads=[io["aff_all"].b, aidx.b], writes=[arow.b])
    kb.dma("sync", io["aff_my"], io["aff_my"].t, arow, arow[0:16, :], "arow")
    collective(kb, "AllGather", ALU.bypass, io["h2_loc"], io["h2_all"], "cc2", 8)
    A = kb.sb("A", [128, 4, 128], F32)
    kb.dma("sync", A, A[:], io["aff_my"], io["aff_my"].t.rearrange("(e q) (a j) -> (q a) e j", e=4, j=128), "A")
    lohi = kb.sb("lohi", [128, 16], F32)
    cmp_ = kb.sb("cmp", [128, 128], F32)
    cnt = kb.sb("cnt", [128, 8], F32)
    pc = kb.ps("pc", [128, 512], F32)
    kb.memset("vector", lohi, lohi[:, 0:4], 0.0, part=True)
    kb.memset("vector", lohi, lohi[:, 4:8], 1.0, part=True)
    for it in range(30):
        kb.tt("vector", lohi, lohi[:, 8:12], lohi, lohi[:, 0:4], lohi, lohi[:, 4:8], ALU.add)
        kb.ts("vector", lohi, lohi[:, 8:12], lohi, lohi[:, 8:12], 0.5, None, ALU.mult)
        for e in range(4):
            kb.ts("vector", cmp_, cmp_[:], A, A[:, e, :], (lohi, lohi[:, 8 + e:9 + e]), 0.0, ALU.is_ge, op1=ALU.add, accum=(cnt, cnt[:, e:e + 1]))
        kb.mm(pc, pc[:, 0:4], cs["ones_f"], cs["ones_f"][:], cnt, cnt[:, 0:4])
        kb.ts("vector", lohi, lohi[:, 12:16], pc, pc[:, 0:4], float(CAP), None, ALU.is_ge)
        kb.tt("vector", cnt, cnt[:, 4:8], lohi, lohi[:, 8:12], lohi, lohi[:, 0:4], ALU.subtract)
        kb.tt("vector", cnt, cnt[:, 4:8], cnt, cnt[:, 4:8], lohi, lohi[:, 12:16], ALU.mult)
        kb.tt("vector", lohi, lohi[:, 0:4], lohi, lohi[:, 0:4], cnt, cnt[:, 4:8], ALU.add)
        kb.tt("vector", cnt, cnt[:, 4:8], lohi, lohi[:, 4:8], lohi, lohi[:, 8:12], ALU.subtract)
        kb.tt("vector", cnt, cnt[:, 4:8], cnt, cnt[:, 4:8], lohi, lohi[:, 12:16], ALU.mult)
        kb.tt("vector", lohi, lohi[:, 4:8], lohi, lohi[:, 8:12], cnt, cnt[:, 4:8], ALU.add)
    mask = kb.sb("mask", [128, 4, 128], F32)
    incl = kb.sb("incl", [128, 4, 128], F32)
    slot = kb.sb("slot", [128, 4, 128], F32)
    sloti = kb.sb("sloti", [128, 4, 128], I32)
    pairs = kb.sb("pairs", [128, 4, 128, 3], F32)
    tot = kb.sb("tot", [128, 8], F32)
    for e in range(4):
        kb.ts("vector", mask, mask[:, e, :], A, A[:, e, :], (lohi, lohi[:, e:e + 1]), None, ALU.is_ge, part=True)
        kb.P.op("vector", lambda en, e=e: en.tensor_tensor_scan(out=incl[:, e, :], data0=cs["ones_f"][:], data1=mask[:, e, :], initial=0.0,
                                                               op0=ALU.mult, op1=ALU.add), reads=[cs["ones_f"].b, mask.b], partial=[incl.b])
        kb.copy("vector", tot, tot[:, e:e + 1], incl, incl[:, e, 127:128], part=True)
    kb.mm(pc, pc[:, 8:12], cs["ustrict"], cs["ustrict"][:], tot, tot[:, 0:4])
    kb.copy("vector", tot, tot[:, 4:8], pc, pc[:, 8:12], part=True)
    for e in range(4):
        kb.tt("vector", slot, slot[:, e, :], incl, incl[:, e, :], mask, mask[:, e, :], ALU.subtract, part=True)
        kb.ts("vector", slot, slot[:, e, :], slot, slot[:, e, :], (tot, tot[:, 4 + e:5 + e]), float(e * CAP), ALU.add, op1=ALU.add, part=True)
        kb.ts("vector", incl, incl[:, e, :], mask, mask[:, e, :], -1.0e6, 1.0e6, ALU.mult, op1=ALU.add, part=True)
        kb.tt("vector", slot, slot[:, e, :], slot, slot[:, e, :], incl, incl[:, e, :], ALU.add, part=True)
        kb.ts("vector", incl, incl[:, e, :], slot, slot[:, e, :], float((e + 1) * CAP), 1.0e6, ALU.is_ge, op1=ALU.mult, part=True)
        kb.tt("vector", slot, slot[:, e, :], slot, slot[:, e, :], incl, incl[:, e, :], ALU.add, part=True)
        kb.copy("vector", sloti, sloti[:, e, :], slot, slot[:, e, :], part=True)
        kb.copy("gpsimd", pairs, pairs[:, e, :, 0], cs["iota_tok"], cs["iota_tok"][:], part=True)
        kb.copy("gpsimd", pairs, pairs[:, e, :, 1], A, A[:, e, :], part=True)
        kb.copy("gpsimd", pairs, pairs[:, e, :, 2], cs["iota_h2row"], cs["iota_h2row"][:], part=True)
    rc = {}

    def breg(en):
        if "r" not in rc:
            rc["r"] = en.to_reg(4 * CAP - 1)
        return rc["r"]

    for e in range(4):
        for j in range(128):
            kb.P.dma("gpsimd", (lambda en, e=e, j=j: en.indirect_dma_start(
                out=io["sel"].t, out_offset=bass.IndirectOffsetOnAxis(ap=sloti[:, e, j:j + 1], axis=0),
                in_=pairs[:, e, j, :], in_offset=None, bounds_check=breg(en), oob_is_err=False)),
                "selsc", reads=[pairs.b, sloti.b], partial=[io["sel"].b])


def moe_phase(kb, io, L):
    with contextlib.ExitStack() as ph:
        kb.ph = ph
        cs = load_consts(kb, io, ["ident_bf", "ones_f", "ustrict", "iota_tok", "iota_h2row"])
        with contextlib.ExitStack() as ph2:
            kb.ph = ph2
            moe_topk(kb, io, cs, L)
            kb.P.barrier()
            kb.P.emit_phase()
        kb.ph = ph
        cs = load_consts(kb, io, ["ident_bf"])
        xeT = kb.sb("xeT", [128, 8, CAP], BF16)
        hidT = kb.sb("hidT", [128, 16, CAP], BF16)
        wdb = kb.sb("wdb", [128, 16, DM], BF16)
        wgb = kb.pool("sb", "wgb", 2, [128, 8, 512], BF16)
        wub = kb.pool("sb", "wub", 2, [128, 8, 512], BF16)
        selT = kb.pool("sb", "selT", 2, [128, 16, 3], F32)
        toki = kb.pool("sb", "toki", 2, [128, 2, 16], I32)
        xe = kb.pool("sb", "xe", 3, [128, DM], BF16)
        sg = kb.pool("sb", "sg", 2, [128, 512], BF16)
        ye = kb.pool("sb", "ye", 2, [128, DM], F32)
        pT = kb.pool("ps", "pT", 2, [128, 4, 128], F32)
        pGt = kb.pool("ps", "pGt", 2, [128, 512], F32)
        pUp = kb.pool("ps", "pUp", 2, [128, 512], F32)
        pYe = kb.ps("pYe", [128, 2, 512], F32)
        wg, wu, wd = io["wg%d" % L], io["wu%d" % L], io["wd%d" % L]
        for e in range(4):
            sT = selT.next()
            kb.dma("sync", sT, sT[:], io["sel"], io["sel"].t[e * CAP:(e + 1) * CAP, :].rearrange("(k p) c -> p k c", p=128), sT.name)
            ti = toki.next()
            kb.copy("vector", ti, ti[:, 0, :], sT, sT[:, :, 0], part=True)
            kb.copy("vector", ti, ti[:, 1, :], sT, sT[:, :, 2], part=True)
            for q in range(4):
                kb.dma("gpsimd", wdb, wdb[:, q * 4:(q + 1) * 4, :], wd, wd.t[e, q * 512:(q + 1) * 512, :].rearrange("(k p) d -> p k d", p=128),
                       "wdb", part=True)
            for k in range(16):
                x = xe.next()
                kb.P.dma("gpsimd", (lambda en, x=x, ti=ti, k=k: en.indirect_dma_start(
                    out=x[:], out_offset=None, in_=io["h2_all"].t, in_offset=bass.IndirectOffsetOnAxis(ap=ti[:, 1, k:k + 1], axis=0))),
                    x.name, reads=[io["h2_all"].b, ti.b], writes=[x.b])
                for half in range(2):
                    p = pT.next()
                    for k4 in range(4):
                        kc = half * 4 + k4
                        kb.mm(p, p[:, k4, :], x, x[:, kc * 128:(kc + 1) * 128], cs["ident_bf"], cs["ident_bf"][:])
                    kb.copy("vector", xeT, xeT[:, half * 4:(half + 1) * 4, k * 128:(k + 1) * 128], p, p[:], part=True)
            for fg in range(4):
                wgt = wgb.next()
                wut = wub.next()
                kb.dma("gpsimd", wgt, wgt[:], wg, wg.t[e, :, fg * 512:(fg + 1) * 512].rearrange("(k p) f -> p k f", p=128), wgt.name)
                kb.dma("gpsimd", wut, wut[:], wu, wu.t[e, :, fg * 512:(fg + 1) * 512].rearrange("(k p) f -> p k f", p=128), wut.name)
                for f4 in range(4):
                    fi = fg * 4 + f4
                    for win in range(4):
                        pg = pGt.next()
                        pu = pUp.next()
                        for kc in range(8):
                            kb.mm(pg, pg[:], wgt, wgt[:, kc, f4 * 128:(f4 + 1) * 128], xeT, xeT[:, kc, win * 512:(win + 1) * 512],
                                  start=(kc == 0), stop=(kc == 7))
                        for kc in range(8):
                            kb.mm(pu, pu[:], wut, wut[:, kc, f4 * 128:(f4 + 1) * 128], xeT, xeT[:, kc, win * 512:(win + 1) * 512],
                                  start=(kc == 0), stop=(kc == 7))
                        s_ = sg.next()
                        kb.act(s_, s_[:], pg, pg[:], AF.Silu)
                        kb.tt("vector", hidT, hidT[:, fi, win * 512:(win + 1) * 512], s_, s_[:], pu, pu[:], ALU.mult, part=True)
            for k in range(16):
                for fi in range(16):
                    for half in range(2):
                        kb.mm(pYe, pYe[:, half, :], hidT, hidT[:, fi, k * 128:(k + 1) * 128], wdb, wdb[:, fi, half * 512:(half + 1) * 512],
                              start=(fi == 0), stop=(fi == 15))
                y = ye.next()
                kb.ts("vector", y, y[:], pYe, pYe[:].rearrange("p a b -> p (a b)"), (sT, sT[:, k, 1:2]), None, ALU.mult)
                kb.P.dma("gpsimd", (lambda en, y=y, ti=ti, k=k: en.indirect_dma_start(
                    out=io["ydense"].t, out_offset=bass.IndirectOffsetOnAxis(ap=ti[:, 0, k:k + 1], axis=0), in_=y[:], in_offset=None,
                    compute_op=ALU.add)), y.name, reads=[y.b, ti.b], writes=[io["ydense"].b])
        kb.P.barrier()
        st = kb.P.emit_phase()
        kb.ph = None
    return st


def phase4(kb, io):
    NT = 32
    TL = 4096
    with contextlib.ExitStack() as ph:
        kb.ph = ph
        cs = load_consts(kb, io, ["ident_bf", "ident_f"])
        hnT = kb.sb("hnT", [128, 8, TL + 2], BF16)
        mT = kb.sb("mT", [128, 8, TL], BF16)
        with contextlib.ExitStack() as ph2:
            kb.ph = ph2
            gbc = kb.sb("gconv_bc", [128, DM], F32)
            kb.dma("sync", gbc, gbc[:], io["g_conv_bc"], io["g_conv_bc"].t, "gconv_bc")
            rows = kb.sb("myrows", [128, 33], I32)
            kb.dma("sync", rows, rows[:], io["myrows"], io["myrows"].t, "myrows")
            hidx = kb.sb("haloidx", [128, 1], I32)
            kb.dma("sync", hidx, hidx[:], io["haloidx"], io["haloidx"].t, "haloidx")
            hmsk = kb.sb("halomsk", [128, 1], F32)
            kb.dma("sync", hmsk, hmsk[:], io["halomsk"], io["halomsk"].t, "halomsk")
            x1p = kb.pool("sb", "x1t", 2, [128, DM], F32)
            ysp = kb.pool("sb", "yst", 2, [128, DM], F32)
            x2p = kb.pool("sb", "x2t", 2, [128, DM], F32)
            sqj = kb.pool("sb", "sqj", 2, [128, DM], BF16)
            stp = kb.pool("sb", "st4", 2, [128, 4], F32)
            hnp = kb.pool("sb", "hn", 2, [128, DM], BF16)
            pT = kb.pool("ps", "pT4", 2, [128, 4, 128], F32)
            for i in range(NT + 1):
                x1 = x1p.next()
                ys = ysp.next()
                if i < NT:
                    kb.dma("sync", x1, x1[:], io["x1_loc"], io["x1_loc"].t[i * 128:(i + 1) * 128, :], x1.name)
                else:
                    kb.P.dma("gpsimd", (lambda en, x1=x1: en.indirect_dma_start(out=x1[:], out_offset=None, in_=io["edges_all"].t,
                             in_offset=bass.IndirectOffsetOnAxis(ap=hidx[:, 0:1], axis=0))), x1.name, reads=[io["edges_all"].b, hidx.b], writes=[x1.b])
                kb.P.dma("gpsimd", (lambda en, ys=ys, i=i: en.indirect_dma_start(out=ys[:], out_offset=None, in_=io["ysum_all"].t,
                         in_offset=bass.IndirectOffsetOnAxis(ap=rows[:, i:i + 1], axis=0))), ys.name, reads=[io["ysum_all"].b, rows.b], writes=[ys.b])
                x2 = x2p.next()
                kb.tt("vector", x2, x2[:], x1, x1[:], ys, ys[:], ALU.add)
                if i == NT:
                    kb.ts("vector", x2, x2[:], x2, x2[:], (hmsk, hmsk[:, 0:1]), None, ALU.mult)
                else:
                    kb.dma("sync", io["x2_loc"], io["x2_loc"].t[i * 128:(i + 1) * 128, :], x2, x2[:], x2.name, part=True)
                st = stp.next()
                sq = sqj.next()
                kb.act(sq, sq[:], x2, x2[:], AF.Square, accum=(st, st[:, 0:1]))
                kb.rstd_from(st, st[:, 2:3], st, st[:, 0:1], DM, st, st[:, 1:2], cs["eps"])
                hn = hnp.next()
                kb.stt(hn, hn[:], x2, x2[:], (st, st[:, 2:3]), gbc, gbc[:], ALU.mult, ALU.mult)
                for half in range(2):
                    p = pT.next()
                    for k4 in range(4):
                        kc = half * 4 + k4
                        kb.mm(p, p[:, k4, :], hn, hn[:, kc * 128:(kc + 1) * 128], cs["ident_bf"], cs["ident_bf"][:])
                    if i < NT:
                        kb.copy("vector", hnT, hnT[:, half * 4:(half + 1) * 4, 1 + i * 128:1 + (i + 1) * 128], p, p[:], part=True)
                    else:
                        kb.copy("vector", hnT, hnT[:, half * 4:(half + 1) * 4, 0:1], p, p[:, :, 0:1], part=True)
                        kb.copy("vector", hnT, hnT[:, half * 4:(half + 1) * 4, TL + 1:TL + 2], p, p[:, :, 1:2], part=True)
            kb.P.barrier()
            kb.P.emit_phase()
        with contextlib.ExitStack() as ph2:
            kb.ph = ph2
            w2b = kb.pool("sb", "w2b", 2, [128, 8, 384], BF16)
            cw = kb.sb("cw3", [128, 8, 3], F32)
            kb.dma("sync", cw, cw[:], io["cw3"], io["cw3"].t, "cw3")
            csb = kb.pool("sb", "csb", 2, [128, 512], F32)
            vv = kb.pool("sb", "vv", 2, [128, 512], F32)
            ca = kb.pool("sb", "ca", 2, [128, 2, 512], F32)
            pB = kb.pool("ps", "pB4", 2, [128, 512], F32)
            pC = kb.pool("ps", "pC4", 2, [128, 512], F32)
            pU = kb.pool("ps", "pU4", 2, [128, 512], F32)
            w2 = io["w2"]
            nwin = 9
            for cc in range(8):
                wt = w2b.next()
                for part in range(3):
                    kb.dma("gpsimd", wt, wt[:, :, part * 128:(part + 1) * 128],
                           w2, w2.t[:, part * DM + cc * 128: part * DM + (cc + 1) * 128].rearrange("(k p) f -> p k f", p=128), wt.name, part=True)
                for w in range(nwin):
                    c0 = 510 * w if w < nwin - 1 else TL + 2 - 512
                    pb, pc, pu = pB.next(), pC.next(), pU.next()
                    for (pp, part) in ((pb, 0), (pc, 1), (pu, 2)):
                        for kc in range(8):
                            kb.mm(pp, pp[:], wt, wt[:, kc, part * 128:(part + 1) * 128], hnT, hnT[:, kc, c0:c0 + 512], start=(kc == 0), stop=(kc == 7))
                    cb = csb.next()
                    kb.copy("scalar", cb, cb[:], pc, pc[:])
                    v = vv.next()
                    kb.tt("vector", v, v[:], cb, cb[:], pu, pu[:], ALU.mult)
                    a = ca.next()
                    kb.act(a, a[:, 0, 0:510], v, v[:, 0:510], AF.Copy, scale=(cw, cw[:, cc, 0:1]), part=True)
                    kb.stt(a, a[:, 0, 0:510], v, v[:, 1:511], (cw, cw[:, cc, 1:2]), a, a[:, 0, 0:510], ALU.mult, ALU.add, part=True)
                    kb.stt(a, a[:, 1, 0:510], v, v[:, 2:512], (cw, cw[:, cc, 2:3]), a, a[:, 0, 0:510], ALU.mult, ALU.add, part=True)
                    kb.tt("vector", mT, mT[:, cc, c0:c0 + 510], a, a[:, 1, 0:510], pb, pb[:, 1:511], ALU.mult, part=True)
            kb.P.barrier()
            kb.P.emit_phase()
        with contextlib.ExitStack() as ph2:
            kb.ph = ph2
            cs = load_consts(kb, io, ["ident_f"])
            r = router_setup(kb, io, 1)
            w3b = kb.sb("w3b", [128, 8, DM], BF16)
            kb.dma("gpsimd", w3b, w3b[:], io["w3"], io["w3"].t.rearrange("(k p) d -> p k d", p=128), "w3b")
            x2p = kb.pool("sb", "x2r", 2, [128, DM], F32)
            x3p = kb.pool("sb", "x3", 2, [128, DM], F32)
            pO = kb.ps("pO4", [128, 2, 512], F32)
            for i in range(NT):
                for half in range(2):
                    for cc in range(8):
                        kb.mm(pO, pO[:, half, :], mT, mT[:, cc, i * 128:(i + 1) * 128], w3b, w3b[:, cc, half * 512:(half + 1) * 512],
                              start=(cc == 0), stop=(cc == 7))
                x2 = x2p.next()
                kb.dma("sync", x2, x2[:], io["x2_loc"], io["x2_loc"].t[i * 128:(i + 1) * 128, :], x2.name)
                x3 = x3p.next()
                kb.tt("vector", x3, x3[:], x2, x2[:], pO, pO[:].rearrange("p a b -> p (a b)"), ALU.add)
                kb.dma("sync", io["x1_loc"], io["x1_loc"].t[i * 128:(i + 1) * 128, :], x3, x3[:], x3.name, part=True)
                router_tile(kb, io, cs, r, x3, i)
            kb.dma("sync", io["aff_loc"], io["aff_loc"].t, r["affT"], r["affT"][:], "affT")
            kb.P.barrier()
            st = kb.P.emit_phase()
        kb.ph = None
    return st


def final_phase(kb, io):
    with contextlib.ExitStack() as ph:
        kb.ph = ph
        rows = kb.sb("myrows", [128, 33], I32)
        kb.dma("sync", rows, rows[:], io["myrows"], io["myrows"].t, "myrows")
        x1p = kb.pool("sb", "x1t", 2, [128, DM], F32)
        ysp = kb.pool("sb", "yst", 2, [128, DM], F32)
        op = kb.pool("sb", "ot", 2, [128, DM], F32)
        for i in range(32):
            x1 = x1p.next()
            ys = ysp.next()
            kb.dma("sync", x1, x1[:], io["x1_loc"], io["x1_loc"].t[i * 128:(i + 1) * 128, :], x1.name)
            kb.P.dma("gpsimd", (lambda en, ys=ys, i=i: en.indirect_dma_start(out=ys[:], out_offset=None, in_=io["ysum_all"].t,
                     in_offset=bass.IndirectOffsetOnAxis(ap=rows[:, i:i + 1], axis=0))), ys.name, reads=[io["ysum_all"].b, rows.b], writes=[ys.b])
            o = op.next()
            kb.tt("vector", o, o[:], x1, x1[:], ys, ys[:], ALU.add)
            kb.dma("sync", io["out"], io["out"].t[i * 128:(i + 1) * 128, :], o, o[:], o.name, part=True)
        kb.P.barrier()
        st = kb.P.emit_phase()
        kb.ph = None
    return st


def collective(kb, kind, op, src, dst, key, nchunks=1):
    rin = src.t.shape[0] // nchunks
    rout = dst.t.shape[0] // nchunks
    for c in range(nchunks):
        sap = src.t[c * rin:(c + 1) * rin, :]
        dap = dst.t[c * rout:(c + 1) * rout, :]
        kb.P.dma("gpsimd", (lambda e, sap=sap, dap=dap: e.collective_compute(kind, op, replica_groups=GROUPS, ins=[sap.opt()], outs=[dap.opt()])),
                 key, reads=[src.b], writes=[] if nchunks > 1 else [dst.b], partial=[dst.b] if nchunks > 1 else [], inc=1)


IN_SPECS = {
    "xT": ([DM, S], F32), "x_tok": ([4096, DM], F32), "w1": ([DM, NC1], F32), "g_attn": ([128, 8], F32), "gqk": ([128, 2], F32),
    "convw": ([128, 4, 6], F32), "dtc": ([8, 2], F32), "Dbc": ([128, 256], F32), "wo": ([2048, DM], F32), "g_wo": ([128, 16], F32),
    "mixidx": ([128, 8, 16], I32), "wr0": ([DM, NE], F32), "wr1": ([DM, NE], F32), "g_ffn_col0": ([128, 8], F32),
    "g_ffn_col1": ([128, 8], F32), "g_ffn_bc0": ([128, DM], F32), "g_ffn_bc1": ([128, DM], F32), "affidx": ([128, 1], I32),
    "myrows": ([128, 33], I32), "haloidx": ([128, 1], I32), "halomsk": ([128, 1], F32), "g_conv_bc": ([128, DM], F32),
    "w2": ([DM, 3 * DM], F32), "cw3": ([128, 8, 3], F32), "w3": ([DM, DM], F32),
    "wg0": ([4, DM, FF], F32), "wu0": ([4, DM, FF], F32), "wd0": ([4, FF, DM], F32),
    "wg1": ([4, DM, FF], F32), "wu1": ([4, DM, FF], F32), "wd1": ([4, FF, DM], F32),
}
CONST_NAMES = ["ident_bf", "ident_f", "negmask", "blockones", "rotm", "sel_ones", "ones_bf", "ones_f", "tri", "smask", "ustrict",
               "iota_tok", "iota_h2row", "cosf", "sinf"]
SCRATCH = {
    "mixA_loc": ([8192, 512], BF16), "mixA_all": ([32768, 512], BF16), "mixS_loc": ([8192, 512], BF16), "mixS_all": ([32768, 512], BF16), "xb_dram": ([DM, S], BF16), "post": ([512, S], BF16), "zs": ([256, S], BF16),
    "dta": ([16, S], F32), "yf": ([S, 256], F32), "x1_loc": ([4096, DM], F32), "x2_loc": ([4096, DM], F32),
    "h2_loc": ([4096, DM], BF16), "h2_all": ([S, DM], BF16), "aff_loc": ([NE, 4096], F32), "aff_all": ([4 * NE, 4096], F32),
    "aff_my": ([NE, 4096], F32), "sel": ([4 * CAP, 3], F32), "ydense": ([S, DM], F32), "ysum_all": ([S, DM], F32),
    "edges_loc": ([2, DM], F32), "edges_all": ([8, DM], F32),
}


def misc_phase(kb, fn):
    with contextlib.ExitStack() as ph:
        kb.ph = ph
        fn()
        kb.P.barrier()
        st = kb.P.emit_phase()
        kb.ph = None
    return st


def build_program(consts, debug=False, upto=99):
    nc = bass.Bass("TRN2", target_bir_lowering=False)
    with contextlib.ExitStack() as st:
        kb = KB(nc, st)
        io = {}
        for n, (shape, dt) in IN_SPECS.items():
            if upto < 3 and n[:2] in ("wg", "wu", "wd"):
                continue
            io[n] = kb.dram(n, shape, dt, "ExternalInput")
        for n in CONST_NAMES:
            io[n] = kb.dram(n, list(consts[n].shape), CONST_DT.get(n, F32), "ExternalInput")
        for n, (shape, dt) in SCRATCH.items():
            io[n] = kb.dram(n, shape, dt)
        io["out"] = kb.dram("out", [4096, DM], F32, "ExternalOutput")
        dbg = {}
        if debug:
            for n, shape in (("dbg_x1", [4096, DM]), ("dbg_x2", [4096, DM]), ("dbg_x3", [4096, DM]), ("dbg_aff0", [NE, 4096]),
                             ("dbg_aff1", [NE, 4096]), ("dbg_sel", [4 * CAP, 3])):
                dbg[n] = kb.dram(n, shape, F32, "ExternalOutput")


        def cp(dst, src, key):
            kb.dma("sync", dst, dst.t, src, src.t, key)

        import os
        if not os.environ.get("K_SKIP1"):
            phase1_attn(kb, io, 0)
            io["_xb_ready"] = True
            phase1_attn(kb, io, 1)
            phase1_ssd_in(kb, io)
            phase1_ssd_scan(kb, io, 0)
            phase1_ssd_scan(kb, io, 1)

        def ph_a():
            collective(kb, "AllGather", ALU.bypass, io["mixS_loc"], io["mixS_all"], "cc0", 8)
            zero_ydense(kb, io)

        misc_phase(kb, ph_a)
        if upto >= 2:
            phase2(kb, io, int(os.environ.get("K_NT", 32)))

            def ph_b(L):
                def f():
                    if L == 0:
                        kb.dma("sync", io["edges_loc"], io["edges_loc"].t[0:1, :], io["x1_loc"], io["x1_loc"].t[0:1, :], "edg", part=True)
                        kb.dma("sync", io["edges_loc"], io["edges_loc"].t[1:2, :], io["x1_loc"], io["x1_loc"].t[4095:4096, :], "edg", part=True)
                        collective(kb, "AllGather", ALU.bypass, io["edges_loc"], io["edges_all"], "cc1")
                    collective(kb, "AllGather", ALU.bypass, io["aff_loc"], io["aff_all"], "cc3")
                    if debug:
                        cp(dbg["dbg_x1" if L == 0 else "dbg_x3"], io["x1_loc"], "dbgx")
                        cp(dbg["dbg_aff%d" % L], io["aff_loc"], "dbga")
                return f
            misc_phase(kb, ph_b(0))
        if upto >= 3:
            moe_phase(kb, io, 0)

            def ph_c():
                collective(kb, "AllReduce", ALU.add, io["ydense"], io["ysum_all"], "cc4", 16)
                if debug:
                    cp(dbg["dbg_sel"], io["sel"], "dbgs")
            misc_phase(kb, ph_c)
        if upto >= 4:
            phase4(kb, io)

            def ph_d():
                zero_ydense(kb, io)
                if debug:
                    cp(dbg["dbg_x2"], io["x2_loc"], "dbgx2")
            misc_phase(kb, ph_d)
            misc_phase(kb, ph_b(1))
        if upto >= 5:
            moe_phase(kb, io, 1)
            misc_phase(kb, lambda: collective(kb, "AllReduce", ALU.add, io["ydense"], io["ysum_all"], "cc5", 16))
            final_phase(kb, io)
        print("semaphores:", len(kb.P.dsems) + 5)
    return nc


def core_inputs(inp, c, consts):
    b, r = c // 4, c % 4
    hg = r
    g = hg // 2
    d = {}
    x = inp["x"]
    d["xT"] = np.ascontiguousarray(x[b].T)
    d["x_tok"] = np.ascontiguousarray(x[b, r * 4096:(r + 1) * 4096])
    w = inp["w_in_even"][0]
    hs = slice(hg * 256, (hg + 1) * 256)
    cols = [w[:, 0:1024][:, hs], w[:, 1024:2048][:, hs], w[:, 2048:3072][:, hs], w[:, 4096:5120][:, hs],
            w[:, 5120 + g * 128:5120 + (g + 1) * 128], w[:, 5376 + g * 128:5376 + (g + 1) * 128], w[:, 3072:4096][:, hs],
            w[:, 5632 + hg * 4:5632 + hg * 4 + 4], w[:, 5648 + hg * 4:5648 + hg * 4 + 4]]
    d["w1"] = np.ascontiguousarray(np.concatenate(cols, 1))
    d["g_attn"] = np.ascontiguousarray(inp["attn_norm"][0].reshape(8, 128).T)
    d["gqk"] = np.ascontiguousarray(np.stack([np.tile(inp["q_norm"][0], 2), np.tile(inp["k_norm"][0], 2)], 1))
    cw_full = inp["ssd_conv_w"][0]; cb = inp["ssd_conv_b"][0]
    chans = np.concatenate([np.arange(hg * 256, hg * 256 + 256), 1024 + g * 128 + np.arange(128), 1280 + g * 128 + np.arange(128)])
    cw = np.concatenate([cw_full[:, chans], cb[None, chans]], 0)
    d["convw"] = np.ascontiguousarray(cw.reshape(6, 4, 128).transpose(2, 1, 0)).astype(np.float32)
    h4 = slice(hg * 4, hg * 4 + 4)
    d["dtc"] = np.stack([np.concatenate([inp["ssd_dt_bias_fwd"][0][h4], inp["ssd_dt_bias_bwd"][0][h4]]),
                         np.concatenate([inp["ssd_a_log_fwd"][0][h4], inp["ssd_a_log_bwd"][0][h4]])], 1).astype(np.float32)
    d["Dbc"] = np.ascontiguousarray(np.tile(np.repeat(inp["ssd_d"][0][h4], 64)[None, :], (128, 1))).astype(np.float32)
    wo = inp["w_out_even"][0]
    perm = np.concatenate([np.concatenate([np.arange(q * 256, (q + 1) * 256), 1024 + np.arange(q * 256, (q + 1) * 256)]) for q in range(4)])
    d["wo"] = np.ascontiguousarray(wo[perm])
    gfull = np.concatenate([np.ones(1024, np.float32), inp["ssd_out_norm"][0]])[perm]
    d["g_wo"] = np.ascontiguousarray(gfull.reshape(16, 128).T).astype(np.float32)
    mi = np.zeros((128, 8, 16), np.int32)
    for wdx in range(8):
        for ck in range(16):
            L = (8 * r + wdx) * 256 + (ck % 2) * 128 + np.arange(128)
            mi[:, wdx, ck] = (L // 1024) * 4096 + (ck // 4) * 1024 + (L % 1024)
    d["mixidx"] = mi
    for L in range(2):
        d["wr%d" % L] = np.ascontiguousarray(inp["router_w"][L])
        d["g_ffn_col%d" % L] = np.ascontiguousarray(inp["ffn_norm"][L].reshape(8, 128).T)
        d["g_ffn_bc%d" % L] = np.ascontiguousarray(np.tile(inp["ffn_norm"][L][None, :], (128, 1)))
        es = slice(4 * r, 4 * r + 4)
        d["wg%d" % L] = np.ascontiguousarray(inp["expert_w_gate"][L, es])
        d["wu%d" % L] = np.ascontiguousarray(inp["expert_w_up"][L, es])
        d["wd%d" % L] = np.ascontiguousarray(inp["expert_w_down"][L, es])
    ai = np.zeros((128, 1), np.int32)
    for e in range(4):
        for q in range(4):
            ai[e * 4 + q, 0] = q * 16 + 4 * r + e
    d["affidx"] = ai
    mr = np.zeros((128, 33), np.int32)
    for i in range(32):
        mr[:, i] = r * 4096 + i * 128 + np.arange(128)
    mr[0, 32] = max(r * 4096 - 1, 0)
    mr[1, 32] = min(r * 4096 + 4096, S - 1)
    d["myrows"] = mr
    hi = np.zeros((128, 1), np.int32); hm = np.zeros((128, 1), np.float32)
    if r > 0:
        hi[0, 0] = 2 * (r - 1) + 1; hm[0, 0] = 1.0
    if r < 3:
        hi[1, 0] = 2 * (r + 1); hm[1, 0] = 1.0
    d["haloidx"] = hi; d["halomsk"] = hm
    d["g_conv_bc"] = np.ascontiguousarray(np.tile(inp["conv_norm"][0][None, :], (128, 1)))
    d["w2"] = np.ascontiguousarray(inp["conv_w_in"][0])
    d["cw3"] = np.ascontiguousarray(inp["conv_w"][0].reshape(3, 8, 128).transpose(2, 1, 0))
    d["w3"] = np.ascontiguousarray(inp["conv_w_out"][0])
    for n in CONST_NAMES:
        d[n] = consts[n]
    return d


def kernel(**inputs):
    from concourse.bass_utils import run_bass_kernel_spmd
    inp = {k: np.asarray(v) for k, v in inputs.items()}
    consts = host_constants()
    nc = build_program(consts)
    in_maps = [core_inputs(inp, c, consts) for c in range(NCORE)]
    res = run_bass_kernel_spmd(nc, in_maps, core_ids=list(range(NCORE)))
    out = np.zeros((2, S, DM), np.float32)
    for c in range(NCORE):
        b, r = c // 4, c % 4
        out[b, r * 4096:(r + 1) * 4096] = res.results[c]["out"]
    return out
```

```python
import contextlib
import numpy as np
import concourse.bass as bass
import concourse.mybir as mybir

F32 = mybir.dt.float32
BF16 = mybir.dt.bfloat16
I32 = mybir.dt.int32
U32 = mybir.dt.uint32
AF = mybir.ActivationFunctionType
ALU = mybir.AluOpType
AX = mybir.AxisListType

ENGS = ("tensor", "vector", "scalar", "gpsimd", "sync")


class Buf:
    __slots__ = ("name", "w", "r", "dsem", "dcount")

    def __init__(self, name):
        self.name = name
        self.w = {}
        self.r = {}
        self.dsem = None
        self.dcount = 0


class Prog:
    def __init__(self, nc, stack):
        self.nc = nc
        self.stack = stack
        self.esem = {e: stack.enter_context(nc.semaphore("es_" + e)) for e in ENGS}
        self.base = {e: 0 for e in ENGS}
        self.dsems = {}
        self.dvals = {}
        self.dfree = []
        self.phase = 0
        self.reset_phase()
        self.same_engine_sync = True
        self.no_self = set()

    def reset_phase(self):
        self.ops = {e: [] for e in ENGS}
        self.seen = {e: {} for e in ENGS}
        self.needed = {e: set() for e in ENGS}

    def _collect(self, eng, reads, writes, partial):
        waits = {}
        def add(d):
            for k, v in d.items():
                if waits.get(k, -1) < v:
                    waits[k] = v
        for b in reads:
            add(b.w)
        for b in writes:
            add(b.w); add(b.r)
        for b in partial:
            add(b.r)
        out = []
        seen = self.seen[eng]
        for k, v in waits.items():
            if k[2] != self.phase:
                continue
            if k[0] == "E" and k[1] == eng and (not self.same_engine_sync or eng in self.no_self):
                continue
            if seen.get(k, -1) >= v:
                continue
            seen[k] = v
            if k[0] == "E":
                self.needed[k[1]].add(v)
            out.append((k, v))
        return out

    def _commit(self, tok, reads, writes, partial):
        k, v = tok
        for b in reads:
            if b.r.get(k, -1) < v:
                b.r[k] = v
        for b in writes:
            b.w = {k: v}
            b.r = {}
        for b in partial:
            if b.w.get(k, -1) < v:
                b.w[k] = v

    def _serial_waits(self, eng, waits):
        import os
        if not os.environ.get("FW_SERIAL"):
            return waits
        if os.environ["FW_SERIAL"] != "1" and eng not in os.environ["FW_SERIAL"].split(","):
            return waits
        have = {k for k, _ in waits}
        for e2 in ENGS:
            if e2 == eng:
                continue
            ee = [o["idx"] for o in self.ops[e2] if o["kind"] == "E"]
            if not ee:
                continue
            k = ("E", e2, self.phase)
            if self.seen[eng].get(k, -1) < ee[-1]:
                self.seen[eng][k] = ee[-1]
                self.needed[e2].add(ee[-1])
                waits.append((k, ee[-1]))
        return waits

    def op(self, eng, emit, reads=(), writes=(), partial=()):
        waits = self._collect(eng, reads, writes, partial)
        waits = self._serial_waits(eng, waits)
        idx = len(self.ops[eng]) + 1
        self.ops[eng].append(dict(waits=waits, emit=emit, kind="E", idx=idx))
        self._commit((("E", eng, self.phase), idx), reads, writes, partial)

    def dma(self, eng, emit, key, reads=(), writes=(), partial=(), inc=16):
        waits = self._collect(eng, reads, writes, partial)
        if key not in self.dsems:
            if self.dfree:
                self.dsems[key], self.dvals[key] = self.dfree.pop(0)
            else:
                self.dsems[key] = self.stack.enter_context(self.nc.semaphore("ds%d_%s" % (self.phase, key)))
                self.dvals[key] = 0
        self.dvals[key] += inc
        v = self.dvals[key]
        idx = len(self.ops[eng]) + 1
        self.ops[eng].append(dict(waits=waits, emit=emit, kind="D", key=key, inc=inc, idx=idx))
        self._commit((("D", key, self.phase), v), reads, writes, partial)

    def barrier(self):
        last = {}
        for e in ENGS:
            ee = [o["idx"] for o in self.ops[e] if o["kind"] == "E"]
            last[e] = ee[-1] if ee else 0
        for e in ENGS:
            waits = []
            for e2 in ENGS:
                if e2 != e and last[e2] > 0:
                    k = ("E", e2, self.phase)
                    if self.seen[e].get(k, -1) < last[e2]:
                        self.seen[e][k] = last[e2]
                        self.needed[e2].add(last[e2])
                        waits.append((k, last[e2]))
            for key, v in self.dvals.items():
                k = ("D", key, self.phase)
                if self.seen[e].get(k, -1) < v:
                    self.seen[e][k] = v
                    waits.append((k, v))
            self.ops[e].append(dict(waits=waits, emit=None, kind="N", idx=len(self.ops[e]) + 1))

    def emit_phase(self):
        nc = self.nc
        val = {}
        for e in ENGS:
            c = self.base[e]
            m = {}
            for o in self.ops[e]:
                if o["kind"] == "E" and o["idx"] in self.needed[e]:
                    c += 1
                    m[o["idx"]] = c
            val[e] = m
        stats = {}

        def replay(e, eng):
            n = 0
            for o in self.ops[e]:
                for (k, v) in o["waits"]:
                    if k[0] == "E":
                        eng.wait_ge(self.esem[k[1]], val[k[1]][v])
                    else:
                        eng.wait_ge(self.dsems[k[1]], v)
                    n += 1
                if o["emit"] is None:
                    continue
                ins = o["emit"](eng)
                n += 1
                if o["kind"] == "E":
                    if o["idx"] in self.needed[e]:
                        ins.then_inc(self.esem[e], 1)
                else:
                    ins.then_inc(self.dsems[o["key"]], o["inc"])
            stats[e] = n

        with nc.Block() as block:
            @block.tensor
            def _(eng):
                replay("tensor", eng)

            @block.vector
            def _(eng):
                replay("vector", eng)

            @block.scalar
            def _(eng):
                replay("scalar", eng)

            @block.gpsimd
            def _(eng):
                replay("gpsimd", eng)

            @block.sync
            def _(eng):
                replay("sync", eng)
        for e in ENGS:
            if val[e]:
                self.base[e] = max(val[e].values())
        for key in list(self.dsems):
            self.dfree.append((self.dsems[key], self.dvals[key]))
        self.dsems = {}
        self.dvals = {}
        self.phase += 1
        self.reset_phase()
        return stats


S = 16384
DM = 1024
NCORE = 8
PAD = 1024
WIN = 512
NW = S // WIN
NCH = S // 128
NE = 16
CAP = 2048
FF = 2048
EPS = 1e-6
NEG = -30000.0
C_Q, C_K, C_V, C_XS, C_B, C_C, C_Z, C_DT, NC1 = 0, 256, 512, 768, 1024, 1152, 1280, 1536, 1544
GROUPS = [[0, 1, 2, 3], [4, 5, 6, 7]]


def sl(start, n, step):
    return slice(start, start + (n - 1) * step + 1, step)


class T:
    def __init__(self, t, name):
        self.t = t
        self.b = Buf(name)
        self.name = name

    def __getitem__(self, k):
        return self.t[k]


class KB:
    def __init__(self, nc, st):
        self.nc = nc
        self.st = st
        self.ph = None
        self.P = Prog(nc, st)
        self.P.no_self = {"tensor"}
        self.uid = 0

    def sb(self, name, shape, dt):
        self.uid += 1
        nm = "s%d_%s" % (self.uid, name)
        return T(self.ph.enter_context(self.nc.sbuf_tensor(nm, shape, dt)), name)

    def ps(self, name, shape, dt=F32):
        self.uid += 1
        nm = "p%d_%s" % (self.uid, name)
        return T(self.ph.enter_context(self.nc.psum_tensor(nm, shape, dt)), name)

    def pool(self, kind, name, n, shape, dt):
        f = self.sb if kind == "sb" else self.ps
        return Pool([f("%s%d" % (name, i), shape, dt) for i in range(n)])

    def dram(self, name, shape, dt, kind=None):
        if kind is None:
            h = self.nc.dram_tensor(name, shape, dt)
        else:
            h = self.nc.dram_tensor(name, shape, dt, kind=kind)
        return T(h.ap(), name)

    def mm(self, out, oap, lt, ltap, rt, rtap, start=True, stop=True):
        self.P.op("tensor", lambda e: e.matmul(oap, lhsT=ltap, rhs=rtap, start=start, stop=stop),
                  reads=[lt.b, rt.b], writes=[out.b])

    def tr(self, out, oap, inp, iap, ident, idap):
        self.P.op("tensor", lambda e: e.transpose(oap, iap, idap), reads=[inp.b, ident.b], writes=[out.b])

    def act(self, out, oap, inp, iap, func, bias=None, scale=None, accum=None, part=False):
        reads = [inp.b]
        kw = {}
        if bias is not None:
            reads.append(bias[0].b)
            kw["bias"] = bias[1]
        if scale is not None:
            if isinstance(scale, tuple):
                reads.append(scale[0].b)
                kw["scale"] = scale[1]
            else:
                kw["scale"] = scale
        writes = [out.b]
        if accum is not None:
            writes.append(accum[0].b)
            kw["accum_out"] = accum[1]
        self.P.op("scalar", lambda e: e.activation(out=oap, in_=iap, func=func, **kw), reads=reads,
                  writes=[] if part else writes, partial=writes if part else [])

    def tt(self, eng, out, oap, a, aap, b, bap, op, part=False):
        self.P.op(eng, lambda e: e.tensor_tensor(out=oap, in0=aap, in1=bap, op=op), reads=[a.b, b.b],
                  writes=[] if part else [out.b], partial=[out.b] if part else [])

    def ts(self, eng, out, oap, a, aap, s1, s2, op0, op1=None, accum=None, part=False):
        reads = [a.b]
        def cv(s):
            if isinstance(s, tuple):
                reads.append(s[0].b)
                return s[1]
            return s
        v1, v2 = cv(s1), cv(s2)
        kw = {}
        writes = [out.b]
        if accum is not None:
            writes.append(accum[0].b)
            kw["accum_out"] = accum[1]
        if op1 is not None:
            kw["op1"] = op1
        self.P.op(eng, lambda e: e.tensor_scalar(out=oap, in0=aap, scalar1=v1, scalar2=v2, op0=op0, **kw), reads=reads,
                  writes=[] if part else writes, partial=writes if part else [])

    def stt(self, out, oap, a, aap, scalar, b, bap, op0, op1, part=False):
        reads = [a.b, b.b]
        sv = scalar
        if isinstance(scalar, tuple):
            reads.append(scalar[0].b)
            sv = scalar[1]
        self.P.op("vector", lambda e: e.scalar_tensor_tensor(out=oap, in0=aap, scalar=sv, in1=bap, op0=op0, op1=op1),
                  reads=reads, writes=[] if part else [out.b], partial=[out.b] if part else [])

    def copy(self, eng, out, oap, a, aap, part=False):
        if eng == "scalar":
            f = lambda e: e.activation(out=oap, in_=aap, func=AF.Copy)
        else:
            f = lambda e: e.tensor_copy(out=oap, in_=aap)
        self.P.op(eng, f, reads=[a.b], writes=[] if part else [out.b], partial=[out.b] if part else [])

    def memset(self, eng, out, oap, val, part=False):
        self.P.op(eng, lambda e: e.memset(oap, val), writes=[] if part else [out.b], partial=[out.b] if part else [])

    def recip(self, out, oap, a, aap):
        self.P.op("vector", lambda e: e.reciprocal(out=oap, in_=aap), reads=[a.b], writes=[out.b])

    def dma(self, q, out, oap, inp, iap, key, part=False, extra_reads=(), **kw):
        self.P.dma(q, lambda e: e.dma_start(out=oap, in_=iap, **kw), key, reads=[inp.b] + [x.b for x in extra_reads],
                   writes=[] if part else [out.b], partial=[out.b] if part else [])

    def rstd_from(self, out, oap, inp, iap, n, tmp, tap, eps):
        self.act(tmp, tap, inp, iap, AF.Ln, bias=(eps, eps[:, 0:1]), scale=1.0 / n)
        self.act(out, oap, tmp, tap, AF.Exp, scale=-0.5)


class Pool:
    def __init__(self, tiles):
        self.tiles = tiles
        self.i = 0

    def next(self):
        t = self.tiles[self.i % len(self.tiles)]
        self.i += 1
        return t


def host_constants():
    import ml_dtypes
    bf = ml_dtypes.bfloat16
    c = {}
    c["ident_bf"] = np.eye(128, dtype=np.float32).astype(bf)
    c["ident_f"] = np.eye(128, dtype=np.float32)
    p = np.arange(128)[:, None]
    f = np.arange(128)[None, :]
    mA = np.where(f <= p, 0.0, NEG).astype(np.float32)
    mB = np.where(f >= p, 0.0, NEG).astype(np.float32)
    mA_first = mA.copy(); mA_first[:64, :] = NEG
    mB_last = mB.copy(); mB_last[64:, :] = NEG
    nm = np.zeros((128, 3, 4, 128), np.float32)
    for v, (a, b) in enumerate([(mA, mB), (mA_first, mB), (mA, mB_last)]):
        nm[:, v, 0] = a; nm[:, v, 1] = b; nm[:, v, 2] = a; nm[:, v, 3] = b
    c["negmask"] = nm.reshape(128, 3, 512).astype(bf)
    bo = np.zeros((128, 128), np.float32); bo[:64, :64] = 1; bo[64:, 64:] = 1
    c["blockones"] = bo.astype(bf)
    rm = np.zeros((128, 128), np.float32)
    for o in (0, 64):
        for i in range(8):
            rm[o + 8 + i, o + i] = -1.0
            rm[o + i, o + 8 + i] = 1.0
    c["rotm"] = rm.astype(bf)
    so = np.zeros((128, 2, 128), np.float32); so[:, 0, :64] = 1; so[:, 1, 64:] = 1
    c["sel_ones"] = so.astype(bf)
    c["ones_bf"] = np.ones((128, 128), np.float32).astype(bf)
    c["ones_f"] = np.ones((128, 128), np.float32)
    t = np.arange(128)[:, None]; l = np.arange(128)[None, :]
    c["tri"] = np.stack([(t <= l), (t >= l)], 1).astype(np.float32)
    mf = np.where(t <= l, 0.0, NEG).astype(np.float32); mb = np.where(t >= l, 0.0, NEG).astype(np.float32)
    c["smask"] = np.stack([np.tile(mf, (1, 4)), np.tile(mb, (1, 4))], 1).astype(np.float32)
    c["ustrict"] = (t < l).astype(np.float32)
    half = 8
    inv_freq = (np.float32(500000.0) ** (-np.arange(half, dtype=np.float32) * np.float32(2.0) / np.float32(16))).astype(np.float32)
    ang = (np.arange(S, dtype=np.float32)[None, :] * inv_freq[:, None]).astype(np.float32)
    cos = np.cos(ang.astype(np.float64)).astype(np.float32); sin = np.sin(ang.astype(np.float64)).astype(np.float32)
    cf = np.ones((128, S), np.float32); sf = np.zeros((128, S), np.float32)
    for o in (0, 64):
        cf[o:o + 8] = cos; cf[o + 8:o + 16] = cos
        sf[o:o + 8] = sin; sf[o + 8:o + 16] = sin
    c["cosf"] = cf; c["sinf"] = sf
    c["iota_tok"] = (np.arange(128)[:, None] * 128 + np.arange(128)[None, :]).astype(np.float32)
    tt_ = np.arange(128)[:, None] * 128 + np.arange(128)[None, :]
    c["iota_h2row"] = (((tt_ % 4096) // 512) * 2048 + (tt_ // 4096) * 512 + (tt_ % 512)).astype(np.float32)
    return c


CONST_DT = {"ident_bf": BF16, "negmask": BF16, "blockones": BF16, "rotm": BF16, "sel_ones": BF16, "ones_bf": BF16}
SMALL_CONSTS_UNUSED = ["ident_bf", "ident_f", "negmask", "blockones", "rotm", "sel_ones", "ones_bf", "ones_f", "tri", "smask",
                "ustrict", "iota_tok"]


def load_consts(kb, io, names):
    cs = {}
    for n in names:
        src = io[n]
        shape = list(src.t.shape)
        t = kb.sb("c_" + n, shape, CONST_DT.get(n, F32))
        kb.dma("sync", t, t[:], src, src.t, "c_" + n)
        cs[n] = t
    eps = kb.sb("c_eps", [128, 1], F32)
    kb.memset("vector", eps, eps[:], EPS)
    cs["eps"] = eps
    return cs


def load_w1(kb, io, w1b, c0, ncols, stage_pool, g_attn):
    w1 = io["w1"]
    for kc in range(8):
        stg = stage_pool.next()
        kb.dma("sync", stg, stg[:, 0:ncols], w1, w1.t[kc * 128:(kc + 1) * 128, c0:c0 + ncols], stg.name)
        kb.ts("vector", w1b, w1b[:, kc, 0:ncols], stg, stg[:, 0:ncols], (g_attn, g_attn[:, kc:kc + 1]), None, ALU.mult, part=True)


def window_front(kb, io, cs, w, xb_pool, sq_pool, rstd_pool, psA):
    xT = io["xT"]
    xb = xb_pool.next()
    xbd = io["xb_dram"]
    xbv = xbd.t.rearrange("(k p) t -> p k t", p=128)[:, :, w * WIN:(w + 1) * WIN]
    if io.get("_xb_ready"):
        kb.dma("sync", xb, xb[:], xbd, xbv, xb.name)
    else:
        kb.dma("gpsimd", xb, xb[:], xT, xT.t.rearrange("(k p) t -> p k t", p=128)[:, :, w * WIN:(w + 1) * WIN], xb.name)
        kb.dma("sync", xbd, xbv, xb, xb[:], xb.name, part=True)
    sq = sq_pool.next()
    kb.act(sq, sq[:], xb, xb[:], AF.Square)
    pss = psA.next()
    for kc in range(8):
        kb.mm(pss, pss[:], cs["ones_bf"], cs["ones_bf"][:], sq, sq[:, kc, :], start=(kc == 0), stop=(kc == 7))
    rstd = rstd_pool.next()
    kb.rstd_from(rstd, rstd[:, 0, :], pss, pss[:], DM, rstd, rstd[:, 1, :], cs["eps"])
    return xb, sq, rstd


def attn_inproj(kb, io, hp, qz, kT, vT):
    if True:
      with contextlib.ExitStack() as ph:
        kb.ph = ph
        cs = load_consts(kb, io, ["blockones", "rotm", "ones_bf"])
        g_attn = kb.sb("g_attn", [128, 8], F32)
        kb.dma("sync", g_attn, g_attn[:], io["g_attn"], io["g_attn"].t, "g_attn")
        gqk = kb.sb("gqk", [128, 2], F32)
        kb.dma("sync", gqk, gqk[:], io["gqk"], io["gqk"].t, "gqk")
        w1b = kb.sb("w1b", [128, 8, 384], BF16)
        stage = kb.pool("sb", "w1stg", 2, [128, 128], F32)
        for j, c0 in enumerate((C_Q + hp * 128, C_K + hp * 128, C_V + hp * 128)):
            w1 = io["w1"]
            for kc in range(8):
                stg = stage.next()
                kb.dma("sync", stg, stg[:], w1, w1.t[kc * 128:(kc + 1) * 128, c0:c0 + 128], stg.name)
                kb.ts("vector", w1b, w1b[:, kc, j * 128:(j + 1) * 128], stg, stg[:], (g_attn, g_attn[:, kc:kc + 1]), None,
                      ALU.mult, part=True)
        for t in (kT, vT):
            kb.memset("gpsimd", t, t[:, 0:PAD], 0.0, part=True)
            kb.memset("gpsimd", t, t[:, PAD + S:], 0.0, part=True)
        kb.memset("gpsimd", qz, qz[64:128, 0, :], 0.0, part=True)
        kb.memset("gpsimd", qz, qz[0:64, 1, :], 0.0, part=True)
        xb_pool = kb.pool("sb", "xb", 2, [128, 8, WIN], BF16)
        sq_pool = kb.pool("sb", "sq", 1, [128, 8, WIN], BF16)
        rstd_pool = kb.pool("sb", "rstd", 2, [128, 2, WIN], F32)
        cos_pool = kb.pool("sb", "cosw", 2, [128, 2, WIN], F32)
        raw_pool = kb.pool("sb", "raw", 2, [128, WIN], F32)
        sqh_pool = kb.pool("sb", "sqh", 2, [128, WIN], BF16)
        rsh_pool = kb.pool("sb", "rsh", 2, [128, 2, WIN], F32)
        qn_pool = kb.pool("sb", "qn", 2, [128, WIN], BF16)
        t12_pool = kb.pool("sb", "t12", 2, [128, 2, WIN], F32)
        psA = kb.pool("ps", "psA", 2, [128, WIN], F32)
        psB = kb.pool("ps", "psB", 2, [128, WIN], F32)
        psC = kb.pool("ps", "psC", 2, [128, WIN], F32)
        for w in range(NW):
            xb, sq, rstd = window_front(kb, io, cs, w, xb_pool, sq_pool, rstd_pool, psA)
            cw = cos_pool.next()
            kb.dma("sync", cw, cw[:, 0, :], io["cosf"], io["cosf"].t[:, w * WIN:(w + 1) * WIN], cw.name, part=True)
            kb.dma("sync", cw, cw[:, 1, :], io["sinf"], io["sinf"].t[:, w * WIN:(w + 1) * WIN], cw.name, part=True)
            for j, dst in enumerate((qz, kT, vT)):
                ps = psB.next()
                for kc in range(8):
                    kb.mm(ps, ps[:], w1b, w1b[:, kc, j * 128:(j + 1) * 128], xb, xb[:, kc, :], start=(kc == 0), stop=(kc == 7))
                off = 0 if j == 0 else PAD
                dap = None if j == 0 else dst[:, off + w * WIN: off + (w + 1) * WIN]
                if j == 2:
                    kb.tt("vector", dst, dap, ps, ps[:], rstd, rstd[:, 0, :], ALU.mult, part=True)
                    continue
                raw = raw_pool.next()
                kb.tt("vector", raw, raw[:], ps, ps[:], rstd, rstd[:, 0, :], ALU.mult)
                sqh = sqh_pool.next()
                kb.act(sqh, sqh[:], raw, raw[:], AF.Square)
                ps2 = psC.next()
                kb.mm(ps2, ps2[:], cs["blockones"], cs["blockones"][:], sqh, sqh[:])
                rsh = rsh_pool.next()
                kb.rstd_from(rsh, rsh[:, 0, :], ps2, ps2[:], 64, rsh, rsh[:, 1, :], cs["eps"])
                qn = qn_pool.next()
                kb.stt(qn, qn[:], raw, raw[:], (gqk, gqk[:, j:j + 1]), rsh, rsh[:, 0, :], ALU.mult, ALU.mult)
                ps3 = psC.next()
                kb.mm(ps3, ps3[:], cs["rotm"], cs["rotm"][:], qn, qn[:])
                t12 = t12_pool.next()
                kb.tt("vector", t12, t12[:, 0, :], qn, qn[:], cw, cw[:, 0, :], ALU.mult, part=True)
                kb.tt("vector", t12, t12[:, 1, :], ps3, ps3[:], cw, cw[:, 1, :], ALU.mult, part=True)
                if j == 0:
                    for h in range(2):
                        hs = slice(h * 64, (h + 1) * 64)
                        kb.tt("vector", qz, qz[hs, h, w * WIN:(w + 1) * WIN], t12, t12[hs, 0, :], t12, t12[hs, 1, :], ALU.add, part=True)
                else:
                    kb.tt("vector", dst, dap, t12, t12[:, 0, :], t12, t12[:, 1, :], ALU.add, part=True)
        if io.get("dbg_qk") is not None:
            kb.dma("sync", io["dbg_qk"], io["dbg_qk"].t[0, 0:64], qz, qz[0:64, 0, :], "dbgq", part=True)
            kb.dma("sync", io["dbg_qk"], io["dbg_qk"].t[0, 64:128], qz, qz[64:128, 1, :], "dbgq", part=True)
            kb.dma("sync", io["dbg_qk"], io["dbg_qk"].t[1], kT, kT[:, PAD:PAD + S], "dbgk", part=True)
            kb.dma("sync", io["dbg_qk"], io["dbg_qk"].t[2], vT, vT[:, PAD:PAD + S], "dbgv", part=True)
        kb.P.barrier()
        stats0 = kb.P.emit_phase()
        kb.ph = None
    return stats0


def attn_core(kb, io, hp, qz, kT, vT):
    import os
    if True:
      with contextlib.ExitStack() as ph:
        kb.ph = ph
        cs = load_consts(kb, io, ["ident_bf", "negmask", "sel_ones"])
        vpad_pool = kb.pool("sb", "vpad", int(os.environ.get("P1_NB", 3)), [128, 2, 2, 128], BF16)
        for t in vpad_pool.tiles:
            kb.memset("vector", t, t[:], 0.0)
        pt_pool = kb.pool("sb", "pt", int(os.environ.get("P1_NB", 3)), [128, 512], BF16)
        accden = kb.sb("accden", [128, 2, 2048], F32)
        rden = kb.sb("rden", [128, 2048], F32)
        attn_o = kb.pool("sb", "attn_o", 2, [128, 2048], BF16)
        psT = kb.pool("ps", "psT", int(os.environ.get("P1_NT", 2)), [128, 4, 128], F32)
        psS = kb.pool("ps", "psS", int(os.environ.get("P1_NS", 2)), [128, 512], F32)
        psO = kb.pool("ps", "psO", int(os.environ.get("P1_NO", 2)), [128, 4, 128], F32)
        mix = io["mixA_loc"]

        def stage1(u):
            d, rho, j, jj = u
            n = S // d
            nb = n // 128
            var = 1 if j == 0 else (2 if j == nb - 1 else 0)
            pT = psT.next()
            kcols = []
            for c in range(2):
                k0 = PAD + rho + d * (128 * j - 64 + 128 * c)
                kcols.append(k0)
                kb.mm(pT, pT[:, c, :], vT, vT[:, sl(k0, 128, d)], cs["ident_bf"], cs["ident_bf"][:])
            vp = vpad_pool.next()
            if MODE >= 1:
                kb.copy("vector", vp, vp[:, :, 0, 0:64], pT, pT[:, 0:2, 0:64], part=True)
            if MODE >= 2:
                kb.copy(os.environ.get("P1_CE", "vector"), vp, vp[:, :, 1, 64:128], pT, pT[:, 0:2, 64:128], part=True)
            sS = psS.next()
            kb.mm(sS, sS[:], cs["ident_bf"], cs["ident_bf"][:], cs["negmask"], cs["negmask"][:, var, :], start=True, stop=False)
            q0 = rho + d * 128 * j
            for h in range(2):
                for c in range(2):
                    sub = 2 * h + c
                    kb.mm(sS, sS[:, sub * 128:(sub + 1) * 128], kT, kT[:, sl(kcols[c], 128, d)],
                          qz, qz[:, h, sl(q0, 128, d)], start=False, stop=True)
            pt = pt_pool.next()
            if MODE >= 3:
                kb.act(pt, pt[:], sS, sS[:], AF.Exp, scale=0.125)
            return (u, vp, pt)

        def stage2(st, first_in_sb):
            u, vp, pt = st
            d, rho, j, jj = u
            if MODE < 4:
                return
            pO = psO.next()
            SUB = os.environ.get("P1_SUB", "")
            k = 0
            for h in range(2 if SUB != "den" else 0):
                for c in range(2):
                    sub = 2 * h + c
                    kb.mm(pO, pO[:, 0, :], vp, vp[:, c, h, :], pt, pt[:, sub * 128:(sub + 1) * 128], start=(k == 0), stop=(k == 3))
                    k += 1
            k = 0
            for h in range(2 if SUB != "pv" else 0):
                for c in range(2):
                    sub = 2 * h + c
                    kb.mm(pO, pO[:, 1, :], cs["sel_ones"], cs["sel_ones"][:, h, :], pt, pt[:, sub * 128:(sub + 1) * 128],
                          start=(k == 0), stop=(k == 3))
                    k += 1
            c0 = rho + d * 128 * jj
            dap = accden[:, :, sl(c0, 128, d)]
            if MODE < 5:
                return
            if first_in_sb:
                kb.copy("vector", accden, dap, pO, pO[:, 0:2, :])
            else:
                kb.tt("vector", accden, dap, accden, dap, pO, pO[:, 0:2, :], ALU.add)

        import os
        MODE = int(os.environ.get("P1_MODE", "9"))
        for sbk in range(int(os.environ.get("P1_NSB", S // 2048))):
            units = []
            for d in (1, 4, 16):
                per = 16 // d
                for rho in range(d):
                    for jj in range(per):
                        units.append((d, rho, sbk * per + jj, jj))
            pend = None
            units = units[:int(os.environ.get("P1_NU", len(units)))]
            for ui, u in enumerate(units):
                st1 = stage1(u)
                if os.environ.get("P1_NOPIPE"):
                    stage2(st1, u[0] == 1)
                    continue
                if pend is not None:
                    stage2(pend[0], pend[1])
                pend = (st1, u[0] == 1)
            if pend is not None:
                stage2(pend[0], pend[1])
            kb.recip(rden, rden[:], accden, accden[:, 1, :])
            ao = attn_o.next()
            kb.tt("gpsimd", ao, ao[:], accden, accden[:, 0, :], rden, rden[:], ALU.mult)
            kb.dma("sync", mix, mix.t.rearrange("(g c) t -> c g t", c=256)[hp * 128:(hp + 1) * 128, sbk * 4:(sbk + 1) * 4, :],
                   ao, ao[:].rearrange("p (g t) -> p g t", t=512), ao.name, part=True)
        kb.P.barrier()
        stats = kb.P.emit_phase()
        kb.ph = None
    return stats


def phase1_attn(kb, io, hp):
    import os
    with contextlib.ExitStack() as outer:
        kb.ph = outer
        qz = kb.sb("qz", [128, 2, S], BF16)
        kT = kb.sb("kT", [128, S + 2 * PAD], BF16)
        vT = kb.sb("vT", [128, S + 2 * PAD], BF16)
        st = attn_inproj(kb, io, hp, qz, kT, vT)
        if os.environ.get("P1_STOP"):
            return st
        st = attn_core(kb, io, hp, qz, kT, vT)
    return st


def phase1_ssd_in(kb, io):
    with contextlib.ExitStack() as ph:
        kb.ph = ph
        cs = load_consts(kb, io, ["ones_bf"])
        g_attn = kb.sb("g_attn", [128, 8], F32)
        kb.dma("sync", g_attn, g_attn[:], io["g_attn"], io["g_attn"].t, "g_attn")
        ncol = NC1 - C_XS
        w1b = kb.sb("w1b", [128, 8, ncol], BF16)
        stage = kb.pool("sb", "w1stg", 2, [128, ncol], F32)
        w1 = io["w1"]
        for kc in range(8):
            stg = stage.next()
            kb.dma("sync", stg, stg[:], w1, w1.t[kc * 128:(kc + 1) * 128, C_XS:NC1], stg.name)
            kb.ts("vector", w1b, w1b[:, kc, :], stg, stg[:], (g_attn, g_attn[:, kc:kc + 1]), None, ALU.mult, part=True)
        cw = kb.sb("convw", [128, 4, 6], F32)
        kb.dma("sync", cw, cw[:], io["convw"], io["convw"].t, "convw")
        dtc = kb.sb("dtc", [8, 2], F32)
        kb.dma("sync", dtc, dtc[:], io["dtc"], io["dtc"].t, "dtc")
        negA = kb.sb("negA", [8, 1], F32)
        kb.act(negA, negA[:], dtc, dtc[:, 1:2], AF.Exp)
        kb.ts("vector", negA, negA[:], negA, negA[:], -1.0, None, ALU.mult)
        one8 = kb.sb("one8", [8, 1], F32)
        kb.memset("vector", one8, one8[:], 1.0)
        xb_pool = kb.pool("sb", "xb", 2, [128, 8, WIN], BF16)
        sq_pool = kb.pool("sb", "sq", 1, [128, 8, WIN], BF16)
        rstd_pool = kb.pool("sb", "rstd", 2, [128, 2, WIN], F32)
        pre_pool = kb.pool("sb", "pre", 2, [128, 4, WIN], F32)
        cacc_pool = kb.pool("sb", "cacc", 2, [128, 4, WIN], F32)
        post_pool = kb.pool("sb", "post", 2, [128, 4, WIN], BF16)
        zs_pool = kb.pool("sb", "zsw", 2, [128, 2, WIN], BF16)
        dt_pool = kb.pool("sb", "dtw", 2, [8, 4, WIN], F32)
        psA = kb.pool("ps", "psA", 2, [128, WIN], F32)
        psB = kb.pool("ps", "psB", 3, [128, WIN], F32)
        xT = io["xT"]
        nwin = 33
        for w in range(nwin):
            t0 = 508 * w - 2 if w < nwin - 1 else S - 510
            lo, hi = max(t0, 0), min(t0 + WIN, S)
            xb = xb_pool.next()
            xbd = io["xb_dram"]
            if lo > t0 or hi < t0 + WIN:
                kb.memset("vector", xb, xb[:], 0.0)
                kb.dma("gpsimd", xb, xb[:, :, lo - t0:hi - t0], xbd, xbd.t.rearrange("(k p) t -> p k t", p=128)[:, :, lo:hi], xb.name)
            else:
                kb.dma("gpsimd", xb, xb[:], xbd, xbd.t.rearrange("(k p) t -> p k t", p=128)[:, :, lo:hi], xb.name)
            sq = sq_pool.next()
            kb.act(sq, sq[:], xb, xb[:], AF.Square)
            pss = psA.next()
            for kc in range(8):
                kb.mm(pss, pss[:], cs["ones_bf"], cs["ones_bf"][:], sq, sq[:, kc, :], start=(kc == 0), stop=(kc == 7))
            rstd = rstd_pool.next()
            kb.rstd_from(rstd, rstd[:, 0, :], pss, pss[:], DM, rstd, rstd[:, 1, :], cs["eps"])
            pre = pre_pool.next()
            for ci in range(4):
                ps = psB.next()
                for kc in range(8):
                    kb.mm(ps, ps[:], w1b, w1b[:, kc, ci * 128:(ci + 1) * 128], xb, xb[:, kc, :], start=(kc == 0), stop=(kc == 7))
                kb.tt("vector", pre, pre[:, ci, :], ps, ps[:], rstd, rstd[:, 0, :], ALU.mult, part=True)
            cacc = cacc_pool.next()
            post = post_pool.next()
            NV = WIN - 4
            for ci in range(4):
                kb.act(cacc, cacc[:, ci, 0:NV], pre, pre[:, ci, 0:NV], AF.Copy, scale=(cw, cw[:, ci, 0:1]), part=True)
                for j in range(1, 5):
                    kb.stt(cacc, cacc[:, ci, 0:NV], pre, pre[:, ci, j:j + NV], (cw, cw[:, ci, j:j + 1]), cacc, cacc[:, ci, 0:NV],
                           ALU.mult, ALU.add, part=True)
            for ci in range(4):
                kb.act(post, post[:, ci, 0:NV], cacc, cacc[:, ci, 0:NV], AF.Silu, bias=(cw, cw[:, ci, 5:6]), part=True)
            vlo, vhi = max(t0 + 2, 0), min(t0 + 2 + NV, S)
            o0 = vlo - (t0 + 2)
            kb.dma("sync", io["post"], io["post"].t.rearrange("(c p) t -> p c t", p=128)[:, :, vlo:vhi], post, post[:, :, o0:o0 + (vhi - vlo)],
                   post.name, part=True)
            zs = zs_pool.next()
            for zi in range(2):
                ps = psB.next()
                c0 = (C_Z - C_XS) + zi * 128
                for kc in range(8):
                    kb.mm(ps, ps[:], w1b, w1b[:, kc, c0:c0 + 128], xb, xb[:, kc, :], start=(kc == 0), stop=(kc == 7))
                kb.tt("vector", pre, pre[:, zi, :], ps, ps[:], rstd, rstd[:, 0, :], ALU.mult, part=True)
                kb.act(zs, zs[:, zi, :], pre, pre[:, zi, :], AF.Silu, part=True)
            kb.dma("sync", io["zs"], io["zs"].t.rearrange("(c p) t -> p c t", p=128)[:, :, vlo:vhi], zs, zs[:, :, o0 + 2:o0 + 2 + (vhi - vlo)],
                   zs.name, part=True)
            ps = psB.next()
            c0 = C_DT - C_XS
            for kc in range(8):
                kb.mm(ps, ps[0:8, :], w1b, w1b[:, kc, c0:c0 + 8], xb, xb[:, kc, :], start=(kc == 0), stop=(kc == 7))
            dtw = dt_pool.next()
            kb.tt("vector", dtw, dtw[:, 0, :], ps, ps[0:8, :], rstd, rstd[0:8, 0, :], ALU.mult, part=True)
            kb.act(dtw, dtw[:, 1, :], dtw, dtw[:, 0, :], AF.Exp, bias=(dtc, dtc[:, 0:1]), part=True)
            kb.act(dtw, dtw[:, 2, :], dtw, dtw[:, 1, :], AF.Ln, bias=(one8, one8[:, 0:1]), part=True)
            kb.ts("vector", dtw, dtw[:, 3, :], dtw, dtw[:, 2, :], (negA, negA[:, 0:1]), None, ALU.mult, part=True)
            kb.dma("sync", io["dta"], io["dta"].t[0:8, vlo:vhi], dtw, dtw[:, 2, o0 + 2:o0 + 2 + (vhi - vlo)], dtw.name, part=True)
            kb.dma("sync", io["dta"], io["dta"].t[8:16, vlo:vhi], dtw, dtw[:, 3, o0 + 2:o0 + 2 + (vhi - vlo)], dtw.name, part=True)
        kb.P.barrier()
        st = kb.P.emit_phase()
        kb.ph = None
    return st


def phase1_ssd_scan(kb, io, dirn, nchunks=NCH):
    with contextlib.ExitStack() as ph:
        kb.ph = ph
        if dirn == 0 and "mixA_all" in io:
            collective(kb, "AllGather", ALU.bypass, io["mixA_loc"], io["mixA_all"], "ccA", 8)
        cs = load_consts(kb, io, ["ident_bf", "ident_f", "ones_f", "tri", "smask"])
        Dbc = kb.sb("Dbc", [128, 256], F32)
        kb.dma("sync", Dbc, Dbc[:], io["Dbc"], io["Dbc"].t, "Dbc")
        H = kb.sb("H", [128, 256], F32)
        Hbf = kb.sb("Hbf", [128, 256], BF16)
        kb.memset("vector", H, H[:], 0.0)
        kb.memset("vector", Hbf, Hbf[:], 0.0)
        inT = kb.pool("sb", "inT", 2, [128, 4, 128], BF16)
        dtaT = kb.pool("sb", "dtaT", 2, [16, 128], F32)
        xsb = kb.pool("sb", "xsb", 2, [128, 384], BF16)
        dta = kb.pool("sb", "dta", 2, [128, 16], F32)
        abc = kb.pool("sb", "abc", 2, [128, 4, 128], F32)
        col = kb.pool("sb", "col", 2, [128, 40], F32)
        LT = kb.pool("sb", "LT", 2, [128, 4, 128], F32)
        MT = kb.pool("sb", "MT", 2, [128, 4, 128], BF16)
        Ysb = kb.pool("sb", "Ysb", 2, [128, 256], F32)
        yout = kb.pool("sb", "yout", 2, [128, 256], F32)
        xsw = kb.pool("sb", "xsw", 2, [128, 256], BF16)
        yprev = kb.pool("sb", "yprev", 2, [128, 256], F32)
        ysum = kb.pool("sb", "ysum", 2, [128, 2, 256], F32)
        ybf = kb.pool("sb", "ybf", 2, [128, 256], BF16)
        zsT = kb.pool("sb", "zsT", 2, [128, 2, 128], BF16)
        ygT = kb.pool("sb", "ygT", 2, [128, 2, 128], BF16)
        pX = kb.ps("pX", [128, 512], F32)
        pD = kb.ps("pD", [128, 512], F32)
        pCS = kb.ps("pCS", [128, 512], F32)
        pG = kb.ps("pG", [128, 512], F32)
        pY = kb.ps("pY", [128, 512], F32)
        pYo = kb.ps("pYo", [128, 512], F32)
        pST = kb.ps("pST", [128, 512], F32)
        pYT = kb.ps("pYT", [128, 512], F32)
        post_v = io["post"].t.rearrange("(c p) t -> p c t", p=128)
        zs_v = io["zs"].t.rearrange("(c p) t -> p c t", p=128)
        order = range(nchunks) if dirn == 0 else range(NCH - 1, NCH - 1 - nchunks, -1)
        def stageA(c):
            tk = slice(c * 128, (c + 1) * 128)
            it = inT.next()
            kb.dma("sync", it, it[:], io["post"], post_v[:, :, tk], it.name)
            dT = dtaT.next()
            kb.dma("sync", dT, dT[:], io["dta"], io["dta"].t[:, tk], dT.name)
            for i in range(3):
                kb.mm(pX, pX[:, i * 128:(i + 1) * 128], it, it[:, i, :], cs["ident_bf"], cs["ident_bf"][:])
            xs = xsb.next()
            kb.copy("vector", xs, xs[:], pX, pX[:, 0:384])
            kb.mm(pD, pD[:, 0:16], dT, dT[:, :], cs["ident_f"], cs["ident_f"][0:16, 0:16])
            dt = dta.next()
            kb.copy("vector", dt, dt[:], pD, pD[:, 0:16])
            dtc = dt[:, 4 * dirn:4 * dirn + 4]
            ac = dt[:, 8 + 4 * dirn:8 + 4 * dirn + 4]
            ab = abc.next()
            for h in range(4):
                kb.act(ab, ab[:, h, :], cs["ones_f"], cs["ones_f"][:], AF.Copy, scale=(dt, dt[:, 8 + 4 * dirn + h:8 + 4 * dirn + h + 1]), part=True)
            kb.mm(pCS, pCS[:], cs["ident_f"], cs["ident_f"][:], cs["smask"], cs["smask"][:, dirn, :], start=True, stop=False)
            for h in range(4):
                kb.mm(pCS, pCS[:, h * 128:(h + 1) * 128], ab, ab[:, h, :], cs["tri"], cs["tri"][:, dirn, :], start=False, stop=True)
            kb.mm(pD, pD[:, 16:20], cs["tri"], cs["tri"][:, dirn, :], dt, ac)
            kb.mm(pD, pD[:, 20:24], cs["ones_f"], cs["ones_f"][:], dt, ac)
            cl = col.next()
            kb.copy("vector", cl, cl[:, 0:8], pD, pD[:, 16:24], part=True)
            kb.ts("vector", cl, cl[:, 8:12], cl, cl[:, 0:4], -1.0, None, ALU.mult, part=True)
            kb.act(cl, cl[:, 12:16], cl, cl[:, 0:4], AF.Exp, part=True)
            kb.tt("vector", cl, cl[:, 16:20], cl, cl[:, 4:8], cl, cl[:, 0:4], ALU.subtract, part=True)
            kb.act(cl, cl[:, 20:24], cl, cl[:, 16:20], AF.Exp, part=True)
            kb.tt("vector", cl, cl[:, 24:28], cl, cl[:, 20:24], dt, dtc, ALU.mult, part=True)
            kb.act(cl, cl[:, 28:32], cl, cl[:, 4:8], AF.Exp, part=True)
            lt = LT.next()
            for h in range(4):
                kb.act(lt, lt[:, h, :], pCS, pCS[:, h * 128:(h + 1) * 128], AF.Exp, bias=(cl, cl[:, 8 + h:9 + h]), part=True)
            kb.mm(pG, pG[:, 0:128], it, it[:, 2, :], it, it[:, 3, :])
            mt = MT.next()
            for h in range(4):
                kb.stt(mt, mt[:, h, :], pG, pG[:, 0:128], (dt, dt[:, 4 * dirn + h:4 * dirn + h + 1]), lt, lt[:, h, :], ALU.mult, ALU.mult, part=True)
            return dict(c=c, tk=tk, it=it, xs=xs, dt=dt, cl=cl, mt=mt)

        def stageB(ctx):
            c, tk, it, xs, dt, cl, mt = ctx['c'], ctx['tk'], ctx['it'], ctx['xs'], ctx['dt'], ctx['cl'], ctx['mt']
            for h in range(4):
                kb.mm(pY, pY[:, h * 64:(h + 1) * 64], mt, mt[:, h, :], xs, xs[:, h * 64:(h + 1) * 64])
            kb.mm(pYo, pYo[:, 0:256], it, it[:, 3, :], Hbf, Hbf[:])
            ysb = Ysb.next()
            kb.copy("vector", ysb, ysb[:], pY, pY[:, 0:256])
            yo = yout.next()
            for h in range(4):
                hs = slice(h * 64, (h + 1) * 64)
                kb.stt(yo, yo[:, hs], pYo, pYo[:, hs], (cl, cl[:, 12 + h:13 + h]), ysb, ysb[:, hs], ALU.mult, ALU.add, part=True)
            xw = xsw.next()
            for h in range(4):
                hs = slice(h * 64, (h + 1) * 64)
                kb.act(xw, xw[:, hs], xs, xs[:, hs], AF.Copy, scale=(cl, cl[:, 24 + h:25 + h]), part=True)
            kb.mm(pST, pST[:, 0:256], xs, xs[:, 256:384], xw, xw[:])
            for h in range(4):
                hs = slice(h * 64, (h + 1) * 64)
                kb.stt(H, H[:, hs], H, H[:, hs], (cl, cl[:, 28 + h:29 + h]), pST, pST[:, hs], ALU.mult, ALU.add)
            kb.copy("scalar", Hbf, Hbf[:], H, H[:])
            if dirn == 0:
                kb.dma("sync", io["yf"], io["yf"].t[tk, :], yo, yo[:], yo.name, part=True)
            else:
                yp = yprev.next()
                kb.dma("sync", yp, yp[:], io["yf"], io["yf"].t[tk, :], yp.name)
                zt = zsT.next()
                kb.dma("sync", zt, zt[:], io["zs"], zs_v[:, :, tk], zt.name)
                ys = ysum.next()
                kb.tt("vector", ys, ys[:, 0, :], xs, xs[:, 0:256], Dbc, Dbc[:], ALU.mult, part=True)
                kb.tt("vector", ys, ys[:, 1, :], yo, yo[:], yp, yp[:], ALU.add, part=True)
                yb = ybf.next()
                kb.tt("vector", yb, yb[:], ys, ys[:, 0, :], ys, ys[:, 1, :], ALU.add)
                for i in range(2):
                    kb.mm(pYT, pYT[:, i * 128:(i + 1) * 128], yb, yb[:, i * 128:(i + 1) * 128], cs["ident_bf"], cs["ident_bf"][:])
                yg = ygT.next()
                kb.tt("vector", yg, yg[:], pYT, pYT[:, 0:256].rearrange("p (c t) -> p c t", c=2), zt, zt[:], ALU.mult)
                mv = io["mixS_loc"].t.rearrange("(g c) t -> c g t", c=256)
                for i in range(2):
                    kb.dma("sync", io["mixS_loc"], mv[i * 128:(i + 1) * 128, c // 4, (c % 4) * 128:(c % 4 + 1) * 128], yg, yg[:, i, :],
                           yg.name, part=True)
        pend = None
        for c in order:
            ctx = stageA(c)
            if pend is not None:
                stageB(pend)
            pend = ctx
        stageB(pend)
        kb.P.barrier()
        st = kb.P.emit_phase()
        kb.ph = None
    return st


def router_setup(kb, io, L):
    r = {}
    r["gbc"] = kb.sb("gffn_bc", [128, DM], F32)
    kb.dma("sync", r["gbc"], r["gbc"][:], io["g_ffn_bc%d" % L], io["g_ffn_bc%d" % L].t, "gffn_bc")
    gcol = kb.sb("gffn_col", [128, 8], F32)
    kb.dma("sync", gcol, gcol[:], io["g_ffn_col%d" % L], io["g_ffn_col%d" % L].t, "gffn_col")
    wr = kb.sb("wr", [128, 8, NE], F32)
    kb.dma("sync", wr, wr[:], io["wr%d" % L], io["wr%d" % L].t.rearrange("(k p) e -> p k e", p=128), "wr")
    r["wrg"] = kb.sb("wrg", [128, 8, NE], F32)
    for kc in range(8):
        kb.ts("vector", r["wrg"], r["wrg"][:, kc, :], wr, wr[:, kc, :], (gcol, gcol[:, kc:kc + 1]), None, ALU.mult, part=True)
    r["affT"] = kb.sb("affT", [NE, 4096], F32)
    r["sqj"] = kb.pool("sb", "sqj", 2, [128, DM], BF16)
    r["st"] = kb.pool("sb", "rst", 2, [128, 8], F32)
    r["h2"] = kb.pool("sb", "h2t", 2, [128, DM], BF16)
    r["xT"] = kb.pool("sb", "x1T", 2, [128, 8, 128], F32)
    r["lg"] = kb.pool("sb", "lg", 2, [128, 3, NE], F32)
    r["pXT"] = kb.pool("ps", "pXT", 2, [128, 4, 128], F32)
    r["pL"] = kb.ps("pL", [128, 512], F32)
    return r


def router_tile(kb, io, cs, r, x1, i):
    st = r["st"].next()
    sqj = r["sqj"].next()
    kb.act(sqj, sqj[:], x1, x1[:], AF.Square, accum=(st, st[:, 0:1]))
    kb.rstd_from(st, st[:, 2:3], st, st[:, 0:1], DM, st, st[:, 1:2], cs["eps"])
    h2 = r["h2"].next()
    kb.stt(h2, h2[:], x1, x1[:], (st, st[:, 2:3]), r["gbc"], r["gbc"][:], ALU.mult, ALU.mult)
    kb.dma("sync", io["h2_loc"], io["h2_loc"].t[i * 128:(i + 1) * 128, :], h2, h2[:], h2.name, part=True)
    xT = r["xT"].next()
    for half in range(2):
        pX = r["pXT"].next()
        for k4 in range(4):
            kc = half * 4 + k4
            kb.mm(pX, pX[:, k4, :], x1, x1[:, kc * 128:(kc + 1) * 128], cs["ident_f"], cs["ident_f"][:])
        kb.copy("vector", xT, xT[:, half * 4:(half + 1) * 4, :], pX, pX[:], part=True)
    pL = r["pL"]
    for kc in range(8):
        kb.mm(pL, pL[:, 0:NE], xT, xT[:, kc, :], r["wrg"], r["wrg"][:, kc, :], start=(kc == 0), stop=(kc == 7))
    lg = r["lg"].next()
    kb.ts("vector", lg, lg[:, 0, :], pL, pL[:, 0:NE], (st, st[:, 2:3]), None, ALU.mult, part=True)
    kb.P.op("vector", lambda e: e.tensor_reduce(out=st[:, 3:4], in_=lg[:, 0, :], axis=AX.X, op=ALU.max), reads=[lg.b], partial=[st.b])
    kb.ts("vector", st, st[:, 4:5], st, st[:, 3:4], -1.0, None, ALU.mult, part=True)
    kb.act(lg, lg[:, 1, :], lg, lg[:, 0, :], AF.Exp, bias=(st, st[:, 4:5]), accum=(st, st[:, 5:6]), part=True)
    kb.P.op("vector", lambda e: e.reciprocal(out=st[:, 6:7], in_=st[:, 5:6]), reads=[st.b], partial=[st.b])
    kb.ts("vector", lg, lg[:, 2, :], lg, lg[:, 1, :], (st, st[:, 6:7]), None, ALU.mult, part=True)
    kb.mm(pL, pL[0:NE, 128:256], lg, lg[:, 2, :], cs["ident_f"], cs["ident_f"][:])
    kb.copy("vector", r["affT"], r["affT"][:, i * 128:(i + 1) * 128], pL, pL[0:NE, 128:256], part=True)


def phase2(kb, io, ntiles=32):
    with contextlib.ExitStack() as ph:
        kb.ph = ph
        cs = load_consts(kb, io, ["ident_f", "ones_bf"])
        r = router_setup(kb, io, 0)
        gwo = kb.sb("gwo", [128, 16], F32)
        kb.dma("sync", gwo, gwo[:], io["g_wo"], io["g_wo"].t, "gwo")
        wob = kb.sb("wob", [128, 16, DM], BF16)
        stage = kb.pool("sb", "wostg", 2, [128, DM], F32)
        for ck in range(16):
            stg = stage.next()
            kb.dma("sync", stg, stg[:], io["wo"], io["wo"].t[ck * 128:(ck + 1) * 128, :], stg.name)
            kb.ts("vector", wob, wob[:, ck, :], stg, stg[:], (gwo, gwo[:, ck:ck + 1]), None, ALU.mult, part=True)
        midx = kb.sb("mixidx", [128, 8, 16], I32)
        kb.dma("sync", midx, midx[:], io["mixidx"], io["mixidx"].t, "mixidx")
        mixT = kb.pool("sb", "mixT", 2, [128, 16, WIN], BF16)
        ysq = kb.pool("sb", "ysq", 2, [128, 8, 128], BF16)
        xt = kb.pool("sb", "xt", 2, [128, DM], F32)
        x1p = kb.pool("sb", "x1", 2, [128, DM], F32)
        rsy = kb.pool("sb", "rsy", 2, [128, 4], F32)
        pA = kb.ps("pA", [128, 2, 512], F32)
        pS = kb.ps("pS", [128, 2, 512], F32)
        pq = kb.ps("pq", [128, 512], F32)
        ssd_ck = [q * 4 + j for q in range(4) for j in (2, 3)]
        att_ck = [q * 4 + j for q in range(4) for j in (0, 1)]
        for w in range((ntiles + 3) // 4):
            mt = mixT.next()
            for ck in range(16):
                src = io["mixA_all"] if ck % 4 < 2 else io["mixS_all"]
                kb.P.dma("gpsimd", (lambda e, mt=mt, ck=ck, w=w, src=src: e.indirect_dma_start(
                    out=mt[:, ck, :], out_offset=None, in_=src.t,
                    in_offset=bass.IndirectOffsetOnAxis(ap=midx[:, w, ck:ck + 1], axis=0))),
                    mt.name, reads=[src.b, midx.b], partial=[mt.b])
            for j in range(min(4, ntiles - 4 * w)):
                i = 4 * w + j
                tk = slice(j * 128, (j + 1) * 128)
                ys = ysq.next()
                for n, ck in enumerate(ssd_ck):
                    kb.act(ys, ys[:, n, :], mt, mt[:, ck, tk], AF.Square, part=True)
                for n in range(8):
                    kb.mm(pq, pq[:, 0:1], ys, ys[:, n, :], cs["ones_bf"], cs["ones_bf"][:, 0:1], start=(n == 0), stop=(n == 7))
                rs = rsy.next()
                kb.rstd_from(rs, rs[:, 1:2], pq, pq[:, 0:1], 1024, rs, rs[:, 0:1], cs["eps"])
                for half in range(2):
                    for n, ck in enumerate(att_ck):
                        kb.mm(pA, pA[:, half, :], mt, mt[:, ck, tk], wob, wob[:, ck, half * 512:(half + 1) * 512], start=(n == 0), stop=(n == 7))
                    for n, ck in enumerate(ssd_ck):
                        kb.mm(pS, pS[:, half, :], mt, mt[:, ck, tk], wob, wob[:, ck, half * 512:(half + 1) * 512], start=(n == 0), stop=(n == 7))
                x = xt.next()
                kb.dma("sync", x, x[:], io["x_tok"], io["x_tok"].t[i * 128:(i + 1) * 128, :], x.name)
                x1 = x1p.next()
                kb.stt(x1, x1[:], pS, pS[:].rearrange("p a b -> p (a b)"), (rs, rs[:, 1:2]), x, x[:], ALU.mult, ALU.add)
                kb.tt("vector", x1, x1[:], x1, x1[:], pA, pA[:].rearrange("p a b -> p (a b)"), ALU.add)
                kb.dma("sync", io["x1_loc"], io["x1_loc"].t[i * 128:(i + 1) * 128, :], x1, x1[:], x1.name, part=True)
                router_tile(kb, io, cs, r, x1, i)
        kb.dma("sync", io["aff_loc"], io["aff_loc"].t, r["affT"], r["affT"][:], "affT")
        kb.P.barrier()
        st = kb.P.emit_phase()
        kb.ph = None
    return st


def zero_ydense(kb, io):
    zt = kb.sb("zt", [128, 4, DM], F32)
    kb.memset("gpsimd", zt, zt[:], 0.0)
    yv = io["ydense"].t.rearrange("(a p) d -> p a d", p=128)
    for a in range(32):
        kb.dma("sync", io["ydense"], yv[:, a * 4:(a + 1) * 4, :], zt, zt[:], "zt", part=True)


def moe_topk(kb, io, cs, L):
    aidx = kb.sb("affidx", [128, 1], I32)
    kb.dma("sync", aidx, aidx[:], io["affidx"], io["affidx"].t, "affidx")
    arow = kb.sb("arow", [128, 4096], F32)
    kb.P.dma("gpsimd", lambda e: e.indirect_dma_start(out=arow[0:16, :], out_offset=None, in_=io["aff_all"].t,
             in_offset=bass.IndirectOffsetOnAxis(ap=aidx[0:16, 0:1], axis=0)), "arow", reads=[io["aff_all"].b, aidx.b], writes=[arow.b])
    kb.dma("sync", io["aff_my"], io["aff_my"].t, arow, arow[0:16, :], "arow")
    collective(kb, "AllGather", ALU.bypass, io["h2_loc"], io["h2_all"], "cc2", 8)
    A = kb.sb("A", [128, 4, 128], F32)
    kb.dma("sync", A, A[:], io["aff_my"], io["aff_my"].t.rearrange("(e q) (a j) -> (q a) e j", e=4, j=128), "A")
    lohi = kb.sb("lohi", [128, 16], F32)
    cmp_ = kb.sb("cmp", [128, 128], F32)
    cnt = kb.sb("cnt", [128, 8], F32)
    pc = kb.ps("pc", [128, 512], F32)
    kb.memset("vector", lohi, lohi[:, 0:4], 0.0, part=True)
    kb.memset("vector", lohi, lohi[:, 4:8], 1.0, part=True)
    for it in range(30):
        kb.tt("vector", lohi, lohi[:, 8:12], lohi, lohi[:, 0:4], lohi, lohi[:, 4:8], ALU.add)
        kb.ts("vector", lohi, lohi[:, 8:12], lohi, lohi[:, 8:12], 0.5, None, ALU.mult)
        for e in range(4):
            kb.ts("vector", cmp_, cmp_[:], A, A[:, e, :], (lohi, lohi[:, 8 + e:9 + e]), 0.0, ALU.is_ge, op1=ALU.add, accum=(cnt, cnt[:, e:e + 1]))
        kb.mm(pc, pc[:, 0:4], cs["ones_f"], cs["ones_f"][:], cnt, cnt[:, 0:4])
        kb.ts("vector", lohi, lohi[:, 12:16], pc, pc[:, 0:4], float(CAP), None, ALU.is_ge)
        kb.tt("vector", cnt, cnt[:, 4:8], lohi, lohi[:, 8:12], lohi, lohi[:, 0:4], ALU.subtract)
        kb.tt("vector", cnt, cnt[:, 4:8], cnt, cnt[:, 4:8], lohi, lohi[:, 12:16], ALU.mult)
        kb.tt("vector", lohi, lohi[:, 0:4], lohi, lohi[:, 0:4], cnt, cnt[:, 4:8], ALU.add)
        kb.tt("vector", cnt, cnt[:, 4:8], lohi, lohi[:, 4:8], lohi, lohi[:, 8:12], ALU.subtract)
        kb.tt("vector", cnt, cnt[:, 4:8], cnt, cnt[:, 4:8], lohi, lohi[:, 12:16], ALU.mult)
        kb.tt("vector", lohi, lohi[:, 4:8], lohi, lohi[:, 8:12], cnt, cnt[:, 4:8], ALU.add)
    mask = kb.sb("mask", [128, 4, 128], F32)
    incl = kb.sb("incl", [128, 4, 128], F32)
    slot = kb.sb("slot", [128, 4, 128], F32)
    sloti = kb.sb("sloti", [128, 4, 128], I32)
    pairs = kb.sb("pairs", [128, 4, 128, 3], F32)
    tot = kb.sb("tot", [128, 8], F32)
    for e in range(4):
        kb.ts("vector", mask, mask[:, e, :], A, A[:, e, :], (lohi, lohi[:, e:e + 1]), None, ALU.is_ge, part=True)
        kb.P.op("vector", lambda en, e=e: en.tensor_tensor_scan(out=incl[:, e, :], data0=cs["ones_f"][:], data1=mask[:, e, :], initial=0.0,
                                                               op0=ALU.mult, op1=ALU.add), reads=[cs["ones_f"].b, mask.b], partial=[incl.b])
        kb.copy("vector", tot, tot[:, e:e + 1], incl, incl[:, e, 127:128], part=True)
    kb.mm(pc, pc[:, 8:12], cs["ustrict"], cs["ustrict"][:], tot, tot[:, 0:4])
    kb.copy("vector", tot, tot[:, 4:8], pc, pc[:, 8:12], part=True)
    for e in range(4):
        kb.tt("vector", slot, slot[:, e, :], incl, incl[:, e, :], mask, mask[:, e, :], ALU.subtract, part=True)
        kb.ts("vector", slot, slot[:, e, :], slot, slot[:, e, :], (tot, tot[:, 4 + e:5 + e]), float(e * CAP), ALU.add, op1=ALU.add, part=True)
        kb.ts("vector", incl, incl[:, e, :], mask, mask[:, e, :], -1.0e6, 1.0e6, ALU.mult, op1=ALU.add, part=True)
        kb.tt("vector", slot, slot[:, e, :], slot, slot[:, e, :], incl, incl[:, e, :], ALU.add, part=True)
        kb.ts("vector", incl, incl[:, e, :], slot, slot[:, e, :], float((e + 1) * CAP), 1.0e6, ALU.is_ge, op1=ALU.mult, part=True)
        kb.tt("vector", slot, slot[:, e, :], slot, slot[:, e, :], incl, incl[:, e, :], ALU.add, part=True)
        kb.copy("vector", sloti, sloti[:, e, :], slot, slot[:, e, :], part=True)
        kb.copy("gpsimd", pairs, pairs[:, e, :, 0], cs["iota_tok"], cs["iota_tok"][:], part=True)
        kb.copy("gpsimd", pairs, pairs[:, e, :, 1], A, A[:, e, :], part=True)
        kb.copy("gpsimd", pairs, pairs[:, e, :, 2], cs["iota_h2row"], cs["iota_h2row"][:], part=True)
    rc = {}

    def breg(en):
        if "r" not in rc:
            rc["r"] = en.to_reg(4 * CAP - 1)
        return rc["r"]

    for e in range(4):
        for j in range(128):
            kb.P.dma("gpsimd", (lambda en, e=e, j=j: en.indirect_dma_start(
                out=io["sel"].t, out_offset=bass.IndirectOffsetOnAxis(ap=sloti[:, e, j:j + 1], axis=0),
                in_=pairs[:, e, j, :], in_offset=None, bounds_check=breg(en), oob_is_err=False)),
                "selsc", reads=[pairs.b, sloti.b], partial=[io["sel"].b])


def moe_phase(kb, io, L):
    with contextlib.ExitStack() as ph:
        kb.ph = ph
        cs = load_consts(kb, io, ["ident_bf", "ones_f", "ustrict", "iota_tok", "iota_h2row"])
        with contextlib.ExitStack() as ph2:
            kb.ph = ph2
            moe_topk(kb, io, cs, L)
            kb.P.barrier()
            kb.P.emit_phase()
        kb.ph = ph
        cs = load_consts(kb, io, ["ident_bf"])
        xeT = kb.sb("xeT", [128, 8, CAP], BF16)
        hidT = kb.sb("hidT", [128, 16, CAP], BF16)
        wdb = kb.sb("wdb", [128, 16, DM], BF16)
        wgb = kb.pool("sb", "wgb", 2, [128, 8, 512], BF16)
        wub = kb.pool("sb", "wub", 2, [128, 8, 512], BF16)
        selT = kb.pool("sb", "selT", 2, [128, 16, 3], F32)
        toki = kb.pool("sb", "toki", 2, [128, 2, 16], I32)
        xe = kb.pool("sb", "xe", 3, [128, DM], BF16)
        sg = kb.pool("sb", "sg", 2, [128, 512], BF16)
        ye = kb.pool("sb", "ye", 2, [128, DM], F32)
        pT = kb.pool("ps", "pT", 2, [128, 4, 128], F32)
        pGt = kb.pool("ps", "pGt", 2, [128, 512], F32)
        pUp = kb.pool("ps", "pUp", 2, [128, 512], F32)
        pYe = kb.ps("pYe", [128, 2, 512], F32)
        wg, wu, wd = io["wg%d" % L], io["wu%d" % L], io["wd%d" % L]
        for e in range(4):
            sT = selT.next()
            kb.dma("sync", sT, sT[:], io["sel"], io["sel"].t[e * CAP:(e + 1) * CAP, :].rearrange("(k p) c -> p k c", p=128), sT.name)
            ti = toki.next()
            kb.copy("vector", ti, ti[:, 0, :], sT, sT[:, :, 0], part=True)
            kb.copy("vector", ti, ti[:, 1, :], sT, sT[:, :, 2], part=True)
            for q in range(4):
                kb.dma("gpsimd", wdb, wdb[:, q * 4:(q + 1) * 4, :], wd, wd.t[e, q * 512:(q + 1) * 512, :].rearrange("(k p) d -> p k d", p=128),
                       "wdb", part=True)
            for k in range(16):
                x = xe.next()
                kb.P.dma("gpsimd", (lambda en, x=x, ti=ti, k=k: en.indirect_dma_start(
                    out=x[:], out_offset=None, in_=io["h2_all"].t, in_offset=bass.IndirectOffsetOnAxis(ap=ti[:, 1, k:k + 1], axis=0))),
                    x.name, reads=[io["h2_all"].b, ti.b], writes=[x.b])
                for half in range(2):
                    p = pT.next()
                    for k4 in range(4):
                        kc = half * 4 + k4
                        kb.mm(p, p[:, k4, :], x, x[:, kc * 128:(kc + 1) * 128], cs["ident_bf"], cs["ident_bf"][:])
                    kb.copy("vector", xeT, xeT[:, half * 4:(half + 1) * 4, k * 128:(k + 1) * 128], p, p[:], part=True)
            for fg in range(4):
                wgt = wgb.next()
                wut = wub.next()
                kb.dma("gpsimd", wgt, wgt[:], wg, wg.t[e, :, fg * 512:(fg + 1) * 512].rearrange("(k p) f -> p k f", p=128), wgt.name)
                kb.dma("gpsimd", wut, wut[:], wu, wu.t[e, :, fg * 512:(fg + 1) * 512].rearrange("(k p) f -> p k f", p=128), wut.name)
                for f4 in range(4):
                    fi = fg * 4 + f4
                    for win in range(4):
                        pg = pGt.next()
                        pu = pUp.next()
                        for kc in range(8):
                            kb.mm(pg, pg[:], wgt, wgt[:, kc, f4 * 128:(f4 + 1) * 128], xeT, xeT[:, kc, win * 512:(win + 1) * 512],
                                  start=(kc == 0), stop=(kc == 7))
                        for kc in range(8):
                            kb.mm(pu, pu[:], wut, wut[:, kc, f4 * 128:(f4 + 1) * 128], xeT, xeT[:, kc, win * 512:(win + 1) * 512],
                                  start=(kc == 0), stop=(kc == 7))
                        s_ = sg.next()
                        kb.act(s_, s_[:], pg, pg[:], AF.Silu)
                        kb.tt("vector", hidT, hidT[:, fi, win * 512:(win + 1) * 512], s_, s_[:], pu, pu[:], ALU.mult, part=True)
            for k in range(16):
                for fi in range(16):
                    for half in range(2):
                        kb.mm(pYe, pYe[:, half, :], hidT, hidT[:, fi, k * 128:(k + 1) * 128], wdb, wdb[:, fi, half * 512:(half + 1) * 512],
                              start=(fi == 0), stop=(fi == 15))
                y = ye.next()
                kb.ts("vector", y, y[:], pYe, pYe[:].rearrange("p a b -> p (a b)"), (sT, sT[:, k, 1:2]), None, ALU.mult)
                kb.P.dma("gpsimd", (lambda en, y=y, ti=ti, k=k: en.indirect_dma_start(
                    out=io["ydense"].t, out_offset=bass.IndirectOffsetOnAxis(ap=ti[:, 0, k:k + 1], axis=0), in_=y[:], in_offset=None,
                    compute_op=ALU.add)), y.name, reads=[y.b, ti.b], writes=[io["ydense"].b])
        kb.P.barrier()
        st = kb.P.emit_phase()
        kb.ph = None
    return st


def phase4(kb, io):
    NT = 32
    TL = 4096
    with contextlib.ExitStack() as ph:
        kb.ph = ph
        cs = load_consts(kb, io, ["ident_bf", "ident_f"])
        hnT = kb.sb("hnT", [128, 8, TL + 2], BF16)
        mT = kb.sb("mT", [128, 8, TL], BF16)
        with contextlib.ExitStack() as ph2:
            kb.ph = ph2
            gbc = kb.sb("gconv_bc", [128, DM], F32)
            kb.dma("sync", gbc, gbc[:], io["g_conv_bc"], io["g_conv_bc"].t, "gconv_bc")
            rows = kb.sb("myrows", [128, 33], I32)
            kb.dma("sync", rows, rows[:], io["myrows"], io["myrows"].t, "myrows")
            hidx = kb.sb("haloidx", [128, 1], I32)
            kb.dma("sync", hidx, hidx[:], io["haloidx"], io["haloidx"].t, "haloidx")
            hmsk = kb.sb("halomsk", [128, 1], F32)
            kb.dma("sync", hmsk, hmsk[:], io["halomsk"], io["halomsk"].t, "halomsk")
            x1p = kb.pool("sb", "x1t", 2, [128, DM], F32)
            ysp = kb.pool("sb", "yst", 2, [128, DM], F32)
            x2p = kb.pool("sb", "x2t", 2, [128, DM], F32)
            sqj = kb.pool("sb", "sqj", 2, [128, DM], BF16)
            stp = kb.pool("sb", "st4", 2, [128, 4], F32)
            hnp = kb.pool("sb", "hn", 2, [128, DM], BF16)
            pT = kb.pool("ps", "pT4", 2, [128, 4, 128], F32)
            for i in range(NT + 1):
                x1 = x1p.next()
                ys = ysp.next()
                if i < NT:
                    kb.dma("sync", x1, x1[:], io["x1_loc"], io["x1_loc"].t[i * 128:(i + 1) * 128, :], x1.name)
                else:
                    kb.P.dma("gpsimd", (lambda en, x1=x1: en.indirect_dma_start(out=x1[:], out_offset=None, in_=io["edges_all"].t,
                             in_offset=bass.IndirectOffsetOnAxis(ap=hidx[:, 0:1], axis=0))), x1.name, reads=[io["edges_all"].b, hidx.b], writes=[x1.b])
                kb.P.dma("gpsimd", (lambda en, ys=ys, i=i: en.indirect_dma_start(out=ys[:], out_offset=None, in_=io["ysum_all"].t,
                         in_offset=bass.IndirectOffsetOnAxis(ap=rows[:, i:i + 1], axis=0))), ys.name, reads=[io["ysum_all"].b, rows.b], writes=[ys.b])
                x2 = x2p.next()
                kb.tt("vector", x2, x2[:], x1, x1[:], ys, ys[:], ALU.add)
                if i == NT:
                    kb.ts("vector", x2, x2[:], x2, x2[:], (hmsk, hmsk[:, 0:1]), None, ALU.mult)
                else:
                    kb.dma("sync", io["x2_loc"], io["x2_loc"].t[i * 128:(i + 1) * 128, :], x2, x2[:], x2.name, part=True)
                st = stp.next()
                sq = sqj.next()
                kb.act(sq, sq[:], x2, x2[:], AF.Square, accum=(st, st[:, 0:1]))
                kb.rstd_from(st, st[:, 2:3], st, st[:, 0:1], DM, st, st[:, 1:2], cs["eps"])
                hn = hnp.next()
                kb.stt(hn, hn[:], x2, x2[:], (st, st[:, 2:3]), gbc, gbc[:], ALU.mult, ALU.mult)
                for half in range(2):
                    p = pT.next()
                    for k4 in range(4):
                        kc = half * 4 + k4
                        kb.mm(p, p[:, k4, :], hn, hn[:, kc * 128:(kc + 1) * 128], cs["ident_bf"], cs["ident_bf"][:])
                    if i < NT:
                        kb.copy("vector", hnT, hnT[:, half * 4:(half + 1) * 4, 1 + i * 128:1 + (i + 1) * 128], p, p[:], part=True)
                    else:
                        kb.copy("vector", hnT, hnT[:, half * 4:(half + 1) * 4, 0:1], p, p[:, :, 0:1], part=True)
                        kb.copy("vector", hnT, hnT[:, half * 4:(half + 1) * 4, TL + 1:TL + 2], p, p[:, :, 1:2], part=True)
            kb.P.barrier()
            kb.P.emit_phase()
        with contextlib.ExitStack() as ph2:
            kb.ph = ph2
            w2b = kb.pool("sb", "w2b", 2, [128, 8, 384], BF16)
            cw = kb.sb("cw3", [128, 8, 3], F32)
            kb.dma("sync", cw, cw[:], io["cw3"], io["cw3"].t, "cw3")
            csb = kb.pool("sb", "csb", 2, [128, 512], F32)
            vv = kb.pool("sb", "vv", 2, [128, 512], F32)
            ca = kb.pool("sb", "ca", 2, [128, 2, 512], F32)
            pB = kb.pool("ps", "pB4", 2, [128, 512], F32)
            pC = kb.pool("ps", "pC4", 2, [128, 512], F32)
            pU = kb.pool("ps", "pU4", 2, [128, 512], F32)
            w2 = io["w2"]
            nwin = 9
            for cc in range(8):
                wt = w2b.next()
                for part in range(3):
                    kb.dma("gpsimd", wt, wt[:, :, part * 128:(part + 1) * 128],
                           w2, w2.t[:, part * DM + cc * 128: part * DM + (cc + 1) * 128].rearrange("(k p) f -> p k f", p=128), wt.name, part=True)
                for w in range(nwin):
                    c0 = 510 * w if w < nwin - 1 else TL + 2 - 512
                    pb, pc, pu = pB.next(), pC.next(), pU.next()
                    for (pp, part) in ((pb, 0), (pc, 1), (pu, 2)):
                        for kc in range(8):
                            kb.mm(pp, pp[:], wt, wt[:, kc, part * 128:(part + 1) * 128], hnT, hnT[:, kc, c0:c0 + 512], start=(kc == 0), stop=(kc == 7))
                    cb = csb.next()
                    kb.copy("scalar", cb, cb[:], pc, pc[:])
                    v = vv.next()
                    kb.tt("vector", v, v[:], cb, cb[:], pu, pu[:], ALU.mult)
                    a = ca.next()
                    kb.act(a, a[:, 0, 0:510], v, v[:, 0:510], AF.Copy, scale=(cw, cw[:, cc, 0:1]), part=True)
                    kb.stt(a, a[:, 0, 0:510], v, v[:, 1:511], (cw, cw[:, cc, 1:2]), a, a[:, 0, 0:510], ALU.mult, ALU.add, part=True)
                    kb.stt(a, a[:, 1, 0:510], v, v[:, 2:512], (cw, cw[:, cc, 2:3]), a, a[:, 0, 0:510], ALU.mult, ALU.add, part=True)
                    kb.tt("vector", mT, mT[:, cc, c0:c0 + 510], a, a[:, 1, 0:510], pb, pb[:, 1:511], ALU.mult, part=True)
            kb.P.barrier()
            kb.P.emit_phase()
        with contextlib.ExitStack() as ph2:
            kb.ph = ph2
            cs = load_consts(kb, io, ["ident_f"])
            r = router_setup(kb, io, 1)
            w3b = kb.sb("w3b", [128, 8, DM], BF16)
            kb.dma("gpsimd", w3b, w3b[:], io["w3"], io["w3"].t.rearrange("(k p) d -> p k d", p=128), "w3b")
            x2p = kb.pool("sb", "x2r", 2, [128, DM], F32)
            x3p = kb.pool("sb", "x3", 2, [128, DM], F32)
            pO = kb.ps("pO4", [128, 2, 512], F32)
            for i in range(NT):
                for half in range(2):
                    for cc in range(8):
                        kb.mm(pO, pO[:, half, :], mT, mT[:, cc, i * 128:(i + 1) * 128], w3b, w3b[:, cc, half * 512:(half + 1) * 512],
                              start=(cc == 0), stop=(cc == 7))
                x2 = x2p.next()
                kb.dma("sync", x2, x2[:], io["x2_loc"], io["x2_loc"].t[i * 128:(i + 1) * 128, :], x2.name)
                x3 = x3p.next()
                kb.tt("vector", x3, x3[:], x2, x2[:], pO, pO[:].rearrange("p a b -> p (a b)"), ALU.add)
                kb.dma("sync", io["x1_loc"], io["x1_loc"].t[i * 128:(i + 1) * 128, :], x3, x3[:], x3.name, part=True)
                router_tile(kb, io, cs, r, x3, i)
            kb.dma("sync", io["aff_loc"], io["aff_loc"].t, r["affT"], r["affT"][:], "affT")
            kb.P.barrier()
            st = kb.P.emit_phase()
        kb.ph = None
    return st


def final_phase(kb, io):
    with contextlib.ExitStack() as ph:
        kb.ph = ph
        rows = kb.sb("myrows", [128, 33], I32)
        kb.dma("sync", rows, rows[:], io["myrows"], io["myrows"].t, "myrows")
        x1p = kb.pool("sb", "x1t", 2, [128, DM], F32)
        ysp = kb.pool("sb", "yst", 2, [128, DM], F32)
        op = kb.pool("sb", "ot", 2, [128, DM], F32)
        for i in range(32):
            x1 = x1p.next()
            ys = ysp.next()
            kb.dma("sync", x1, x1[:], io["x1_loc"], io["x1_loc"].t[i * 128:(i + 1) * 128, :], x1.name)
            kb.P.dma("gpsimd", (lambda en, ys=ys, i=i: en.indirect_dma_start(out=ys[:], out_offset=None, in_=io["ysum_all"].t,
                     in_offset=bass.IndirectOffsetOnAxis(ap=rows[:, i:i + 1], axis=0))), ys.name, reads=[io["ysum_all"].b, rows.b], writes=[ys.b])
            o = op.next()
            kb.tt("vector", o, o[:], x1, x1[:], ys, ys[:], ALU.add)
            kb.dma("sync", io["out"], io["out"].t[i * 128:(i + 1) * 128, :], o, o[:], o.name, part=True)
        kb.P.barrier()
        st = kb.P.emit_phase()
        kb.ph = None
    return st


def collective(kb, kind, op, src, dst, key, nchunks=1):
    rin = src.t.shape[0] // nchunks
    rout = dst.t.shape[0] // nchunks
    for c in range(nchunks):
        sap = src.t[c * rin:(c + 1) * rin, :]
        dap = dst.t[c * rout:(c + 1) * rout, :]
        kb.P.dma("gpsimd", (lambda e, sap=sap, dap=dap: e.collective_compute(kind, op, replica_groups=GROUPS, ins=[sap.opt()], outs=[dap.opt()])),
                 key, reads=[src.b], writes=[] if nchunks > 1 else [dst.b], partial=[dst.b] if nchunks > 1 else [], inc=1)


IN_SPECS = {
    "xT": ([DM, S], F32), "x_tok": ([4096, DM], F32), "w1": ([DM, NC1], F32), "g_attn": ([128, 8], F32), "gqk": ([128, 2], F32),
    "convw": ([128, 4, 6], F32), "dtc": ([8, 2], F32), "Dbc": ([128, 256], F32), "wo": ([2048, DM], F32), "g_wo": ([128, 16], F32),
    "mixidx": ([128, 8, 16], I32), "wr0": ([DM, NE], F32), "wr1": ([DM, NE], F32), "g_ffn_col0": ([128, 8], F32),
    "g_ffn_col1": ([128, 8], F32), "g_ffn_bc0": ([128, DM], F32), "g_ffn_bc1": ([128, DM], F32), "affidx": ([128, 1], I32),
    "myrows": ([128, 33], I32), "haloidx": ([128, 1], I32), "halomsk": ([128, 1], F32), "g_conv_bc": ([128, DM], F32),
    "w2": ([DM, 3 * DM], F32), "cw3": ([128, 8, 3], F32), "w3": ([DM, DM], F32),
    "wg0": ([4, DM, FF], F32), "wu0": ([4, DM, FF], F32), "wd0": ([4, FF, DM], F32),
    "wg1": ([4, DM, FF], F32), "wu1": ([4, DM, FF], F32), "wd1": ([4, FF, DM], F32),
}
CONST_NAMES = ["ident_bf", "ident_f", "negmask", "blockones", "rotm", "sel_ones", "ones_bf", "ones_f", "tri", "smask", "ustrict",
               "iota_tok", "iota_h2row", "cosf", "sinf"]
SCRATCH = {
    "mixA_loc": ([8192, 512], BF16), "mixA_all": ([32768, 512], BF16), "mixS_loc": ([8192, 512], BF16), "mixS_all": ([32768, 512], BF16), "xb_dram": ([DM, S], BF16), "post": ([512, S], BF16), "zs": ([256, S], BF16),
    "dta": ([16, S], F32), "yf": ([S, 256], F32), "x1_loc": ([4096, DM], F32), "x2_loc": ([4096, DM], F32),
    "h2_loc": ([4096, DM], BF16), "h2_all": ([S, DM], BF16), "aff_loc": ([NE, 4096], F32), "aff_all": ([4 * NE, 4096], F32),
    "aff_my": ([NE, 4096], F32), "sel": ([4 * CAP, 3], F32), "ydense": ([S, DM], F32), "ysum_all": ([S, DM], F32),
    "edges_loc": ([2, DM], F32), "edges_all": ([8, DM], F32),
}


def misc_phase(kb, fn):
    with contextlib.ExitStack() as ph:
        kb.ph = ph
        fn()
        kb.P.barrier()
        st = kb.P.emit_phase()
        kb.ph = None
    return st


def build_program(consts, debug=False, upto=99):
    nc = bass.Bass("TRN2", target_bir_lowering=False)
    with contextlib.ExitStack() as st:
        kb = KB(nc, st)
        io = {}
        for n, (shape, dt) in IN_SPECS.items():
            if upto < 3 and n[:2] in ("wg", "wu", "wd"):
                continue
            io[n] = kb.dram(n, shape, dt, "ExternalInput")
        for n in CONST_NAMES:
            io[n] = kb.dram(n, list(consts[n].shape), CONST_DT.get(n, F32), "ExternalInput")
        for n, (shape, dt) in SCRATCH.items():
            io[n] = kb.dram(n, shape, dt)
        io["out"] = kb.dram("out", [4096, DM], F32, "ExternalOutput")
        dbg = {}
        if debug:
            for n, shape in (("dbg_x1", [4096, DM]), ("dbg_x2", [4096, DM]), ("dbg_x3", [4096, DM]), ("dbg_aff0", [NE, 4096]),
                             ("dbg_aff1", [NE, 4096]), ("dbg_sel", [4 * CAP, 3])):
                dbg[n] = kb.dram(n, shape, F32, "ExternalOutput")


        def cp(dst, src, key):
            kb.dma("sync", dst, dst.t, src, src.t, key)

        import os
        if not os.environ.get("K_SKIP1"):
            phase1_attn(kb, io, 0)
            io["_xb_ready"] = True
            phase1_attn(kb, io, 1)
            phase1_ssd_in(kb, io)
            phase1_ssd_scan(kb, io, 0)
            phase1_ssd_scan(kb, io, 1)

        def ph_a():
            collective(kb, "AllGather", ALU.bypass, io["mixS_loc"], io["mixS_all"], "cc0", 8)
            zero_ydense(kb, io)

        misc_phase(kb, ph_a)
        if upto >= 2:
            phase2(kb, io, int(os.environ.get("K_NT", 32)))

            def ph_b(L):
                def f():
                    if L == 0:
                        kb.dma("sync", io["edges_loc"], io["edges_loc"].t[0:1, :], io["x1_loc"], io["x1_loc"].t[0:1, :], "edg", part=True)
                        kb.dma("sync", io["edges_loc"], io["edges_loc"].t[1:2, :], io["x1_loc"], io["x1_loc"].t[4095:4096, :], "edg", part=True)
                        collective(kb, "AllGather", ALU.bypass, io["edges_loc"], io["edges_all"], "cc1")
                    collective(kb, "AllGather", ALU.bypass, io["aff_loc"], io["aff_all"], "cc3")
                    if debug:
                        cp(dbg["dbg_x1" if L == 0 else "dbg_x3"], io["x1_loc"], "dbgx")
                        cp(dbg["dbg_aff%d" % L], io["aff_loc"], "dbga")
                return f
            misc_phase(kb, ph_b(0))
        if upto >= 3:
            moe_phase(kb, io, 0)

            def ph_c():
                collective(kb, "AllReduce", ALU.add, io["ydense"], io["ysum_all"], "cc4", 16)
                if debug:
                    cp(dbg["dbg_sel"], io["sel"], "dbgs")
            misc_phase(kb, ph_c)
        if upto >= 4:
            phase4(kb, io)

            def ph_d():
                zero_ydense(kb, io)
                if debug:
                    cp(dbg["dbg_x2"], io["x2_loc"], "dbgx2")
            misc_phase(kb, ph_d)
            misc_phase(kb, ph_b(1))
        if upto >= 5:
            moe_phase(kb, io, 1)
            misc_phase(kb, lambda: collective(kb, "AllReduce", ALU.add, io["ydense"], io["ysum_all"], "cc5", 16))
            final_phase(kb, io)
        print("semaphores:", len(kb.P.dsems) + 5)
    return nc


def core_inputs(inp, c, consts):
    b, r = c // 4, c % 4
    hg = r
    g = hg // 2
    d = {}
    x = inp["x"]
    d["xT"] = np.ascontiguousarray(x[b].T)
    d["x_tok"] = np.ascontiguousarray(x[b, r * 4096:(r + 1) * 4096])
    w = inp["w_in_even"][0]
    hs = slice(hg * 256, (hg + 1) * 256)
    cols = [w[:, 0:1024][:, hs], w[:, 1024:2048][:, hs], w[:, 2048:3072][:, hs], w[:, 4096:5120][:, hs],
            w[:, 5120 + g * 128:5120 + (g + 1) * 128], w[:, 5376 + g * 128:5376 + (g + 1) * 128], w[:, 3072:4096][:, hs],
            w[:, 5632 + hg * 4:5632 + hg * 4 + 4], w[:, 5648 + hg * 4:5648 + hg * 4 + 4]]
    d["w1"] = np.ascontiguousarray(np.concatenate(cols, 1))
    d["g_attn"] = np.ascontiguousarray(inp["attn_norm"][0].reshape(8, 128).T)
    d["gqk"] = np.ascontiguousarray(np.stack([np.tile(inp["q_norm"][0], 2), np.tile(inp["k_norm"][0], 2)], 1))
    cw_full = inp["ssd_conv_w"][0]; cb = inp["ssd_conv_b"][0]
    chans = np.concatenate([np.arange(hg * 256, hg * 256 + 256), 1024 + g * 128 + np.arange(128), 1280 + g * 128 + np.arange(128)])
    cw = np.concatenate([cw_full[:, chans], cb[None, chans]], 0)
    d["convw"] = np.ascontiguousarray(cw.reshape(6, 4, 128).transpose(2, 1, 0)).astype(np.float32)
    h4 = slice(hg * 4, hg * 4 + 4)
    d["dtc"] = np.stack([np.concatenate([inp["ssd_dt_bias_fwd"][0][h4], inp["ssd_dt_bias_bwd"][0][h4]]),
                         np.concatenate([inp["ssd_a_log_fwd"][0][h4], inp["ssd_a_log_bwd"][0][h4]])], 1).astype(np.float32)
    d["Dbc"] = np.ascontiguousarray(np.tile(np.repeat(inp["ssd_d"][0][h4], 64)[None, :], (128, 1))).astype(np.float32)
    wo = inp["w_out_even"][0]
    perm = np.concatenate([np.concatenate([np.arange(q * 256, (q + 1) * 256), 1024 + np.arange(q * 256, (q + 1) * 256)]) for q in range(4)])
    d["wo"] = np.ascontiguousarray(wo[perm])
    gfull = np.concatenate([np.ones(1024, np.float32), inp["ssd_out_norm"][0]])[perm]
    d["g_wo"] = np.ascontiguousarray(gfull.reshape(16, 128).T).astype(np.float32)
    mi = np.zeros((128, 8, 16), np.int32)
    for wdx in range(8):
        for ck in range(16):
            L = (8 * r + wdx) * 256 + (ck % 2) * 128 + np.arange(128)
            mi[:, wdx, ck] = (L // 1024) * 4096 + (ck // 4) * 1024 + (L % 1024)
    d["mixidx"] = mi
    for L in range(2):
        d["wr%d" % L] = np.ascontiguousarray(inp["router_w"][L])
        d["g_ffn_col%d" % L] = np.ascontiguousarray(inp["ffn_norm"][L].reshape(8, 128).T)
        d["g_ffn_bc%d" % L] = np.ascontiguousarray(np.tile(inp["ffn_norm"][L][None, :], (128, 1)))
        es = slice(4 * r, 4 * r + 4)
        d["wg%d" % L] = np.ascontiguousarray(inp["expert_w_gate"][L, es])
        d["wu%d" % L] = np.ascontiguousarray(inp["expert_w_up"][L, es])
        d["wd%d" % L] = np.ascontiguousarray(inp["expert_w_down"][L, es])
    ai = np.zeros((128, 1), np.int32)
    for e in range(4):
        for q in range(4):
            ai[e * 4 + q, 0] = q * 16 + 4 * r + e
    d["affidx"] = ai
    mr = np.zeros((128, 33), np.int32)
    for i in range(32):
        mr[:, i] = r * 4096 + i * 128 + np.arange(128)
    mr[0, 32] = max(r * 4096 - 1, 0)
    mr[1, 32] = min(r * 4096 + 4096, S - 1)
    d["myrows"] = mr
    hi = np.zeros((128, 1), np.int32); hm = np.zeros((128, 1), np.float32)
    if r > 0:
        hi[0, 0] = 2 * (r - 1) + 1; hm[0, 0] = 1.0
    if r < 3:
        hi[1, 0] = 2 * (r + 1); hm[1, 0] = 1.0
    d["haloidx"] = hi; d["halomsk"] = hm
    d["g_conv_bc"] = np.ascontiguousarray(np.tile(inp["conv_norm"][0][None, :], (128, 1)))
    d["w2"] = np.ascontiguousarray(inp["conv_w_in"][0])
    d["cw3"] = np.ascontiguousarray(inp["conv_w"][0].reshape(3, 8, 128).transpose(2, 1, 0))
    d["w3"] = np.ascontiguousarray(inp["conv_w_out"][0])
    for n in CONST_NAMES:
        d[n] = consts[n]
    return d


def kernel(**inputs):
    from concourse.bass_utils import run_bass_kernel_spmd
    inp = {k: np.asarray(v) for k, v in inputs.items()}
    consts = host_constants()
    nc = build_program(consts)
    in_maps = [core_inputs(inp, c, consts) for c in range(NCORE)]
    res = run_bass_kernel_spmd(nc, in_maps, core_ids=list(range(NCORE)))
    out = np.zeros((2, S, DM), np.float32)
    for c in range(NCORE):
        b, r = c // 4, c % 4
        out[b, r * 4096:(r + 1) * 4096] = res.results[c]["out"]
    return out
```

```python
import contextlib
import numpy as np
import concourse.bass as bass
import concourse.mybir as mybir

F32 = mybir.dt.float32
BF16 = mybir.dt.bfloat16
I32 = mybir.dt.int32
U32 = mybir.dt.uint32
AF = mybir.ActivationFunctionType
ALU = mybir.AluOpType
AX = mybir.AxisListType

ENGS = ("tensor", "vector", "scalar", "gpsimd", "sync")


class Buf:
    __slots__ = ("name", "w", "r", "dsem", "dcount")

    def __init__(self, name):
        self.name = name
        self.w = {}
        self.r = {}
        self.dsem = None
        self.dcount = 0


class Prog:
    def __init__(self, nc, stack):
        self.nc = nc
        self.stack = stack
        self.esem = {e: stack.enter_context(nc.semaphore("es_" + e)) for e in ENGS}
        self.base = {e: 0 for e in ENGS}
        self.dsems = {}
        self.dvals = {}
        self.dfree = []
        self.phase = 0
        self.reset_phase()
        self.same_engine_sync = True
        self.no_self = set()

    def reset_phase(self):
        self.ops = {e: [] for e in ENGS}
        self.seen = {e: {} for e in ENGS}
        self.needed = {e: set() for e in ENGS}

    def _collect(self, eng, reads, writes, partial):
        waits = {}
        def add(d):
            for k, v in d.items():
                if waits.get(k, -1) < v:
                    waits[k] = v
        for b in reads:
            add(b.w)
        for b in writes:
            add(b.w); add(b.r)
        for b in partial:
            add(b.r)
        out = []
        seen = self.seen[eng]
        for k, v in waits.items():
            if k[2] != self.phase:
                continue
            if k[0] == "E" and k[1] == eng and (not self.same_engine_sync or eng in self.no_self):
                continue
            if seen.get(k, -1) >= v:
                continue
            seen[k] = v
            if k[0] == "E":
                self.needed[k[1]].add(v)
            out.append((k, v))
        return out

    def _commit(self, tok, reads, writes, partial):
        k, v = tok
        for b in reads:
            if b.r.get(k, -1) < v:
                b.r[k] = v
        for b in writes:
            b.w = {k: v}
            b.r = {}
        for b in partial:
            if b.w.get(k, -1) < v:
                b.w[k] = v

    def _serial_waits(self, eng, waits):
        import os
        if not os.environ.get("FW_SERIAL"):
            return waits
        if os.environ["FW_SERIAL"] != "1" and eng not in os.environ["FW_SERIAL"].split(","):
            return waits
        have = {k for k, _ in waits}
        for e2 in ENGS:
            if e2 == eng:
                continue
            ee = [o["idx"] for o in self.ops[e2] if o["kind"] == "E"]
            if not ee:
                continue
            k = ("E", e2, self.phase)
            if self.seen[eng].get(k, -1) < ee[-1]:
                self.seen[eng][k] = ee[-1]
                self.needed[e2].add(ee[-1])
                waits.append((k, ee[-1]))
        return waits

    def op(self, eng, emit, reads=(), writes=(), partial=()):
        waits = self._collect(eng, reads, writes, partial)
        waits = self._serial_waits(eng, waits)
        idx = len(self.ops[eng]) + 1
        self.ops[eng].append(dict(waits=waits, emit=emit, kind="E", idx=idx))
        self._commit((("E", eng, self.phase), idx), reads, writes, partial)

    def dma(self, eng, emit, key, reads=(), writes=(), partial=(), inc=16):
        waits = self._collect(eng, reads, writes, partial)
        if key not in self.dsems:
            if self.dfree:
                self.dsems[key], self.dvals[key] = self.dfree.pop(0)
            else:
                self.dsems[key] = self.stack.enter_context(self.nc.semaphore("ds%d_%s" % (self.phase, key)))
                self.dvals[key] = 0
        self.dvals[key] += inc
        v = self.dvals[key]
        idx = len(self.ops[eng]) + 1
        self.ops[eng].append(dict(waits=waits, emit=emit, kind="D", key=key, inc=inc, idx=idx))
        self._commit((("D", key, self.phase), v), reads, writes, partial)

    def barrier(self):
        last = {}
        for e in ENGS:
            ee = [o["idx"] for o in self.ops[e] if o["kind"] == "E"]
            last[e] = ee[-1] if ee else 0
        for e in ENGS:
            waits = []
            for e2 in ENGS:
                if e2 != e and last[e2] > 0:
                    k = ("E", e2, self.phase)
                    if self.seen[e].get(k, -1) < last[e2]:
                        self.seen[e][k] = last[e2]
                        self.needed[e2].add(last[e2])
                        waits.append((k, last[e2]))
            for key, v in self.dvals.items():
                k = ("D", key, self.phase)
                if self.seen[e].get(k, -1) < v:
                    self.seen[e][k] = v
                    waits.append((k, v))
            self.ops[e].append(dict(waits=waits, emit=None, kind="N", idx=len(self.ops[e]) + 1))

    def emit_phase(self):
        nc = self.nc
        val = {}
        for e in ENGS:
            c = self.base[e]
            m = {}
            for o in self.ops[e]:
                if o["kind"] == "E" and o["idx"] in self.needed[e]:
                    c += 1
                    m[o["idx"]] = c
            val[e] = m
        stats = {}

        def replay(e, eng):
            n = 0
            for o in self.ops[e]:
                for (k, v) in o["waits"]:
                    if k[0] == "E":
                        eng.wait_ge(self.esem[k[1]], val[k[1]][v])
                    else:
                        eng.wait_ge(self.dsems[k[1]], v)
                    n += 1
                if o["emit"] is None:
                    continue
                ins = o["emit"](eng)
                n += 1
                if o["kind"] == "E":
                    if o["idx"] in self.needed[e]:
                        ins.then_inc(self.esem[e], 1)
                else:
                    ins.then_inc(self.dsems[o["key"]], o["inc"])
            stats[e] = n

        with nc.Block() as block:
            @block.tensor
            def _(eng):
                replay("tensor", eng)

            @block.vector
            def _(eng):
                replay("vector", eng)

            @block.scalar
            def _(eng):
                replay("scalar", eng)

            @block.gpsimd
            def _(eng):
                replay("gpsimd", eng)

            @block.sync
            def _(eng):
                replay("sync", eng)
        for e in ENGS:
            if val[e]:
                self.base[e] = max(val[e].values())
        for key in list(self.dsems):
            self.dfree.append((self.dsems[key], self.dvals[key]))
        self.dsems = {}
        self.dvals = {}
        self.phase += 1
        self.reset_phase()
        return stats


S = 16384
DM = 1024
NCORE = 8
PAD = 1024
WIN = 512
NW = S // WIN
NCH = S // 128
NE = 16
CAP = 2048
FF = 2048
EPS = 1e-6
NEG = -30000.0
C_Q, C_K, C_V, C_XS, C_B, C_C, C_Z, C_DT, NC1 = 0, 256, 512, 768, 1024, 1152, 1280, 1536, 1544
GROUPS = [[0, 1, 2, 3], [4, 5, 6, 7]]


def sl(start, n, step):
    return slice(start, start + (n - 1) * step + 1, step)


class T:
    def __init__(self, t, name):
        self.t = t
        self.b = Buf(name)
        self.name = name

    def __getitem__(self, k):
        return self.t[k]


class KB:
    def __init__(self, nc, st):
        self.nc = nc
        self.st = st
        self.ph = None
        self.P = Prog(nc, st)
        self.P.no_self = {"tensor"}
        self.uid = 0

    def sb(self, name, shape, dt):
        self.uid += 1
        nm = "s%d_%s" % (self.uid, name)
        return T(self.ph.enter_context(self.nc.sbuf_tensor(nm, shape, dt)), name)

    def ps(self, name, shape, dt=F32):
        self.uid += 1
        nm = "p%d_%s" % (self.uid, name)
        return T(self.ph.enter_context(self.nc.psum_tensor(nm, shape, dt)), name)

    def pool(self, kind, name, n, shape, dt):
        f = self.sb if kind == "sb" else self.ps
        return Pool([f("%s%d" % (name, i), shape, dt) for i in range(n)])

    def dram(self, name, shape, dt, kind=None):
        if kind is None:
            h = self.nc.dram_tensor(name, shape, dt)
        else:
            h = self.nc.dram_tensor(name, shape, dt, kind=kind)
        return T(h.ap(), name)

    def mm(self, out, oap, lt, ltap, rt, rtap, start=True, stop=True):
        self.P.op("tensor", lambda e: e.matmul(oap, lhsT=ltap, rhs=rtap, start=start, stop=stop),
                  reads=[lt.b, rt.b], writes=[out.b])

    def tr(self, out, oap, inp, iap, ident, idap):
        self.P.op("tensor", lambda e: e.transpose(oap, iap, idap), reads=[inp.b, ident.b], writes=[out.b])

    def act(self, out, oap, inp, iap, func, bias=None, scale=None, accum=None, part=False):
        reads = [inp.b]
        kw = {}
        if bias is not None:
            reads.append(bias[0].b)
            kw["bias"] = bias[1]
        if scale is not None:
            if isinstance(scale, tuple):
                reads.append(scale[0].b)
                kw["scale"] = scale[1]
            else:
                kw["scale"] = scale
        writes = [out.b]
        if accum is not None:
            writes.append(accum[0].b)
            kw["accum_out"] = accum[1]
        self.P.op("scalar", lambda e: e.activation(out=oap, in_=iap, func=func, **kw), reads=reads,
                  writes=[] if part else writes, partial=writes if part else [])

    def tt(self, eng, out, oap, a, aap, b, bap, op, part=False):
        self.P.op(eng, lambda e: e.tensor_tensor(out=oap, in0=aap, in1=bap, op=op), reads=[a.b, b.b],
                  writes=[] if part else [out.b], partial=[out.b] if part else [])

    def ts(self, eng, out, oap, a, aap, s1, s2, op0, op1=None, accum=None, part=False):
        reads = [a.b]
        def cv(s):
            if isinstance(s, tuple):
                reads.append(s[0].b)
                return s[1]
            return s
        v1, v2 = cv(s1), cv(s2)
        kw = {}
        writes = [out.b]
        if accum is not None:
            writes.append(accum[0].b)
            kw["accum_out"] = accum[1]
        if op1 is not None:
            kw["op1"] = op1
        self.P.op(eng, lambda e: e.tensor_scalar(out=oap, in0=aap, scalar1=v1, scalar2=v2, op0=op0, **kw), reads=reads,
                  writes=[] if part else writes, partial=writes if part else [])

    def stt(self, out, oap, a, aap, scalar, b, bap, op0, op1, part=False):
        reads = [a.b, b.b]
        sv = scalar
        if isinstance(scalar, tuple):
            reads.append(scalar[0].b)
            sv = scalar[1]
        self.P.op("vector", lambda e: e.scalar_tensor_tensor(out=oap, in0=aap, scalar=sv, in1=bap, op0=op0, op1=op1),
                  reads=reads, writes=[] if part else [out.b], partial=[out.b] if part else [])

    def copy(self, eng, out, oap, a, aap, part=False):
        if eng == "scalar":
            f = lambda e: e.activation(out=oap, in_=aap, func=AF.Copy)
        else:
            f = lambda e: e.tensor_copy(out=oap, in_=aap)
        self.P.op(eng, f, reads=[a.b], writes=[] if part else [out.b], partial=[out.b] if part else [])

    def memset(self, eng, out, oap, val, part=False):
        self.P.op(eng, lambda e: e.memset(oap, val), writes=[] if part else [out.b], partial=[out.b] if part else [])

    def recip(self, out, oap, a, aap):
        self.P.op("vector", lambda e: e.reciprocal(out=oap, in_=aap), reads=[a.b], writes=[out.b])

    def dma(self, q, out, oap, inp, iap, key, part=False, extra_reads=(), **kw):
        self.P.dma(q, lambda e: e.dma_start(out=oap, in_=iap, **kw), key, reads=[inp.b] + [x.b for x in extra_reads],
                   writes=[] if part else [out.b], partial=[out.b] if part else [])

    def rstd_from(self, out, oap, inp, iap, n, tmp, tap, eps):
        self.act(tmp, tap, inp, iap, AF.Ln, bias=(eps, eps[:, 0:1]), scale=1.0 / n)
        self.act(out, oap, tmp, tap, AF.Exp, scale=-0.5)


class Pool:
    def __init__(self, tiles):
        self.tiles = tiles
        self.i = 0

    def next(self):
        t = self.tiles[self.i % len(self.tiles)]
        self.i += 1
        return t


def host_constants():
    import ml_dtypes
    bf = ml_dtypes.bfloat16
    c = {}
    c["ident_bf"] = np.eye(128, dtype=np.float32).astype(bf)
    c["ident_f"] = np.eye(128, dtype=np.float32)
    p = np.arange(128)[:, None]
    f = np.arange(128)[None, :]
    mA = np.where(f <= p, 0.0, NEG).astype(np.float32)
    mB = np.where(f >= p, 0.0, NEG).astype(np.float32)
    mA_first = mA.copy(); mA_first[:64, :] = NEG
    mB_last = mB.copy(); mB_last[64:, :] = NEG
    nm = np.zeros((128, 3, 4, 128), np.float32)
    for v, (a, b) in enumerate([(mA, mB), (mA_first, mB), (mA, mB_last)]):
        nm[:, v, 0] = a; nm[:, v, 1] = b; nm[:, v, 2] = a; nm[:, v, 3] = b
    c["negmask"] = nm.reshape(128, 3, 512).astype(bf)
    bo = np.zeros((128, 128), np.float32); bo[:64, :64] = 1; bo[64:, 64:] = 1
    c["blockones"] = bo.astype(bf)
    rm = np.zeros((128, 128), np.float32)
    for o in (0, 64):
        for i in range(8):
            rm[o + 8 + i, o + i] = -1.0
            rm[o + i, o + 8 + i] = 1.0
    c["rotm"] = rm.astype(bf)
    so = np.zeros((128, 2, 128), np.float32); so[:, 0, :64] = 1; so[:, 1, 64:] = 1
    c["sel_ones"] = so.astype(bf)
    c["ones_bf"] = np.ones((128, 128), np.float32).astype(bf)
    c["ones_f"] = np.ones((128, 128), np.float32)
    t = np.arange(128)[:, None]; l = np.arange(128)[None, :]
    c["tri"] = np.stack([(t <= l), (t >= l)], 1).astype(np.float32)
    mf = np.where(t <= l, 0.0, NEG).astype(np.float32); mb = np.where(t >= l, 0.0, NEG).astype(np.float32)
    c["smask"] = np.stack([np.tile(mf, (1, 4)), np.tile(mb, (1, 4))], 1).astype(np.float32)
    c["ustrict"] = (t < l).astype(np.float32)
    half = 8
    inv_freq = (np.float32(500000.0) ** (-np.arange(half, dtype=np.float32) * np.float32(2.0) / np.float32(16))).astype(np.float32)
    ang = (np.arange(S, dtype=np.float32)[None, :] * inv_freq[:, None]).astype(np.float32)
    cos = np.cos(ang.astype(np.float64)).astype(np.float32); sin = np.sin(ang.astype(np.float64)).astype(np.float32)
    cf = np.ones((128, S), np.float32); sf = np.zeros((128, S), np.float32)
    for o in (0, 64):
        cf[o:o + 8] = cos; cf[o + 8:o + 16] = cos
        sf[o:o + 8] = sin; sf[o + 8:o + 16] = sin
    c["cosf"] = cf; c["sinf"] = sf
    c["iota_tok"] = (np.arange(128)[:, None] * 128 + np.arange(128)[None, :]).astype(np.float32)
    tt_ = np.arange(128)[:, None] * 128 + np.arange(128)[None, :]
    c["iota_h2row"] = (((tt_ % 4096) // 512) * 2048 + (tt_ // 4096) * 512 + (tt_ % 512)).astype(np.float32)
    return c


CONST_DT = {"ident_bf": BF16, "negmask": BF16, "blockones": BF16, "rotm": BF16, "sel_ones": BF16, "ones_bf": BF16}
SMALL_CONSTS_UNUSED = ["ident_bf", "ident_f", "negmask", "blockones", "rotm", "sel_ones", "ones_bf", "ones_f", "tri", "smask",
                "ustrict", "iota_tok"]


def load_consts(kb, io, names):
    cs = {}
    for n in names:
        src = io[n]
        shape = list(src.t.shape)
        t = kb.sb("c_" + n, shape, CONST_DT.get(n, F32))
        kb.dma("sync", t, t[:], src, src.t, "c_" + n)
        cs[n] = t
    eps = kb.sb("c_eps", [128, 1], F32)
    kb.memset("vector", eps, eps[:], EPS)
    cs["eps"] = eps
    return cs


def load_w1(kb, io, w1b, c0, ncols, stage_pool, g_attn):
    w1 = io["w1"]
    for kc in range(8):
        stg = stage_pool.next()
        kb.dma("sync", stg, stg[:, 0:ncols], w1, w1.t[kc * 128:(kc + 1) * 128, c0:c0 + ncols], stg.name)
        kb.ts("vector", w1b, w1b[:, kc, 0:ncols], stg, stg[:, 0:ncols], (g_attn, g_attn[:, kc:kc + 1]), None, ALU.mult, part=True)


def window_front(kb, io, cs, w, xb_pool, sq_pool, rstd_pool, psA):
    xT = io["xT"]
    xb = xb_pool.next()
    kb.dma("gpsimd", xb, xb[:], xT, xT.t.rearrange("(k p) t -> p k t", p=128)[:, :, w * WIN:(w + 1) * WIN], xb.name)
    sq = sq_pool.next()
    kb.act(sq, sq[:], xb, xb[:], AF.Square)
    pss = psA.next()
    for kc in range(8):
        kb.mm(pss, pss[:], cs["ones_bf"], cs["ones_bf"][:], sq, sq[:, kc, :], start=(kc == 0), stop=(kc == 7))
    rstd = rstd_pool.next()
    kb.rstd_from(rstd, rstd[:, 0, :], pss, pss[:], DM, rstd, rstd[:, 1, :], cs["eps"])
    return xb, sq, rstd


def attn_inproj(kb, io, hp, qz, kT, vT):
    if True:
      with contextlib.ExitStack() as ph:
        kb.ph = ph
        cs = load_consts(kb, io, ["blockones", "rotm", "ones_bf"])
        g_attn = kb.sb("g_attn", [128, 8], F32)
        kb.dma("sync", g_attn, g_attn[:], io["g_attn"], io["g_attn"].t, "g_attn")
        gqk = kb.sb("gqk", [128, 2], F32)
        kb.dma("sync", gqk, gqk[:], io["gqk"], io["gqk"].t, "gqk")
        w1b = kb.sb("w1b", [128, 8, 384], BF16)
        stage = kb.pool("sb", "w1stg", 2, [128, 128], F32)
        for j, c0 in enumerate((C_Q + hp * 128, C_K + hp * 128, C_V + hp * 128)):
            w1 = io["w1"]
            for kc in range(8):
                stg = stage.next()
                kb.dma("sync", stg, stg[:], w1, w1.t[kc * 128:(kc + 1) * 128, c0:c0 + 128], stg.name)
                kb.ts("vector", w1b, w1b[:, kc, j * 128:(j + 1) * 128], stg, stg[:], (g_attn, g_attn[:, kc:kc + 1]), None,
                      ALU.mult, part=True)
        for t in (kT, vT):
            kb.memset("gpsimd", t, t[:, 0:PAD], 0.0, part=True)
            kb.memset("gpsimd", t, t[:, PAD + S:], 0.0, part=True)
        kb.memset("gpsimd", qz, qz[64:128, 0, :], 0.0, part=True)
        kb.memset("gpsimd", qz, qz[0:64, 1, :], 0.0, part=True)
        xb_pool = kb.pool("sb", "xb", 2, [128, 8, WIN], BF16)
        sq_pool = kb.pool("sb", "sq", 1, [128, 8, WIN], BF16)
        rstd_pool = kb.pool("sb", "rstd", 2, [128, 2, WIN], F32)
        cos_pool = kb.pool("sb", "cosw", 2, [128, 2, WIN], F32)
        raw_pool = kb.pool("sb", "raw", 2, [128, WIN], F32)
        sqh_pool = kb.pool("sb", "sqh", 2, [128, WIN], BF16)
        rsh_pool = kb.pool("sb", "rsh", 2, [128, 2, WIN], F32)
        qn_pool = kb.pool("sb", "qn", 2, [128, WIN], BF16)
        t12_pool = kb.pool("sb", "t12", 2, [128, 2, WIN], F32)
        psA = kb.pool("ps", "psA", 2, [128, WIN], F32)
        psB = kb.pool("ps", "psB", 2, [128, WIN], F32)
        psC = kb.pool("ps", "psC", 2, [128, WIN], F32)
        for w in range(NW):
            xb, sq, rstd = window_front(kb, io, cs, w, xb_pool, sq_pool, rstd_pool, psA)
            cw = cos_pool.next()
            kb.dma("sync", cw, cw[:, 0, :], io["cosf"], io["cosf"].t[:, w * WIN:(w + 1) * WIN], cw.name, part=True)
            kb.dma("sync", cw, cw[:, 1, :], io["sinf"], io["sinf"].t[:, w * WIN:(w + 1) * WIN], cw.name, part=True)
            for j, dst in enumerate((qz, kT, vT)):
                ps = psB.next()
                for kc in range(8):
                    kb.mm(ps, ps[:], w1b, w1b[:, kc, j * 128:(j + 1) * 128], xb, xb[:, kc, :], start=(kc == 0), stop=(kc == 7))
                off = 0 if j == 0 else PAD
                dap = None if j == 0 else dst[:, off + w * WIN: off + (w + 1) * WIN]
                if j == 2:
                    kb.tt("vector", dst, dap, ps, ps[:], rstd, rstd[:, 0, :], ALU.mult, part=True)
                    continue
                raw = raw_pool.next()
                kb.tt("vector", raw, raw[:], ps, ps[:], rstd, rstd[:, 0, :], ALU.mult)
                sqh = sqh_pool.next()
                kb.act(sqh, sqh[:], raw, raw[:], AF.Square)
                ps2 = psC.next()
                kb.mm(ps2, ps2[:], cs["blockones"], cs["blockones"][:], sqh, sqh[:])
                rsh = rsh_pool.next()
                kb.rstd_from(rsh, rsh[:, 0, :], ps2, ps2[:], 64, rsh, rsh[:, 1, :], cs["eps"])
                qn = qn_pool.next()
                kb.stt(qn, qn[:], raw, raw[:], (gqk, gqk[:, j:j + 1]), rsh, rsh[:, 0, :], ALU.mult, ALU.mult)
                ps3 = psC.next()
                kb.mm(ps3, ps3[:], cs["rotm"], cs["rotm"][:], qn, qn[:])
                t12 = t12_pool.next()
                kb.tt("vector", t12, t12[:, 0, :], qn, qn[:], cw, cw[:, 0, :], ALU.mult, part=True)
                kb.tt("vector", t12, t12[:, 1, :], ps3, ps3[:], cw, cw[:, 1, :], ALU.mult, part=True)
                if j == 0:
                    for h in range(2):
                        hs = slice(h * 64, (h + 1) * 64)
                        kb.tt("vector", qz, qz[hs, h, w * WIN:(w + 1) * WIN], t12, t12[hs, 0, :], t12, t12[hs, 1, :], ALU.add, part=True)
                else:
                    kb.tt("vector", dst, dap, t12, t12[:, 0, :], t12, t12[:, 1, :], ALU.add, part=True)
        if io.get("dbg_qk") is not None:
            kb.dma("sync", io["dbg_qk"], io["dbg_qk"].t[0, 0:64], qz, qz[0:64, 0, :], "dbgq", part=True)
            kb.dma("sync", io["dbg_qk"], io["dbg_qk"].t[0, 64:128], qz, qz[64:128, 1, :], "dbgq", part=True)
            kb.dma("sync", io["dbg_qk"], io["dbg_qk"].t[1], kT, kT[:, PAD:PAD + S], "dbgk", part=True)
            kb.dma("sync", io["dbg_qk"], io["dbg_qk"].t[2], vT, vT[:, PAD:PAD + S], "dbgv", part=True)
        kb.P.barrier()
        stats0 = kb.P.emit_phase()
        kb.ph = None
    return stats0


def attn_core(kb, io, hp, qz, kT, vT):
    import os
    if True:
      with contextlib.ExitStack() as ph:
        kb.ph = ph
        cs = load_consts(kb, io, ["ident_bf", "negmask", "sel_ones"])
        vpad_pool = kb.pool("sb", "vpad", int(os.environ.get("P1_NB", 3)), [128, 2, 2, 128], BF16)
        for t in vpad_pool.tiles:
            kb.memset("vector", t, t[:], 0.0)
        pt_pool = kb.pool("sb", "pt", int(os.environ.get("P1_NB", 3)), [128, 512], BF16)
        accden = kb.sb("accden", [128, 2, 2048], F32)
        rden = kb.sb("rden", [128, 2048], F32)
        attn_o = kb.pool("sb", "attn_o", 2, [128, 2048], BF16)
        psT = kb.pool("ps", "psT", int(os.environ.get("P1_NT", 2)), [128, 4, 128], F32)
        psS = kb.pool("ps", "psS", int(os.environ.get("P1_NS", 2)), [128, 512], F32)
        psO = kb.pool("ps", "psO", int(os.environ.get("P1_NO", 2)), [128, 4, 128], F32)
        mix = io["mixA_loc"]

        def stage1(u):
            d, rho, j, jj = u
            n = S // d
            nb = n // 128
            var = 1 if j == 0 else (2 if j == nb - 1 else 0)
            pT = psT.next()
            kcols = []
            for c in range(2):
                k0 = PAD + rho + d * (128 * j - 64 + 128 * c)
                kcols.append(k0)
                kb.mm(pT, pT[:, c, :], vT, vT[:, sl(k0, 128, d)], cs["ident_bf"], cs["ident_bf"][:])
            vp = vpad_pool.next()
            if MODE >= 1:
                kb.copy("vector", vp, vp[:, :, 0, 0:64], pT, pT[:, 0:2, 0:64], part=True)
            if MODE >= 2:
                kb.copy(os.environ.get("P1_CE", "vector"), vp, vp[:, :, 1, 64:128], pT, pT[:, 0:2, 64:128], part=True)
            sS = psS.next()
            kb.mm(sS, sS[:], cs["ident_bf"], cs["ident_bf"][:], cs["negmask"], cs["negmask"][:, var, :], start=True, stop=False)
            q0 = rho + d * 128 * j
            for h in range(2):
                for c in range(2):
                    sub = 2 * h + c
                    kb.mm(sS, sS[:, sub * 128:(sub + 1) * 128], kT, kT[:, sl(kcols[c], 128, d)],
                          qz, qz[:, h, sl(q0, 128, d)], start=False, stop=True)
            pt = pt_pool.next()
            if MODE >= 3:
                kb.act(pt, pt[:], sS, sS[:], AF.Exp, scale=0.125)
            return (u, vp, pt)

        def stage2(st, first_in_sb):
            u, vp, pt = st
            d, rho, j, jj = u
            if MODE < 4:
                return
            pO = psO.next()
            SUB = os.environ.get("P1_SUB", "")
            k = 0
            for h in range(2 if SUB != "den" else 0):
                for c in range(2):
                    sub = 2 * h + c
                    kb.mm(pO, pO[:, 0, :], vp, vp[:, c, h, :], pt, pt[:, sub * 128:(sub + 1) * 128], start=(k == 0), stop=(k == 3))
                    k += 1
            k = 0
            for h in range(2 if SUB != "pv" else 0):
                for c in range(2):
                    sub = 2 * h + c
                    kb.mm(pO, pO[:, 1, :], cs["sel_ones"], cs["sel_ones"][:, h, :], pt, pt[:, sub * 128:(sub + 1) * 128],
                          start=(k == 0), stop=(k == 3))
                    k += 1
            c0 = rho + d * 128 * jj
            dap = accden[:, :, sl(c0, 128, d)]
            if MODE < 5:
                return
            if first_in_sb:
                kb.copy("vector", accden, dap, pO, pO[:, 0:2, :])
            else:
                kb.tt("vector", accden, dap, accden, dap, pO, pO[:, 0:2, :], ALU.add)

        import os
        MODE = int(os.environ.get("P1_MODE", "9"))
        for sbk in range(int(os.environ.get("P1_NSB", S // 2048))):
            units = []
            for d in (1, 4, 16):
                per = 16 // d
                for rho in range(d):
                    for jj in range(per):
                        units.append((d, rho, sbk * per + jj, jj))
            pend = None
            units = units[:int(os.environ.get("P1_NU", len(units)))]
            for ui, u in enumerate(units):
                st1 = stage1(u)
                if os.environ.get("P1_NOPIPE"):
                    stage2(st1, u[0] == 1)
                    continue
                if pend is not None:
                    stage2(pend[0], pend[1])
                pend = (st1, u[0] == 1)
            if pend is not None:
                stage2(pend[0], pend[1])
            kb.recip(rden, rden[:], accden, accden[:, 1, :])
            ao = attn_o.next()
            kb.tt("gpsimd", ao, ao[:], accden, accden[:, 0, :], rden, rden[:], ALU.mult)
            kb.dma("sync", mix, mix.t.rearrange("(g c) t -> c g t", c=256)[hp * 128:(hp + 1) * 128, sbk * 4:(sbk + 1) * 4, :],
                   ao, ao[:].rearrange("p (g t) -> p g t", t=512), ao.name, part=True)
        kb.P.barrier()
        stats = kb.P.emit_phase()
        kb.ph = None
    return stats


def phase1_attn(kb, io, hp):
    import os
    with contextlib.ExitStack() as outer:
        kb.ph = outer
        qz = kb.sb("qz", [128, 2, S], BF16)
        kT = kb.sb("kT", [128, S + 2 * PAD], BF16)
        vT = kb.sb("vT", [128, S + 2 * PAD], BF16)
        st = attn_inproj(kb, io, hp, qz, kT, vT)
        if os.environ.get("P1_STOP"):
            return st
        st = attn_core(kb, io, hp, qz, kT, vT)
    return st


def phase1_ssd_in(kb, io):
    with contextlib.ExitStack() as ph:
        kb.ph = ph
        if "mixA_all" in io:
            collective(kb, "AllGather", ALU.bypass, io["mixA_loc"], io["mixA_all"], "ccA", 8)
        cs = load_consts(kb, io, ["ones_bf"])
        g_attn = kb.sb("g_attn", [128, 8], F32)
        kb.dma("sync", g_attn, g_attn[:], io["g_attn"], io["g_attn"].t, "g_attn")
        ncol = NC1 - C_XS
        w1b = kb.sb("w1b", [128, 8, ncol], BF16)
        stage = kb.pool("sb", "w1stg", 2, [128, ncol], F32)
        w1 = io["w1"]
        for kc in range(8):
            stg = stage.next()
            kb.dma("sync", stg, stg[:], w1, w1.t[kc * 128:(kc + 1) * 128, C_XS:NC1], stg.name)
            kb.ts("vector", w1b, w1b[:, kc, :], stg, stg[:], (g_attn, g_attn[:, kc:kc + 1]), None, ALU.mult, part=True)
        cw = kb.sb("convw", [128, 4, 6], F32)
        kb.dma("sync", cw, cw[:], io["convw"], io["convw"].t, "convw")
        dtc = kb.sb("dtc", [8, 2], F32)
        kb.dma("sync", dtc, dtc[:], io["dtc"], io["dtc"].t, "dtc")
        negA = kb.sb("negA", [8, 1], F32)
        kb.act(negA, negA[:], dtc, dtc[:, 1:2], AF.Exp)
        kb.ts("vector", negA, negA[:], negA, negA[:], -1.0, None, ALU.mult)
        one8 = kb.sb("one8", [8, 1], F32)
        kb.memset("vector", one8, one8[:], 1.0)
        xb_pool = kb.pool("sb", "xb", 2, [128, 8, WIN], BF16)
        sq_pool = kb.pool("sb", "sq", 1, [128, 8, WIN], BF16)
        rstd_pool = kb.pool("sb", "rstd", 2, [128, 2, WIN], F32)
        pre_pool = kb.pool("sb", "pre", 2, [128, 4, WIN], F32)
        cacc_pool = kb.pool("sb", "cacc", 2, [128, 4, WIN], F32)
        post_pool = kb.pool("sb", "post", 2, [128, 4, WIN], BF16)
        zs_pool = kb.pool("sb", "zsw", 2, [128, 2, WIN], BF16)
        dt_pool = kb.pool("sb", "dtw", 2, [8, 4, WIN], F32)
        psA = kb.pool("ps", "psA", 2, [128, WIN], F32)
        psB = kb.pool("ps", "psB", 3, [128, WIN], F32)
        xT = io["xT"]
        nwin = 33
        for w in range(nwin):
            t0 = 508 * w - 2 if w < nwin - 1 else S - 510
            lo, hi = max(t0, 0), min(t0 + WIN, S)
            xb = xb_pool.next()
            if lo > t0 or hi < t0 + WIN:
                kb.memset("vector", xb, xb[:], 0.0)
                kb.dma("gpsimd", xb, xb[:, :, lo - t0:hi - t0], xT, xT.t.rearrange("(k p) t -> p k t", p=128)[:, :, lo:hi], xb.name)
            else:
                kb.dma("gpsimd", xb, xb[:], xT, xT.t.rearrange("(k p) t -> p k t", p=128)[:, :, lo:hi], xb.name)
            sq = sq_pool.next()
            kb.act(sq, sq[:], xb, xb[:], AF.Square)
            pss = psA.next()
            for kc in range(8):
                kb.mm(pss, pss[:], cs["ones_bf"], cs["ones_bf"][:], sq, sq[:, kc, :], start=(kc == 0), stop=(kc == 7))
            rstd = rstd_pool.next()
            kb.rstd_from(rstd, rstd[:, 0, :], pss, pss[:], DM, rstd, rstd[:, 1, :], cs["eps"])
            pre = pre_pool.next()
            for ci in range(4):
                ps = psB.next()
                for kc in range(8):
                    kb.mm(ps, ps[:], w1b, w1b[:, kc, ci * 128:(ci + 1) * 128], xb, xb[:, kc, :], start=(kc == 0), stop=(kc == 7))
                kb.tt("vector", pre, pre[:, ci, :], ps, ps[:], rstd, rstd[:, 0, :], ALU.mult, part=True)
            cacc = cacc_pool.next()
            post = post_pool.next()
            NV = WIN - 4
            for ci in range(4):
                kb.act(cacc, cacc[:, ci, 0:NV], pre, pre[:, ci, 0:NV], AF.Copy, scale=(cw, cw[:, ci, 0:1]), part=True)
                for j in range(1, 5):
                    kb.stt(cacc, cacc[:, ci, 0:NV], pre, pre[:, ci, j:j + NV], (cw, cw[:, ci, j:j + 1]), cacc, cacc[:, ci, 0:NV],
                           ALU.mult, ALU.add, part=True)
            for ci in range(4):
                kb.act(post, post[:, ci, 0:NV], cacc, cacc[:, ci, 0:NV], AF.Silu, bias=(cw, cw[:, ci, 5:6]), part=True)
            vlo, vhi = max(t0 + 2, 0), min(t0 + 2 + NV, S)
            o0 = vlo - (t0 + 2)
            kb.dma("sync", io["post"], io["post"].t.rearrange("(c p) t -> p c t", p=128)[:, :, vlo:vhi], post, post[:, :, o0:o0 + (vhi - vlo)],
                   post.name, part=True)
            zs = zs_pool.next()
            for zi in range(2):
                ps = psB.next()
                c0 = (C_Z - C_XS) + zi * 128
                for kc in range(8):
                    kb.mm(ps, ps[:], w1b, w1b[:, kc, c0:c0 + 128], xb, xb[:, kc, :], start=(kc == 0), stop=(kc == 7))
                kb.tt("vector", pre, pre[:, zi, :], ps, ps[:], rstd, rstd[:, 0, :], ALU.mult, part=True)
                kb.act(zs, zs[:, zi, :], pre, pre[:, zi, :], AF.Silu, part=True)
            kb.dma("sync", io["zs"], io["zs"].t.rearrange("(c p) t -> p c t", p=128)[:, :, vlo:vhi], zs, zs[:, :, o0 + 2:o0 + 2 + (vhi - vlo)],
                   zs.name, part=True)
            ps = psB.next()
            c0 = C_DT - C_XS
            for kc in range(8):
                kb.mm(ps, ps[0:8, :], w1b, w1b[:, kc, c0:c0 + 8], xb, xb[:, kc, :], start=(kc == 0), stop=(kc == 7))
            dtw = dt_pool.next()
            kb.tt("vector", dtw, dtw[:, 0, :], ps, ps[0:8, :], rstd, rstd[0:8, 0, :], ALU.mult, part=True)
            kb.act(dtw, dtw[:, 1, :], dtw, dtw[:, 0, :], AF.Exp, bias=(dtc, dtc[:, 0:1]), part=True)
            kb.act(dtw, dtw[:, 2, :], dtw, dtw[:, 1, :], AF.Ln, bias=(one8, one8[:, 0:1]), part=True)
            kb.ts("vector", dtw, dtw[:, 3, :], dtw, dtw[:, 2, :], (negA, negA[:, 0:1]), None, ALU.mult, part=True)
            kb.dma("sync", io["dta"], io["dta"].t[0:8, vlo:vhi], dtw, dtw[:, 2, o0 + 2:o0 + 2 + (vhi - vlo)], dtw.name, part=True)
            kb.dma("sync", io["dta"], io["dta"].t[8:16, vlo:vhi], dtw, dtw[:, 3, o0 + 2:o0 + 2 + (vhi - vlo)], dtw.name, part=True)
        kb.P.barrier()
        st = kb.P.emit_phase()
        kb.ph = None
    return st


def phase1_ssd_scan(kb, io, dirn, nchunks=NCH):
    with contextlib.ExitStack() as ph:
        kb.ph = ph
        cs = load_consts(kb, io, ["ident_bf", "ident_f", "ones_f", "tri", "smask"])
        Dbc = kb.sb("Dbc", [128, 256], F32)
        kb.dma("sync", Dbc, Dbc[:], io["Dbc"], io["Dbc"].t, "Dbc")
        H = kb.sb("H", [128, 256], F32)
        Hbf = kb.sb("Hbf", [128, 256], BF16)
        kb.memset("vector", H, H[:], 0.0)
        kb.memset("vector", Hbf, Hbf[:], 0.0)
        inT = kb.pool("sb", "inT", 2, [128, 4, 128], BF16)
        dtaT = kb.pool("sb", "dtaT", 2, [16, 128], F32)
        xsb = kb.pool("sb", "xsb", 2, [128, 384], BF16)
        dta = kb.pool("sb", "dta", 2, [128, 16], F32)
        abc = kb.pool("sb", "abc", 2, [128, 4, 128], F32)
        col = kb.pool("sb", "col", 2, [128, 40], F32)
        LT = kb.pool("sb", "LT", 2, [128, 4, 128], F32)
        MT = kb.pool("sb", "MT", 2, [128, 4, 128], BF16)
        Ysb = kb.pool("sb", "Ysb", 2, [128, 256], F32)
        yout = kb.pool("sb", "yout", 2, [128, 256], F32)
        xsw = kb.pool("sb", "xsw", 2, [128, 256], BF16)
        yprev = kb.pool("sb", "yprev", 2, [128, 256], F32)
        ysum = kb.pool("sb", "ysum", 2, [128, 2, 256], F32)
        ybf = kb.pool("sb", "ybf", 2, [128, 256], BF16)
        zsT = kb.pool("sb", "zsT", 2, [128, 2, 128], BF16)
        ygT = kb.pool("sb", "ygT", 2, [128, 2, 128], BF16)
        pX = kb.ps("pX", [128, 512], F32)
        pD = kb.ps("pD", [128, 512], F32)
        pCS = kb.ps("pCS", [128, 512], F32)
        pG = kb.ps("pG", [128, 512], F32)
        pY = kb.ps("pY", [128, 512], F32)
        pYo = kb.ps("pYo", [128, 512], F32)
        pST = kb.ps("pST", [128, 512], F32)
        pYT = kb.ps("pYT", [128, 512], F32)
        post_v = io["post"].t.rearrange("(c p) t -> p c t", p=128)
        zs_v = io["zs"].t.rearrange("(c p) t -> p c t", p=128)
        order = range(nchunks) if dirn == 0 else range(NCH - 1, NCH - 1 - nchunks, -1)
        def stageA(c):
            tk = slice(c * 128, (c + 1) * 128)
            it = inT.next()
            kb.dma("sync", it, it[:], io["post"], post_v[:, :, tk], it.name)
            dT = dtaT.next()
            kb.dma("sync", dT, dT[:], io["dta"], io["dta"].t[:, tk], dT.name)
            for i in range(3):
                kb.mm(pX, pX[:, i * 128:(i + 1) * 128], it, it[:, i, :], cs["ident_bf"], cs["ident_bf"][:])
            xs = xsb.next()
            kb.copy("vector", xs, xs[:], pX, pX[:, 0:384])
            kb.mm(pD, pD[:, 0:16], dT, dT[:, :], cs["ident_f"], cs["ident_f"][0:16, 0:16])
            dt = dta.next()
            kb.copy("vector", dt, dt[:], pD, pD[:, 0:16])
            dtc = dt[:, 4 * dirn:4 * dirn + 4]
            ac = dt[:, 8 + 4 * dirn:8 + 4 * dirn + 4]
            ab = abc.next()
            for h in range(4):
                kb.act(ab, ab[:, h, :], cs["ones_f"], cs["ones_f"][:], AF.Copy, scale=(dt, dt[:, 8 + 4 * dirn + h:8 + 4 * dirn + h + 1]), part=True)
            kb.mm(pCS, pCS[:], cs["ident_f"], cs["ident_f"][:], cs["smask"], cs["smask"][:, dirn, :], start=True, stop=False)
            for h in range(4):
                kb.mm(pCS, pCS[:, h * 128:(h + 1) * 128], ab, ab[:, h, :], cs["tri"], cs["tri"][:, dirn, :], start=False, stop=True)
            kb.mm(pD, pD[:, 16:20], cs["tri"], cs["tri"][:, dirn, :], dt, ac)
            kb.mm(pD, pD[:, 20:24], cs["ones_f"], cs["ones_f"][:], dt, ac)
            cl = col.next()
            kb.copy("vector", cl, cl[:, 0:8], pD, pD[:, 16:24], part=True)
            kb.ts("vector", cl, cl[:, 8:12], cl, cl[:, 0:4], -1.0, None, ALU.mult, part=True)
            kb.act(cl, cl[:, 12:16], cl, cl[:, 0:4], AF.Exp, part=True)
            kb.tt("vector", cl, cl[:, 16:20], cl, cl[:, 4:8], cl, cl[:, 0:4], ALU.subtract, part=True)
            kb.act(cl, cl[:, 20:24], cl, cl[:, 16:20], AF.Exp, part=True)
            kb.tt("vector", cl, cl[:, 24:28], cl, cl[:, 20:24], dt, dtc, ALU.mult, part=True)
            kb.act(cl, cl[:, 28:32], cl, cl[:, 4:8], AF.Exp, part=True)
            lt = LT.next()
            for h in range(4):
                kb.act(lt, lt[:, h, :], pCS, pCS[:, h * 128:(h + 1) * 128], AF.Exp, bias=(cl, cl[:, 8 + h:9 + h]), part=True)
            kb.mm(pG, pG[:, 0:128], it, it[:, 2, :], it, it[:, 3, :])
            mt = MT.next()
            for h in range(4):
                kb.stt(mt, mt[:, h, :], pG, pG[:, 0:128], (dt, dt[:, 4 * dirn + h:4 * dirn + h + 1]), lt, lt[:, h, :], ALU.mult, ALU.mult, part=True)
            return dict(c=c, tk=tk, it=it, xs=xs, dt=dt, cl=cl, mt=mt)

        def stageB(ctx):
            c, tk, it, xs, dt, cl, mt = ctx['c'], ctx['tk'], ctx['it'], ctx['xs'], ctx['dt'], ctx['cl'], ctx['mt']
            for h in range(4):
                kb.mm(pY, pY[:, h * 64:(h + 1) * 64], mt, mt[:, h, :], xs, xs[:, h * 64:(h + 1) * 64])
            kb.mm(pYo, pYo[:, 0:256], it, it[:, 3, :], Hbf, Hbf[:])
            ysb = Ysb.next()
            kb.copy("vector", ysb, ysb[:], pY, pY[:, 0:256])
            yo = yout.next()
            for h in range(4):
                hs = slice(h * 64, (h + 1) * 64)
                kb.stt(yo, yo[:, hs], pYo, pYo[:, hs], (cl, cl[:, 12 + h:13 + h]), ysb, ysb[:, hs], ALU.mult, ALU.add, part=True)
            xw = xsw.next()
            for h in range(4):
                hs = slice(h * 64, (h + 1) * 64)
                kb.act(xw, xw[:, hs], xs, xs[:, hs], AF.Copy, scale=(cl, cl[:, 24 + h:25 + h]), part=True)
            kb.mm(pST, pST[:, 0:256], xs, xs[:, 256:384], xw, xw[:])
            for h in range(4):
                hs = slice(h * 64, (h + 1) * 64)
                kb.stt(H, H[:, hs], H, H[:, hs], (cl, cl[:, 28 + h:29 + h]), pST, pST[:, hs], ALU.mult, ALU.add)
            kb.copy("scalar", Hbf, Hbf[:], H, H[:])
            if dirn == 0:
                kb.dma("sync", io["yf"], io["yf"].t[tk, :], yo, yo[:], yo.name, part=True)
            else:
                yp = yprev.next()
                kb.dma("sync", yp, yp[:], io["yf"], io["yf"].t[tk, :], yp.name)
                zt = zsT.next()
                kb.dma("sync", zt, zt[:], io["zs"], zs_v[:, :, tk], zt.name)
                ys = ysum.next()
                kb.tt("vector", ys, ys[:, 0, :], xs, xs[:, 0:256], Dbc, Dbc[:], ALU.mult, part=True)
                kb.tt("vector", ys, ys[:, 1, :], yo, yo[:], yp, yp[:], ALU.add, part=True)
                yb = ybf.next()
                kb.tt("vector", yb, yb[:], ys, ys[:, 0, :], ys, ys[:, 1, :], ALU.add)
                for i in range(2):
                    kb.mm(pYT, pYT[:, i * 128:(i + 1) * 128], yb, yb[:, i * 128:(i + 1) * 128], cs["ident_bf"], cs["ident_bf"][:])
                yg = ygT.next()
                kb.tt("vector", yg, yg[:], pYT, pYT[:, 0:256].rearrange("p (c t) -> p c t", c=2), zt, zt[:], ALU.mult)
                mv = io["mixS_loc"].t.rearrange("(g c) t -> c g t", c=256)
                for i in range(2):
                    kb.dma("sync", io["mixS_loc"], mv[i * 128:(i + 1) * 128, c // 4, (c % 4) * 128:(c % 4 + 1) * 128], yg, yg[:, i, :],
                           yg.name, part=True)
        pend = None
        for c in order:
            ctx = stageA(c)
            if pend is not None:
                stageB(pend)
            pend = ctx
        stageB(pend)
        kb.P.barrier()
        st = kb.P.emit_phase()
        kb.ph = None
    return st


def router_setup(kb, io, L):
    r = {}
    r["gbc"] = kb.sb("gffn_bc", [128, DM], F32)
    kb.dma("sync", r["gbc"], r["gbc"][:], io["g_ffn_bc%d" % L], io["g_ffn_bc%d" % L].t, "gffn_bc")
    gcol = kb.sb("gffn_col", [128, 8], F32)
    kb.dma("sync", gcol, gcol[:], io["g_ffn_col%d" % L], io["g_ffn_col%d" % L].t, "gffn_col")
    wr = kb.sb("wr", [128, 8, NE], F32)
    kb.dma("sync", wr, wr[:], io["wr%d" % L], io["wr%d" % L].t.rearrange("(k p) e -> p k e", p=128), "wr")
    r["wrg"] = kb.sb("wrg", [128, 8, NE], F32)
    for kc in range(8):
        kb.ts("vector", r["wrg"], r["wrg"][:, kc, :], wr, wr[:, kc, :], (gcol, gcol[:, kc:kc + 1]), None, ALU.mult, part=True)
    r["affT"] = kb.sb("affT", [NE, 4096], F32)
    r["sqj"] = kb.pool("sb", "sqj", 2, [128, DM], BF16)
    r["st"] = kb.pool("sb", "rst", 2, [128, 8], F32)
    r["h2"] = kb.pool("sb", "h2t", 2, [128, DM], BF16)
    r["xT"] = kb.pool("sb", "x1T", 2, [128, 8, 128], F32)
    r["lg"] = kb.pool("sb", "lg", 2, [128, 3, NE], F32)
    r["pXT"] = kb.pool("ps", "pXT", 2, [128, 4, 128], F32)
    r["pL"] = kb.ps("pL", [128, 512], F32)
    return r


def router_tile(kb, io, cs, r, x1, i):
    st = r["st"].next()
    sqj = r["sqj"].next()
    kb.act(sqj, sqj[:], x1, x1[:], AF.Square, accum=(st, st[:, 0:1]))
    kb.rstd_from(st, st[:, 2:3], st, st[:, 0:1], DM, st, st[:, 1:2], cs["eps"])
    h2 = r["h2"].next()
    kb.stt(h2, h2[:], x1, x1[:], (st, st[:, 2:3]), r["gbc"], r["gbc"][:], ALU.mult, ALU.mult)
    kb.dma("sync", io["h2_loc"], io["h2_loc"].t[i * 128:(i + 1) * 128, :], h2, h2[:], h2.name, part=True)
    xT = r["xT"].next()
    for half in range(2):
        pX = r["pXT"].next()
        for k4 in range(4):
            kc = half * 4 + k4
            kb.mm(pX, pX[:, k4, :], x1, x1[:, kc * 128:(kc + 1) * 128], cs["ident_f"], cs["ident_f"][:])
        kb.copy("vector", xT, xT[:, half * 4:(half + 1) * 4, :], pX, pX[:], part=True)
    pL = r["pL"]
    for kc in range(8):
        kb.mm(pL, pL[:, 0:NE], xT, xT[:, kc, :], r["wrg"], r["wrg"][:, kc, :], start=(kc == 0), stop=(kc == 7))
    lg = r["lg"].next()
    kb.ts("vector", lg, lg[:, 0, :], pL, pL[:, 0:NE], (st, st[:, 2:3]), None, ALU.mult, part=True)
    kb.P.op("vector", lambda e: e.tensor_reduce(out=st[:, 3:4], in_=lg[:, 0, :], axis=AX.X, op=ALU.max), reads=[lg.b], partial=[st.b])
    kb.ts("vector", st, st[:, 4:5], st, st[:, 3:4], -1.0, None, ALU.mult, part=True)
    kb.act(lg, lg[:, 1, :], lg, lg[:, 0, :], AF.Exp, bias=(st, st[:, 4:5]), accum=(st, st[:, 5:6]), part=True)
    kb.P.op("vector", lambda e: e.reciprocal(out=st[:, 6:7], in_=st[:, 5:6]), reads=[st.b], partial=[st.b])
    kb.ts("vector", lg, lg[:, 2, :], lg, lg[:, 1, :], (st, st[:, 6:7]), None, ALU.mult, part=True)
    kb.mm(pL, pL[0:NE, 128:256], lg, lg[:, 2, :], cs["ident_f"], cs["ident_f"][:])
    kb.copy("vector", r["affT"], r["affT"][:, i * 128:(i + 1) * 128], pL, pL[0:NE, 128:256], part=True)


def phase2(kb, io, ntiles=32):
    with contextlib.ExitStack() as ph:
        kb.ph = ph
        cs = load_consts(kb, io, ["ident_f", "ones_bf"])
        r = router_setup(kb, io, 0)
        gwo = kb.sb("gwo", [128, 16], F32)
        kb.dma("sync", gwo, gwo[:], io["g_wo"], io["g_wo"].t, "gwo")
        wob = kb.sb("wob", [128, 16, DM], BF16)
        stage = kb.pool("sb", "wostg", 2, [128, DM], F32)
        for ck in range(16):
            stg = stage.next()
            kb.dma("sync", stg, stg[:], io["wo"], io["wo"].t[ck * 128:(ck + 1) * 128, :], stg.name)
            kb.ts("vector", wob, wob[:, ck, :], stg, stg[:], (gwo, gwo[:, ck:ck + 1]), None, ALU.mult, part=True)
        midx = kb.sb("mixidx", [128, 8, 16], I32)
        kb.dma("sync", midx, midx[:], io["mixidx"], io["mixidx"].t, "mixidx")
        mixT = kb.pool("sb", "mixT", 2, [128, 16, WIN], BF16)
        ysq = kb.pool("sb", "ysq", 2, [128, 8, 128], BF16)
        xt = kb.pool("sb", "xt", 2, [128, DM], F32)
        x1p = kb.pool("sb", "x1", 2, [128, DM], F32)
        rsy = kb.pool("sb", "rsy", 2, [128, 4], F32)
        pA = kb.ps("pA", [128, 2, 512], F32)
        pS = kb.ps("pS", [128, 2, 512], F32)
        pq = kb.ps("pq", [128, 512], F32)
        ssd_ck = [q * 4 + j for q in range(4) for j in (2, 3)]
        att_ck = [q * 4 + j for q in range(4) for j in (0, 1)]
        for w in range((ntiles + 3) // 4):
            mt = mixT.next()
            for ck in range(16):
                src = io["mixA_all"] if ck % 4 < 2 else io["mixS_all"]
                kb.P.dma("gpsimd", (lambda e, mt=mt, ck=ck, w=w, src=src: e.indirect_dma_start(
                    out=mt[:, ck, :], out_offset=None, in_=src.t,
                    in_offset=bass.IndirectOffsetOnAxis(ap=midx[:, w, ck:ck + 1], axis=0))),
                    mt.name, reads=[src.b, midx.b], partial=[mt.b])
            for j in range(min(4, ntiles - 4 * w)):
                i = 4 * w + j
                tk = slice(j * 128, (j + 1) * 128)
                ys = ysq.next()
                for n, ck in enumerate(ssd_ck):
                    kb.act(ys, ys[:, n, :], mt, mt[:, ck, tk], AF.Square, part=True)
                for n in range(8):
                    kb.mm(pq, pq[:, 0:1], ys, ys[:, n, :], cs["ones_bf"], cs["ones_bf"][:, 0:1], start=(n == 0), stop=(n == 7))
                rs = rsy.next()
                kb.rstd_from(rs, rs[:, 1:2], pq, pq[:, 0:1], 1024, rs, rs[:, 0:1], cs["eps"])
                for half in range(2):
                    for n, ck in enumerate(att_ck):
                        kb.mm(pA, pA[:, half, :], mt, mt[:, ck, tk], wob, wob[:, ck, half * 512:(half + 1) * 512], start=(n == 0), stop=(n == 7))
                    for n, ck in enumerate(ssd_ck):
                        kb.mm(pS, pS[:, half, :], mt, mt[:, ck, tk], wob, wob[:, ck, half * 512:(half + 1) * 512], start=(n == 0), stop=(n == 7))
                x = xt.next()
                kb.dma("sync", x, x[:], io["x_tok"], io["x_tok"].t[i * 128:(i + 1) * 128, :], x.name)
                x1 = x1p.next()
                kb.stt(x1, x1[:], pS, pS[:].rearrange("p a b -> p (a b)"), (rs, rs[:, 1:2]), x, x[:], ALU.mult, ALU.add)
                kb.tt("vector", x1, x1[:], x1, x1[:], pA, pA[:].rearrange("p a b -> p (a b)"), ALU.add)
                kb.dma("sync", io["x1_loc"], io["x1_loc"].t[i * 128:(i + 1) * 128, :], x1, x1[:], x1.name, part=True)
                router_tile(kb, io, cs, r, x1, i)
        kb.dma("sync", io["aff_loc"], io["aff_loc"].t, r["affT"], r["affT"][:], "affT")
        kb.P.barrier()
        st = kb.P.emit_phase()
        kb.ph = None
    return st


def zero_ydense(kb, io):
    zt = kb.sb("zt", [128, 4, DM], F32)
    kb.memset("gpsimd", zt, zt[:], 0.0)
    yv = io["ydense"].t.rearrange("(a p) d -> p a d", p=128)
    for a in range(32):
        kb.dma("sync", io["ydense"], yv[:, a * 4:(a + 1) * 4, :], zt, zt[:], "zt", part=True)


def moe_topk(kb, io, cs, L):
    aidx = kb.sb("affidx", [128, 1], I32)
    kb.dma("sync", aidx, aidx[:], io["affidx"], io["affidx"].t, "affidx")
    arow = kb.sb("arow", [128, 4096], F32)
    kb.P.dma("gpsimd", lambda e: e.indirect_dma_start(out=arow[0:16, :], out_offset=None, in_=io["aff_all"].t,
             in_offset=bass.IndirectOffsetOnAxis(ap=aidx[0:16, 0:1], axis=0)), "arow", reads=[io["aff_all"].b, aidx.b], writes=[arow.b])
    kb.dma("sync", io["aff_my"], io["aff_my"].t, arow, arow[0:16, :], "arow")
    collective(kb, "AllGather", ALU.bypass, io["h2_loc"], io["h2_all"], "cc2", 8)
    A = kb.sb("A", [128, 4, 128], F32)
    kb.dma("sync", A, A[:], io["aff_my"], io["aff_my"].t.rearrange("(e q) (a j) -> (q a) e j", e=4, j=128), "A")
    lohi = kb.sb("lohi", [128, 16], F32)
    cmp_ = kb.sb("cmp", [128, 128], F32)
    cnt = kb.sb("cnt", [128, 8], F32)
    pc = kb.ps("pc", [128, 512], F32)
    kb.memset("vector", lohi, lohi[:, 0:4], 0.0, part=True)
    kb.memset("vector", lohi, lohi[:, 4:8], 1.0, part=True)
    for it in range(30):
        kb.tt("vector", lohi, lohi[:, 8:12], lohi, lohi[:, 0:4], lohi, lohi[:, 4:8], ALU.add)
        kb.ts("vector", lohi, lohi[:, 8:12], lohi, lohi[:, 8:12], 0.5, None, ALU.mult)
        for e in range(4):
            kb.ts("vector", cmp_, cmp_[:], A, A[:, e, :], (lohi, lohi[:, 8 + e:9 + e]), 0.0, ALU.is_ge, op1=ALU.add, accum=(cnt, cnt[:, e:e + 1]))
        kb.mm(pc, pc[:, 0:4], cs["ones_f"], cs["ones_f"][:], cnt, cnt[:, 0:4])
        kb.ts("vector", lohi, lohi[:, 12:16], pc, pc[:, 0:4], float(CAP), None, ALU.is_ge)
        kb.tt("vector", cnt, cnt[:, 4:8], lohi, lohi[:, 8:12], lohi, lohi[:, 0:4], ALU.subtract)
        kb.tt("vector", cnt, cnt[:, 4:8], cnt, cnt[:, 4:8], lohi, lohi[:, 12:16], ALU.mult)
        kb.tt("vector", lohi, lohi[:, 0:4], lohi, lohi[:, 0:4], cnt, cnt[:, 4:8], ALU.add)
        kb.tt("vector", cnt, cnt[:, 4:8], lohi, lohi[:, 4:8], lohi, lohi[:, 8:12], ALU.subtract)
        kb.tt("vector", cnt, cnt[:, 4:8], cnt, cnt[:, 4:8], lohi, lohi[:, 12:16], ALU.mult)
        kb.tt("vector", lohi, lohi[:, 4:8], lohi, lohi[:, 8:12], cnt, cnt[:, 4:8], ALU.add)
    mask = kb.sb("mask", [128, 4, 128], F32)
    incl = kb.sb("incl", [128, 4, 128], F32)
    slot = kb.sb("slot", [128, 4, 128], F32)
    sloti = kb.sb("sloti", [128, 4, 128], I32)
    pairs = kb.sb("pairs", [128, 4, 128, 3], F32)
    tot = kb.sb("tot", [128, 8], F32)
    for e in range(4):
        kb.ts("vector", mask, mask[:, e, :], A, A[:, e, :], (lohi, lohi[:, e:e + 1]), None, ALU.is_ge, part=True)
        kb.P.op("vector", lambda en, e=e: en.tensor_tensor_scan(out=incl[:, e, :], data0=cs["ones_f"][:], data1=mask[:, e, :], initial=0.0,
                                                               op0=ALU.mult, op1=ALU.add), reads=[cs["ones_f"].b, mask.b], partial=[incl.b])
        kb.copy("vector", tot, tot[:, e:e + 1], incl, incl[:, e, 127:128], part=True)
    kb.mm(pc, pc[:, 8:12], cs["ustrict"], cs["ustrict"][:], tot, tot[:, 0:4])
    kb.copy("vector", tot, tot[:, 4:8], pc, pc[:, 8:12], part=True)
    for e in range(4):
        kb.tt("vector", slot, slot[:, e, :], incl, incl[:, e, :], mask, mask[:, e, :], ALU.subtract, part=True)
        kb.ts("vector", slot, slot[:, e, :], slot, slot[:, e, :], (tot, tot[:, 4 + e:5 + e]), float(e * CAP), ALU.add, op1=ALU.add, part=True)
        kb.ts("vector", incl, incl[:, e, :], mask, mask[:, e, :], -1.0e6, 1.0e6, ALU.mult, op1=ALU.add, part=True)
        kb.tt("vector", slot, slot[:, e, :], slot, slot[:, e, :], incl, incl[:, e, :], ALU.add, part=True)
        kb.ts("vector", incl, incl[:, e, :], slot, slot[:, e, :], float((e + 1) * CAP), 1.0e6, ALU.is_ge, op1=ALU.mult, part=True)
        kb.tt("vector", slot, slot[:, e, :], slot, slot[:, e, :], incl, incl[:, e, :], ALU.add, part=True)
        kb.copy("vector", sloti, sloti[:, e, :], slot, slot[:, e, :], part=True)
        kb.copy("gpsimd", pairs, pairs[:, e, :, 0], cs["iota_tok"], cs["iota_tok"][:], part=True)
        kb.copy("gpsimd", pairs, pairs[:, e, :, 1], A, A[:, e, :], part=True)
        kb.copy("gpsimd", pairs, pairs[:, e, :, 2], cs["iota_h2row"], cs["iota_h2row"][:], part=True)
    rc = {}

    def breg(en):
        if "r" not in rc:
            rc["r"] = en.to_reg(4 * CAP - 1)
        return rc["r"]

    for e in range(4):
        for j in range(128):
            kb.P.dma("gpsimd", (lambda en, e=e, j=j: en.indirect_dma_start(
                out=io["sel"].t, out_offset=bass.IndirectOffsetOnAxis(ap=sloti[:, e, j:j + 1], axis=0),
                in_=pairs[:, e, j, :], in_offset=None, bounds_check=breg(en), oob_is_err=False)),
                "selsc", reads=[pairs.b, sloti.b], partial=[io["sel"].b])


def moe_phase(kb, io, L):
    with contextlib.ExitStack() as ph:
        kb.ph = ph
        cs = load_consts(kb, io, ["ident_bf", "ones_f", "ustrict", "iota_tok", "iota_h2row"])
        with contextlib.ExitStack() as ph2:
            kb.ph = ph2
            moe_topk(kb, io, cs, L)
            kb.P.barrier()
            kb.P.emit_phase()
        kb.ph = ph
        cs = load_consts(kb, io, ["ident_bf"])
        xeT = kb.sb("xeT", [128, 8, CAP], BF16)
        hidT = kb.sb("hidT", [128, 16, CAP], BF16)
        wdb = kb.sb("wdb", [128, 16, DM], BF16)
        wgb = kb.pool("sb", "wgb", 2, [128, 8, 512], BF16)
        wub = kb.pool("sb", "wub", 2, [128, 8, 512], BF16)
        selT = kb.pool("sb", "selT", 2, [128, 16, 3], F32)
        toki = kb.pool("sb", "toki", 2, [128, 2, 16], I32)
        xe = kb.pool("sb", "xe", 3, [128, DM], BF16)
        sg = kb.pool("sb", "sg", 2, [128, 512], BF16)
        ye = kb.pool("sb", "ye", 2, [128, DM], F32)
        pT = kb.pool("ps", "pT", 2, [128, 4, 128], F32)
        pGt = kb.pool("ps", "pGt", 2, [128, 512], F32)
        pUp = kb.pool("ps", "pUp", 2, [128, 512], F32)
        pYe = kb.ps("pYe", [128, 2, 512], F32)
        wg, wu, wd = io["wg%d" % L], io["wu%d" % L], io["wd%d" % L]
        for e in range(4):
            sT = selT.next()
            kb.dma("sync", sT, sT[:], io["sel"], io["sel"].t[e * CAP:(e + 1) * CAP, :].rearrange("(k p) c -> p k c", p=128), sT.name)
            ti = toki.next()
            kb.copy("vector", ti, ti[:, 0, :], sT, sT[:, :, 0], part=True)
            kb.copy("vector", ti, ti[:, 1, :], sT, sT[:, :, 2], part=True)
            for q in range(4):
                kb.dma("gpsimd", wdb, wdb[:, q * 4:(q + 1) * 4, :], wd, wd.t[e, q * 512:(q + 1) * 512, :].rearrange("(k p) d -> p k d", p=128),
                       "wdb", part=True)
            for k in range(16):
                x = xe.next()
                kb.P.dma("gpsimd", (lambda en, x=x, ti=ti, k=k: en.indirect_dma_start(
                    out=x[:], out_offset=None, in_=io["h2_all"].t, in_offset=bass.IndirectOffsetOnAxis(ap=ti[:, 1, k:k + 1], axis=0))),
                    x.name, reads=[io["h2_all"].b, ti.b], writes=[x.b])
                for half in range(2):
                    p = pT.next()
                    for k4 in range(4):
                        kc = half * 4 + k4
                        kb.mm(p, p[:, k4, :], x, x[:, kc * 128:(kc + 1) * 128], cs["ident_bf"], cs["ident_bf"][:])
                    kb.copy("vector", xeT, xeT[:, half * 4:(half + 1) * 4, k * 128:(k + 1) * 128], p, p[:], part=True)
            for fg in range(4):
                wgt = wgb.next()
                wut = wub.next()
                kb.dma("gpsimd", wgt, wgt[:], wg, wg.t[e, :, fg * 512:(fg + 1) * 512].rearrange("(k p) f -> p k f", p=128), wgt.name)
                kb.dma("gpsimd", wut, wut[:], wu, wu.t[e, :, fg * 512:(fg + 1) * 512].rearrange("(k p) f -> p k f", p=128), wut.name)
                for f4 in range(4):
                    fi = fg * 4 + f4
                    for win in range(4):
                        pg = pGt.next()
                        pu = pUp.next()
                        for kc in range(8):
                            kb.mm(pg, pg[:], wgt, wgt[:, kc, f4 * 128:(f4 + 1) * 128], xeT, xeT[:, kc, win * 512:(win + 1) * 512],
                                  start=(kc == 0), stop=(kc == 7))
                        for kc in range(8):
                            kb.mm(pu, pu[:], wut, wut[:, kc, f4 * 128:(f4 + 1) * 128], xeT, xeT[:, kc, win * 512:(win + 1) * 512],
                                  start=(kc == 0), stop=(kc == 7))
                        s_ = sg.next()
                        kb.act(s_, s_[:], pg, pg[:], AF.Silu)
                        kb.tt("vector", hidT, hidT[:, fi, win * 512:(win + 1) * 512], s_, s_[:], pu, pu[:], ALU.mult, part=True)
            for k in range(16):
                for half in range(2):
                    for fi in range(16):
                        kb.mm(pYe, pYe[:, half, :], hidT, hidT[:, fi, k * 128:(k + 1) * 128], wdb, wdb[:, fi, half * 512:(half + 1) * 512],
                              start=(fi == 0), stop=(fi == 15))
                y = ye.next()
                kb.ts("vector", y, y[:], pYe, pYe[:].rearrange("p a b -> p (a b)"), (sT, sT[:, k, 1:2]), None, ALU.mult)
                kb.P.dma("gpsimd", (lambda en, y=y, ti=ti, k=k: en.indirect_dma_start(
                    out=io["ydense"].t, out_offset=bass.IndirectOffsetOnAxis(ap=ti[:, 0, k:k + 1], axis=0), in_=y[:], in_offset=None,
                    compute_op=ALU.add)), y.name, reads=[y.b, ti.b], writes=[io["ydense"].b])
        kb.P.barrier()
        st = kb.P.emit_phase()
        kb.ph = None
    return st


def phase4(kb, io):
    NT = 32
    TL = 4096
    with contextlib.ExitStack() as ph:
        kb.ph = ph
        cs = load_consts(kb, io, ["ident_bf", "ident_f"])
        hnT = kb.sb("hnT", [128, 8, TL + 2], BF16)
        mT = kb.sb("mT", [128, 8, TL], BF16)
        with contextlib.ExitStack() as ph2:
            kb.ph = ph2
            gbc = kb.sb("gconv_bc", [128, DM], F32)
            kb.dma("sync", gbc, gbc[:], io["g_conv_bc"], io["g_conv_bc"].t, "gconv_bc")
            rows = kb.sb("myrows", [128, 33], I32)
            kb.dma("sync", rows, rows[:], io["myrows"], io["myrows"].t, "myrows")
            hidx = kb.sb("haloidx", [128, 1], I32)
            kb.dma("sync", hidx, hidx[:], io["haloidx"], io["haloidx"].t, "haloidx")
            hmsk = kb.sb("halomsk", [128, 1], F32)
            kb.dma("sync", hmsk, hmsk[:], io["halomsk"], io["halomsk"].t, "halomsk")
            x1p = kb.pool("sb", "x1t", 2, [128, DM], F32)
            ysp = kb.pool("sb", "yst", 2, [128, DM], BF16)
            x2p = kb.pool("sb", "x2t", 2, [128, DM], F32)
            sqj = kb.pool("sb", "sqj", 2, [128, DM], BF16)
            stp = kb.pool("sb", "st4", 2, [128, 4], F32)
            hnp = kb.pool("sb", "hn", 2, [128, DM], BF16)
            pT = kb.pool("ps", "pT4", 2, [128, 4, 128], F32)
            for i in range(NT + 1):
                x1 = x1p.next()
                ys = ysp.next()
                if i < NT:
                    kb.dma("sync", x1, x1[:], io["x1_loc"], io["x1_loc"].t[i * 128:(i + 1) * 128, :], x1.name)
                else:
                    kb.P.dma("gpsimd", (lambda en, x1=x1: en.indirect_dma_start(out=x1[:], out_offset=None, in_=io["edges_all"].t,
                             in_offset=bass.IndirectOffsetOnAxis(ap=hidx[:, 0:1], axis=0))), x1.name, reads=[io["edges_all"].b, hidx.b], writes=[x1.b])
                kb.P.dma("gpsimd", (lambda en, ys=ys, i=i: en.indirect_dma_start(out=ys[:], out_offset=None, in_=io["ysum_all"].t,
                         in_offset=bass.IndirectOffsetOnAxis(ap=rows[:, i:i + 1], axis=0))), ys.name, reads=[io["ysum_all"].b, rows.b], writes=[ys.b])
                x2 = x2p.next()
                kb.tt("vector", x2, x2[:], x1, x1[:], ys, ys[:], ALU.add)
                if i == NT:
                    kb.ts("vector", x2, x2[:], x2, x2[:], (hmsk, hmsk[:, 0:1]), None, ALU.mult)
                else:
                    kb.dma("sync", io["x2_loc"], io["x2_loc"].t[i * 128:(i + 1) * 128, :], x2, x2[:], x2.name, part=True)
                st = stp.next()
                sq = sqj.next()
                kb.act(sq, sq[:], x2, x2[:], AF.Square, accum=(st, st[:, 0:1]))
                kb.rstd_from(st, st[:, 2:3], st, st[:, 0:1], DM, st, st[:, 1:2], cs["eps"])
                hn = hnp.next()
                kb.stt(hn, hn[:], x2, x2[:], (st, st[:, 2:3]), gbc, gbc[:], ALU.mult, ALU.mult)
                for half in range(2):
                    p = pT.next()
                    for k4 in range(4):
                        kc = half * 4 + k4
                        kb.mm(p, p[:, k4, :], hn, hn[:, kc * 128:(kc + 1) * 128], cs["ident_bf"], cs["ident_bf"][:])
                    if i < NT:
                        kb.copy("vector", hnT, hnT[:, half * 4:(half + 1) * 4, 1 + i * 128:1 + (i + 1) * 128], p, p[:], part=True)
                    else:
                        kb.copy("vector", hnT, hnT[:, half * 4:(half + 1) * 4, 0:1], p, p[:, :, 0:1], part=True)
                        kb.copy("vector", hnT, hnT[:, half * 4:(half + 1) * 4, TL + 1:TL + 2], p, p[:, :, 1:2], part=True)
            kb.P.barrier()
            kb.P.emit_phase()
        with contextlib.ExitStack() as ph2:
            kb.ph = ph2
            w2b = kb.pool("sb", "w2b", 2, [128, 8, 384], BF16)
            cw = kb.sb("cw3", [128, 8, 3], F32)
            kb.dma("sync", cw, cw[:], io["cw3"], io["cw3"].t, "cw3")
            csb = kb.pool("sb", "csb", 2, [128, 512], F32)
            vv = kb.pool("sb", "vv", 2, [128, 512], F32)
            ca = kb.pool("sb", "ca", 2, [128, 2, 512], F32)
            pB = kb.pool("ps", "pB4", 2, [128, 512], F32)
            pC = kb.pool("ps", "pC4", 2, [128, 512], F32)
            pU = kb.pool("ps", "pU4", 2, [128, 512], F32)
            w2 = io["w2"]
            nwin = 9
            for cc in range(8):
                wt = w2b.next()
                for part in range(3):
                    kb.dma("gpsimd", wt, wt[:, :, part * 128:(part + 1) * 128],
                           w2, w2.t[:, part * DM + cc * 128: part * DM + (cc + 1) * 128].rearrange("(k p) f -> p k f", p=128), wt.name, part=True)
                for w in range(nwin):
                    c0 = 510 * w if w < nwin - 1 else TL + 2 - 512
                    pb, pc, pu = pB.next(), pC.next(), pU.next()
                    for (pp, part) in ((pb, 0), (pc, 1), (pu, 2)):
                        for kc in range(8):
                            kb.mm(pp, pp[:], wt, wt[:, kc, part * 128:(part + 1) * 128], hnT, hnT[:, kc, c0:c0 + 512], start=(kc == 0), stop=(kc == 7))
                    cb = csb.next()
                    kb.copy("scalar", cb, cb[:], pc, pc[:])
                    v = vv.next()
                    kb.tt("vector", v, v[:], cb, cb[:], pu, pu[:], ALU.mult)
                    a = ca.next()
                    kb.act(a, a[:, 0, 0:510], v, v[:, 0:510], AF.Copy, scale=(cw, cw[:, cc, 0:1]), part=True)
                    kb.stt(a, a[:, 0, 0:510], v, v[:, 1:511], (cw, cw[:, cc, 1:2]), a, a[:, 0, 0:510], ALU.mult, ALU.add, part=True)
                    kb.stt(a, a[:, 1, 0:510], v, v[:, 2:512], (cw, cw[:, cc, 2:3]), a, a[:, 0, 0:510], ALU.mult, ALU.add, part=True)
                    kb.tt("vector", mT, mT[:, cc, c0:c0 + 510], a, a[:, 1, 0:510], pb, pb[:, 1:511], ALU.mult, part=True)
            kb.P.barrier()
            kb.P.emit_phase()
        with contextlib.ExitStack() as ph2:
            kb.ph = ph2
            cs = load_consts(kb, io, ["ident_f"])
            r = router_setup(kb, io, 1)
            w3b = kb.sb("w3b", [128, 8, DM], BF16)
            kb.dma("gpsimd", w3b, w3b[:], io["w3"], io["w3"].t.rearrange("(k p) d -> p k d", p=128), "w3b")
            x2p = kb.pool("sb", "x2r", 2, [128, DM], F32)
            x3p = kb.pool("sb", "x3", 2, [128, DM], F32)
            pO = kb.ps("pO4", [128, 2, 512], F32)
            for i in range(NT):
                for half in range(2):
                    for cc in range(8):
                        kb.mm(pO, pO[:, half, :], mT, mT[:, cc, i * 128:(i + 1) * 128], w3b, w3b[:, cc, half * 512:(half + 1) * 512],
                              start=(cc == 0), stop=(cc == 7))
                x2 = x2p.next()
                kb.dma("sync", x2, x2[:], io["x2_loc"], io["x2_loc"].t[i * 128:(i + 1) * 128, :], x2.name)
                x3 = x3p.next()
                kb.tt("vector", x3, x3[:], x2, x2[:], pO, pO[:].rearrange("p a b -> p (a b)"), ALU.add)
                kb.dma("sync", io["x1_loc"], io["x1_loc"].t[i * 128:(i + 1) * 128, :], x3, x3[:], x3.name, part=True)
                router_tile(kb, io, cs, r, x3, i)
            kb.dma("sync", io["aff_loc"], io["aff_loc"].t, r["affT"], r["affT"][:], "affT")
            kb.P.barrier()
            st = kb.P.emit_phase()
        kb.ph = None
    return st


def final_phase(kb, io):
    with contextlib.ExitStack() as ph:
        kb.ph = ph
        rows = kb.sb("myrows", [128, 33], I32)
        kb.dma("sync", rows, rows[:], io["myrows"], io["myrows"].t, "myrows")
        x1p = kb.pool("sb", "x1t", 2, [128, DM], F32)
        ysp = kb.pool("sb", "yst", 2, [128, DM], BF16)
        op = kb.pool("sb", "ot", 2, [128, DM], F32)
        for i in range(32):
            x1 = x1p.next()
            ys = ysp.next()
            kb.dma("sync", x1, x1[:], io["x1_loc"], io["x1_loc"].t[i * 128:(i + 1) * 128, :], x1.name)
            kb.P.dma("gpsimd", (lambda en, ys=ys, i=i: en.indirect_dma_start(out=ys[:], out_offset=None, in_=io["ysum_all"].t,
                     in_offset=bass.IndirectOffsetOnAxis(ap=rows[:, i:i + 1], axis=0))), ys.name, reads=[io["ysum_all"].b, rows.b], writes=[ys.b])
            o = op.next()
            kb.tt("vector", o, o[:], x1, x1[:], ys, ys[:], ALU.add)
            kb.dma("sync", io["out"], io["out"].t[i * 128:(i + 1) * 128, :], o, o[:], o.name, part=True)
        kb.P.barrier()
        st = kb.P.emit_phase()
        kb.ph = None
    return st


def collective(kb, kind, op, src, dst, key, nchunks=1):
    rin = src.t.shape[0] // nchunks
    rout = dst.t.shape[0] // nchunks
    for c in range(nchunks):
        sap = src.t[c * rin:(c + 1) * rin, :]
        dap = dst.t[c * rout:(c + 1) * rout, :]
        kb.P.dma("gpsimd", (lambda e, sap=sap, dap=dap: e.collective_compute(kind, op, replica_groups=GROUPS, ins=[sap.opt()], outs=[dap.opt()])),
                 key, reads=[src.b], writes=[] if nchunks > 1 else [dst.b], partial=[dst.b] if nchunks > 1 else [], inc=1)


IN_SPECS = {
    "xT": ([DM, S], F32), "x_tok": ([4096, DM], F32), "w1": ([DM, NC1], F32), "g_attn": ([128, 8], F32), "gqk": ([128, 2], F32),
    "convw": ([128, 4, 6], F32), "dtc": ([8, 2], F32), "Dbc": ([128, 256], F32), "wo": ([2048, DM], F32), "g_wo": ([128, 16], F32),
    "mixidx": ([128, 8, 16], I32), "wr0": ([DM, NE], F32), "wr1": ([DM, NE], F32), "g_ffn_col0": ([128, 8], F32),
    "g_ffn_col1": ([128, 8], F32), "g_ffn_bc0": ([128, DM], F32), "g_ffn_bc1": ([128, DM], F32), "affidx": ([128, 1], I32),
    "myrows": ([128, 33], I32), "haloidx": ([128, 1], I32), "halomsk": ([128, 1], F32), "g_conv_bc": ([128, DM], F32),
    "w2": ([DM, 3 * DM], F32), "cw3": ([128, 8, 3], F32), "w3": ([DM, DM], F32),
    "wg0": ([4, DM, FF], F32), "wu0": ([4, DM, FF], F32), "wd0": ([4, FF, DM], F32),
    "wg1": ([4, DM, FF], F32), "wu1": ([4, DM, FF], F32), "wd1": ([4, FF, DM], F32),
}
CONST_NAMES = ["ident_bf", "ident_f", "negmask", "blockones", "rotm", "sel_ones", "ones_bf", "ones_f", "tri", "smask", "ustrict",
               "iota_tok", "iota_h2row", "cosf", "sinf"]
SCRATCH = {
    "mixA_loc": ([8192, 512], BF16), "mixA_all": ([32768, 512], BF16), "mixS_loc": ([8192, 512], BF16), "mixS_all": ([32768, 512], BF16), "post": ([512, S], BF16), "zs": ([256, S], BF16),
    "dta": ([16, S], F32), "yf": ([S, 256], F32), "x1_loc": ([4096, DM], F32), "x2_loc": ([4096, DM], F32),
    "h2_loc": ([4096, DM], BF16), "h2_all": ([S, DM], BF16), "aff_loc": ([NE, 4096], F32), "aff_all": ([4 * NE, 4096], F32),
    "aff_my": ([NE, 4096], F32), "sel": ([4 * CAP, 3], F32), "ydense": ([S, DM], F32), "ydense_bf": ([S, DM], BF16), "ysum_all": ([S, DM], BF16),
    "edges_loc": ([2, DM], F32), "edges_all": ([8, DM], F32),
}


def moe_allreduce(kb, io, key):
    for a in range(16):
        rs = slice(a * 1024, (a + 1) * 1024)
        kb.dma("gpsimd", io["ydense_bf"], io["ydense_bf"].t[rs, :], io["ydense"], io["ydense"].t[rs, :], "ycast", part=True)
    collective(kb, "AllReduce", ALU.add, io["ydense_bf"], io["ysum_all"], key, 8)


def misc_phase(kb, fn):
    with contextlib.ExitStack() as ph:
        kb.ph = ph
        fn()
        kb.P.barrier()
        st = kb.P.emit_phase()
        kb.ph = None
    return st


def build_program(consts, debug=False, upto=99):
    nc = bass.Bass("TRN2", target_bir_lowering=False)
    with contextlib.ExitStack() as st:
        kb = KB(nc, st)
        io = {}
        for n, (shape, dt) in IN_SPECS.items():
            if upto < 3 and n[:2] in ("wg", "wu", "wd"):
                continue
            io[n] = kb.dram(n, shape, dt, "ExternalInput")
        for n in CONST_NAMES:
            io[n] = kb.dram(n, list(consts[n].shape), CONST_DT.get(n, F32), "ExternalInput")
        for n, (shape, dt) in SCRATCH.items():
            io[n] = kb.dram(n, shape, dt)
        io["out"] = kb.dram("out", [4096, DM], F32, "ExternalOutput")
        dbg = {}
        if debug:
            for n, shape in (("dbg_x1", [4096, DM]), ("dbg_x2", [4096, DM]), ("dbg_x3", [4096, DM]), ("dbg_aff0", [NE, 4096]),
                             ("dbg_aff1", [NE, 4096]), ("dbg_sel", [4 * CAP, 3])):
                dbg[n] = kb.dram(n, shape, F32, "ExternalOutput")


        def cp(dst, src, key):
            kb.dma("sync", dst, dst.t, src, src.t, key)

        import os
        if not os.environ.get("K_SKIP1"):
            phase1_attn(kb, io, 0)
            phase1_attn(kb, io, 1)
            phase1_ssd_in(kb, io)
            phase1_ssd_scan(kb, io, 0)
            phase1_ssd_scan(kb, io, 1)

        def ph_a():
            collective(kb, "AllGather", ALU.bypass, io["mixS_loc"], io["mixS_all"], "cc0", 8)
            zero_ydense(kb, io)

        misc_phase(kb, ph_a)
        if upto >= 2:
            phase2(kb, io, int(os.environ.get("K_NT", 32)))

            def ph_b(L):
                def f():
                    if L == 0:
                        kb.dma("sync", io["edges_loc"], io["edges_loc"].t[0:1, :], io["x1_loc"], io["x1_loc"].t[0:1, :], "edg", part=True)
                        kb.dma("sync", io["edges_loc"], io["edges_loc"].t[1:2, :], io["x1_loc"], io["x1_loc"].t[4095:4096, :], "edg", part=True)
                        collective(kb, "AllGather", ALU.bypass, io["edges_loc"], io["edges_all"], "cc1")
                    collective(kb, "AllGather", ALU.bypass, io["aff_loc"], io["aff_all"], "cc3")
                    if debug:
                        cp(dbg["dbg_x1" if L == 0 else "dbg_x3"], io["x1_loc"], "dbgx")
                        cp(dbg["dbg_aff%d" % L], io["aff_loc"], "dbga")
                return f
            misc_phase(kb, ph_b(0))
        if upto >= 3:
            moe_phase(kb, io, 0)

            def ph_c():
                moe_allreduce(kb, io, "cc4")
                if debug:
                    cp(dbg["dbg_sel"], io["sel"], "dbgs")
            misc_phase(kb, ph_c)
        if upto >= 4:
            phase4(kb, io)

            def ph_d():
                zero_ydense(kb, io)
                if debug:
                    cp(dbg["dbg_x2"], io["x2_loc"], "dbgx2")
            misc_phase(kb, ph_d)
            misc_phase(kb, ph_b(1))
        if upto >= 5:
            moe_phase(kb, io, 1)
            misc_phase(kb, lambda: moe_allreduce(kb, io, "cc5"))
            final_phase(kb, io)
        print("semaphores:", len(kb.P.dsems) + 5)
    return nc


def core_inputs(inp, c, consts):
    b, r = c // 4, c % 4
    hg = r
    g = hg // 2
    d = {}
    x = inp["x"]
    d["xT"] = np.ascontiguousarray(x[b].T)
    d["x_tok"] = np.ascontiguousarray(x[b, r * 4096:(r + 1) * 4096])
    w = inp["w_in_even"][0]
    hs = slice(hg * 256, (hg + 1) * 256)
    cols = [w[:, 0:1024][:, hs], w[:, 1024:2048][:, hs], w[:, 2048:3072][:, hs], w[:, 4096:5120][:, hs],
            w[:, 5120 + g * 128:5120 + (g + 1) * 128], w[:, 5376 + g * 128:5376 + (g + 1) * 128], w[:, 3072:4096][:, hs],
            w[:, 5632 + hg * 4:5632 + hg * 4 + 4], w[:, 5648 + hg * 4:5648 + hg * 4 + 4]]
    d["w1"] = np.ascontiguousarray(np.concatenate(cols, 1))
    d["g_attn"] = np.ascontiguousarray(inp["attn_norm"][0].reshape(8, 128).T)
    d["gqk"] = np.ascontiguousarray(np.stack([np.tile(inp["q_norm"][0], 2), np.tile(inp["k_norm"][0], 2)], 1))
    cw_full = inp["ssd_conv_w"][0]; cb = inp["ssd_conv_b"][0]
    chans = np.concatenate([np.arange(hg * 256, hg * 256 + 256), 1024 + g * 128 + np.arange(128), 1280 + g * 128 + np.arange(128)])
    cw = np.concatenate([cw_full[:, chans], cb[None, chans]], 0)
    d["convw"] = np.ascontiguousarray(cw.reshape(6, 4, 128).transpose(2, 1, 0)).astype(np.float32)
    h4 = slice(hg * 4, hg * 4 + 4)
    d["dtc"] = np.stack([np.concatenate([inp["ssd_dt_bias_fwd"][0][h4], inp["ssd_dt_bias_bwd"][0][h4]]),
                         np.concatenate([inp["ssd_a_log_fwd"][0][h4], inp["ssd_a_log_bwd"][0][h4]])], 1).astype(np.float32)
    d["Dbc"] = np.ascontiguousarray(np.tile(np.repeat(inp["ssd_d"][0][h4], 64)[None, :], (128, 1))).astype(np.float32)
    wo = inp["w_out_even"][0]
    perm = np.concatenate([np.concatenate([np.arange(q * 256, (q + 1) * 256), 1024 + np.arange(q * 256, (q + 1) * 256)]) for q in range(4)])
    d["wo"] = np.ascontiguousarray(wo[perm])
    gfull = np.concatenate([np.ones(1024, np.float32), inp["ssd_out_norm"][0]])[perm]
    d["g_wo"] = np.ascontiguousarray(gfull.reshape(16, 128).T).astype(np.float32)
    mi = np.zeros((128, 8, 16), np.int32)
    for wdx in range(8):
        for ck in range(16):
            L = (8 * r + wdx) * 256 + (ck % 2) * 128 + np.arange(128)
            mi[:, wdx, ck] = (L // 1024) * 4096 + (ck // 4) * 1024 + (L % 1024)
    d["mixidx"] = mi
    for L in range(2):
        d["wr%d" % L] = np.ascontiguousarray(inp["router_w"][L])
        d["g_ffn_col%d" % L] = np.ascontiguousarray(inp["ffn_norm"][L].reshape(8, 128).T)
        d["g_ffn_bc%d" % L] = np.ascontiguousarray(np.tile(inp["ffn_norm"][L][None, :], (128, 1)))
        es = slice(4 * r, 4 * r + 4)
        d["wg%d" % L] = np.ascontiguousarray(inp["expert_w_gate"][L, es])
        d["wu%d" % L] = np.ascontiguousarray(inp["expert_w_up"][L, es])
        d["wd%d" % L] = np.ascontiguousarray(inp["expert_w_down"][L, es])
    ai = np.zeros((128, 1), np.int32)
    for e in range(4):
        for q in range(4):
            ai[e * 4 + q, 0] = q * 16 + 4 * r + e
    d["affidx"] = ai
    mr = np.zeros((128, 33), np.int32)
    for i in range(32):
        mr[:, i] = r * 4096 + i * 128 + np.arange(128)
    mr[0, 32] = max(r * 4096 - 1, 0)
    mr[1, 32] = min(r * 4096 + 4096, S - 1)
    d["myrows"] = mr
    hi = np.zeros((128, 1), np.int32); hm = np.zeros((128, 1), np.float32)
    if r > 0:
        hi[0, 0] = 2 * (r - 1) + 1; hm[0, 0] = 1.0
    if r < 3:
        hi[1, 0] = 2 * (r + 1); hm[1, 0] = 1.0
    d["haloidx"] = hi; d["halomsk"] = hm
    d["g_conv_bc"] = np.ascontiguousarray(np.tile(inp["conv_norm"][0][None, :], (128, 1)))
    d["w2"] = np.ascontiguousarray(inp["conv_w_in"][0])
    d["cw3"] = np.ascontiguousarray(inp["conv_w"][0].reshape(3, 8, 128).transpose(2, 1, 0))
    d["w3"] = np.ascontiguousarray(inp["conv_w_out"][0])
    for n in CONST_NAMES:
        d[n] = consts[n]
    return d


def kernel(**inputs):
    from concourse.bass_utils import run_bass_kernel_spmd
    inp = {k: np.asarray(v) for k, v in inputs.items()}
    consts = host_constants()
    nc = build_program(consts)
    in_maps = [core_inputs(inp, c, consts) for c in range(NCORE)]
    res = run_bass_kernel_spmd(nc, in_maps, core_ids=list(range(NCORE)))
    out = np.zeros((2, S, DM), np.float32)
    for c in range(NCORE):
        b, r = c // 4, c % 4
        out[b, r * 4096:(r + 1) * 4096] = res.results[c]["out"]
    return out
```

```python
import contextlib
import numpy as np
import concourse.bass as bass
import concourse.mybir as mybir

F32 = mybir.dt.float32
BF16 = mybir.dt.bfloat16
I32 = mybir.dt.int32
U32 = mybir.dt.uint32
AF = mybir.ActivationFunctionType
ALU = mybir.AluOpType
AX = mybir.AxisListType

ENGS = ("tensor", "vector", "scalar", "gpsimd", "sync")


class Buf:
    __slots__ = ("name", "w", "r", "dsem", "dcount")

    def __init__(self, name):
        self.name = name
        self.w = {}
        self.r = {}
        self.dsem = None
        self.dcount = 0


class Prog:
    def __init__(self, nc, stack):
        self.nc = nc
        self.stack = stack
        self.esem = {e: stack.enter_context(nc.semaphore("es_" + e)) for e in ENGS}
        self.base = {e: 0 for e in ENGS}
        self.dsems = {}
        self.dvals = {}
        self.dfree = []
        self.phase = 0
        self.reset_phase()
        self.same_engine_sync = True
        self.no_self = set()

    def reset_phase(self):
        self.ops = {e: [] for e in ENGS}
        self.seen = {e: {} for e in ENGS}
        self.needed = {e: set() for e in ENGS}

    def _collect(self, eng, reads, writes, partial):
        waits = {}
        def add(d):
            for k, v in d.items():
                if waits.get(k, -1) < v:
                    waits[k] = v
        for b in reads:
            add(b.w)
        for b in writes:
            add(b.w); add(b.r)
        for b in partial:
            add(b.r)
        out = []
        seen = self.seen[eng]
        for k, v in waits.items():
            if k[2] != self.phase:
                continue
            if k[0] == "E" and k[1] == eng and (not self.same_engine_sync or eng in self.no_self):
                continue
            if seen.get(k, -1) >= v:
                continue
            seen[k] = v
            if k[0] == "E":
                self.needed[k[1]].add(v)
            out.append((k, v))
        return out

    def _commit(self, tok, reads, writes, partial):
        k, v = tok
        for b in reads:
            if b.r.get(k, -1) < v:
                b.r[k] = v
        for b in writes:
            b.w = {k: v}
            b.r = {}
        for b in partial:
            if b.w.get(k, -1) < v:
                b.w[k] = v

    def _serial_waits(self, eng, waits):
        import os
        if not os.environ.get("FW_SERIAL"):
            return waits
        if os.environ["FW_SERIAL"] != "1" and eng not in os.environ["FW_SERIAL"].split(","):
            return waits
        have = {k for k, _ in waits}
        for e2 in ENGS:
            if e2 == eng:
                continue
            ee = [o["idx"] for o in self.ops[e2] if o["kind"] == "E"]
            if not ee:
                continue
            k = ("E", e2, self.phase)
            if self.seen[eng].get(k, -1) < ee[-1]:
                self.seen[eng][k] = ee[-1]
                self.needed[e2].add(ee[-1])
                waits.append((k, ee[-1]))
        return waits

    def op(self, eng, emit, reads=(), writes=(), partial=()):
        waits = self._collect(eng, reads, writes, partial)
        waits = self._serial_waits(eng, waits)
        idx = len(self.ops[eng]) + 1
        self.ops[eng].append(dict(waits=waits, emit=emit, kind="E", idx=idx))
        self._commit((("E", eng, self.phase), idx), reads, writes, partial)

    def dma(self, eng, emit, key, reads=(), writes=(), partial=(), inc=16):
        waits = self._collect(eng, reads, writes, partial)
        if key not in self.dsems:
            if self.dfree:
                self.dsems[key], self.dvals[key] = self.dfree.pop(0)
            else:
                self.dsems[key] = self.stack.enter_context(self.nc.semaphore("ds%d_%s" % (self.phase, key)))
                self.dvals[key] = 0
        self.dvals[key] += inc
        v = self.dvals[key]
        idx = len(self.ops[eng]) + 1
        self.ops[eng].append(dict(waits=waits, emit=emit, kind="D", key=key, inc=inc, idx=idx))
        self._commit((("D", key, self.phase), v), reads, writes, partial)

    def barrier(self):
        last = {}
        for e in ENGS:
            ee = [o["idx"] for o in self.ops[e] if o["kind"] == "E"]
            last[e] = ee[-1] if ee else 0
        for e in ENGS:
            waits = []
            for e2 in ENGS:
                if e2 != e and last[e2] > 0:
                    k = ("E", e2, self.phase)
                    if self.seen[e].get(k, -1) < last[e2]:
                        self.seen[e][k] = last[e2]
                        self.needed[e2].add(last[e2])
                        waits.append((k, last[e2]))
            for key, v in self.dvals.items():
                k = ("D", key, self.phase)
                if self.seen[e].get(k, -1) < v:
                    self.seen[e][k] = v
                    waits.append((k, v))
            self.ops[e].append(dict(waits=waits, emit=None, kind="N", idx=len(self.ops[e]) + 1))

    def emit_phase(self):
        nc = self.nc
        val = {}
        for e in ENGS:
            c = self.base[e]
            m = {}
            for o in self.ops[e]:
                if o["kind"] == "E" and o["idx"] in self.needed[e]:
                    c += 1
                    m[o["idx"]] = c
            val[e] = m
        stats = {}

        def replay(e, eng):
            n = 0
            for o in self.ops[e]:
                for (k, v) in o["waits"]:
                    if k[0] == "E":
                        eng.wait_ge(self.esem[k[1]], val[k[1]][v])
                    else:
                        eng.wait_ge(self.dsems[k[1]], v)
                    n += 1
                if o["emit"] is None:
                    continue
                ins = o["emit"](eng)
                n += 1
                if o["kind"] == "E":
                    if o["idx"] in self.needed[e]:
                        ins.then_inc(self.esem[e], 1)
                else:
                    ins.then_inc(self.dsems[o["key"]], o["inc"])
            stats[e] = n

        with nc.Block() as block:
            @block.tensor
            def _(eng):
                replay("tensor", eng)

            @block.vector
            def _(eng):
                replay("vector", eng)

            @block.scalar
            def _(eng):
                replay("scalar", eng)

            @block.gpsimd
            def _(eng):
                replay("gpsimd", eng)

            @block.sync
            def _(eng):
                replay("sync", eng)
        for e in ENGS:
            if val[e]:
                self.base[e] = max(val[e].values())
        for key in list(self.dsems):
            self.dfree.append((self.dsems[key], self.dvals[key]))
        self.dsems = {}
        self.dvals = {}
        self.phase += 1
        self.reset_phase()
        return stats


S = 16384
DM = 1024
NCORE = 8
PAD = 1024
WIN = 512
NW = S // WIN
NCH = S // 128
NE = 16
CAP = 2048
FF = 2048
EPS = 1e-6
NEG = -30000.0
C_Q, C_K, C_V, C_XS, C_B, C_C, C_Z, C_DT, NC1 = 0, 256, 512, 768, 1024, 1152, 1280, 1536, 1544
GROUPS = [[0, 1, 2, 3], [4, 5, 6, 7]]


def sl(start, n, step):
    return slice(start, start + (n - 1) * step + 1, step)


class T:
    def __init__(self, t, name):
        self.t = t
        self.b = Buf(name)
        self.name = name

    def __getitem__(self, k):
        return self.t[k]


class KB:
    def __init__(self, nc, st):
        self.nc = nc
        self.st = st
        self.ph = None
        self.P = Prog(nc, st)
        self.P.no_self = {"tensor"}
        self.uid = 0

    def sb(self, name, shape, dt):
        self.uid += 1
        nm = "s%d_%s" % (self.uid, name)
        return T(self.ph.enter_context(self.nc.sbuf_tensor(nm, shape, dt)), name)

    def ps(self, name, shape, dt=F32):
        self.uid += 1
        nm = "p%d_%s" % (self.uid, name)
        return T(self.ph.enter_context(self.nc.psum_tensor(nm, shape, dt)), name)

    def pool(self, kind, name, n, shape, dt):
        f = self.sb if kind == "sb" else self.ps
        return Pool([f("%s%d" % (name, i), shape, dt) for i in range(n)])

    def dram(self, name, shape, dt, kind=None):
        if kind is None:
            h = self.nc.dram_tensor(name, shape, dt)
        else:
            h = self.nc.dram_tensor(name, shape, dt, kind=kind)
        return T(h.ap(), name)

    def mm(self, out, oap, lt, ltap, rt, rtap, start=True, stop=True):
        self.P.op("tensor", lambda e: e.matmul(oap, lhsT=ltap, rhs=rtap, start=start, stop=stop),
                  reads=[lt.b, rt.b], writes=[out.b])

    def tr(self, out, oap, inp, iap, ident, idap):
        self.P.op("tensor", lambda e: e.transpose(oap, iap, idap), reads=[inp.b, ident.b], writes=[out.b])

    def act(self, out, oap, inp, iap, func, bias=None, scale=None, accum=None, part=False):
        reads = [inp.b]
        kw = {}
        if bias is not None:
            reads.append(bias[0].b)
            kw["bias"] = bias[1]
        if scale is not None:
            if isinstance(scale, tuple):
                reads.append(scale[0].b)
                kw["scale"] = scale[1]
            else:
                kw["scale"] = scale
        writes = [out.b]
        if accum is not None:
            writes.append(accum[0].b)
            kw["accum_out"] = accum[1]
        self.P.op("scalar", lambda e: e.activation(out=oap, in_=iap, func=func, **kw), reads=reads,
                  writes=[] if part else writes, partial=writes if part else [])

    def tt(self, eng, out, oap, a, aap, b, bap, op, part=False):
        self.P.op(eng, lambda e: e.tensor_tensor(out=oap, in0=aap, in1=bap, op=op), reads=[a.b, b.b],
                  writes=[] if part else [out.b], partial=[out.b] if part else [])

    def ts(self, eng, out, oap, a, aap, s1, s2, op0, op1=None, accum=None, part=False):
        reads = [a.b]
        def cv(s):
            if isinstance(s, tuple):
                reads.append(s[0].b)
                return s[1]
            return s
        v1, v2 = cv(s1), cv(s2)
        kw = {}
        writes = [out.b]
        if accum is not None:
            writes.append(accum[0].b)
            kw["accum_out"] = accum[1]
        if op1 is not None:
            kw["op1"] = op1
        self.P.op(eng, lambda e: e.tensor_scalar(out=oap, in0=aap, scalar1=v1, scalar2=v2, op0=op0, **kw), reads=reads,
                  writes=[] if part else writes, partial=writes if part else [])

    def stt(self, out, oap, a, aap, scalar, b, bap, op0, op1, part=False):
        reads = [a.b, b.b]
        sv = scalar
        if isinstance(scalar, tuple):
            reads.append(scalar[0].b)
            sv = scalar[1]
        self.P.op("vector", lambda e: e.scalar_tensor_tensor(out=oap, in0=aap, scalar=sv, in1=bap, op0=op0, op1=op1),
                  reads=reads, writes=[] if part else [out.b], partial=[out.b] if part else [])

    def copy(self, eng, out, oap, a, aap, part=False):
        if eng == "scalar":
            f = lambda e: e.activation(out=oap, in_=aap, func=AF.Copy)
        else:
            f = lambda e: e.tensor_copy(out=oap, in_=aap)
        self.P.op(eng, f, reads=[a.b], writes=[] if part else [out.b], partial=[out.b] if part else [])

    def memset(self, eng, out, oap, val, part=False):
        self.P.op(eng, lambda e: e.memset(oap, val), writes=[] if part else [out.b], partial=[out.b] if part else [])

    def recip(self, out, oap, a, aap):
        self.P.op("vector", lambda e: e.reciprocal(out=oap, in_=aap), reads=[a.b], writes=[out.b])

    def dma(self, q, out, oap, inp, iap, key, part=False, extra_reads=(), **kw):
        self.P.dma(q, lambda e: e.dma_start(out=oap, in_=iap, **kw), key, reads=[inp.b] + [x.b for x in extra_reads],
                   writes=[] if part else [out.b], partial=[out.b] if part else [])

    def rstd_from(self, out, oap, inp, iap, n, tmp, tap, eps):
        self.act(tmp, tap, inp, iap, AF.Ln, bias=(eps, eps[:, 0:1]), scale=1.0 / n)
        self.act(out, oap, tmp, tap, AF.Exp, scale=-0.5)


class Pool:
    def __init__(self, tiles):
        self.tiles = tiles
        self.i = 0

    def next(self):
        t = self.tiles[self.i % len(self.tiles)]
        self.i += 1
        return t


def host_constants():
    import ml_dtypes
    bf = ml_dtypes.bfloat16
    c = {}
    c["ident_bf"] = np.eye(128, dtype=np.float32).astype(bf)
    c["ident_f"] = np.eye(128, dtype=np.float32)
    p = np.arange(128)[:, None]
    f = np.arange(128)[None, :]
    mA = np.where(f <= p, 0.0, NEG).astype(np.float32)
    mB = np.where(f >= p, 0.0, NEG).astype(np.float32)
    mA_first = mA.copy(); mA_first[:64, :] = NEG
    mB_last = mB.copy(); mB_last[64:, :] = NEG
    nm = np.zeros((128, 3, 4, 128), np.float32)
    for v, (a, b) in enumerate([(mA, mB), (mA_first, mB), (mA, mB_last)]):
        nm[:, v, 0] = a; nm[:, v, 1] = b; nm[:, v, 2] = a; nm[:, v, 3] = b
    c["negmask"] = nm.reshape(128, 3, 512).astype(bf)
    bo = np.zeros((128, 128), np.float32); bo[:64, :64] = 1; bo[64:, 64:] = 1
    c["blockones"] = bo.astype(bf)
    rm = np.zeros((128, 128), np.float32)
    for o in (0, 64):
        for i in range(8):
            rm[o + 8 + i, o + i] = -1.0
            rm[o + i, o + 8 + i] = 1.0
    c["rotm"] = rm.astype(bf)
    so = np.zeros((128, 2, 128), np.float32); so[:, 0, :64] = 1; so[:, 1, 64:] = 1
    c["sel_ones"] = so.astype(bf)
    c["ones_bf"] = np.ones((128, 128), np.float32).astype(bf)
    c["ones_f"] = np.ones((128, 128), np.float32)
    t = np.arange(128)[:, None]; l = np.arange(128)[None, :]
    c["tri"] = np.stack([(t <= l), (t >= l)], 1).astype(np.float32)
    mf = np.where(t <= l, 0.0, NEG).astype(np.float32); mb = np.where(t >= l, 0.0, NEG).astype(np.float32)
    c["smask"] = np.stack([np.tile(mf, (1, 4)), np.tile(mb, (1, 4))], 1).astype(np.float32)
    c["ustrict"] = (t < l).astype(np.float32)
    half = 8
    inv_freq = (np.float32(500000.0) ** (-np.arange(half, dtype=np.float32) * np.float32(2.0) / np.float32(16))).astype(np.float32)
    ang = (np.arange(S, dtype=np.float32)[None, :] * inv_freq[:, None]).astype(np.float32)
    cos = np.cos(ang.astype(np.float64)).astype(np.float32); sin = np.sin(ang.astype(np.float64)).astype(np.float32)
    cf = np.ones((128, S), np.float32); sf = np.zeros((128, S), np.float32)
    for o in (0, 64):
        cf[o:o + 8] = cos; cf[o + 8:o + 16] = cos
        sf[o:o + 8] = sin; sf[o + 8:o + 16] = sin
    c["cosf"] = cf; c["sinf"] = sf
    c["iota_tok"] = (np.arange(128)[:, None] * 128 + np.arange(128)[None, :]).astype(np.float32)
    tt_ = np.arange(128)[:, None] * 128 + np.arange(128)[None, :]
    c["iota_h2row"] = (((tt_ % 4096) // 512) * 2048 + (tt_ // 4096) * 512 + (tt_ % 512)).astype(np.float32)
    return c


CONST_DT = {"ident_bf": BF16, "negmask": BF16, "blockones": BF16, "rotm": BF16, "sel_ones": BF16, "ones_bf": BF16}
SMALL_CONSTS_UNUSED = ["ident_bf", "ident_f", "negmask", "blockones", "rotm", "sel_ones", "ones_bf", "ones_f", "tri", "smask",
                "ustrict", "iota_tok"]


def load_consts(kb, io, names):
    cs = {}
    for n in names:
        src = io[n]
        shape = list(src.t.shape)
        t = kb.sb("c_" + n, shape, CONST_DT.get(n, F32))
        kb.dma("sync", t, t[:], src, src.t, "c_" + n)
        cs[n] = t
    eps = kb.sb("c_eps", [128, 1], F32)
    kb.memset("vector", eps, eps[:], EPS)
    cs["eps"] = eps
    return cs


def load_w1(kb, io, w1b, c0, ncols, stage_pool, g_attn):
    w1 = io["w1"]
    for kc in range(8):
        stg = stage_pool.next()
        kb.dma("sync", stg, stg[:, 0:ncols], w1, w1.t[kc * 128:(kc + 1) * 128, c0:c0 + ncols], stg.name)
        kb.ts("vector", w1b, w1b[:, kc, 0:ncols], stg, stg[:, 0:ncols], (g_attn, g_attn[:, kc:kc + 1]), None, ALU.mult, part=True)


def window_front(kb, io, cs, w, xb_pool, sq_pool, rstd_pool, psA):
    xT = io["xT"]
    xb = xb_pool.next()
    kb.dma("gpsimd", xb, xb[:], xT, xT.t.rearrange("(k p) t -> p k t", p=128)[:, :, w * WIN:(w + 1) * WIN], xb.name)
    sq = sq_pool.next()
    kb.act(sq, sq[:], xb, xb[:], AF.Square)
    pss = psA.next()
    for kc in range(8):
        kb.mm(pss, pss[:], cs["ones_bf"], cs["ones_bf"][:], sq, sq[:, kc, :], start=(kc == 0), stop=(kc == 7))
    rstd = rstd_pool.next()
    kb.rstd_from(rstd, rstd[:, 0, :], pss, pss[:], DM, rstd, rstd[:, 1, :], cs["eps"])
    return xb, sq, rstd


def attn_inproj(kb, io, hp, qz, kT, vT):
    if True:
      with contextlib.ExitStack() as ph:
        kb.ph = ph
        cs = load_consts(kb, io, ["blockones", "rotm", "ones_bf"])
        g_attn = kb.sb("g_attn", [128, 8], F32)
        kb.dma("sync", g_attn, g_attn[:], io["g_attn"], io["g_attn"].t, "g_attn")
        gqk = kb.sb("gqk", [128, 2], F32)
        kb.dma("sync", gqk, gqk[:], io["gqk"], io["gqk"].t, "gqk")
        w1b = kb.sb("w1b", [128, 8, 384], BF16)
        stage = kb.pool("sb", "w1stg", 2, [128, 128], F32)
        for j, c0 in enumerate((C_Q + hp * 128, C_K + hp * 128, C_V + hp * 128)):
            w1 = io["w1"]
            for kc in range(8):
                stg = stage.next()
                kb.dma("sync", stg, stg[:], w1, w1.t[kc * 128:(kc + 1) * 128, c0:c0 + 128], stg.name)
                kb.ts("vector", w1b, w1b[:, kc, j * 128:(j + 1) * 128], stg, stg[:], (g_attn, g_attn[:, kc:kc + 1]), None,
                      ALU.mult, part=True)
        for t in (kT, vT):
            kb.memset("gpsimd", t, t[:, 0:PAD], 0.0, part=True)
            kb.memset("gpsimd", t, t[:, PAD + S:], 0.0, part=True)
        kb.memset("gpsimd", qz, qz[64:128, 0, :], 0.0, part=True)
        kb.memset("gpsimd", qz, qz[0:64, 1, :], 0.0, part=True)
        xb_pool = kb.pool("sb", "xb", 2, [128, 8, WIN], BF16)
        sq_pool = kb.pool("sb", "sq", 1, [128, 8, WIN], BF16)
        rstd_pool = kb.pool("sb", "rstd", 2, [128, 2, WIN], F32)
        cos_pool = kb.pool("sb", "cosw", 2, [128, 2, WIN], F32)
        raw_pool = kb.pool("sb", "raw", 2, [128, WIN], F32)
        sqh_pool = kb.pool("sb", "sqh", 2, [128, WIN], BF16)
        rsh_pool = kb.pool("sb", "rsh", 2, [128, 2, WIN], F32)
        qn_pool = kb.pool("sb", "qn", 2, [128, WIN], BF16)
        t12_pool = kb.pool("sb", "t12", 2, [128, 2, WIN], F32)
        psA = kb.pool("ps", "psA", 2, [128, WIN], F32)
        psB = kb.pool("ps", "psB", 2, [128, WIN], F32)
        psC = kb.pool("ps", "psC", 2, [128, WIN], F32)
        for w in range(NW):
            xb, sq, rstd = window_front(kb, io, cs, w, xb_pool, sq_pool, rstd_pool, psA)
            cw = cos_pool.next()
            kb.dma("sync", cw, cw[:, 0, :], io["cosf"], io["cosf"].t[:, w * WIN:(w + 1) * WIN], cw.name, part=True)
            kb.dma("sync", cw, cw[:, 1, :], io["sinf"], io["sinf"].t[:, w * WIN:(w + 1) * WIN], cw.name, part=True)
            for j, dst in enumerate((qz, kT, vT)):
                ps = psB.next()
                for kc in range(8):
                    kb.mm(ps, ps[:], w1b, w1b[:, kc, j * 128:(j + 1) * 128], xb, xb[:, kc, :], start=(kc == 0), stop=(kc == 7))
                off = 0 if j == 0 else PAD
                dap = None if j == 0 else dst[:, off + w * WIN: off + (w + 1) * WIN]
                if j == 2:
                    kb.tt("vector", dst, dap, ps, ps[:], rstd, rstd[:, 0, :], ALU.mult, part=True)
                    continue
                raw = raw_pool.next()
                kb.tt("vector", raw, raw[:], ps, ps[:], rstd, rstd[:, 0, :], ALU.mult)
                sqh = sqh_pool.next()
                kb.act(sqh, sqh[:], raw, raw[:], AF.Square)
                ps2 = psC.next()
                kb.mm(ps2, ps2[:], cs["blockones"], cs["blockones"][:], sqh, sqh[:])
                rsh = rsh_pool.next()
                kb.rstd_from(rsh, rsh[:, 0, :], ps2, ps2[:], 64, rsh, rsh[:, 1, :], cs["eps"])
                qn = qn_pool.next()
                kb.stt(qn, qn[:], raw, raw[:], (gqk, gqk[:, j:j + 1]), rsh, rsh[:, 0, :], ALU.mult, ALU.mult)
                ps3 = psC.next()
                kb.mm(ps3, ps3[:], cs["rotm"], cs["rotm"][:], qn, qn[:])
                t12 = t12_pool.next()
                kb.tt("vector", t12, t12[:, 0, :], qn, qn[:], cw, cw[:, 0, :], ALU.mult, part=True)
                kb.tt("vector", t12, t12[:, 1, :], ps3, ps3[:], cw, cw[:, 1, :], ALU.mult, part=True)
                if j == 0:
                    for h in range(2):
                        hs = slice(h * 64, (h + 1) * 64)
                        kb.tt("vector", qz, qz[hs, h, w * WIN:(w + 1) * WIN], t12, t12[hs, 0, :], t12, t12[hs, 1, :], ALU.add, part=True)
                else:
                    kb.tt("vector", dst, dap, t12, t12[:, 0, :], t12, t12[:, 1, :], ALU.add, part=True)
        if io.get("dbg_qk") is not None:
            kb.dma("sync", io["dbg_qk"], io["dbg_qk"].t[0, 0:64], qz, qz[0:64, 0, :], "dbgq", part=True)
            kb.dma("sync", io["dbg_qk"], io["dbg_qk"].t[0, 64:128], qz, qz[64:128, 1, :], "dbgq", part=True)
            kb.dma("sync", io["dbg_qk"], io["dbg_qk"].t[1], kT, kT[:, PAD:PAD + S], "dbgk", part=True)
            kb.dma("sync", io["dbg_qk"], io["dbg_qk"].t[2], vT, vT[:, PAD:PAD + S], "dbgv", part=True)
        kb.P.barrier()
        stats0 = kb.P.emit_phase()
        kb.ph = None
    return stats0


def attn_core(kb, io, hp, qz, kT, vT):
    import os
    if True:
      with contextlib.ExitStack() as ph:
        kb.ph = ph
        cs = load_consts(kb, io, ["ident_bf", "negmask", "sel_ones"])
        vpad_pool = kb.pool("sb", "vpad", int(os.environ.get("P1_NB", 3)), [128, 2, 2, 128], BF16)
        for t in vpad_pool.tiles:
            kb.memset("vector", t, t[:], 0.0)
        pt_pool = kb.pool("sb", "pt", int(os.environ.get("P1_NB", 3)), [128, 512], BF16)
        accden = kb.sb("accden", [128, 2, 2048], F32)
        rden = kb.sb("rden", [128, 2048], F32)
        attn_o = kb.pool("sb", "attn_o", 2, [128, 2048], BF16)
        psT = kb.pool("ps", "psT", int(os.environ.get("P1_NT", 2)), [128, 4, 128], F32)
        psS = kb.pool("ps", "psS", int(os.environ.get("P1_NS", 2)), [128, 512], F32)
        psO = kb.pool("ps", "psO", int(os.environ.get("P1_NO", 2)), [128, 4, 128], F32)
        mix = io["mixA_loc"]

        def stage1(u):
            d, rho, j, jj = u
            n = S // d
            nb = n // 128
            var = 1 if j == 0 else (2 if j == nb - 1 else 0)
            pT = psT.next()
            kcols = []
            for c in range(2):
                k0 = PAD + rho + d * (128 * j - 64 + 128 * c)
                kcols.append(k0)
                kb.mm(pT, pT[:, c, :], vT, vT[:, sl(k0, 128, d)], cs["ident_bf"], cs["ident_bf"][:])
            vp = vpad_pool.next()
            if MODE >= 1:
                kb.copy("vector", vp, vp[:, :, 0, 0:64], pT, pT[:, 0:2, 0:64], part=True)
            if MODE >= 2:
                kb.copy(os.environ.get("P1_CE", "vector"), vp, vp[:, :, 1, 64:128], pT, pT[:, 0:2, 64:128], part=True)
            sS = psS.next()
            kb.mm(sS, sS[:], cs["ident_bf"], cs["ident_bf"][:], cs["negmask"], cs["negmask"][:, var, :], start=True, stop=False)
            q0 = rho + d * 128 * j
            for h in range(2):
                for c in range(2):
                    sub = 2 * h + c
                    kb.mm(sS, sS[:, sub * 128:(sub + 1) * 128], kT, kT[:, sl(kcols[c], 128, d)],
                          qz, qz[:, h, sl(q0, 128, d)], start=False, stop=True)
            pt = pt_pool.next()
            if MODE >= 3:
                kb.act(pt, pt[:], sS, sS[:], AF.Exp, scale=0.125)
            return (u, vp, pt)

        def stage2(st, first_in_sb):
            u, vp, pt = st
            d, rho, j, jj = u
            if MODE < 4:
                return
            pO = psO.next()
            SUB = os.environ.get("P1_SUB", "")
            k = 0
            for h in range(2 if SUB != "den" else 0):
                for c in range(2):
                    sub = 2 * h + c
                    kb.mm(pO, pO[:, 0, :], vp, vp[:, c, h, :], pt, pt[:, sub * 128:(sub + 1) * 128], start=(k == 0), stop=(k == 3))
                    k += 1
            k = 0
            for h in range(2 if SUB != "pv" else 0):
                for c in range(2):
                    sub = 2 * h + c
                    kb.mm(pO, pO[:, 1, :], cs["sel_ones"], cs["sel_ones"][:, h, :], pt, pt[:, sub * 128:(sub + 1) * 128],
                          start=(k == 0), stop=(k == 3))
                    k += 1
            c0 = rho + d * 128 * jj
            dap = accden[:, :, sl(c0, 128, d)]
            if MODE < 5:
                return
            if first_in_sb:
                kb.copy("vector", accden, dap, pO, pO[:, 0:2, :])
            else:
                kb.tt("vector", accden, dap, accden, dap, pO, pO[:, 0:2, :], ALU.add)

        import os
        MODE = int(os.environ.get("P1_MODE", "9"))
        for sbk in range(int(os.environ.get("P1_NSB", S // 2048))):
            units = []
            for d in (1, 4, 16):
                per = 16 // d
                for rho in range(d):
                    for jj in range(per):
                        units.append((d, rho, sbk * per + jj, jj))
            pend = None
            units = units[:int(os.environ.get("P1_NU", len(units)))]
            for ui, u in enumerate(units):
                st1 = stage1(u)
                if os.environ.get("P1_NOPIPE"):
                    stage2(st1, u[0] == 1)
                    continue
                if pend is not None:
                    stage2(pend[0], pend[1])
                pend = (st1, u[0] == 1)
            if pend is not None:
                stage2(pend[0], pend[1])
            kb.recip(rden, rden[:], accden, accden[:, 1, :])
            ao = attn_o.next()
            kb.tt("gpsimd", ao, ao[:], accden, accden[:, 0, :], rden, rden[:], ALU.mult)
            kb.dma("sync", mix, mix.t.rearrange("(g c) t -> c g t", c=256)[hp * 128:(hp + 1) * 128, sbk * 4:(sbk + 1) * 4, :],
                   ao, ao[:].rearrange("p (g t) -> p g t", t=512), ao.name, part=True)
        kb.P.barrier()
        stats = kb.P.emit_phase()
        kb.ph = None
    return stats


def phase1_attn(kb, io, hp):
    import os
    with contextlib.ExitStack() as outer:
        kb.ph = outer
        qz = kb.sb("qz", [128, 2, S], BF16)
        kT = kb.sb("kT", [128, S + 2 * PAD], BF16)
        vT = kb.sb("vT", [128, S + 2 * PAD], BF16)
        st = attn_inproj(kb, io, hp, qz, kT, vT)
        if os.environ.get("P1_STOP"):
            return st
        st = attn_core(kb, io, hp, qz, kT, vT)
    return st


def phase1_ssd_in(kb, io):
    with contextlib.ExitStack() as ph:
        kb.ph = ph
        if "mixA_all" in io:
            collective(kb, "AllGather", ALU.bypass, io["mixA_loc"], io["mixA_all"], "ccA", 8)
        cs = load_consts(kb, io, ["ones_bf"])
        g_attn = kb.sb("g_attn", [128, 8], F32)
        kb.dma("sync", g_attn, g_attn[:], io["g_attn"], io["g_attn"].t, "g_attn")
        ncol = NC1 - C_XS
        w1b = kb.sb("w1b", [128, 8, ncol], BF16)
        stage = kb.pool("sb", "w1stg", 2, [128, ncol], F32)
        w1 = io["w1"]
        for kc in range(8):
            stg = stage.next()
            kb.dma("sync", stg, stg[:], w1, w1.t[kc * 128:(kc + 1) * 128, C_XS:NC1], stg.name)
            kb.ts("vector", w1b, w1b[:, kc, :], stg, stg[:], (g_attn, g_attn[:, kc:kc + 1]), None, ALU.mult, part=True)
        cw = kb.sb("convw", [128, 4, 6], F32)
        kb.dma("sync", cw, cw[:], io["convw"], io["convw"].t, "convw")
        dtc = kb.sb("dtc", [8, 2], F32)
        kb.dma("sync", dtc, dtc[:], io["dtc"], io["dtc"].t, "dtc")
        negA = kb.sb("negA", [8, 1], F32)
        kb.act(negA, negA[:], dtc, dtc[:, 1:2], AF.Exp)
        kb.ts("vector", negA, negA[:], negA, negA[:], -1.0, None, ALU.mult)
        one8 = kb.sb("one8", [8, 1], F32)
        kb.memset("vector", one8, one8[:], 1.0)
        xb_pool = kb.pool("sb", "xb", 2, [128, 8, WIN], BF16)
        sq_pool = kb.pool("sb", "sq", 1, [128, 8, WIN], BF16)
        rstd_pool = kb.pool("sb", "rstd", 2, [128, 2, WIN], F32)
        pre_pool = kb.pool("sb", "pre", 2, [128, 4, WIN], F32)
        cacc_pool = kb.pool("sb", "cacc", 2, [128, 4, WIN], F32)
        post_pool = kb.pool("sb", "post", 2, [128, 4, WIN], BF16)
        zs_pool = kb.pool("sb", "zsw", 2, [128, 2, WIN], BF16)
        dt_pool = kb.pool("sb", "dtw", 2, [8, 4, WIN], F32)
        psA = kb.pool("ps", "psA", 2, [128, WIN], F32)
        psB = kb.pool("ps", "psB", 3, [128, WIN], F32)
        xT = io["xT"]
        nwin = 33
        for w in range(nwin):
            t0 = 508 * w - 2 if w < nwin - 1 else S - 510
            lo, hi = max(t0, 0), min(t0 + WIN, S)
            xb = xb_pool.next()
            if lo > t0 or hi < t0 + WIN:
                kb.memset("vector", xb, xb[:], 0.0)
                kb.dma("gpsimd", xb, xb[:, :, lo - t0:hi - t0], xT, xT.t.rearrange("(k p) t -> p k t", p=128)[:, :, lo:hi], xb.name)
            else:
                kb.dma("gpsimd", xb, xb[:], xT, xT.t.rearrange("(k p) t -> p k t", p=128)[:, :, lo:hi], xb.name)
            sq = sq_pool.next()
            kb.act(sq, sq[:], xb, xb[:], AF.Square)
            pss = psA.next()
            for kc in range(8):
                kb.mm(pss, pss[:], cs["ones_bf"], cs["ones_bf"][:], sq, sq[:, kc, :], start=(kc == 0), stop=(kc == 7))
            rstd = rstd_pool.next()
            kb.rstd_from(rstd, rstd[:, 0, :], pss, pss[:], DM, rstd, rstd[:, 1, :], cs["eps"])
            pre = pre_pool.next()
            for ci in range(4):
                ps = psB.next()
                for kc in range(8):
                    kb.mm(ps, ps[:], w1b, w1b[:, kc, ci * 128:(ci + 1) * 128], xb, xb[:, kc, :], start=(kc == 0), stop=(kc == 7))
                kb.tt("vector", pre, pre[:, ci, :], ps, ps[:], rstd, rstd[:, 0, :], ALU.mult, part=True)
            cacc = cacc_pool.next()
            post = post_pool.next()
            NV = WIN - 4
            for ci in range(4):
                kb.act(cacc, cacc[:, ci, 0:NV], pre, pre[:, ci, 0:NV], AF.Copy, scale=(cw, cw[:, ci, 0:1]), part=True)
                for j in range(1, 5):
                    kb.stt(cacc, cacc[:, ci, 0:NV], pre, pre[:, ci, j:j + NV], (cw, cw[:, ci, j:j + 1]), cacc, cacc[:, ci, 0:NV],
                           ALU.mult, ALU.add, part=True)
            for ci in range(4):
                kb.act(post, post[:, ci, 0:NV], cacc, cacc[:, ci, 0:NV], AF.Silu, bias=(cw, cw[:, ci, 5:6]), part=True)
            vlo, vhi = max(t0 + 2, 0), min(t0 + 2 + NV, S)
            o0 = vlo - (t0 + 2)
            kb.dma("sync", io["post"], io["post"].t.rearrange("(c p) t -> p c t", p=128)[:, :, vlo:vhi], post, post[:, :, o0:o0 + (vhi - vlo)],
                   post.name, part=True)
            zs = zs_pool.next()
            for zi in range(2):
                ps = psB.next()
                c0 = (C_Z - C_XS) + zi * 128
                for kc in range(8):
                    kb.mm(ps, ps[:], w1b, w1b[:, kc, c0:c0 + 128], xb, xb[:, kc, :], start=(kc == 0), stop=(kc == 7))
                kb.tt("vector", pre, pre[:, zi, :], ps, ps[:], rstd, rstd[:, 0, :], ALU.mult, part=True)
                kb.act(zs, zs[:, zi, :], pre, pre[:, zi, :], AF.Silu, part=True)
            kb.dma("sync", io["zs"], io["zs"].t.rearrange("(c p) t -> p c t", p=128)[:, :, vlo:vhi], zs, zs[:, :, o0 + 2:o0 + 2 + (vhi - vlo)],
                   zs.name, part=True)
            ps = psB.next()
            c0 = C_DT - C_XS
            for kc in range(8):
                kb.mm(ps, ps[0:8, :], w1b, w1b[:, kc, c0:c0 + 8], xb, xb[:, kc, :], start=(kc == 0), stop=(kc == 7))
            dtw = dt_pool.next()
            kb.tt("vector", dtw, dtw[:, 0, :], ps, ps[0:8, :], rstd, rstd[0:8, 0, :], ALU.mult, part=True)
            kb.act(dtw, dtw[:, 1, :], dtw, dtw[:, 0, :], AF.Exp, bias=(dtc, dtc[:, 0:1]), part=True)
            kb.act(dtw, dtw[:, 2, :], dtw, dtw[:, 1, :], AF.Ln, bias=(one8, one8[:, 0:1]), part=True)
            kb.ts("vector", dtw, dtw[:, 3, :], dtw, dtw[:, 2, :], (negA, negA[:, 0:1]), None, ALU.mult, part=True)
            kb.dma("sync", io["dta"], io["dta"].t[0:8, vlo:vhi], dtw, dtw[:, 2, o0 + 2:o0 + 2 + (vhi - vlo)], dtw.name, part=True)
            kb.dma("sync", io["dta"], io["dta"].t[8:16, vlo:vhi], dtw, dtw[:, 3, o0 + 2:o0 + 2 + (vhi - vlo)], dtw.name, part=True)
        kb.P.barrier()
        st = kb.P.emit_phase()
        kb.ph = None
    return st


def phase1_ssd_scan(kb, io, dirn, nchunks=NCH):
    with contextlib.ExitStack() as ph:
        kb.ph = ph
        cs = load_consts(kb, io, ["ident_bf", "ident_f", "ones_f", "tri", "smask"])
        Dbc = kb.sb("Dbc", [128, 256], F32)
        kb.dma("sync", Dbc, Dbc[:], io["Dbc"], io["Dbc"].t, "Dbc")
        H = kb.sb("H", [128, 256], F32)
        Hbf = kb.sb("Hbf", [128, 256], BF16)
        kb.memset("vector", H, H[:], 0.0)
        kb.memset("vector", Hbf, Hbf[:], 0.0)
        inT = kb.pool("sb", "inT", 2, [128, 4, 128], BF16)
        dtaT = kb.pool("sb", "dtaT", 2, [16, 128], F32)
        xsb = kb.pool("sb", "xsb", 2, [128, 384], BF16)
        dta = kb.pool("sb", "dta", 2, [128, 16], F32)
        abc = kb.pool("sb", "abc", 2, [128, 4, 128], F32)
        col = kb.pool("sb", "col", 2, [128, 40], F32)
        LT = kb.pool("sb", "LT", 2, [128, 4, 128], F32)
        MT = kb.pool("sb", "MT", 2, [128, 4, 128], BF16)
        Ysb = kb.pool("sb", "Ysb", 2, [128, 256], F32)
        yout = kb.pool("sb", "yout", 2, [128, 256], F32)
        xsw = kb.pool("sb", "xsw", 2, [128, 256], BF16)
        yprev = kb.pool("sb", "yprev", 2, [128, 256], F32)
        ysum = kb.pool("sb", "ysum", 2, [128, 2, 256], F32)
        ybf = kb.pool("sb", "ybf", 2, [128, 256], BF16)
        zsT = kb.pool("sb", "zsT", 2, [128, 2, 128], BF16)
        ygT = kb.pool("sb", "ygT", 2, [128, 2, 128], BF16)
        pX = kb.ps("pX", [128, 512], F32)
        pD = kb.ps("pD", [128, 512], F32)
        pCS = kb.ps("pCS", [128, 512], F32)
        pG = kb.ps("pG", [128, 512], F32)
        pY = kb.ps("pY", [128, 512], F32)
        pYo = kb.ps("pYo", [128, 512], F32)
        pST = kb.ps("pST", [128, 512], F32)
        pYT = kb.ps("pYT", [128, 512], F32)
        post_v = io["post"].t.rearrange("(c p) t -> p c t", p=128)
        zs_v = io["zs"].t.rearrange("(c p) t -> p c t", p=128)
        order = range(nchunks) if dirn == 0 else range(NCH - 1, NCH - 1 - nchunks, -1)
        def stageA(c):
            tk = slice(c * 128, (c + 1) * 128)
            it = inT.next()
            kb.dma("sync", it, it[:], io["post"], post_v[:, :, tk], it.name)
            dT = dtaT.next()
            kb.dma("sync", dT, dT[:], io["dta"], io["dta"].t[:, tk], dT.name)
            for i in range(3):
                kb.mm(pX, pX[:, i * 128:(i + 1) * 128], it, it[:, i, :], cs["ident_bf"], cs["ident_bf"][:])
            xs = xsb.next()
            kb.copy("vector", xs, xs[:], pX, pX[:, 0:384])
            kb.mm(pD, pD[:, 0:16], dT, dT[:, :], cs["ident_f"], cs["ident_f"][0:16, 0:16])
            dt = dta.next()
            kb.copy("vector", dt, dt[:], pD, pD[:, 0:16])
            dtc = dt[:, 4 * dirn:4 * dirn + 4]
            ac = dt[:, 8 + 4 * dirn:8 + 4 * dirn + 4]
            ab = abc.next()
            for h in range(4):
                kb.act(ab, ab[:, h, :], cs["ones_f"], cs["ones_f"][:], AF.Copy, scale=(dt, dt[:, 8 + 4 * dirn + h:8 + 4 * dirn + h + 1]), part=True)
            kb.mm(pCS, pCS[:], cs["ident_f"], cs["ident_f"][:], cs["smask"], cs["smask"][:, dirn, :], start=True, stop=False)
            for h in range(4):
                kb.mm(pCS, pCS[:, h * 128:(h + 1) * 128], ab, ab[:, h, :], cs["tri"], cs["tri"][:, dirn, :], start=False, stop=True)
            kb.mm(pD, pD[:, 16:20], cs["tri"], cs["tri"][:, dirn, :], dt, ac)
            kb.mm(pD, pD[:, 20:24], cs["ones_f"], cs["ones_f"][:], dt, ac)
            cl = col.next()
            kb.copy("vector", cl, cl[:, 0:8], pD, pD[:, 16:24], part=True)
            kb.ts("vector", cl, cl[:, 8:12], cl, cl[:, 0:4], -1.0, None, ALU.mult, part=True)
            kb.act(cl, cl[:, 12:16], cl, cl[:, 0:4], AF.Exp, part=True)
            kb.tt("vector", cl, cl[:, 16:20], cl, cl[:, 4:8], cl, cl[:, 0:4], ALU.subtract, part=True)
            kb.act(cl, cl[:, 20:24], cl, cl[:, 16:20], AF.Exp, part=True)
            kb.tt("vector", cl, cl[:, 24:28], cl, cl[:, 20:24], dt, dtc, ALU.mult, part=True)
            kb.act(cl, cl[:, 28:32], cl, cl[:, 4:8], AF.Exp, part=True)
            lt = LT.next()
            for h in range(4):
                kb.act(lt, lt[:, h, :], pCS, pCS[:, h * 128:(h + 1) * 128], AF.Exp, bias=(cl, cl[:, 8 + h:9 + h]), part=True)
            kb.mm(pG, pG[:, 0:128], it, it[:, 2, :], it, it[:, 3, :])
            mt = MT.next()
            for h in range(4):
                kb.stt(mt, mt[:, h, :], pG, pG[:, 0:128], (dt, dt[:, 4 * dirn + h:4 * dirn + h + 1]), lt, lt[:, h, :], ALU.mult, ALU.mult, part=True)
            return dict(c=c, tk=tk, it=it, xs=xs, dt=dt, cl=cl, mt=mt)

        def stageB(ctx):
            c, tk, it, xs, dt, cl, mt = ctx['c'], ctx['tk'], ctx['it'], ctx['xs'], ctx['dt'], ctx['cl'], ctx['mt']
            for h in range(4):
                kb.mm(pY, pY[:, h * 64:(h + 1) * 64], mt, mt[:, h, :], xs, xs[:, h * 64:(h + 1) * 64])
            kb.mm(pYo, pYo[:, 0:256], it, it[:, 3, :], Hbf, Hbf[:])
            ysb = Ysb.next()
            kb.copy("vector", ysb, ysb[:], pY, pY[:, 0:256])
            yo = yout.next()
            for h in range(4):
                hs = slice(h * 64, (h + 1) * 64)
                kb.stt(yo, yo[:, hs], pYo, pYo[:, hs], (cl, cl[:, 12 + h:13 + h]), ysb, ysb[:, hs], ALU.mult, ALU.add, part=True)
            xw = xsw.next()
            for h in range(4):
                hs = slice(h * 64, (h + 1) * 64)
                kb.act(xw, xw[:, hs], xs, xs[:, hs], AF.Copy, scale=(cl, cl[:, 24 + h:25 + h]), part=True)
            kb.mm(pST, pST[:, 0:256], xs, xs[:, 256:384], xw, xw[:])
            for h in range(4):
                hs = slice(h * 64, (h + 1) * 64)
                kb.stt(H, H[:, hs], H, H[:, hs], (cl, cl[:, 28 + h:29 + h]), pST, pST[:, hs], ALU.mult, ALU.add)
            kb.copy("scalar", Hbf, Hbf[:], H, H[:])
            if dirn == 0:
                kb.dma("sync", io["yf"], io["yf"].t[tk, :], yo, yo[:], yo.name, part=True)
            else:
                yp = yprev.next()
                kb.dma("sync", yp, yp[:], io["yf"], io["yf"].t[tk, :], yp.name)
                zt = zsT.next()
                kb.dma("sync", zt, zt[:], io["zs"], zs_v[:, :, tk], zt.name)
                ys = ysum.next()
                kb.tt("vector", ys, ys[:, 0, :], xs, xs[:, 0:256], Dbc, Dbc[:], ALU.mult, part=True)
                kb.tt("vector", ys, ys[:, 1, :], yo, yo[:], yp, yp[:], ALU.add, part=True)
                yb = ybf.next()
                kb.tt("vector", yb, yb[:], ys, ys[:, 0, :], ys, ys[:, 1, :], ALU.add)
                for i in range(2):
                    kb.mm(pYT, pYT[:, i * 128:(i + 1) * 128], yb, yb[:, i * 128:(i + 1) * 128], cs["ident_bf"], cs["ident_bf"][:])
                yg = ygT.next()
                kb.tt("vector", yg, yg[:], pYT, pYT[:, 0:256].rearrange("p (c t) -> p c t", c=2), zt, zt[:], ALU.mult)
                mv = io["mixS_loc"].t.rearrange("(g c) t -> c g t", c=256)
                for i in range(2):
                    kb.dma("sync", io["mixS_loc"], mv[i * 128:(i + 1) * 128, c // 4, (c % 4) * 128:(c % 4 + 1) * 128], yg, yg[:, i, :],
                           yg.name, part=True)
        pend = None
        for c in order:
            ctx = stageA(c)
            if pend is not None:
                stageB(pend)
            pend = ctx
        stageB(pend)
        kb.P.barrier()
        st = kb.P.emit_phase()
        kb.ph = None
    return st


def router_setup(kb, io, L):
    r = {}
    r["gbc"] = kb.sb("gffn_bc", [128, DM], F32)
    kb.dma("sync", r["gbc"], r["gbc"][:], io["g_ffn_bc%d" % L], io["g_ffn_bc%d" % L].t, "gffn_bc")
    gcol = kb.sb("gffn_col", [128, 8], F32)
    kb.dma("sync", gcol, gcol[:], io["g_ffn_col%d" % L], io["g_ffn_col%d" % L].t, "gffn_col")
    wr = kb.sb("wr", [128, 8, NE], F32)
    kb.dma("sync", wr, wr[:], io["wr%d" % L], io["wr%d" % L].t.rearrange("(k p) e -> p k e", p=128), "wr")
    r["wrg"] = kb.sb("wrg", [128, 8, NE], F32)
    for kc in range(8):
        kb.ts("vector", r["wrg"], r["wrg"][:, kc, :], wr, wr[:, kc, :], (gcol, gcol[:, kc:kc + 1]), None, ALU.mult, part=True)
    r["affT"] = kb.sb("affT", [NE, 4096], F32)
    r["sqj"] = kb.pool("sb", "sqj", 2, [128, DM], BF16)
    r["st"] = kb.pool("sb", "rst", 2, [128, 8], F32)
    r["h2"] = kb.pool("sb", "h2t", 2, [128, DM], BF16)
    r["xT"] = kb.pool("sb", "x1T", 2, [128, 8, 128], F32)
    r["lg"] = kb.pool("sb", "lg", 2, [128, 3, NE], F32)
    r["pXT"] = kb.pool("ps", "pXT", 2, [128, 4, 128], F32)
    r["pL"] = kb.ps("pL", [128, 512], F32)
    return r


def router_tile(kb, io, cs, r, x1, i):
    st = r["st"].next()
    sqj = r["sqj"].next()
    kb.act(sqj, sqj[:], x1, x1[:], AF.Square, accum=(st, st[:, 0:1]))
    kb.rstd_from(st, st[:, 2:3], st, st[:, 0:1], DM, st, st[:, 1:2], cs["eps"])
    h2 = r["h2"].next()
    kb.stt(h2, h2[:], x1, x1[:], (st, st[:, 2:3]), r["gbc"], r["gbc"][:], ALU.mult, ALU.mult)
    kb.dma("sync", io["h2_loc"], io["h2_loc"].t[i * 128:(i + 1) * 128, :], h2, h2[:], h2.name, part=True)
    xT = r["xT"].next()
    for half in range(2):
        pX = r["pXT"].next()
        for k4 in range(4):
            kc = half * 4 + k4
            kb.mm(pX, pX[:, k4, :], x1, x1[:, kc * 128:(kc + 1) * 128], cs["ident_f"], cs["ident_f"][:])
        kb.copy("vector", xT, xT[:, half * 4:(half + 1) * 4, :], pX, pX[:], part=True)
    pL = r["pL"]
    for kc in range(8):
        kb.mm(pL, pL[:, 0:NE], xT, xT[:, kc, :], r["wrg"], r["wrg"][:, kc, :], start=(kc == 0), stop=(kc == 7))
    lg = r["lg"].next()
    kb.ts("vector", lg, lg[:, 0, :], pL, pL[:, 0:NE], (st, st[:, 2:3]), None, ALU.mult, part=True)
    kb.P.op("vector", lambda e: e.tensor_reduce(out=st[:, 3:4], in_=lg[:, 0, :], axis=AX.X, op=ALU.max), reads=[lg.b], partial=[st.b])
    kb.ts("vector", st, st[:, 4:5], st, st[:, 3:4], -1.0, None, ALU.mult, part=True)
    kb.act(lg, lg[:, 1, :], lg, lg[:, 0, :], AF.Exp, bias=(st, st[:, 4:5]), accum=(st, st[:, 5:6]), part=True)
    kb.P.op("vector", lambda e: e.reciprocal(out=st[:, 6:7], in_=st[:, 5:6]), reads=[st.b], partial=[st.b])
    kb.ts("vector", lg, lg[:, 2, :], lg, lg[:, 1, :], (st, st[:, 6:7]), None, ALU.mult, part=True)
    kb.mm(pL, pL[0:NE, 128:256], lg, lg[:, 2, :], cs["ident_f"], cs["ident_f"][:])
    kb.copy("vector", r["affT"], r["affT"][:, i * 128:(i + 1) * 128], pL, pL[0:NE, 128:256], part=True)


def phase2(kb, io, ntiles=32):
    with contextlib.ExitStack() as ph:
        kb.ph = ph
        cs = load_consts(kb, io, ["ident_f", "ones_bf"])
        r = router_setup(kb, io, 0)
        gwo = kb.sb("gwo", [128, 16], F32)
        kb.dma("sync", gwo, gwo[:], io["g_wo"], io["g_wo"].t, "gwo")
        wob = kb.sb("wob", [128, 16, DM], BF16)
        stage = kb.pool("sb", "wostg", 2, [128, DM], F32)
        for ck in range(16):
            stg = stage.next()
            kb.dma("sync", stg, stg[:], io["wo"], io["wo"].t[ck * 128:(ck + 1) * 128, :], stg.name)
            kb.ts("vector", wob, wob[:, ck, :], stg, stg[:], (gwo, gwo[:, ck:ck + 1]), None, ALU.mult, part=True)
        midx = kb.sb("mixidx", [128, 8, 16], I32)
        kb.dma("sync", midx, midx[:], io["mixidx"], io["mixidx"].t, "mixidx")
        mixT = kb.pool("sb", "mixT", 2, [128, 16, WIN], BF16)
        ysq = kb.pool("sb", "ysq", 2, [128, 8, 128], BF16)
        xt = kb.pool("sb", "xt", 2, [128, DM], F32)
        x1p = kb.pool("sb", "x1", 2, [128, DM], F32)
        rsy = kb.pool("sb", "rsy", 2, [128, 4], F32)
        pA = kb.ps("pA", [128, 2, 512], F32)
        pS = kb.ps("pS", [128, 2, 512], F32)
        pq = kb.ps("pq", [128, 512], F32)
        ssd_ck = [q * 4 + j for q in range(4) for j in (2, 3)]
        att_ck = [q * 4 + j for q in range(4) for j in (0, 1)]
        for w in range((ntiles + 3) // 4):
            mt = mixT.next()
            for ck in range(16):
                src = io["mixA_all"] if ck % 4 < 2 else io["mixS_all"]
                kb.P.dma("gpsimd", (lambda e, mt=mt, ck=ck, w=w, src=src: e.indirect_dma_start(
                    out=mt[:, ck, :], out_offset=None, in_=src.t,
                    in_offset=bass.IndirectOffsetOnAxis(ap=midx[:, w, ck:ck + 1], axis=0))),
                    mt.name, reads=[src.b, midx.b], partial=[mt.b])
            for j in range(min(4, ntiles - 4 * w)):
                i = 4 * w + j
                tk = slice(j * 128, (j + 1) * 128)
                ys = ysq.next()
                for n, ck in enumerate(ssd_ck):
                    kb.act(ys, ys[:, n, :], mt, mt[:, ck, tk], AF.Square, part=True)
                for n in range(8):
                    kb.mm(pq, pq[:, 0:1], ys, ys[:, n, :], cs["ones_bf"], cs["ones_bf"][:, 0:1], start=(n == 0), stop=(n == 7))
                rs = rsy.next()
                kb.rstd_from(rs, rs[:, 1:2], pq, pq[:, 0:1], 1024, rs, rs[:, 0:1], cs["eps"])
                for half in range(2):
                    for n, ck in enumerate(att_ck):
                        kb.mm(pA, pA[:, half, :], mt, mt[:, ck, tk], wob, wob[:, ck, half * 512:(half + 1) * 512], start=(n == 0), stop=(n == 7))
                    for n, ck in enumerate(ssd_ck):
                        kb.mm(pS, pS[:, half, :], mt, mt[:, ck, tk], wob, wob[:, ck, half * 512:(half + 1) * 512], start=(n == 0), stop=(n == 7))
                x = xt.next()
                kb.dma("sync", x, x[:], io["x_tok"], io["x_tok"].t[i * 128:(i + 1) * 128, :], x.name)
                x1 = x1p.next()
                kb.stt(x1, x1[:], pS, pS[:].rearrange("p a b -> p (a b)"), (rs, rs[:, 1:2]), x, x[:], ALU.mult, ALU.add)
                kb.tt("vector", x1, x1[:], x1, x1[:], pA, pA[:].rearrange("p a b -> p (a b)"), ALU.add)
                kb.dma("sync", io["x1_loc"], io["x1_loc"].t[i * 128:(i + 1) * 128, :], x1, x1[:], x1.name, part=True)
                router_tile(kb, io, cs, r, x1, i)
        kb.dma("sync", io["aff_loc"], io["aff_loc"].t, r["affT"], r["affT"][:], "affT")
        kb.P.barrier()
        st = kb.P.emit_phase()
        kb.ph = None
    return st


def zero_ydense(kb, io):
    zt = kb.sb("zt", [128, 4, DM], F32)
    kb.memset("gpsimd", zt, zt[:], 0.0)
    yv = io["ydense"].t.rearrange("(a p) d -> p a d", p=128)
    for a in range(32):
        kb.dma("sync", io["ydense"], yv[:, a * 4:(a + 1) * 4, :], zt, zt[:], "zt", part=True)


def moe_topk(kb, io, cs, L):
    aidx = kb.sb("affidx", [128, 1], I32)
    kb.dma("sync", aidx, aidx[:], io["affidx"], io["affidx"].t, "affidx")
    arow = kb.sb("arow", [128, 4096], F32)
    kb.P.dma("gpsimd", lambda e: e.indirect_dma_start(out=arow[0:16, :], out_offset=None, in_=io["aff_all"].t,
             in_offset=bass.IndirectOffsetOnAxis(ap=aidx[0:16, 0:1], axis=0)), "arow", reads=[io["aff_all"].b, aidx.b], writes=[arow.b])
    kb.dma("sync", io["aff_my"], io["aff_my"].t, arow, arow[0:16, :], "arow")
    collective(kb, "AllGather", ALU.bypass, io["h2_loc"], io["h2_all"], "cc2", 8)
    A = kb.sb("A", [128, 4, 128], F32)
    kb.dma("sync", A, A[:], io["aff_my"], io["aff_my"].t.rearrange("(e q) (a j) -> (q a) e j", e=4, j=128), "A")
    lohi = kb.sb("lohi", [128, 16], F32)
    cmp_ = kb.sb("cmp", [128, 128], F32)
    cnt = kb.sb("cnt", [128, 8], F32)
    pc = kb.ps("pc", [128, 512], F32)
    kb.memset("vector", lohi, lohi[:, 0:4], 0.0, part=True)
    kb.memset("vector", lohi, lohi[:, 4:8], 1.0, part=True)
    for it in range(30):
        kb.tt("vector", lohi, lohi[:, 8:12], lohi, lohi[:, 0:4], lohi, lohi[:, 4:8], ALU.add)
        kb.ts("vector", lohi, lohi[:, 8:12], lohi, lohi[:, 8:12], 0.5, None, ALU.mult)
        for e in range(4):
            kb.ts("vector", cmp_, cmp_[:], A, A[:, e, :], (lohi, lohi[:, 8 + e:9 + e]), 0.0, ALU.is_ge, op1=ALU.add, accum=(cnt, cnt[:, e:e + 1]))
        kb.mm(pc, pc[:, 0:4], cs["ones_f"], cs["ones_f"][:], cnt, cnt[:, 0:4])
        kb.ts("vector", lohi, lohi[:, 12:16], pc, pc[:, 0:4], float(CAP), None, ALU.is_ge)
        kb.tt("vector", cnt, cnt[:, 4:8], lohi, lohi[:, 8:12], lohi, lohi[:, 0:4], ALU.subtract)
        kb.tt("vector", cnt, cnt[:, 4:8], cnt, cnt[:, 4:8], lohi, lohi[:, 12:16], ALU.mult)
        kb.tt("vector", lohi, lohi[:, 0:4], lohi, lohi[:, 0:4], cnt, cnt[:, 4:8], ALU.add)
        kb.tt("vector", cnt, cnt[:, 4:8], lohi, lohi[:, 4:8], lohi, lohi[:, 8:12], ALU.subtract)
        kb.tt("vector", cnt, cnt[:, 4:8], cnt, cnt[:, 4:8], lohi, lohi[:, 12:16], ALU.mult)
        kb.tt("vector", lohi, lohi[:, 4:8], lohi, lohi[:, 8:12], cnt, cnt[:, 4:8], ALU.add)
    mask = kb.sb("mask", [128, 4, 128], F32)
    incl = kb.sb("incl", [128, 4, 128], F32)
    slot = kb.sb("slot", [128, 4, 128], F32)
    sloti = kb.sb("sloti", [128, 4, 128], I32)
    pairs = kb.sb("pairs", [128, 4, 128, 3], F32)
    tot = kb.sb("tot", [128, 8], F32)
    for e in range(4):
        kb.ts("vector", mask, mask[:, e, :], A, A[:, e, :], (lohi, lohi[:, e:e + 1]), None, ALU.is_ge, part=True)
        kb.P.op("vector", lambda en, e=e: en.tensor_tensor_scan(out=incl[:, e, :], data0=cs["ones_f"][:], data1=mask[:, e, :], initial=0.0,
                                                               op0=ALU.mult, op1=ALU.add), reads=[cs["ones_f"].b, mask.b], partial=[incl.b])
        kb.copy("vector", tot, tot[:, e:e + 1], incl, incl[:, e, 127:128], part=True)
    kb.mm(pc, pc[:, 8:12], cs["ustrict"], cs["ustrict"][:], tot, tot[:, 0:4])
    kb.copy("vector", tot, tot[:, 4:8], pc, pc[:, 8:12], part=True)
    for e in range(4):
        kb.tt("vector", slot, slot[:, e, :], incl, incl[:, e, :], mask, mask[:, e, :], ALU.subtract, part=True)
        kb.ts("vector", slot, slot[:, e, :], slot, slot[:, e, :], (tot, tot[:, 4 + e:5 + e]), float(e * CAP), ALU.add, op1=ALU.add, part=True)
        kb.ts("vector", incl, incl[:, e, :], mask, mask[:, e, :], -1.0e6, 1.0e6, ALU.mult, op1=ALU.add, part=True)
        kb.tt("vector", slot, slot[:, e, :], slot, slot[:, e, :], incl, incl[:, e, :], ALU.add, part=True)
        kb.ts("vector", incl, incl[:, e, :], slot, slot[:, e, :], float((e + 1) * CAP), 1.0e6, ALU.is_ge, op1=ALU.mult, part=True)
        kb.tt("vector", slot, slot[:, e, :], slot, slot[:, e, :], incl, incl[:, e, :], ALU.add, part=True)
        kb.copy("vector", sloti, sloti[:, e, :], slot, slot[:, e, :], part=True)
        kb.copy("gpsimd", pairs, pairs[:, e, :, 0], cs["iota_tok"], cs["iota_tok"][:], part=True)
        kb.copy("gpsimd", pairs, pairs[:, e, :, 1], A, A[:, e, :], part=True)
        kb.copy("gpsimd", pairs, pairs[:, e, :, 2], cs["iota_h2row"], cs["iota_h2row"][:], part=True)
    rc = {}

    def breg(en):
        if "r" not in rc:
            rc["r"] = en.to_reg(4 * CAP - 1)
        return rc["r"]

    for e in range(4):
        for j in range(128):
            kb.P.dma("gpsimd", (lambda en, e=e, j=j: en.indirect_dma_start(
                out=io["sel"].t, out_offset=bass.IndirectOffsetOnAxis(ap=sloti[:, e, j:j + 1], axis=0),
                in_=pairs[:, e, j, :], in_offset=None, bounds_check=breg(en), oob_is_err=False)),
                "selsc", reads=[pairs.b, sloti.b], partial=[io["sel"].b])


def moe_phase(kb, io, L):
    with contextlib.ExitStack() as ph:
        kb.ph = ph
        cs = load_consts(kb, io, ["ident_bf", "ones_f", "ustrict", "iota_tok", "iota_h2row"])
        with contextlib.ExitStack() as ph2:
            kb.ph = ph2
            moe_topk(kb, io, cs, L)
            kb.P.barrier()
            kb.P.emit_phase()
        kb.ph = ph
        cs = load_consts(kb, io, ["ident_bf"])
        xeT = kb.sb("xeT", [128, 8, CAP], BF16)
        hidT = kb.sb("hidT", [128, 16, CAP], BF16)
        wdb = kb.sb("wdb", [128, 16, DM], BF16)
        wgb = kb.pool("sb", "wgb", 2, [128, 8, 512], BF16)
        wub = kb.pool("sb", "wub", 2, [128, 8, 512], BF16)
        selT = kb.pool("sb", "selT", 2, [128, 16, 3], F32)
        toki = kb.pool("sb", "toki", 2, [128, 2, 16], I32)
        xe = kb.pool("sb", "xe", 3, [128, DM], BF16)
        sg = kb.pool("sb", "sg", 2, [128, 512], BF16)
        ye = kb.pool("sb", "ye", 2, [128, DM], F32)
        pT = kb.pool("ps", "pT", 2, [128, 4, 128], F32)
        pGt = kb.pool("ps", "pGt", 2, [128, 512], F32)
        pUp = kb.pool("ps", "pUp", 2, [128, 512], F32)
        pYe = kb.ps("pYe", [128, 2, 512], F32)
        wg, wu, wd = io["wg%d" % L], io["wu%d" % L], io["wd%d" % L]
        for e in range(4):
            sT = selT.next()
            kb.dma("sync", sT, sT[:], io["sel"], io["sel"].t[e * CAP:(e + 1) * CAP, :].rearrange("(k p) c -> p k c", p=128), sT.name)
            ti = toki.next()
            kb.copy("vector", ti, ti[:, 0, :], sT, sT[:, :, 0], part=True)
            kb.copy("vector", ti, ti[:, 1, :], sT, sT[:, :, 2], part=True)
            for q in range(4):
                kb.dma("gpsimd", wdb, wdb[:, q * 4:(q + 1) * 4, :], wd, wd.t[e, q * 512:(q + 1) * 512, :].rearrange("(k p) d -> p k d", p=128),
                       "wdb", part=True)
            for k in range(16):
                x = xe.next()
                kb.P.dma("gpsimd", (lambda en, x=x, ti=ti, k=k: en.indirect_dma_start(
                    out=x[:], out_offset=None, in_=io["h2_all"].t, in_offset=bass.IndirectOffsetOnAxis(ap=ti[:, 1, k:k + 1], axis=0))),
                    x.name, reads=[io["h2_all"].b, ti.b], writes=[x.b])
                for half in range(2):
                    p = pT.next()
                    for k4 in range(4):
                        kc = half * 4 + k4
                        kb.mm(p, p[:, k4, :], x, x[:, kc * 128:(kc + 1) * 128], cs["ident_bf"], cs["ident_bf"][:])
                    kb.copy("vector", xeT, xeT[:, half * 4:(half + 1) * 4, k * 128:(k + 1) * 128], p, p[:], part=True)
            for fg in range(4):
                wgt = wgb.next()
                wut = wub.next()
                kb.dma("gpsimd", wgt, wgt[:], wg, wg.t[e, :, fg * 512:(fg + 1) * 512].rearrange("(k p) f -> p k f", p=128), wgt.name)
                kb.dma("gpsimd", wut, wut[:], wu, wu.t[e, :, fg * 512:(fg + 1) * 512].rearrange("(k p) f -> p k f", p=128), wut.name)
                for f4 in range(4):
                    fi = fg * 4 + f4
                    for win in range(4):
                        pg = pGt.next()
                        pu = pUp.next()
                        for kc in range(8):
                            kb.mm(pg, pg[:], wgt, wgt[:, kc, f4 * 128:(f4 + 1) * 128], xeT, xeT[:, kc, win * 512:(win + 1) * 512],
                                  start=(kc == 0), stop=(kc == 7))
                        for kc in range(8):
                            kb.mm(pu, pu[:], wut, wut[:, kc, f4 * 128:(f4 + 1) * 128], xeT, xeT[:, kc, win * 512:(win + 1) * 512],
                                  start=(kc == 0), stop=(kc == 7))
                        s_ = sg.next()
                        kb.act(s_, s_[:], pg, pg[:], AF.Silu)
                        kb.tt("vector", hidT, hidT[:, fi, win * 512:(win + 1) * 512], s_, s_[:], pu, pu[:], ALU.mult, part=True)
            for k in range(16):
                for half in range(2):
                    for fi in range(16):
                        kb.mm(pYe, pYe[:, half, :], hidT, hidT[:, fi, k * 128:(k + 1) * 128], wdb, wdb[:, fi, half * 512:(half + 1) * 512],
                              start=(fi == 0), stop=(fi == 15))
                y = ye.next()
                kb.ts("vector", y, y[:], pYe, pYe[:].rearrange("p a b -> p (a b)"), (sT, sT[:, k, 1:2]), None, ALU.mult)
                kb.P.dma("gpsimd", (lambda en, y=y, ti=ti, k=k: en.indirect_dma_start(
                    out=io["ydense"].t, out_offset=bass.IndirectOffsetOnAxis(ap=ti[:, 0, k:k + 1], axis=0), in_=y[:], in_offset=None,
                    compute_op=ALU.add)), y.name, reads=[y.b, ti.b], writes=[io["ydense"].b])
        kb.P.barrier()
        st = kb.P.emit_phase()
        kb.ph = None
    return st


def phase4(kb, io):
    NT = 32
    TL = 4096
    with contextlib.ExitStack() as ph:
        kb.ph = ph
        cs = load_consts(kb, io, ["ident_bf", "ident_f"])
        hnT = kb.sb("hnT", [128, 8, TL + 2], BF16)
        mT = kb.sb("mT", [128, 8, TL], BF16)
        with contextlib.ExitStack() as ph2:
            kb.ph = ph2
            gbc = kb.sb("gconv_bc", [128, DM], F32)
            kb.dma("sync", gbc, gbc[:], io["g_conv_bc"], io["g_conv_bc"].t, "gconv_bc")
            rows = kb.sb("myrows", [128, 33], I32)
            kb.dma("sync", rows, rows[:], io["myrows"], io["myrows"].t, "myrows")
            hidx = kb.sb("haloidx", [128, 1], I32)
            kb.dma("sync", hidx, hidx[:], io["haloidx"], io["haloidx"].t, "haloidx")
            hmsk = kb.sb("halomsk", [128, 1], F32)
            kb.dma("sync", hmsk, hmsk[:], io["halomsk"], io["halomsk"].t, "halomsk")
            x1p = kb.pool("sb", "x1t", 2, [128, DM], F32)
            ysp = kb.pool("sb", "yst", 2, [128, DM], BF16)
            x2p = kb.pool("sb", "x2t", 2, [128, DM], F32)
            sqj = kb.pool("sb", "sqj", 2, [128, DM], BF16)
            stp = kb.pool("sb", "st4", 2, [128, 4], F32)
            hnp = kb.pool("sb", "hn", 2, [128, DM], BF16)
            pT = kb.pool("ps", "pT4", 2, [128, 4, 128], F32)
            for i in range(NT + 1):
                x1 = x1p.next()
                ys = ysp.next()
                if i < NT:
                    kb.dma("sync", x1, x1[:], io["x1_loc"], io["x1_loc"].t[i * 128:(i + 1) * 128, :], x1.name)
                else:
                    kb.P.dma("gpsimd", (lambda en, x1=x1: en.indirect_dma_start(out=x1[:], out_offset=None, in_=io["edges_all"].t,
                             in_offset=bass.IndirectOffsetOnAxis(ap=hidx[:, 0:1], axis=0))), x1.name, reads=[io["edges_all"].b, hidx.b], writes=[x1.b])
                kb.P.dma("gpsimd", (lambda en, ys=ys, i=i: en.indirect_dma_start(out=ys[:], out_offset=None, in_=io["ysum_all"].t,
                         in_offset=bass.IndirectOffsetOnAxis(ap=rows[:, i:i + 1], axis=0))), ys.name, reads=[io["ysum_all"].b, rows.b], writes=[ys.b])
                x2 = x2p.next()
                kb.tt("vector", x2, x2[:], x1, x1[:], ys, ys[:], ALU.add)
                if i == NT:
                    kb.ts("vector", x2, x2[:], x2, x2[:], (hmsk, hmsk[:, 0:1]), None, ALU.mult)
                else:
                    kb.dma("sync", io["x2_loc"], io["x2_loc"].t[i * 128:(i + 1) * 128, :], x2, x2[:], x2.name, part=True)
                st = stp.next()
                sq = sqj.next()
                kb.act(sq, sq[:], x2, x2[:], AF.Square, accum=(st, st[:, 0:1]))
                kb.rstd_from(st, st[:, 2:3], st, st[:, 0:1], DM, st, st[:, 1:2], cs["eps"])
                hn = hnp.next()
                kb.stt(hn, hn[:], x2, x2[:], (st, st[:, 2:3]), gbc, gbc[:], ALU.mult, ALU.mult)
                for half in range(2):
                    p = pT.next()
                    for k4 in range(4):
                        kc = half * 4 + k4
                        kb.mm(p, p[:, k4, :], hn, hn[:, kc * 128:(kc + 1) * 128], cs["ident_bf"], cs["ident_bf"][:])
                    if i < NT:
                        kb.copy("vector", hnT, hnT[:, half * 4:(half + 1) * 4, 1 + i * 128:1 + (i + 1) * 128], p, p[:], part=True)
                    else:
                        kb.copy("vector", hnT, hnT[:, half * 4:(half + 1) * 4, 0:1], p, p[:, :, 0:1], part=True)
                        kb.copy("vector", hnT, hnT[:, half * 4:(half + 1) * 4, TL + 1:TL + 2], p, p[:, :, 1:2], part=True)
            kb.P.barrier()
            kb.P.emit_phase()
        with contextlib.ExitStack() as ph2:
            kb.ph = ph2
            w2b = kb.pool("sb", "w2b", 2, [128, 8, 384], BF16)
            cw = kb.sb("cw3", [128, 8, 3], F32)
            kb.dma("sync", cw, cw[:], io["cw3"], io["cw3"].t, "cw3")
            csb = kb.pool("sb", "csb", 2, [128, 512], F32)
            vv = kb.pool("sb", "vv", 2, [128, 512], F32)
            ca = kb.pool("sb", "ca", 2, [128, 2, 512], F32)
            pB = kb.pool("ps", "pB4", 2, [128, 512], F32)
            pC = kb.pool("ps", "pC4", 2, [128, 512], F32)
            pU = kb.pool("ps", "pU4", 2, [128, 512], F32)
            w2 = io["w2"]
            nwin = 9
            for cc in range(8):
                wt = w2b.next()
                for part in range(3):
                    kb.dma("gpsimd", wt, wt[:, :, part * 128:(part + 1) * 128],
                           w2, w2.t[:, part * DM + cc * 128: part * DM + (cc + 1) * 128].rearrange("(k p) f -> p k f", p=128), wt.name, part=True)
                for w in range(nwin):
                    c0 = 510 * w if w < nwin - 1 else TL + 2 - 512
                    pb, pc, pu = pB.next(), pC.next(), pU.next()
                    for (pp, part) in ((pb, 0), (pc, 1), (pu, 2)):
                        for kc in range(8):
                            kb.mm(pp, pp[:], wt, wt[:, kc, part * 128:(part + 1) * 128], hnT, hnT[:, kc, c0:c0 + 512], start=(kc == 0), stop=(kc == 7))
                    cb = csb.next()
                    kb.copy("scalar", cb, cb[:], pc, pc[:])
                    v = vv.next()
                    kb.tt("vector", v, v[:], cb, cb[:], pu, pu[:], ALU.mult)
                    a = ca.next()
                    kb.act(a, a[:, 0, 0:510], v, v[:, 0:510], AF.Copy, scale=(cw, cw[:, cc, 0:1]), part=True)
                    kb.stt(a, a[:, 0, 0:510], v, v[:, 1:511], (cw, cw[:, cc, 1:2]), a, a[:, 0, 0:510], ALU.mult, ALU.add, part=True)
                    kb.stt(a, a[:, 1, 0:510], v, v[:, 2:512], (cw, cw[:, cc, 2:3]), a, a[:, 0, 0:510], ALU.mult, ALU.add, part=True)
                    kb.tt("vector", mT, mT[:, cc, c0:c0 + 510], a, a[:, 1, 0:510], pb, pb[:, 1:511], ALU.mult, part=True)
            kb.P.barrier()
            kb.P.emit_phase()
        with contextlib.ExitStack() as ph2:
            kb.ph = ph2
            cs = load_consts(kb, io, ["ident_f"])
            r = router_setup(kb, io, 1)
            w3b = kb.sb("w3b", [128, 8, DM], BF16)
            kb.dma("gpsimd", w3b, w3b[:], io["w3"], io["w3"].t.rearrange("(k p) d -> p k d", p=128), "w3b")
            x2p = kb.pool("sb", "x2r", 2, [128, DM], F32)
            x3p = kb.pool("sb", "x3", 2, [128, DM], F32)
            pO = kb.ps("pO4", [128, 2, 512], F32)
            for i in range(NT):
                for half in range(2):
                    for cc in range(8):
                        kb.mm(pO, pO[:, half, :], mT, mT[:, cc, i * 128:(i + 1) * 128], w3b, w3b[:, cc, half * 512:(half + 1) * 512],
                              start=(cc == 0), stop=(cc == 7))
                x2 = x2p.next()
                kb.dma("sync", x2, x2[:], io["x2_loc"], io["x2_loc"].t[i * 128:(i + 1) * 128, :], x2.name)
                x3 = x3p.next()
                kb.tt("vector", x3, x3[:], x2, x2[:], pO, pO[:].rearrange("p a b -> p (a b)"), ALU.add)
                kb.dma("sync", io["x1_loc"], io["x1_loc"].t[i * 128:(i + 1) * 128, :], x3, x3[:], x3.name, part=True)
                router_tile(kb, io, cs, r, x3, i)
            kb.dma("sync", io["aff_loc"], io["aff_loc"].t, r["affT"], r["affT"][:], "affT")
            kb.P.barrier()
            st = kb.P.emit_phase()
        kb.ph = None
    return st


def final_phase(kb, io):
    with contextlib.ExitStack() as ph:
        kb.ph = ph
        rows = kb.sb("myrows", [128, 33], I32)
        kb.dma("sync", rows, rows[:], io["myrows"], io["myrows"].t, "myrows")
        x1p = kb.pool("sb", "x1t", 2, [128, DM], F32)
        ysp = kb.pool("sb", "yst", 2, [128, DM], BF16)
        op = kb.pool("sb", "ot", 2, [128, DM], F32)
        for i in range(32):
            x1 = x1p.next()
            ys = ysp.next()
            kb.dma("sync", x1, x1[:], io["x1_loc"], io["x1_loc"].t[i * 128:(i + 1) * 128, :], x1.name)
            kb.P.dma("gpsimd", (lambda en, ys=ys, i=i: en.indirect_dma_start(out=ys[:], out_offset=None, in_=io["ysum_all"].t,
                     in_offset=bass.IndirectOffsetOnAxis(ap=rows[:, i:i + 1], axis=0))), ys.name, reads=[io["ysum_all"].b, rows.b], writes=[ys.b])
            o = op.next()
            kb.tt("vector", o, o[:], x1, x1[:], ys, ys[:], ALU.add)
            kb.dma("sync", io["out"], io["out"].t[i * 128:(i + 1) * 128, :], o, o[:], o.name, part=True)
        kb.P.barrier()
        st = kb.P.emit_phase()
        kb.ph = None
    return st


def collective(kb, kind, op, src, dst, key, nchunks=1):
    rin = src.t.shape[0] // nchunks
    rout = dst.t.shape[0] // nchunks
    for c in range(nchunks):
        sap = src.t[c * rin:(c + 1) * rin, :]
        dap = dst.t[c * rout:(c + 1) * rout, :]
        kb.P.dma("gpsimd", (lambda e, sap=sap, dap=dap: e.collective_compute(kind, op, replica_groups=GROUPS, ins=[sap.opt()], outs=[dap.opt()])),
                 key, reads=[src.b], writes=[] if nchunks > 1 else [dst.b], partial=[dst.b] if nchunks > 1 else [], inc=1)


IN_SPECS = {
    "xT": ([DM, S], F32), "x_tok": ([4096, DM], F32), "w1": ([DM, NC1], F32), "g_attn": ([128, 8], F32), "gqk": ([128, 2], F32),
    "convw": ([128, 4, 6], F32), "dtc": ([8, 2], F32), "Dbc": ([128, 256], F32), "wo": ([2048, DM], F32), "g_wo": ([128, 16], F32),
    "mixidx": ([128, 8, 16], I32), "wr0": ([DM, NE], F32), "wr1": ([DM, NE], F32), "g_ffn_col0": ([128, 8], F32),
    "g_ffn_col1": ([128, 8], F32), "g_ffn_bc0": ([128, DM], F32), "g_ffn_bc1": ([128, DM], F32), "affidx": ([128, 1], I32),
    "myrows": ([128, 33], I32), "haloidx": ([128, 1], I32), "halomsk": ([128, 1], F32), "g_conv_bc": ([128, DM], F32),
    "w2": ([DM, 3 * DM], F32), "cw3": ([128, 8, 3], F32), "w3": ([DM, DM], F32),
    "wg0": ([4, DM, FF], F32), "wu0": ([4, DM, FF], F32), "wd0": ([4, FF, DM], F32),
    "wg1": ([4, DM, FF], F32), "wu1": ([4, DM, FF], F32), "wd1": ([4, FF, DM], F32),
}
CONST_NAMES = ["ident_bf", "ident_f", "negmask", "blockones", "rotm", "sel_ones", "ones_bf", "ones_f", "tri", "smask", "ustrict",
               "iota_tok", "iota_h2row", "cosf", "sinf"]
SCRATCH = {
    "mixA_loc": ([8192, 512], BF16), "mixA_all": ([32768, 512], BF16), "mixS_loc": ([8192, 512], BF16), "mixS_all": ([32768, 512], BF16), "post": ([512, S], BF16), "zs": ([256, S], BF16),
    "dta": ([16, S], F32), "yf": ([S, 256], F32), "x1_loc": ([4096, DM], F32), "x2_loc": ([4096, DM], F32),
    "h2_loc": ([4096, DM], BF16), "h2_all": ([S, DM], BF16), "aff_loc": ([NE, 4096], F32), "aff_all": ([4 * NE, 4096], F32),
    "aff_my": ([NE, 4096], F32), "sel": ([4 * CAP, 3], F32), "ydense": ([S, DM], F32), "ydense_bf": ([S, DM], BF16), "ysum_all": ([S, DM], BF16),
    "edges_loc": ([2, DM], F32), "edges_all": ([8, DM], F32),
}


def moe_allreduce(kb, io, key):
    for c in range(8):
        tc = T(io["ydense_bf"].t[c * 2048:(c + 1) * 2048, :], "ybf%d" % c)
        dc = T(io["ysum_all"].t[c * 2048:(c + 1) * 2048, :], "ysum%d" % c)
        for a in range(2):
            rs = slice((2 * c + a) * 1024, (2 * c + a + 1) * 1024)
            kb.dma("gpsimd", tc, io["ydense_bf"].t[rs, :], io["ydense"], io["ydense"].t[rs, :], "ycast", part=True)
        collective(kb, "AllReduce", ALU.add, tc, dc, key, 1)


def misc_phase(kb, fn):
    with contextlib.ExitStack() as ph:
        kb.ph = ph
        fn()
        kb.P.barrier()
        st = kb.P.emit_phase()
        kb.ph = None
    return st


def build_program(consts, debug=False, upto=99):
    nc = bass.Bass("TRN2", target_bir_lowering=False)
    with contextlib.ExitStack() as st:
        kb = KB(nc, st)
        io = {}
        for n, (shape, dt) in IN_SPECS.items():
            if upto < 3 and n[:2] in ("wg", "wu", "wd"):
                continue
            io[n] = kb.dram(n, shape, dt, "ExternalInput")
        for n in CONST_NAMES:
            io[n] = kb.dram(n, list(consts[n].shape), CONST_DT.get(n, F32), "ExternalInput")
        for n, (shape, dt) in SCRATCH.items():
            io[n] = kb.dram(n, shape, dt)
        io["out"] = kb.dram("out", [4096, DM], F32, "ExternalOutput")
        dbg = {}
        if debug:
            for n, shape in (("dbg_x1", [4096, DM]), ("dbg_x2", [4096, DM]), ("dbg_x3", [4096, DM]), ("dbg_aff0", [NE, 4096]),
                             ("dbg_aff1", [NE, 4096]), ("dbg_sel", [4 * CAP, 3])):
                dbg[n] = kb.dram(n, shape, F32, "ExternalOutput")


        def cp(dst, src, key):
            kb.dma("sync", dst, dst.t, src, src.t, key)

        import os
        if not os.environ.get("K_SKIP1"):
            phase1_attn(kb, io, 0)
            phase1_attn(kb, io, 1)
            phase1_ssd_in(kb, io)
            phase1_ssd_scan(kb, io, 0)
            phase1_ssd_scan(kb, io, 1)

        def ph_a():
            collective(kb, "AllGather", ALU.bypass, io["mixS_loc"], io["mixS_all"], "cc0", 8)
            zero_ydense(kb, io)

        misc_phase(kb, ph_a)
        if upto >= 2:
            phase2(kb, io, int(os.environ.get("K_NT", 32)))

            def ph_b(L):
                def f():
                    if L == 0:
                        kb.dma("sync", io["edges_loc"], io["edges_loc"].t[0:1, :], io["x1_loc"], io["x1_loc"].t[0:1, :], "edg", part=True)
                        kb.dma("sync", io["edges_loc"], io["edges_loc"].t[1:2, :], io["x1_loc"], io["x1_loc"].t[4095:4096, :], "edg", part=True)
                        collective(kb, "AllGather", ALU.bypass, io["edges_loc"], io["edges_all"], "cc1")
                    collective(kb, "AllGather", ALU.bypass, io["aff_loc"], io["aff_all"], "cc3")
                    if debug:
                        cp(dbg["dbg_x1" if L == 0 else "dbg_x3"], io["x1_loc"], "dbgx")
                        cp(dbg["dbg_aff%d" % L], io["aff_loc"], "dbga")
                return f
            misc_phase(kb, ph_b(0))
        if upto >= 3:
            moe_phase(kb, io, 0)

            def ph_c():
                moe_allreduce(kb, io, "cc4")
                if debug:
                    cp(dbg["dbg_sel"], io["sel"], "dbgs")
            misc_phase(kb, ph_c)
        if upto >= 4:
            phase4(kb, io)

            def ph_d():
                zero_ydense(kb, io)
                if debug:
                    cp(dbg["dbg_x2"], io["x2_loc"], "dbgx2")
            misc_phase(kb, ph_d)
            misc_phase(kb, ph_b(1))
        if upto >= 5:
            moe_phase(kb, io, 1)
            misc_phase(kb, lambda: moe_allreduce(kb, io, "cc5"))
            final_phase(kb, io)
        print("semaphores:", len(kb.P.dsems) + 5)
    return nc


def core_inputs(inp, c, consts):
    b, r = c // 4, c % 4
    hg = r
    g = hg // 2
    d = {}
    x = inp["x"]
    d["xT"] = np.ascontiguousarray(x[b].T)
    d["x_tok"] = np.ascontiguousarray(x[b, r * 4096:(r + 1) * 4096])
    w = inp["w_in_even"][0]
    hs = slice(hg * 256, (hg + 1) * 256)
    cols = [w[:, 0:1024][:, hs], w[:, 1024:2048][:, hs], w[:, 2048:3072][:, hs], w[:, 4096:5120][:, hs],
            w[:, 5120 + g * 128:5120 + (g + 1) * 128], w[:, 5376 + g * 128:5376 + (g + 1) * 128], w[:, 3072:4096][:, hs],
            w[:, 5632 + hg * 4:5632 + hg * 4 + 4], w[:, 5648 + hg * 4:5648 + hg * 4 + 4]]
    d["w1"] = np.ascontiguousarray(np.concatenate(cols, 1))
    d["g_attn"] = np.ascontiguousarray(inp["attn_norm"][0].reshape(8, 128).T)
    d["gqk"] = np.ascontiguousarray(np.stack([np.tile(inp["q_norm"][0], 2), np.tile(inp["k_norm"][0], 2)], 1))
    cw_full = inp["ssd_conv_w"][0]; cb = inp["ssd_conv_b"][0]
    chans = np.concatenate([np.arange(hg * 256, hg * 256 + 256), 1024 + g * 128 + np.arange(128), 1280 + g * 128 + np.arange(128)])
    cw = np.concatenate([cw_full[:, chans], cb[None, chans]], 0)
    d["convw"] = np.ascontiguousarray(cw.reshape(6, 4, 128).transpose(2, 1, 0)).astype(np.float32)
    h4 = slice(hg * 4, hg * 4 + 4)
    d["dtc"] = np.stack([np.concatenate([inp["ssd_dt_bias_fwd"][0][h4], inp["ssd_dt_bias_bwd"][0][h4]]),
                         np.concatenate([inp["ssd_a_log_fwd"][0][h4], inp["ssd_a_log_bwd"][0][h4]])], 1).astype(np.float32)
    d["Dbc"] = np.ascontiguousarray(np.tile(np.repeat(inp["ssd_d"][0][h4], 64)[None, :], (128, 1))).astype(np.float32)
    wo = inp["w_out_even"][0]
    perm = np.concatenate([np.concatenate([np.arange(q * 256, (q + 1) * 256), 1024 + np.arange(q * 256, (q + 1) * 256)]) for q in range(4)])
    d["wo"] = np.ascontiguousarray(wo[perm])
    gfull = np.concatenate([np.ones(1024, np.float32), inp["ssd_out_norm"][0]])[perm]
    d["g_wo"] = np.ascontiguousarray(gfull.reshape(16, 128).T).astype(np.float32)
    mi = np.zeros((128, 8, 16), np.int32)
    for wdx in range(8):
        for ck in range(16):
            L = (8 * r + wdx) * 256 + (ck % 2) * 128 + np.arange(128)
            mi[:, wdx, ck] = (L // 1024) * 4096 + (ck // 4) * 1024 + (L % 1024)
    d["mixidx"] = mi
    for L in range(2):
        d["wr%d" % L] = np.ascontiguousarray(inp["router_w"][L])
        d["g_ffn_col%d" % L] = np.ascontiguousarray(inp["ffn_norm"][L].reshape(8, 128).T)
        d["g_ffn_bc%d" % L] = np.ascontiguousarray(np.tile(inp["ffn_norm"][L][None, :], (128, 1)))
        es = slice(4 * r, 4 * r + 4)
        d["wg%d" % L] = np.ascontiguousarray(inp["expert_w_gate"][L, es])
        d["wu%d" % L] = np.ascontiguousarray(inp["expert_w_up"][L, es])
        d["wd%d" % L] = np.ascontiguousarray(inp["expert_w_down"][L, es])
    ai = np.zeros((128, 1), np.int32)
    for e in range(4):
        for q in range(4):
            ai[e * 4 + q, 0] = q * 16 + 4 * r + e
    d["affidx"] = ai
    mr = np.zeros((128, 33), np.int32)
    for i in range(32):
        mr[:, i] = r * 4096 + i * 128 + np.arange(128)
    mr[0, 32] = max(r * 4096 - 1, 0)
    mr[1, 32] = min(r * 4096 + 4096, S - 1)
    d["myrows"] = mr
    hi = np.zeros((128, 1), np.int32); hm = np.zeros((128, 1), np.float32)
    if r > 0:
        hi[0, 0] = 2 * (r - 1) + 1; hm[0, 0] = 1.0
    if r < 3:
        hi[1, 0] = 2 * (r + 1); hm[1, 0] = 1.0
    d["haloidx"] = hi; d["halomsk"] = hm
    d["g_conv_bc"] = np.ascontiguousarray(np.tile(inp["conv_norm"][0][None, :], (128, 1)))
    d["w2"] = np.ascontiguousarray(inp["conv_w_in"][0])
    d["cw3"] = np.ascontiguousarray(inp["conv_w"][0].reshape(3, 8, 128).transpose(2, 1, 0))
    d["w3"] = np.ascontiguousarray(inp["conv_w_out"][0])
    for n in CONST_NAMES:
        d[n] = consts[n]
    return d


def kernel(**inputs):
    from concourse.bass_utils import run_bass_kernel_spmd
    inp = {k: np.asarray(v) for k, v in inputs.items()}
    consts = host_constants()
    nc = build_program(consts)
    in_maps = [core_inputs(inp, c, consts) for c in range(NCORE)]
    res = run_bass_kernel_spmd(nc, in_maps, core_ids=list(range(NCORE)))
    out = np.zeros((2, S, DM), np.float32)
    for c in range(NCORE):
        b, r = c // 4, c % 4
        out[b, r * 4096:(r + 1) * 4096] = res.results[c]["out"]
    return out
```

```python
import contextlib
import numpy as np
import concourse.bass as bass
import concourse.mybir as mybir

F32 = mybir.dt.float32
BF16 = mybir.dt.bfloat16
I32 = mybir.dt.int32
U32 = mybir.dt.uint32
AF = mybir.ActivationFunctionType
ALU = mybir.AluOpType
AX = mybir.AxisListType

ENGS = ("tensor", "vector", "scalar", "gpsimd", "sync")


class Buf:
    __slots__ = ("name", "w", "r", "dsem", "dcount")

    def __init__(self, name):
        self.name = name
        self.w = {}
        self.r = {}
        self.dsem = None
        self.dcount = 0


class Prog:
    def __init__(self, nc, stack):
        self.nc = nc
        self.stack = stack
        self.esem = {e: stack.enter_context(nc.semaphore("es_" + e)) for e in ENGS}
        self.base = {e: 0 for e in ENGS}
        self.dsems = {}
        self.dvals = {}
        self.dfree = []
        self.phase = 0
        self.reset_phase()
        self.same_engine_sync = True
        self.no_self = set()

    def reset_phase(self):
        self.ops = {e: [] for e in ENGS}
        self.seen = {e: {} for e in ENGS}
        self.needed = {e: set() for e in ENGS}

    def _collect(self, eng, reads, writes, partial):
        waits = {}
        def add(d):
            for k, v in d.items():
                if waits.get(k, -1) < v:
                    waits[k] = v
        for b in reads:
            add(b.w)
        for b in writes:
            add(b.w); add(b.r)
        for b in partial:
            add(b.r)
        out = []
        seen = self.seen[eng]
        for k, v in waits.items():
            if k[2] != self.phase:
                continue
            if k[0] == "E" and k[1] == eng and (not self.same_engine_sync or eng in self.no_self):
                continue
            if seen.get(k, -1) >= v:
                continue
            seen[k] = v
            if k[0] == "E":
                self.needed[k[1]].add(v)
            out.append((k, v))
        return out

    def _commit(self, tok, reads, writes, partial):
        k, v = tok
        for b in reads:
            if b.r.get(k, -1) < v:
                b.r[k] = v
        for b in writes:
            b.w = {k: v}
            b.r = {}
        for b in partial:
            if b.w.get(k, -1) < v:
                b.w[k] = v

    def _serial_waits(self, eng, waits):
        import os
        if not os.environ.get("FW_SERIAL"):
            return waits
        if os.environ["FW_SERIAL"] != "1" and eng not in os.environ["FW_SERIAL"].split(","):
            return waits
        have = {k for k, _ in waits}
        for e2 in ENGS:
            if e2 == eng:
                continue
            ee = [o["idx"] for o in self.ops[e2] if o["kind"] == "E"]
            if not ee:
                continue
            k = ("E", e2, self.phase)
            if self.seen[eng].get(k, -1) < ee[-1]:
                self.seen[eng][k] = ee[-1]
                self.needed[e2].add(ee[-1])
                waits.append((k, ee[-1]))
        return waits

    def op(self, eng, emit, reads=(), writes=(), partial=()):
        waits = self._collect(eng, reads, writes, partial)
        waits = self._serial_waits(eng, waits)
        idx = len(self.ops[eng]) + 1
        self.ops[eng].append(dict(waits=waits, emit=emit, kind="E", idx=idx))
        self._commit((("E", eng, self.phase), idx), reads, writes, partial)

    def dma(self, eng, emit, key, reads=(), writes=(), partial=(), inc=16):
        waits = self._collect(eng, reads, writes, partial)
        if key not in self.dsems:
            if self.dfree:
                self.dsems[key], self.dvals[key] = self.dfree.pop(0)
            else:
                self.dsems[key] = self.stack.enter_context(self.nc.semaphore("ds%d_%s" % (self.phase, key)))
                self.dvals[key] = 0
        self.dvals[key] += inc
        v = self.dvals[key]
        idx = len(self.ops[eng]) + 1
        self.ops[eng].append(dict(waits=waits, emit=emit, kind="D", key=key, inc=inc, idx=idx))
        self._commit((("D", key, self.phase), v), reads, writes, partial)

    def barrier(self):
        last = {}
        for e in ENGS:
            ee = [o["idx"] for o in self.ops[e] if o["kind"] == "E"]
            last[e] = ee[-1] if ee else 0
        for e in ENGS:
            waits = []
            for e2 in ENGS:
                if e2 != e and last[e2] > 0:
                    k = ("E", e2, self.phase)
                    if self.seen[e].get(k, -1) < last[e2]:
                        self.seen[e][k] = last[e2]
                        self.needed[e2].add(last[e2])
                        waits.append((k, last[e2]))
            for key, v in self.dvals.items():
                k = ("D", key, self.phase)
                if self.seen[e].get(k, -1) < v:
                    self.seen[e][k] = v
                    waits.append((k, v))
            self.ops[e].append(dict(waits=waits, emit=None, kind="N", idx=len(self.ops[e]) + 1))

    def emit_phase(self):
        nc = self.nc
        val = {}
        for e in ENGS:
            c = self.base[e]
            m = {}
            for o in self.ops[e]:
                if o["kind"] == "E" and o["idx"] in self.needed[e]:
                    c += 1
                    m[o["idx"]] = c
            val[e] = m
        stats = {}

        def replay(e, eng):
            n = 0
            for o in self.ops[e]:
                for (k, v) in o["waits"]:
                    if k[0] == "E":
                        eng.wait_ge(self.esem[k[1]], val[k[1]][v])
                    else:
                        eng.wait_ge(self.dsems[k[1]], v)
                    n += 1
                if o["emit"] is None:
                    continue
                ins = o["emit"](eng)
                n += 1
                if o["kind"] == "E":
                    if o["idx"] in self.needed[e]:
                        ins.then_inc(self.esem[e], 1)
                else:
                    ins.then_inc(self.dsems[o["key"]], o["inc"])
            stats[e] = n

        with nc.Block() as block:
            @block.tensor
            def _(eng):
                replay("tensor", eng)

            @block.vector
            def _(eng):
                replay("vector", eng)

            @block.scalar
            def _(eng):
                replay("scalar", eng)

            @block.gpsimd
            def _(eng):
                replay("gpsimd", eng)

            @block.sync
            def _(eng):
                replay("sync", eng)
        for e in ENGS:
            if val[e]:
                self.base[e] = max(val[e].values())
        for key in list(self.dsems):
            self.dfree.append((self.dsems[key], self.dvals[key]))
        self.dsems = {}
        self.dvals = {}
        self.phase += 1
        self.reset_phase()
        return stats


S = 16384
DM = 1024
NCORE = 8
PAD = 1024
WIN = 512
NW = S // WIN
NCH = S // 128
NE = 16
CAP = 2048
FF = 2048
EPS = 1e-6
NEG = -30000.0
C_Q, C_K, C_V, C_XS, C_B, C_C, C_Z, C_DT, NC1 = 0, 256, 512, 768, 1024, 1152, 1280, 1536, 1544
GROUPS = [[0, 1, 2, 3], [4, 5, 6, 7]]


def sl(start, n, step):
    return slice(start, start + (n - 1) * step + 1, step)


class T:
    def __init__(self, t, name):
        self.t = t
        self.b = Buf(name)
        self.name = name

    def __getitem__(self, k):
        return self.t[k]


class KB:
    def __init__(self, nc, st):
        self.nc = nc
        self.st = st
        self.ph = None
        self.P = Prog(nc, st)
        self.P.no_self = {"tensor"}
        self.uid = 0

    def sb(self, name, shape, dt):
        self.uid += 1
        nm = "s%d_%s" % (self.uid, name)
        return T(self.ph.enter_context(self.nc.sbuf_tensor(nm, shape, dt)), name)

    def ps(self, name, shape, dt=F32):
        self.uid += 1
        nm = "p%d_%s" % (self.uid, name)
        return T(self.ph.enter_context(self.nc.psum_tensor(nm, shape, dt)), name)

    def pool(self, kind, name, n, shape, dt):
        f = self.sb if kind == "sb" else self.ps
        return Pool([f("%s%d" % (name, i), shape, dt) for i in range(n)])

    def dram(self, name, shape, dt, kind=None):
        if kind is None:
            h = self.nc.dram_tensor(name, shape, dt)
        else:
            h = self.nc.dram_tensor(name, shape, dt, kind=kind)
        return T(h.ap(), name)

    def mm(self, out, oap, lt, ltap, rt, rtap, start=True, stop=True):
        self.P.op("tensor", lambda e: e.matmul(oap, lhsT=ltap, rhs=rtap, start=start, stop=stop),
                  reads=[lt.b, rt.b], writes=[out.b])

    def tr(self, out, oap, inp, iap, ident, idap):
        self.P.op("tensor", lambda e: e.transpose(oap, iap, idap), reads=[inp.b, ident.b], writes=[out.b])

    def act(self, out, oap, inp, iap, func, bias=None, scale=None, accum=None, part=False):
        reads = [inp.b]
        kw = {}
        if bias is not None:
            reads.append(bias[0].b)
            kw["bias"] = bias[1]
        if scale is not None:
            if isinstance(scale, tuple):
                reads.append(scale[0].b)
                kw["scale"] = scale[1]
            else:
                kw["scale"] = scale
        writes = [out.b]
        if accum is not None:
            writes.append(accum[0].b)
            kw["accum_out"] = accum[1]
        self.P.op("scalar", lambda e: e.activation(out=oap, in_=iap, func=func, **kw), reads=reads,
                  writes=[] if part else writes, partial=writes if part else [])

    def tt(self, eng, out, oap, a, aap, b, bap, op, part=False):
        self.P.op(eng, lambda e: e.tensor_tensor(out=oap, in0=aap, in1=bap, op=op), reads=[a.b, b.b],
                  writes=[] if part else [out.b], partial=[out.b] if part else [])

    def ts(self, eng, out, oap, a, aap, s1, s2, op0, op1=None, accum=None, part=False):
        reads = [a.b]
        def cv(s):
            if isinstance(s, tuple):
                reads.append(s[0].b)
                return s[1]
            return s
        v1, v2 = cv(s1), cv(s2)
        kw = {}
        writes = [out.b]
        if accum is not None:
            writes.append(accum[0].b)
            kw["accum_out"] = accum[1]
        if op1 is not None:
            kw["op1"] = op1
        self.P.op(eng, lambda e: e.tensor_scalar(out=oap, in0=aap, scalar1=v1, scalar2=v2, op0=op0, **kw), reads=reads,
                  writes=[] if part else writes, partial=writes if part else [])

    def stt(self, out, oap, a, aap, scalar, b, bap, op0, op1, part=False):
        reads = [a.b, b.b]
        sv = scalar
        if isinstance(scalar, tuple):
            reads.append(scalar[0].b)
            sv = scalar[1]
        self.P.op("vector", lambda e: e.scalar_tensor_tensor(out=oap, in0=aap, scalar=sv, in1=bap, op0=op0, op1=op1),
                  reads=reads, writes=[] if part else [out.b], partial=[out.b] if part else [])

    def copy(self, eng, out, oap, a, aap, part=False):
        if eng == "scalar":
            f = lambda e: e.activation(out=oap, in_=aap, func=AF.Copy)
        else:
            f = lambda e: e.tensor_copy(out=oap, in_=aap)
        self.P.op(eng, f, reads=[a.b], writes=[] if part else [out.b], partial=[out.b] if part else [])

    def memset(self, eng, out, oap, val, part=False):
        self.P.op(eng, lambda e: e.memset(oap, val), writes=[] if part else [out.b], partial=[out.b] if part else [])

    def recip(self, out, oap, a, aap):
        self.P.op("vector", lambda e: e.reciprocal(out=oap, in_=aap), reads=[a.b], writes=[out.b])

    def dma(self, q, out, oap, inp, iap, key, part=False, extra_reads=(), **kw):
        self.P.dma(q, lambda e: e.dma_start(out=oap, in_=iap, **kw), key, reads=[inp.b] + [x.b for x in extra_reads],
                   writes=[] if part else [out.b], partial=[out.b] if part else [])

    def rstd_from(self, out, oap, inp, iap, n, tmp, tap, eps):
        self.act(tmp, tap, inp, iap, AF.Ln, bias=(eps, eps[:, 0:1]), scale=1.0 / n)
        self.act(out, oap, tmp, tap, AF.Exp, scale=-0.5)


class Pool:
    def __init__(self, tiles):
        self.tiles = tiles
        self.i = 0

    def next(self):
        t = self.tiles[self.i % len(self.tiles)]
        self.i += 1
        return t


def host_constants():
    import ml_dtypes
    bf = ml_dtypes.bfloat16
    c = {}
    c["ident_bf"] = np.eye(128, dtype=np.float32).astype(bf)
    c["ident_f"] = np.eye(128, dtype=np.float32)
    p = np.arange(128)[:, None]
    f = np.arange(128)[None, :]
    mA = np.where(f <= p, 0.0, NEG).astype(np.float32)
    mB = np.where(f >= p, 0.0, NEG).astype(np.float32)
    mA_first = mA.copy(); mA_first[:64, :] = NEG
    mB_last = mB.copy(); mB_last[64:, :] = NEG
    nm = np.zeros((128, 3, 4, 128), np.float32)
    for v, (a, b) in enumerate([(mA, mB), (mA_first, mB), (mA, mB_last)]):
        nm[:, v, 0] = a; nm[:, v, 1] = b; nm[:, v, 2] = a; nm[:, v, 3] = b
    c["negmask"] = nm.reshape(128, 3, 512).astype(bf)
    bo = np.zeros((128, 128), np.float32); bo[:64, :64] = 1; bo[64:, 64:] = 1
    c["blockones"] = bo.astype(bf)
    rm = np.zeros((128, 128), np.float32)
    for o in (0, 64):
        for i in range(8):
            rm[o + 8 + i, o + i] = -1.0
            rm[o + i, o + 8 + i] = 1.0
    c["rotm"] = rm.astype(bf)
    so = np.zeros((128, 2, 128), np.float32); so[:, 0, :64] = 1; so[:, 1, 64:] = 1
    c["sel_ones"] = so.astype(bf)
    c["ones_bf"] = np.ones((128, 128), np.float32).astype(bf)
    c["ones_f"] = np.ones((128, 128), np.float32)
    t = np.arange(128)[:, None]; l = np.arange(128)[None, :]
    c["tri"] = np.stack([(t <= l), (t >= l)], 1).astype(np.float32)
    mf = np.where(t <= l, 0.0, NEG).astype(np.float32); mb = np.where(t >= l, 0.0, NEG).astype(np.float32)
    c["smask"] = np.stack([np.tile(mf, (1, 4)), np.tile(mb, (1, 4))], 1).astype(np.float32)
    c["ustrict"] = (t < l).astype(np.float32)
    half = 8
    inv_freq = (np.float32(500000.0) ** (-np.arange(half, dtype=np.float32) * np.float32(2.0) / np.float32(16))).astype(np.float32)
    ang = (np.arange(S, dtype=np.float32)[None, :] * inv_freq[:, None]).astype(np.float32)
    cos = np.cos(ang.astype(np.float64)).astype(np.float32); sin = np.sin(ang.astype(np.float64)).astype(np.float32)
    cf = np.ones((128, S), np.float32); sf = np.zeros((128, S), np.float32)
    for o in (0, 64):
        cf[o:o + 8] = cos; cf[o + 8:o + 16] = cos
        sf[o:o + 8] = sin; sf[o + 8:o + 16] = sin
    c["cosf"] = cf; c["sinf"] = sf
    c["iota_tok"] = (np.arange(128)[:, None] * 128 + np.arange(128)[None, :]).astype(np.float32)
    tt_ = np.arange(128)[:, None] * 128 + np.arange(128)[None, :]
    c["iota_h2row"] = (((tt_ % 4096) // 512) * 2048 + (tt_ // 4096) * 512 + (tt_ % 512)).astype(np.float32)
    return c


CONST_DT = {"ident_bf": BF16, "negmask": BF16, "blockones": BF16, "rotm": BF16, "sel_ones": BF16, "ones_bf": BF16}
SMALL_CONSTS_UNUSED = ["ident_bf", "ident_f", "negmask", "blockones", "rotm", "sel_ones", "ones_bf", "ones_f", "tri", "smask",
                "ustrict", "iota_tok"]


def load_consts(kb, io, names):
    cs = {}
    for n in names:
        src = io[n]
        shape = list(src.t.shape)
        t = kb.sb("c_" + n, shape, CONST_DT.get(n, F32))
        kb.dma("sync", t, t[:], src, src.t, "c_" + n)
        cs[n] = t
    eps = kb.sb("c_eps", [128, 1], F32)
    kb.memset("vector", eps, eps[:], EPS)
    cs["eps"] = eps
    return cs


def load_w1(kb, io, w1b, c0, ncols, stage_pool, g_attn):
    w1 = io["w1"]
    for kc in range(8):
        stg = stage_pool.next()
        kb.dma("sync", stg, stg[:, 0:ncols], w1, w1.t[kc * 128:(kc + 1) * 128, c0:c0 + ncols], stg.name)
        kb.ts("vector", w1b, w1b[:, kc, 0:ncols], stg, stg[:, 0:ncols], (g_attn, g_attn[:, kc:kc + 1]), None, ALU.mult, part=True)


def window_front(kb, io, cs, w, xb_pool, sq_pool, rstd_pool, psA):
    xT = io["xT"]
    xb = xb_pool.next()
    kb.dma("gpsimd", xb, xb[:], xT, xT.t.rearrange("(k p) t -> p k t", p=128)[:, :, w * WIN:(w + 1) * WIN], xb.name)
    sq = sq_pool.next()
    kb.act(sq, sq[:], xb, xb[:], AF.Square)
    pss = psA.next()
    for kc in range(8):
        kb.mm(pss, pss[:], cs["ones_bf"], cs["ones_bf"][:], sq, sq[:, kc, :], start=(kc == 0), stop=(kc == 7))
    rstd = rstd_pool.next()
    kb.rstd_from(rstd, rstd[:, 0, :], pss, pss[:], DM, rstd, rstd[:, 1, :], cs["eps"])
    return xb, sq, rstd


def attn_inproj(kb, io, hp, qz, kT, vT):
    if True:
      with contextlib.ExitStack() as ph:
        kb.ph = ph
        cs = load_consts(kb, io, ["blockones", "rotm", "ones_bf"])
        g_attn = kb.sb("g_attn", [128, 8], F32)
        kb.dma("sync", g_attn, g_attn[:], io["g_attn"], io["g_attn"].t, "g_attn")
        gqk = kb.sb("gqk", [128, 2], F32)
        kb.dma("sync", gqk, gqk[:], io["gqk"], io["gqk"].t, "gqk")
        w1b = kb.sb("w1b", [128, 8, 384], BF16)
        stage = kb.pool("sb", "w1stg", 2, [128, 128], F32)
        for j, c0 in enumerate((C_Q + hp * 128, C_K + hp * 128, C_V + hp * 128)):
            w1 = io["w1"]
            for kc in range(8):
                stg = stage.next()
                kb.dma("sync", stg, stg[:], w1, w1.t[kc * 128:(kc + 1) * 128, c0:c0 + 128], stg.name)
                kb.ts("vector", w1b, w1b[:, kc, j * 128:(j + 1) * 128], stg, stg[:], (g_attn, g_attn[:, kc:kc + 1]), None,
                      ALU.mult, part=True)
        for t in (kT, vT):
            kb.memset("gpsimd", t, t[:, 0:PAD], 0.0, part=True)
            kb.memset("gpsimd", t, t[:, PAD + S:], 0.0, part=True)
        kb.memset("gpsimd", qz, qz[64:128, 0, :], 0.0, part=True)
        kb.memset("gpsimd", qz, qz[0:64, 1, :], 0.0, part=True)
        xb_pool = kb.pool("sb", "xb", 2, [128, 8, WIN], BF16)
        sq_pool = kb.pool("sb", "sq", 1, [128, 8, WIN], BF16)
        rstd_pool = kb.pool("sb", "rstd", 2, [128, 2, WIN], F32)
        cos_pool = kb.pool("sb", "cosw", 2, [128, 2, WIN], F32)
        raw_pool = kb.pool("sb", "raw", 2, [128, WIN], F32)
        sqh_pool = kb.pool("sb", "sqh", 2, [128, WIN], BF16)
        rsh_pool = kb.pool("sb", "rsh", 2, [128, 2, WIN], F32)
        qn_pool = kb.pool("sb", "qn", 2, [128, WIN], BF16)
        t12_pool = kb.pool("sb", "t12", 2, [128, 2, WIN], F32)
        psA = kb.pool("ps", "psA", 2, [128, WIN], F32)
        psB = kb.pool("ps", "psB", 2, [128, WIN], F32)
        psC = kb.pool("ps", "psC", 2, [128, WIN], F32)
        for w in range(NW):
            xb, sq, rstd = window_front(kb, io, cs, w, xb_pool, sq_pool, rstd_pool, psA)
            cw = cos_pool.next()
            kb.dma("sync", cw, cw[:, 0, :], io["cosf"], io["cosf"].t[:, w * WIN:(w + 1) * WIN], cw.name, part=True)
            kb.dma("sync", cw, cw[:, 1, :], io["sinf"], io["sinf"].t[:, w * WIN:(w + 1) * WIN], cw.name, part=True)
            for j, dst in enumerate((qz, kT, vT)):
                ps = psB.next()
                for kc in range(8):
                    kb.mm(ps, ps[:], w1b, w1b[:, kc, j * 128:(j + 1) * 128], xb, xb[:, kc, :], start=(kc == 0), stop=(kc == 7))
                off = 0 if j == 0 else PAD
                dap = None if j == 0 else dst[:, off + w * WIN: off + (w + 1) * WIN]
                if j == 2:
                    kb.tt("vector", dst, dap, ps, ps[:], rstd, rstd[:, 0, :], ALU.mult, part=True)
                    continue
                raw = raw_pool.next()
                kb.tt("vector", raw, raw[:], ps, ps[:], rstd, rstd[:, 0, :], ALU.mult)
                sqh = sqh_pool.next()
                kb.act(sqh, sqh[:], raw, raw[:], AF.Square)
                ps2 = psC.next()
                kb.mm(ps2, ps2[:], cs["blockones"], cs["blockones"][:], sqh, sqh[:])
                rsh = rsh_pool.next()
                kb.rstd_from(rsh, rsh[:, 0, :], ps2, ps2[:], 64, rsh, rsh[:, 1, :], cs["eps"])
                qn = qn_pool.next()
                kb.stt(qn, qn[:], raw, raw[:], (gqk, gqk[:, j:j + 1]), rsh, rsh[:, 0, :], ALU.mult, ALU.mult)
                ps3 = psC.next()
                kb.mm(ps3, ps3[:], cs["rotm"], cs["rotm"][:], qn, qn[:])
                t12 = t12_pool.next()
                kb.tt("vector", t12, t12[:, 0, :], qn, qn[:], cw, cw[:, 0, :], ALU.mult, part=True)
                kb.tt("vector", t12, t12[:, 1, :], ps3, ps3[:], cw, cw[:, 1, :], ALU.mult, part=True)
                if j == 0:
                    for h in range(2):
                        hs = slice(h * 64, (h + 1) * 64)
                        kb.tt("vector", qz, qz[hs, h, w * WIN:(w + 1) * WIN], t12, t12[hs, 0, :], t12, t12[hs, 1, :], ALU.add, part=True)
                else:
                    kb.tt("vector", dst, dap, t12, t12[:, 0, :], t12, t12[:, 1, :], ALU.add, part=True)
        if io.get("dbg_qk") is not None:
            kb.dma("sync", io["dbg_qk"], io["dbg_qk"].t[0, 0:64], qz, qz[0:64, 0, :], "dbgq", part=True)
            kb.dma("sync", io["dbg_qk"], io["dbg_qk"].t[0, 64:128], qz, qz[64:128, 1, :], "dbgq", part=True)
            kb.dma("sync", io["dbg_qk"], io["dbg_qk"].t[1], kT, kT[:, PAD:PAD + S], "dbgk", part=True)
            kb.dma("sync", io["dbg_qk"], io["dbg_qk"].t[2], vT, vT[:, PAD:PAD + S], "dbgv", part=True)
        kb.P.barrier()
        stats0 = kb.P.emit_phase()
        kb.ph = None
    return stats0


def attn_core(kb, io, hp, qz, kT, vT):
    import os
    if True:
      with contextlib.ExitStack() as ph:
        kb.ph = ph
        cs = load_consts(kb, io, ["ident_bf", "negmask", "sel_ones"])
        vpad_pool = kb.pool("sb", "vpad", int(os.environ.get("P1_NB", 3)), [128, 2, 2, 128], BF16)
        for t in vpad_pool.tiles:
            kb.memset("vector", t, t[:], 0.0)
        pt_pool = kb.pool("sb", "pt", int(os.environ.get("P1_NB", 3)), [128, 512], BF16)
        accden = kb.sb("accden", [128, 2, 2048], F32)
        rden = kb.sb("rden", [128, 2048], F32)
        attn_o = kb.pool("sb", "attn_o", 2, [128, 2048], BF16)
        psT = kb.pool("ps", "psT", int(os.environ.get("P1_NT", 2)), [128, 4, 128], F32)
        psS = kb.pool("ps", "psS", int(os.environ.get("P1_NS", 2)), [128, 512], F32)
        psO = kb.pool("ps", "psO", int(os.environ.get("P1_NO", 2)), [128, 4, 128], F32)
        mix = io["mixA_loc"]

        def stage1(u):
            d, rho, j, jj = u
            n = S // d
            nb = n // 128
            var = 1 if j == 0 else (2 if j == nb - 1 else 0)
            pT = psT.next()
            kcols = []
            for c in range(2):
                k0 = PAD + rho + d * (128 * j - 64 + 128 * c)
                kcols.append(k0)
                kb.mm(pT, pT[:, c, :], vT, vT[:, sl(k0, 128, d)], cs["ident_bf"], cs["ident_bf"][:])
            vp = vpad_pool.next()
            if MODE >= 1:
                kb.copy("vector", vp, vp[:, :, 0, 0:64], pT, pT[:, 0:2, 0:64], part=True)
            if MODE >= 2:
                kb.copy(os.environ.get("P1_CE", "vector"), vp, vp[:, :, 1, 64:128], pT, pT[:, 0:2, 64:128], part=True)
            sS = psS.next()
            kb.mm(sS, sS[:], cs["ident_bf"], cs["ident_bf"][:], cs["negmask"], cs["negmask"][:, var, :], start=True, stop=False)
            q0 = rho + d * 128 * j
            for h in range(2):
                for c in range(2):
                    sub = 2 * h + c
                    kb.mm(sS, sS[:, sub * 128:(sub + 1) * 128], kT, kT[:, sl(kcols[c], 128, d)],
                          qz, qz[:, h, sl(q0, 128, d)], start=False, stop=True)
            pt = pt_pool.next()
            if MODE >= 3:
                kb.act(pt, pt[:], sS, sS[:], AF.Exp, scale=0.125)
            return (u, vp, pt)

        def stage2(st, first_in_sb):
            u, vp, pt = st
            d, rho, j, jj = u
            if MODE < 4:
                return
            pO = psO.next()
            SUB = os.environ.get("P1_SUB", "")
            k = 0
            for h in range(2 if SUB != "den" else 0):
                for c in range(2):
                    sub = 2 * h + c
                    kb.mm(pO, pO[:, 0, :], vp, vp[:, c, h, :], pt, pt[:, sub * 128:(sub + 1) * 128], start=(k == 0), stop=(k == 3))
                    k += 1
            k = 0
            for h in range(2 if SUB != "pv" else 0):
                for c in range(2):
                    sub = 2 * h + c
                    kb.mm(pO, pO[:, 1, :], cs["sel_ones"], cs["sel_ones"][:, h, :], pt, pt[:, sub * 128:(sub + 1) * 128],
                          start=(k == 0), stop=(k == 3))
                    k += 1
            c0 = rho + d * 128 * jj
            dap = accden[:, :, sl(c0, 128, d)]
            if MODE < 5:
                return
            if first_in_sb:
                kb.copy("vector", accden, dap, pO, pO[:, 0:2, :])
            else:
                kb.tt("vector", accden, dap, accden, dap, pO, pO[:, 0:2, :], ALU.add)

        import os
        MODE = int(os.environ.get("P1_MODE", "9"))
        for sbk in range(int(os.environ.get("P1_NSB", S // 2048))):
            units = []
            for d in (1, 4, 16):
                per = 16 // d
                for rho in range(d):
                    for jj in range(per):
                        units.append((d, rho, sbk * per + jj, jj))
            pend = None
            units = units[:int(os.environ.get("P1_NU", len(units)))]
            for ui, u in enumerate(units):
                st1 = stage1(u)
                if os.environ.get("P1_NOPIPE"):
                    stage2(st1, u[0] == 1)
                    continue
                if pend is not None:
                    stage2(pend[0], pend[1])
                pend = (st1, u[0] == 1)
            if pend is not None:
                stage2(pend[0], pend[1])
            kb.recip(rden, rden[:], accden, accden[:, 1, :])
            ao = attn_o.next()
            kb.tt("gpsimd", ao, ao[:], accden, accden[:, 0, :], rden, rden[:], ALU.mult)
            kb.dma("sync", mix, mix.t.rearrange("(g c) t -> c g t", c=256)[hp * 128:(hp + 1) * 128, sbk * 4:(sbk + 1) * 4, :],
                   ao, ao[:].rearrange("p (g t) -> p g t", t=512), ao.name, part=True)
        kb.P.barrier()
        stats = kb.P.emit_phase()
        kb.ph = None
    return stats


def phase1_attn(kb, io, hp):
    import os
    with contextlib.ExitStack() as outer:
        kb.ph = outer
        qz = kb.sb("qz", [128, 2, S], BF16)
        kT = kb.sb("kT", [128, S + 2 * PAD], BF16)
        vT = kb.sb("vT", [128, S + 2 * PAD], BF16)
        st = attn_inproj(kb, io, hp, qz, kT, vT)
        if os.environ.get("P1_STOP"):
            return st
        st = attn_core(kb, io, hp, qz, kT, vT)
    return st


def phase1_ssd_in(kb, io):
    with contextlib.ExitStack() as ph:
        kb.ph = ph
        if "mixA_all" in io:
            collective(kb, "AllGather", ALU.bypass, io["mixA_loc"], io["mixA_all"], "ccA", 8)
        cs = load_consts(kb, io, ["ones_bf"])
        g_attn = kb.sb("g_attn", [128, 8], F32)
        kb.dma("sync", g_attn, g_attn[:], io["g_attn"], io["g_attn"].t, "g_attn")
        ncol = NC1 - C_XS
        w1b = kb.sb("w1b", [128, 8, ncol], BF16)
        stage = kb.pool("sb", "w1stg", 2, [128, ncol], F32)
        w1 = io["w1"]
        for kc in range(8):
            stg = stage.next()
            kb.dma("sync", stg, stg[:], w1, w1.t[kc * 128:(kc + 1) * 128, C_XS:NC1], stg.name)
            kb.ts("vector", w1b, w1b[:, kc, :], stg, stg[:], (g_attn, g_attn[:, kc:kc + 1]), None, ALU.mult, part=True)
        cw = kb.sb("convw", [128, 4, 6], F32)
        kb.dma("sync", cw, cw[:], io["convw"], io["convw"].t, "convw")
        dtc = kb.sb("dtc", [8, 2], F32)
        kb.dma("sync", dtc, dtc[:], io["dtc"], io["dtc"].t, "dtc")
        negA = kb.sb("negA", [8, 1], F32)
        kb.act(negA, negA[:], dtc, dtc[:, 1:2], AF.Exp)
        kb.ts("vector", negA, negA[:], negA, negA[:], -1.0, None, ALU.mult)
        one8 = kb.sb("one8", [8, 1], F32)
        kb.memset("vector", one8, one8[:], 1.0)
        xb_pool = kb.pool("sb", "xb", 2, [128, 8, WIN], BF16)
        sq_pool = kb.pool("sb", "sq", 1, [128, 8, WIN], BF16)
        rstd_pool = kb.pool("sb", "rstd", 2, [128, 2, WIN], F32)
        pre_pool = kb.pool("sb", "pre", 2, [128, 4, WIN], F32)
        cacc_pool = kb.pool("sb", "cacc", 2, [128, 4, WIN], F32)
        post_pool = kb.pool("sb", "post", 2, [128, 4, WIN], BF16)
        zs_pool = kb.pool("sb", "zsw", 2, [128, 2, WIN], BF16)
        dt_pool = kb.pool("sb", "dtw", 2, [8, 4, WIN], F32)
        psA = kb.pool("ps", "psA", 2, [128, WIN], F32)
        psB = kb.pool("ps", "psB", 3, [128, WIN], F32)
        xT = io["xT"]
        nwin = 33
        for w in range(nwin):
            t0 = 508 * w - 2 if w < nwin - 1 else S - 510
            lo, hi = max(t0, 0), min(t0 + WIN, S)
            xb = xb_pool.next()
            if lo > t0 or hi < t0 + WIN:
                kb.memset("vector", xb, xb[:], 0.0)
                kb.dma("gpsimd", xb, xb[:, :, lo - t0:hi - t0], xT, xT.t.rearrange("(k p) t -> p k t", p=128)[:, :, lo:hi], xb.name)
            else:
                kb.dma("gpsimd", xb, xb[:], xT, xT.t.rearrange("(k p) t -> p k t", p=128)[:, :, lo:hi], xb.name)
            sq = sq_pool.next()
            kb.act(sq, sq[:], xb, xb[:], AF.Square)
            pss = psA.next()
            for kc in range(8):
                kb.mm(pss, pss[:], cs["ones_bf"], cs["ones_bf"][:], sq, sq[:, kc, :], start=(kc == 0), stop=(kc == 7))
            rstd = rstd_pool.next()
            kb.rstd_from(rstd, rstd[:, 0, :], pss, pss[:], DM, rstd, rstd[:, 1, :], cs["eps"])
            pre = pre_pool.next()
            for ci in range(4):
                ps = psB.next()
                for kc in range(8):
                    kb.mm(ps, ps[:], w1b, w1b[:, kc, ci * 128:(ci + 1) * 128], xb, xb[:, kc, :], start=(kc == 0), stop=(kc == 7))
                kb.tt("vector", pre, pre[:, ci, :], ps, ps[:], rstd, rstd[:, 0, :], ALU.mult, part=True)
            cacc = cacc_pool.next()
            post = post_pool.next()
            NV = WIN - 4
            for ci in range(4):
                kb.act(cacc, cacc[:, ci, 0:NV], pre, pre[:, ci, 0:NV], AF.Copy, scale=(cw, cw[:, ci, 0:1]), part=True)
                for j in range(1, 5):
                    kb.stt(cacc, cacc[:, ci, 0:NV], pre, pre[:, ci, j:j + NV], (cw, cw[:, ci, j:j + 1]), cacc, cacc[:, ci, 0:NV],
                           ALU.mult, ALU.add, part=True)
            for ci in range(4):
                kb.act(post, post[:, ci, 0:NV], cacc, cacc[:, ci, 0:NV], AF.Silu, bias=(cw, cw[:, ci, 5:6]), part=True)
            vlo, vhi = max(t0 + 2, 0), min(t0 + 2 + NV, S)
            o0 = vlo - (t0 + 2)
            kb.dma("sync", io["post"], io["post"].t.rearrange("(c p) t -> p c t", p=128)[:, :, vlo:vhi], post, post[:, :, o0:o0 + (vhi - vlo)],
                   post.name, part=True)
            zs = zs_pool.next()
            for zi in range(2):
                ps = psB.next()
                c0 = (C_Z - C_XS) + zi * 128
                for kc in range(8):
                    kb.mm(ps, ps[:], w1b, w1b[:, kc, c0:c0 + 128], xb, xb[:, kc, :], start=(kc == 0), stop=(kc == 7))
                kb.tt("vector", pre, pre[:, zi, :], ps, ps[:], rstd, rstd[:, 0, :], ALU.mult, part=True)
                kb.act(zs, zs[:, zi, :], pre, pre[:, zi, :], AF.Silu, part=True)
            kb.dma("sync", io["zs"], io["zs"].t.rearrange("(c p) t -> p c t", p=128)[:, :, vlo:vhi], zs, zs[:, :, o0 + 2:o0 + 2 + (vhi - vlo)],
                   zs.name, part=True)
            ps = psB.next()
            c0 = C_DT - C_XS
            for kc in range(8):
                kb.mm(ps, ps[0:8, :], w1b, w1b[:, kc, c0:c0 + 8], xb, xb[:, kc, :], start=(kc == 0), stop=(kc == 7))
            dtw = dt_pool.next()
            kb.tt("vector", dtw, dtw[:, 0, :], ps, ps[0:8, :], rstd, rstd[0:8, 0, :], ALU.mult, part=True)
            kb.act(dtw, dtw[:, 1, :], dtw, dtw[:, 0, :], AF.Exp, bias=(dtc, dtc[:, 0:1]), part=True)
            kb.act(dtw, dtw[:, 2, :], dtw, dtw[:, 1, :], AF.Ln, bias=(one8, one8[:, 0:1]), part=True)
            kb.ts("vector", dtw, dtw[:, 3, :], dtw, dtw[:, 2, :], (negA, negA[:, 0:1]), None, ALU.mult, part=True)
            kb.dma("sync", io["dta"], io["dta"].t[0:8, vlo:vhi], dtw, dtw[:, 2, o0 + 2:o0 + 2 + (vhi - vlo)], dtw.name, part=True)
            kb.dma("sync", io["dta"], io["dta"].t[8:16, vlo:vhi], dtw, dtw[:, 3, o0 + 2:o0 + 2 + (vhi - vlo)], dtw.name, part=True)
        kb.P.barrier()
        st = kb.P.emit_phase()
        kb.ph = None
    return st


def phase1_ssd_scan(kb, io, dirn, nchunks=NCH):
    with contextlib.ExitStack() as ph:
        kb.ph = ph
        cs = load_consts(kb, io, ["ident_bf", "ident_f", "ones_f", "tri", "smask"])
        Dbc = kb.sb("Dbc", [128, 256], F32)
        kb.dma("sync", Dbc, Dbc[:], io["Dbc"], io["Dbc"].t, "Dbc")
        H = kb.sb("H", [128, 256], F32)
        Hbf = kb.sb("Hbf", [128, 256], BF16)
        kb.memset("vector", H, H[:], 0.0)
        kb.memset("vector", Hbf, Hbf[:], 0.0)
        inT = kb.pool("sb", "inT", 2, [128, 4, 128], BF16)
        dtaT = kb.pool("sb", "dtaT", 2, [16, 128], F32)
        xsb = kb.pool("sb", "xsb", 2, [128, 384], BF16)
        dta = kb.pool("sb", "dta", 2, [128, 16], F32)
        abc = kb.pool("sb", "abc", 2, [128, 4, 128], F32)
        col = kb.pool("sb", "col", 2, [128, 40], F32)
        LT = kb.pool("sb", "LT", 2, [128, 4, 128], F32)
        MT = kb.pool("sb", "MT", 2, [128, 4, 128], BF16)
        Ysb = kb.pool("sb", "Ysb", 2, [128, 256], F32)
        yout = kb.pool("sb", "yout", 2, [128, 256], F32)
        xsw = kb.pool("sb", "xsw", 2, [128, 256], BF16)
        yprev = kb.pool("sb", "yprev", 2, [128, 256], F32)
        ysum = kb.pool("sb", "ysum", 2, [128, 2, 256], F32)
        ybf = kb.pool("sb", "ybf", 2, [128, 256], BF16)
        zsT = kb.pool("sb", "zsT", 2, [128, 2, 128], BF16)
        ygT = kb.pool("sb", "ygT", 2, [128, 2, 128], BF16)
        pX = kb.ps("pX", [128, 512], F32)
        pD = kb.ps("pD", [128, 512], F32)
        pCS = kb.ps("pCS", [128, 512], F32)
        pG = kb.ps("pG", [128, 512], F32)
        pY = kb.ps("pY", [128, 512], F32)
        pYo = kb.ps("pYo", [128, 512], F32)
        pST = kb.ps("pST", [128, 512], F32)
        pYT = kb.ps("pYT", [128, 512], F32)
        post_v = io["post"].t.rearrange("(c p) t -> p c t", p=128)
        zs_v = io["zs"].t.rearrange("(c p) t -> p c t", p=128)
        order = range(nchunks) if dirn == 0 else range(NCH - 1, NCH - 1 - nchunks, -1)
        def stageA(c):
            tk = slice(c * 128, (c + 1) * 128)
            it = inT.next()
            kb.dma("sync", it, it[:], io["post"], post_v[:, :, tk], it.name)
            dT = dtaT.next()
            kb.dma("sync", dT, dT[:], io["dta"], io["dta"].t[:, tk], dT.name)
            for i in range(3):
                kb.mm(pX, pX[:, i * 128:(i + 1) * 128], it, it[:, i, :], cs["ident_bf"], cs["ident_bf"][:])
            xs = xsb.next()
            kb.copy("vector", xs, xs[:], pX, pX[:, 0:384])
            kb.mm(pD, pD[:, 0:16], dT, dT[:, :], cs["ident_f"], cs["ident_f"][0:16, 0:16])
            dt = dta.next()
            kb.copy("vector", dt, dt[:], pD, pD[:, 0:16])
            dtc = dt[:, 4 * dirn:4 * dirn + 4]
            ac = dt[:, 8 + 4 * dirn:8 + 4 * dirn + 4]
            ab = abc.next()
            for h in range(4):
                kb.act(ab, ab[:, h, :], cs["ones_f"], cs["ones_f"][:], AF.Copy, scale=(dt, dt[:, 8 + 4 * dirn + h:8 + 4 * dirn + h + 1]), part=True)
            kb.mm(pCS, pCS[:], cs["ident_f"], cs["ident_f"][:], cs["smask"], cs["smask"][:, dirn, :], start=True, stop=False)
            for h in range(4):
                kb.mm(pCS, pCS[:, h * 128:(h + 1) * 128], ab, ab[:, h, :], cs["tri"], cs["tri"][:, dirn, :], start=False, stop=True)
            kb.mm(pD, pD[:, 16:20], cs["tri"], cs["tri"][:, dirn, :], dt, ac)
            kb.mm(pD, pD[:, 20:24], cs["ones_f"], cs["ones_f"][:], dt, ac)
            cl = col.next()
            kb.copy("vector", cl, cl[:, 0:8], pD, pD[:, 16:24], part=True)
            kb.ts("vector", cl, cl[:, 8:12], cl, cl[:, 0:4], -1.0, None, ALU.mult, part=True)
            kb.act(cl, cl[:, 12:16], cl, cl[:, 0:4], AF.Exp, part=True)
            kb.tt("vector", cl, cl[:, 16:20], cl, cl[:, 4:8], cl, cl[:, 0:4], ALU.subtract, part=True)
            kb.act(cl, cl[:, 20:24], cl, cl[:, 16:20], AF.Exp, part=True)
            kb.tt("vector", cl, cl[:, 24:28], cl, cl[:, 20:24], dt, dtc, ALU.mult, part=True)
            kb.act(cl, cl[:, 28:32], cl, cl[:, 4:8], AF.Exp, part=True)
            lt = LT.next()
            for h in range(4):
                kb.act(lt, lt[:, h, :], pCS, pCS[:, h * 128:(h + 1) * 128], AF.Exp, bias=(cl, cl[:, 8 + h:9 + h]), part=True)
            kb.mm(pG, pG[:, 0:128], it, it[:, 2, :], it, it[:, 3, :])
            mt = MT.next()
            for h in range(4):
                kb.stt(mt, mt[:, h, :], pG, pG[:, 0:128], (dt, dt[:, 4 * dirn + h:4 * dirn + h + 1]), lt, lt[:, h, :], ALU.mult, ALU.mult, part=True)
            return dict(c=c, tk=tk, it=it, xs=xs, dt=dt, cl=cl, mt=mt)

        def stageB(ctx):
            c, tk, it, xs, dt, cl, mt = ctx['c'], ctx['tk'], ctx['it'], ctx['xs'], ctx['dt'], ctx['cl'], ctx['mt']
            for h in range(4):
                kb.mm(pY, pY[:, h * 64:(h + 1) * 64], mt, mt[:, h, :], xs, xs[:, h * 64:(h + 1) * 64])
            kb.mm(pYo, pYo[:, 0:256], it, it[:, 3, :], Hbf, Hbf[:])
            ysb = Ysb.next()
            kb.copy("vector", ysb, ysb[:], pY, pY[:, 0:256])
            yo = yout.next()
            for h in range(4):
                hs = slice(h * 64, (h + 1) * 64)
                kb.stt(yo, yo[:, hs], pYo, pYo[:, hs], (cl, cl[:, 12 + h:13 + h]), ysb, ysb[:, hs], ALU.mult, ALU.add, part=True)
            xw = xsw.next()
            for h in range(4):
                hs = slice(h * 64, (h + 1) * 64)
                kb.act(xw, xw[:, hs], xs, xs[:, hs], AF.Copy, scale=(cl, cl[:, 24 + h:25 + h]), part=True)
            kb.mm(pST, pST[:, 0:256], xs, xs[:, 256:384], xw, xw[:])
            for h in range(4):
                hs = slice(h * 64, (h + 1) * 64)
                kb.stt(H, H[:, hs], H, H[:, hs], (cl, cl[:, 28 + h:29 + h]), pST, pST[:, hs], ALU.mult, ALU.add)
            kb.copy("scalar", Hbf, Hbf[:], H, H[:])
            if dirn == 0:
                kb.dma("sync", io["yf"], io["yf"].t[tk, :], yo, yo[:], yo.name, part=True)
            else:
                yp = yprev.next()
                kb.dma("sync", yp, yp[:], io["yf"], io["yf"].t[tk, :], yp.name)
                zt = zsT.next()
                kb.dma("sync", zt, zt[:], io["zs"], zs_v[:, :, tk], zt.name)
                ys = ysum.next()
                kb.tt("vector", ys, ys[:, 0, :], xs, xs[:, 0:256], Dbc, Dbc[:], ALU.mult, part=True)
                kb.tt("vector", ys, ys[:, 1, :], yo, yo[:], yp, yp[:], ALU.add, part=True)
                yb = ybf.next()
                kb.tt("vector", yb, yb[:], ys, ys[:, 0, :], ys, ys[:, 1, :], ALU.add)
                for i in range(2):
                    kb.mm(pYT, pYT[:, i * 128:(i + 1) * 128], yb, yb[:, i * 128:(i + 1) * 128], cs["ident_bf"], cs["ident_bf"][:])
                yg = ygT.next()
                kb.tt("vector", yg, yg[:], pYT, pYT[:, 0:256].rearrange("p (c t) -> p c t", c=2), zt, zt[:], ALU.mult)
                mv = io["mixS_loc"].t.rearrange("(g c) t -> c g t", c=256)
                for i in range(2):
                    kb.dma("sync", io["mixS_loc"], mv[i * 128:(i + 1) * 128, c // 4, (c % 4) * 128:(c % 4 + 1) * 128], yg, yg[:, i, :],
                           yg.name, part=True)
        pend = None
        for c in order:
            ctx = stageA(c)
            if pend is not None:
                stageB(pend)
            pend = ctx
        stageB(pend)
        kb.P.barrier()
        st = kb.P.emit_phase()
        kb.ph = None
    return st


def router_setup(kb, io, L):
    r = {}
    r["gbc"] = kb.sb("gffn_bc", [128, DM], F32)
    kb.dma("sync", r["gbc"], r["gbc"][:], io["g_ffn_bc%d" % L], io["g_ffn_bc%d" % L].t, "gffn_bc")
    gcol = kb.sb("gffn_col", [128, 8], F32)
    kb.dma("sync", gcol, gcol[:], io["g_ffn_col%d" % L], io["g_ffn_col%d" % L].t, "gffn_col")
    wr = kb.sb("wr", [128, 8, NE], F32)
    kb.dma("sync", wr, wr[:], io["wr%d" % L], io["wr%d" % L].t.rearrange("(k p) e -> p k e", p=128), "wr")
    r["wrg"] = kb.sb("wrg", [128, 8, NE], F32)
    for kc in range(8):
        kb.ts("vector", r["wrg"], r["wrg"][:, kc, :], wr, wr[:, kc, :], (gcol, gcol[:, kc:kc + 1]), None, ALU.mult, part=True)
    r["affT"] = kb.sb("affT", [NE, 4096], F32)
    r["sqj"] = kb.pool("sb", "sqj", 2, [128, DM], BF16)
    r["st"] = kb.pool("sb", "rst", 2, [128, 8], F32)
    r["h2"] = kb.pool("sb", "h2t", 2, [128, DM], BF16)
    r["xT"] = kb.pool("sb", "x1T", 2, [128, 8, 128], F32)
    r["lg"] = kb.pool("sb", "lg", 2, [128, 3, NE], F32)
    r["pXT"] = kb.pool("ps", "pXT", 2, [128, 4, 128], F32)
    r["pL"] = kb.ps("pL", [128, 512], F32)
    return r


def router_tile(kb, io, cs, r, x1, i):
    st = r["st"].next()
    sqj = r["sqj"].next()
    kb.act(sqj, sqj[:], x1, x1[:], AF.Square, accum=(st, st[:, 0:1]))
    kb.rstd_from(st, st[:, 2:3], st, st[:, 0:1], DM, st, st[:, 1:2], cs["eps"])
    h2 = r["h2"].next()
    kb.stt(h2, h2[:], x1, x1[:], (st, st[:, 2:3]), r["gbc"], r["gbc"][:], ALU.mult, ALU.mult)
    kb.dma("sync", io["h2_loc"], io["h2_loc"].t[i * 128:(i + 1) * 128, :], h2, h2[:], h2.name, part=True)
    xT = r["xT"].next()
    for half in range(2):
        pX = r["pXT"].next()
        for k4 in range(4):
            kc = half * 4 + k4
            kb.mm(pX, pX[:, k4, :], x1, x1[:, kc * 128:(kc + 1) * 128], cs["ident_f"], cs["ident_f"][:])
        kb.copy("vector", xT, xT[:, half * 4:(half + 1) * 4, :], pX, pX[:], part=True)
    pL = r["pL"]
    for kc in range(8):
        kb.mm(pL, pL[:, 0:NE], xT, xT[:, kc, :], r["wrg"], r["wrg"][:, kc, :], start=(kc == 0), stop=(kc == 7))
    lg = r["lg"].next()
    kb.ts("vector", lg, lg[:, 0, :], pL, pL[:, 0:NE], (st, st[:, 2:3]), None, ALU.mult, part=True)
    kb.P.op("vector", lambda e: e.tensor_reduce(out=st[:, 3:4], in_=lg[:, 0, :], axis=AX.X, op=ALU.max), reads=[lg.b], partial=[st.b])
    kb.ts("vector", st, st[:, 4:5], st, st[:, 3:4], -1.0, None, ALU.mult, part=True)
    kb.act(lg, lg[:, 1, :], lg, lg[:, 0, :], AF.Exp, bias=(st, st[:, 4:5]), accum=(st, st[:, 5:6]), part=True)
    kb.P.op("vector", lambda e: e.reciprocal(out=st[:, 6:7], in_=st[:, 5:6]), reads=[st.b], partial=[st.b])
    kb.ts("vector", lg, lg[:, 2, :], lg, lg[:, 1, :], (st, st[:, 6:7]), None, ALU.mult, part=True)
    kb.mm(pL, pL[0:NE, 128:256], lg, lg[:, 2, :], cs["ident_f"], cs["ident_f"][:])
    kb.copy("vector", r["affT"], r["affT"][:, i * 128:(i + 1) * 128], pL, pL[0:NE, 128:256], part=True)


def phase2(kb, io, ntiles=32):
    with contextlib.ExitStack() as ph:
        kb.ph = ph
        cs = load_consts(kb, io, ["ident_f", "ones_bf"])
        r = router_setup(kb, io, 0)
        gwo = kb.sb("gwo", [128, 16], F32)
        kb.dma("sync", gwo, gwo[:], io["g_wo"], io["g_wo"].t, "gwo")
        wob = kb.sb("wob", [128, 16, DM], BF16)
        stage = kb.pool("sb", "wostg", 2, [128, DM], F32)
        for ck in range(16):
            stg = stage.next()
            kb.dma("sync", stg, stg[:], io["wo"], io["wo"].t[ck * 128:(ck + 1) * 128, :], stg.name)
            kb.ts("vector", wob, wob[:, ck, :], stg, stg[:], (gwo, gwo[:, ck:ck + 1]), None, ALU.mult, part=True)
        midx = kb.sb("mixidx", [128, 8, 16], I32)
        kb.dma("sync", midx, midx[:], io["mixidx"], io["mixidx"].t, "mixidx")
        mixT = kb.pool("sb", "mixT", 2, [128, 16, WIN], BF16)
        ysq = kb.pool("sb", "ysq", 2, [128, 8, 128], BF16)
        xt = kb.pool("sb", "xt", 2, [128, DM], F32)
        x1p = kb.pool("sb", "x1", 2, [128, DM], F32)
        rsy = kb.pool("sb", "rsy", 2, [128, 4], F32)
        pA = kb.ps("pA", [128, 2, 512], F32)
        pS = kb.ps("pS", [128, 2, 512], F32)
        pq = kb.ps("pq", [128, 512], F32)
        ssd_ck = [q * 4 + j for q in range(4) for j in (2, 3)]
        att_ck = [q * 4 + j for q in range(4) for j in (0, 1)]
        for w in range((ntiles + 3) // 4):
            mt = mixT.next()
            for ck in range(16):
                src = io["mixA_all"] if ck % 4 < 2 else io["mixS_all"]
                kb.P.dma("gpsimd", (lambda e, mt=mt, ck=ck, w=w, src=src: e.indirect_dma_start(
                    out=mt[:, ck, :], out_offset=None, in_=src.t,
                    in_offset=bass.IndirectOffsetOnAxis(ap=midx[:, w, ck:ck + 1], axis=0))),
                    mt.name, reads=[src.b, midx.b], partial=[mt.b])
            for j in range(min(4, ntiles - 4 * w)):
                i = 4 * w + j
                tk = slice(j * 128, (j + 1) * 128)
                ys = ysq.next()
                for n, ck in enumerate(ssd_ck):
                    kb.act(ys, ys[:, n, :], mt, mt[:, ck, tk], AF.Square, part=True)
                for n in range(8):
                    kb.mm(pq, pq[:, 0:1], ys, ys[:, n, :], cs["ones_bf"], cs["ones_bf"][:, 0:1], start=(n == 0), stop=(n == 7))
                rs = rsy.next()
                kb.rstd_from(rs, rs[:, 1:2], pq, pq[:, 0:1], 1024, rs, rs[:, 0:1], cs["eps"])
                for half in range(2):
                    for n, ck in enumerate(att_ck):
                        kb.mm(pA, pA[:, half, :], mt, mt[:, ck, tk], wob, wob[:, ck, half * 512:(half + 1) * 512], start=(n == 0), stop=(n == 7))
                    for n, ck in enumerate(ssd_ck):
                        kb.mm(pS, pS[:, half, :], mt, mt[:, ck, tk], wob, wob[:, ck, half * 512:(half + 1) * 512], start=(n == 0), stop=(n == 7))
                x = xt.next()
                kb.dma("sync", x, x[:], io["x_tok"], io["x_tok"].t[i * 128:(i + 1) * 128, :], x.name)
                x1 = x1p.next()
                kb.stt(x1, x1[:], pS, pS[:].rearrange("p a b -> p (a b)"), (rs, rs[:, 1:2]), x, x[:], ALU.mult, ALU.add)
                kb.tt("vector", x1, x1[:], x1, x1[:], pA, pA[:].rearrange("p a b -> p (a b)"), ALU.add)
                kb.dma("sync", io["x1_loc"], io["x1_loc"].t[i * 128:(i + 1) * 128, :], x1, x1[:], x1.name, part=True)
                router_tile(kb, io, cs, r, x1, i)
        kb.dma("sync", io["aff_loc"], io["aff_loc"].t, r["affT"], r["affT"][:], "affT")
        kb.P.barrier()
        st = kb.P.emit_phase()
        kb.ph = None
    return st


def zero_ydense(kb, io):
    zt = kb.sb("zt", [128, 4, DM], F32)
    kb.memset("gpsimd", zt, zt[:], 0.0)
    yv = io["ydense"].t.rearrange("(a p) d -> p a d", p=128)
    for a in range(32):
        kb.dma("sync", io["ydense"], yv[:, a * 4:(a + 1) * 4, :], zt, zt[:], "zt", part=True)


def moe_topk(kb, io, cs, L):
    aidx = kb.sb("affidx", [128, 1], I32)
    kb.dma("sync", aidx, aidx[:], io["affidx"], io["affidx"].t, "affidx")
    arow = kb.sb("arow", [128, 4096], F32)
    kb.P.dma("gpsimd", lambda e: e.indirect_dma_start(out=arow[0:16, :], out_offset=None, in_=io["aff_all"].t,
             in_offset=bass.IndirectOffsetOnAxis(ap=aidx[0:16, 0:1], axis=0)), "arow", reads=[io["aff_all"].b, aidx.b], writes=[arow.b])
    kb.dma("sync", io["aff_my"], io["aff_my"].t, arow, arow[0:16, :], "arow")
    collective(kb, "AllGather", ALU.bypass, io["h2_loc"], io["h2_all"], "cc2", 8)
    A = kb.sb("A", [128, 4, 128], F32)
    kb.dma("sync", A, A[:], io["aff_my"], io["aff_my"].t.rearrange("(e q) (a j) -> (q a) e j", e=4, j=128), "A")
    lohi = kb.sb("lohi", [128, 16], F32)
    cmp_ = kb.sb("cmp", [128, 128], F32)
    cnt = kb.sb("cnt", [128, 8], F32)
    pc = kb.ps("pc", [128, 512], F32)
    kb.memset("vector", lohi, lohi[:, 0:4], 0.0, part=True)
    kb.memset("vector", lohi, lohi[:, 4:8], 1.0, part=True)
    for it in range(30):
        kb.tt("vector", lohi, lohi[:, 8:12], lohi, lohi[:, 0:4], lohi, lohi[:, 4:8], ALU.add)
        kb.ts("vector", lohi, lohi[:, 8:12], lohi, lohi[:, 8:12], 0.5, None, ALU.mult)
        for e in range(4):
            kb.ts("vector", cmp_, cmp_[:], A, A[:, e, :], (lohi, lohi[:, 8 + e:9 + e]), 0.0, ALU.is_ge, op1=ALU.add, accum=(cnt, cnt[:, e:e + 1]))
        kb.mm(pc, pc[:, 0:4], cs["ones_f"], cs["ones_f"][:], cnt, cnt[:, 0:4])
        kb.ts("vector", lohi, lohi[:, 12:16], pc, pc[:, 0:4], float(CAP), None, ALU.is_ge)
        kb.tt("vector", cnt, cnt[:, 4:8], lohi, lohi[:, 8:12], lohi, lohi[:, 0:4], ALU.subtract)
        kb.tt("vector", cnt, cnt[:, 4:8], cnt, cnt[:, 4:8], lohi, lohi[:, 12:16], ALU.mult)
        kb.tt("vector", lohi, lohi[:, 0:4], lohi, lohi[:, 0:4], cnt, cnt[:, 4:8], ALU.add)
        kb.tt("vector", cnt, cnt[:, 4:8], lohi, lohi[:, 4:8], lohi, lohi[:, 8:12], ALU.subtract)
        kb.tt("vector", cnt, cnt[:, 4:8], cnt, cnt[:, 4:8], lohi, lohi[:, 12:16], ALU.mult)
        kb.tt("vector", lohi, lohi[:, 4:8], lohi, lohi[:, 8:12], cnt, cnt[:, 4:8], ALU.add)
    mask = kb.sb("mask", [128, 4, 128], F32)
    incl = kb.sb("incl", [128, 4, 128], F32)
    slot = kb.sb("slot", [128, 4, 128], F32)
    sloti = kb.sb("sloti", [128, 4, 128], I32)
    pairs = kb.sb("pairs", [128, 4, 128, 3], F32)
    tot = kb.sb("tot", [128, 8], F32)
    for e in range(4):
        kb.ts("vector", mask, mask[:, e, :], A, A[:, e, :], (lohi, lohi[:, e:e + 1]), None, ALU.is_ge, part=True)
        kb.P.op("vector", lambda en, e=e: en.tensor_tensor_scan(out=incl[:, e, :], data0=cs["ones_f"][:], data1=mask[:, e, :], initial=0.0,
                                                               op0=ALU.mult, op1=ALU.add), reads=[cs["ones_f"].b, mask.b], partial=[incl.b])
        kb.copy("vector", tot, tot[:, e:e + 1], incl, incl[:, e, 127:128], part=True)
    kb.mm(pc, pc[:, 8:12], cs["ustrict"], cs["ustrict"][:], tot, tot[:, 0:4])
    kb.copy("vector", tot, tot[:, 4:8], pc, pc[:, 8:12], part=True)
    for e in range(4):
        kb.tt("vector", slot, slot[:, e, :], incl, incl[:, e, :], mask, mask[:, e, :], ALU.subtract, part=True)
        kb.ts("vector", slot, slot[:, e, :], slot, slot[:, e, :], (tot, tot[:, 4 + e:5 + e]), float(e * CAP), ALU.add, op1=ALU.add, part=True)
        kb.ts("vector", incl, incl[:, e, :], mask, mask[:, e, :], -1.0e6, 1.0e6, ALU.mult, op1=ALU.add, part=True)
        kb.tt("vector", slot, slot[:, e, :], slot, slot[:, e, :], incl, incl[:, e, :], ALU.add, part=True)
        kb.ts("vector", incl, incl[:, e, :], slot, slot[:, e, :], float((e + 1) * CAP), 1.0e6, ALU.is_ge, op1=ALU.mult, part=True)
        kb.tt("vector", slot, slot[:, e, :], slot, slot[:, e, :], incl, incl[:, e, :], ALU.add, part=True)
        kb.copy("vector", sloti, sloti[:, e, :], slot, slot[:, e, :], part=True)
        kb.copy("gpsimd", pairs, pairs[:, e, :, 0], cs["iota_tok"], cs["iota_tok"][:], part=True)
        kb.copy("gpsimd", pairs, pairs[:, e, :, 1], A, A[:, e, :], part=True)
        kb.copy("gpsimd", pairs, pairs[:, e, :, 2], cs["iota_h2row"], cs["iota_h2row"][:], part=True)
    rc = {}

    def breg(en):
        if "r" not in rc:
            rc["r"] = en.to_reg(4 * CAP - 1)
        return rc["r"]

    for e in range(4):
        for j in range(128):
            kb.P.dma("gpsimd", (lambda en, e=e, j=j: en.indirect_dma_start(
                out=io["sel"].t, out_offset=bass.IndirectOffsetOnAxis(ap=sloti[:, e, j:j + 1], axis=0),
                in_=pairs[:, e, j, :], in_offset=None, bounds_check=breg(en), oob_is_err=False)),
                "selsc", reads=[pairs.b, sloti.b], partial=[io["sel"].b])


def moe_phase(kb, io, L):
    with contextlib.ExitStack() as ph:
        kb.ph = ph
        cs = load_consts(kb, io, ["ident_bf", "ones_f", "ustrict", "iota_tok", "iota_h2row"])
        with contextlib.ExitStack() as ph2:
            kb.ph = ph2
            moe_topk(kb, io, cs, L)
            kb.P.barrier()
            kb.P.emit_phase()
        kb.ph = ph
        cs = load_consts(kb, io, ["ident_bf"])
        xeT = kb.sb("xeT", [128, 8, CAP], BF16)
        hidT = kb.sb("hidT", [128, 16, CAP], BF16)
        wdb = kb.sb("wdb", [128, 16, DM], BF16)
        wgb = kb.pool("sb", "wgb", 2, [128, 8, 512], BF16)
        wub = kb.pool("sb", "wub", 2, [128, 8, 512], BF16)
        selT = kb.pool("sb", "selT", 2, [128, 16, 3], F32)
        toki = kb.pool("sb", "toki", 2, [128, 2, 16], I32)
        xe = kb.pool("sb", "xe", 3, [128, DM], BF16)
        sg = kb.pool("sb", "sg", 2, [128, 512], BF16)
        ye = kb.pool("sb", "ye", 2, [128, DM], F32)
        pT = kb.pool("ps", "pT", 2, [128, 4, 128], F32)
        pGt = kb.pool("ps", "pGt", 2, [128, 512], F32)
        pUp = kb.pool("ps", "pUp", 2, [128, 512], F32)
        pYe = kb.ps("pYe", [128, 2, 512], F32)
        wg, wu, wd = io["wg%d" % L], io["wu%d" % L], io["wd%d" % L]
        for e in range(4):
            sT = selT.next()
            kb.dma("sync", sT, sT[:], io["sel"], io["sel"].t[e * CAP:(e + 1) * CAP, :].rearrange("(k p) c -> p k c", p=128), sT.name)
            ti = toki.next()
            kb.copy("vector", ti, ti[:, 0, :], sT, sT[:, :, 0], part=True)
            kb.copy("vector", ti, ti[:, 1, :], sT, sT[:, :, 2], part=True)
            for q in range(4):
                kb.dma("gpsimd", wdb, wdb[:, q * 4:(q + 1) * 4, :], wd, wd.t[e, q * 512:(q + 1) * 512, :].rearrange("(k p) d -> p k d", p=128),
                       "wdb", part=True)
            for k in range(16):
                x = xe.next()
                kb.P.dma("gpsimd", (lambda en, x=x, ti=ti, k=k: en.indirect_dma_start(
                    out=x[:], out_offset=None, in_=io["h2_all"].t, in_offset=bass.IndirectOffsetOnAxis(ap=ti[:, 1, k:k + 1], axis=0))),
                    x.name, reads=[io["h2_all"].b, ti.b], writes=[x.b])
                for half in range(2):
                    p = pT.next()
                    for k4 in range(4):
                        kc = half * 4 + k4
                        kb.mm(p, p[:, k4, :], x, x[:, kc * 128:(kc + 1) * 128], cs["ident_bf"], cs["ident_bf"][:])
                    kb.copy("vector", xeT, xeT[:, half * 4:(half + 1) * 4, k * 128:(k + 1) * 128], p, p[:], part=True)
            for fg in range(4):
                wgt = wgb.next()
                wut = wub.next()
                kb.dma("gpsimd", wgt, wgt[:], wg, wg.t[e, :, fg * 512:(fg + 1) * 512].rearrange("(k p) f -> p k f", p=128), wgt.name)
                kb.dma("gpsimd", wut, wut[:], wu, wu.t[e, :, fg * 512:(fg + 1) * 512].rearrange("(k p) f -> p k f", p=128), wut.name)
                for f4 in range(4):
                    fi = fg * 4 + f4
                    for win in range(4):
                        pg = pGt.next()
                        pu = pUp.next()
                        for kc in range(8):
                            kb.mm(pg, pg[:], wgt, wgt[:, kc, f4 * 128:(f4 + 1) * 128], xeT, xeT[:, kc, win * 512:(win + 1) * 512],
                                  start=(kc == 0), stop=(kc == 7))
                        for kc in range(8):
                            kb.mm(pu, pu[:], wut, wut[:, kc, f4 * 128:(f4 + 1) * 128], xeT, xeT[:, kc, win * 512:(win + 1) * 512],
                                  start=(kc == 0), stop=(kc == 7))
                        s_ = sg.next()
                        kb.act(s_, s_[:], pg, pg[:], AF.Silu)
                        kb.tt("vector", hidT, hidT[:, fi, win * 512:(win + 1) * 512], s_, s_[:], pu, pu[:], ALU.mult, part=True)
            for k in range(16):
                for half in range(2):
                    for fi in range(16):
                        kb.mm(pYe, pYe[:, half, :], hidT, hidT[:, fi, k * 128:(k + 1) * 128], wdb, wdb[:, fi, half * 512:(half + 1) * 512],
                              start=(fi == 0), stop=(fi == 15))
                y = ye.next()
                kb.ts("vector", y, y[:], pYe, pYe[:].rearrange("p a b -> p (a b)"), (sT, sT[:, k, 1:2]), None, ALU.mult)
                kb.P.dma("gpsimd", (lambda en, y=y, ti=ti, k=k: en.indirect_dma_start(
                    out=io["ydense"].t, out_offset=bass.IndirectOffsetOnAxis(ap=ti[:, 0, k:k + 1], axis=0), in_=y[:], in_offset=None,
                    compute_op=ALU.add)), y.name, reads=[y.b, ti.b], writes=[io["ydense"].b])
        kb.P.barrier()
        st = kb.P.emit_phase()
        kb.ph = None
    return st


def phase4(kb, io):
    NT = 32
    TL = 4096
    with contextlib.ExitStack() as ph:
        kb.ph = ph
        cs = load_consts(kb, io, ["ident_bf", "ident_f"])
        hnT = kb.sb("hnT", [128, 8, TL + 2], BF16)
        mT = kb.sb("mT", [128, 8, TL], BF16)
        with contextlib.ExitStack() as ph2:
            kb.ph = ph2
            gbc = kb.sb("gconv_bc", [128, DM], F32)
            kb.dma("sync", gbc, gbc[:], io["g_conv_bc"], io["g_conv_bc"].t, "gconv_bc")
            rows = kb.sb("myrows", [128, 33], I32)
            kb.dma("sync", rows, rows[:], io["myrows"], io["myrows"].t, "myrows")
            hidx = kb.sb("haloidx", [128, 1], I32)
            kb.dma("sync", hidx, hidx[:], io["haloidx"], io["haloidx"].t, "haloidx")
            hmsk = kb.sb("halomsk", [128, 1], F32)
            kb.dma("sync", hmsk, hmsk[:], io["halomsk"], io["halomsk"].t, "halomsk")
            x1p = kb.pool("sb", "x1t", 2, [128, DM], F32)
            ysp = kb.pool("sb", "yst", 2, [128, DM], BF16)
            x2p = kb.pool("sb", "x2t", 2, [128, DM], F32)
            sqj = kb.pool("sb", "sqj", 2, [128, DM], BF16)
            stp = kb.pool("sb", "st4", 2, [128, 4], F32)
            hnp = kb.pool("sb", "hn", 2, [128, DM], BF16)
            pT = kb.pool("ps", "pT4", 2, [128, 4, 128], F32)
            for i in range(NT + 1):
                x1 = x1p.next()
                ys = ysp.next()
                if i < NT:
                    kb.dma("sync", x1, x1[:], io["x1_loc"], io["x1_loc"].t[i * 128:(i + 1) * 128, :], x1.name)
                else:
                    kb.P.dma("gpsimd", (lambda en, x1=x1: en.indirect_dma_start(out=x1[:], out_offset=None, in_=io["edges_all"].t,
                             in_offset=bass.IndirectOffsetOnAxis(ap=hidx[:, 0:1], axis=0))), x1.name, reads=[io["edges_all"].b, hidx.b], writes=[x1.b])
                kb.P.dma("gpsimd", (lambda en, ys=ys, i=i: en.indirect_dma_start(out=ys[:], out_offset=None, in_=io["ysum_all"].t,
                         in_offset=bass.IndirectOffsetOnAxis(ap=rows[:, i:i + 1], axis=0))), ys.name, reads=[io["ysum_all"].b, rows.b], writes=[ys.b])
                x2 = x2p.next()
                kb.tt("vector", x2, x2[:], x1, x1[:], ys, ys[:], ALU.add)
                if i == NT:
                    kb.ts("vector", x2, x2[:], x2, x2[:], (hmsk, hmsk[:, 0:1]), None, ALU.mult)
                else:
                    kb.dma("sync", io["x2_loc"], io["x2_loc"].t[i * 128:(i + 1) * 128, :], x2, x2[:], x2.name, part=True)
                st = stp.next()
                sq = sqj.next()
                kb.act(sq, sq[:], x2, x2[:], AF.Square, accum=(st, st[:, 0:1]))
                kb.rstd_from(st, st[:, 2:3], st, st[:, 0:1], DM, st, st[:, 1:2], cs["eps"])
                hn = hnp.next()
                kb.stt(hn, hn[:], x2, x2[:], (st, st[:, 2:3]), gbc, gbc[:], ALU.mult, ALU.mult)
                for half in range(2):
                    p = pT.next()
                    for k4 in range(4):
                        kc = half * 4 + k4
                        kb.mm(p, p[:, k4, :], hn, hn[:, kc * 128:(kc + 1) * 128], cs["ident_bf"], cs["ident_bf"][:])
                    if i < NT:
                        kb.copy("vector", hnT, hnT[:, half * 4:(half + 1) * 4, 1 + i * 128:1 + (i + 1) * 128], p, p[:], part=True)
                    else:
                        kb.copy("vector", hnT, hnT[:, half * 4:(half + 1) * 4, 0:1], p, p[:, :, 0:1], part=True)
                        kb.copy("vector", hnT, hnT[:, half * 4:(half + 1) * 4, TL + 1:TL + 2], p, p[:, :, 1:2], part=True)
            kb.P.barrier()
            kb.P.emit_phase()
        with contextlib.ExitStack() as ph2:
            kb.ph = ph2
            zero_ydense(kb, io)
            w2b = kb.pool("sb", "w2b", 2, [128, 8, 384], BF16)
            cw = kb.sb("cw3", [128, 8, 3], F32)
            kb.dma("sync", cw, cw[:], io["cw3"], io["cw3"].t, "cw3")
            csb = kb.pool("sb", "csb", 2, [128, 512], F32)
            vv = kb.pool("sb", "vv", 2, [128, 512], F32)
            ca = kb.pool("sb", "ca", 2, [128, 2, 512], F32)
            pB = kb.pool("ps", "pB4", 2, [128, 512], F32)
            pC = kb.pool("ps", "pC4", 2, [128, 512], F32)
            pU = kb.pool("ps", "pU4", 2, [128, 512], F32)
            w2 = io["w2"]
            nwin = 9
            for cc in range(8):
                wt = w2b.next()
                for part in range(3):
                    kb.dma("gpsimd", wt, wt[:, :, part * 128:(part + 1) * 128],
                           w2, w2.t[:, part * DM + cc * 128: part * DM + (cc + 1) * 128].rearrange("(k p) f -> p k f", p=128), wt.name, part=True)
                for w in range(nwin):
                    c0 = 510 * w if w < nwin - 1 else TL + 2 - 512
                    pb, pc, pu = pB.next(), pC.next(), pU.next()
                    for (pp, part) in ((pb, 0), (pc, 1), (pu, 2)):
                        for kc in range(8):
                            kb.mm(pp, pp[:], wt, wt[:, kc, part * 128:(part + 1) * 128], hnT, hnT[:, kc, c0:c0 + 512], start=(kc == 0), stop=(kc == 7))
                    cb = csb.next()
                    kb.copy("scalar", cb, cb[:], pc, pc[:])
                    v = vv.next()
                    kb.tt("vector", v, v[:], cb, cb[:], pu, pu[:], ALU.mult)
                    a = ca.next()
                    kb.act(a, a[:, 0, 0:510], v, v[:, 0:510], AF.Copy, scale=(cw, cw[:, cc, 0:1]), part=True)
                    kb.stt(a, a[:, 0, 0:510], v, v[:, 1:511], (cw, cw[:, cc, 1:2]), a, a[:, 0, 0:510], ALU.mult, ALU.add, part=True)
                    kb.stt(a, a[:, 1, 0:510], v, v[:, 2:512], (cw, cw[:, cc, 2:3]), a, a[:, 0, 0:510], ALU.mult, ALU.add, part=True)
                    kb.tt("vector", mT, mT[:, cc, c0:c0 + 510], a, a[:, 1, 0:510], pb, pb[:, 1:511], ALU.mult, part=True)
            kb.P.barrier()
            kb.P.emit_phase()
        with contextlib.ExitStack() as ph2:
            kb.ph = ph2
            cs = load_consts(kb, io, ["ident_f"])
            r = router_setup(kb, io, 1)
            w3b = kb.sb("w3b", [128, 8, DM], BF16)
            kb.dma("gpsimd", w3b, w3b[:], io["w3"], io["w3"].t.rearrange("(k p) d -> p k d", p=128), "w3b")
            x2p = kb.pool("sb", "x2r", 2, [128, DM], F32)
            x3p = kb.pool("sb", "x3", 2, [128, DM], F32)
            pO = kb.ps("pO4", [128, 2, 512], F32)
            for i in range(NT):
                for half in range(2):
                    for cc in range(8):
                        kb.mm(pO, pO[:, half, :], mT, mT[:, cc, i * 128:(i + 1) * 128], w3b, w3b[:, cc, half * 512:(half + 1) * 512],
                              start=(cc == 0), stop=(cc == 7))
                x2 = x2p.next()
                kb.dma("sync", x2, x2[:], io["x2_loc"], io["x2_loc"].t[i * 128:(i + 1) * 128, :], x2.name)
                x3 = x3p.next()
                kb.tt("vector", x3, x3[:], x2, x2[:], pO, pO[:].rearrange("p a b -> p (a b)"), ALU.add)
                kb.dma("sync", io["x1_loc"], io["x1_loc"].t[i * 128:(i + 1) * 128, :], x3, x3[:], x3.name, part=True)
                router_tile(kb, io, cs, r, x3, i)
            kb.dma("sync", io["aff_loc"], io["aff_loc"].t, r["affT"], r["affT"][:], "affT")
            kb.P.barrier()
            st = kb.P.emit_phase()
        kb.ph = None
    return st


def final_phase(kb, io):
    with contextlib.ExitStack() as ph:
        kb.ph = ph
        rows = kb.sb("myrows", [128, 33], I32)
        kb.dma("sync", rows, rows[:], io["myrows"], io["myrows"].t, "myrows")
        x1p = kb.pool("sb", "x1t", 2, [128, DM], F32)
        ysp = kb.pool("sb", "yst", 2, [128, DM], BF16)
        op = kb.pool("sb", "ot", 2, [128, DM], F32)
        for i in range(32):
            x1 = x1p.next()
            ys = ysp.next()
            kb.dma("sync", x1, x1[:], io["x1_loc"], io["x1_loc"].t[i * 128:(i + 1) * 128, :], x1.name)
            kb.P.dma("gpsimd", (lambda en, ys=ys, i=i: en.indirect_dma_start(out=ys[:], out_offset=None, in_=io["ysum_all"].t,
                     in_offset=bass.IndirectOffsetOnAxis(ap=rows[:, i:i + 1], axis=0))), ys.name, reads=[io["ysum_all"].b, rows.b], writes=[ys.b])
            o = op.next()
            kb.tt("vector", o, o[:], x1, x1[:], ys, ys[:], ALU.add)
            kb.dma("sync", io["out"], io["out"].t[i * 128:(i + 1) * 128, :], o, o[:], o.name, part=True)
        kb.P.barrier()
        st = kb.P.emit_phase()
        kb.ph = None
    return st


def collective(kb, kind, op, src, dst, key, nchunks=1):
    rin = src.t.shape[0] // nchunks
    rout = dst.t.shape[0] // nchunks
    for c in range(nchunks):
        sap = src.t[c * rin:(c + 1) * rin, :]
        dap = dst.t[c * rout:(c + 1) * rout, :]
        kb.P.dma("gpsimd", (lambda e, sap=sap, dap=dap: e.collective_compute(kind, op, replica_groups=GROUPS, ins=[sap.opt()], outs=[dap.opt()])),
                 key, reads=[src.b], writes=[] if nchunks > 1 else [dst.b], partial=[dst.b] if nchunks > 1 else [], inc=1)


IN_SPECS = {
    "xT": ([DM, S], F32), "x_tok": ([4096, DM], F32), "w1": ([DM, NC1], F32), "g_attn": ([128, 8], F32), "gqk": ([128, 2], F32),
    "convw": ([128, 4, 6], F32), "dtc": ([8, 2], F32), "Dbc": ([128, 256], F32), "wo": ([2048, DM], F32), "g_wo": ([128, 16], F32),
    "mixidx": ([128, 8, 16], I32), "wr0": ([DM, NE], F32), "wr1": ([DM, NE], F32), "g_ffn_col0": ([128, 8], F32),
    "g_ffn_col1": ([128, 8], F32), "g_ffn_bc0": ([128, DM], F32), "g_ffn_bc1": ([128, DM], F32), "affidx": ([128, 1], I32),
    "myrows": ([128, 33], I32), "haloidx": ([128, 1], I32), "halomsk": ([128, 1], F32), "g_conv_bc": ([128, DM], F32),
    "w2": ([DM, 3 * DM], F32), "cw3": ([128, 8, 3], F32), "w3": ([DM, DM], F32),
    "wg0": ([4, DM, FF], F32), "wu0": ([4, DM, FF], F32), "wd0": ([4, FF, DM], F32),
    "wg1": ([4, DM, FF], F32), "wu1": ([4, DM, FF], F32), "wd1": ([4, FF, DM], F32),
}
CONST_NAMES = ["ident_bf", "ident_f", "negmask", "blockones", "rotm", "sel_ones", "ones_bf", "ones_f", "tri", "smask", "ustrict",
               "iota_tok", "iota_h2row", "cosf", "sinf"]
SCRATCH = {
    "mixA_loc": ([8192, 512], BF16), "mixA_all": ([32768, 512], BF16), "mixS_loc": ([8192, 512], BF16), "mixS_all": ([32768, 512], BF16), "post": ([512, S], BF16), "zs": ([256, S], BF16),
    "dta": ([16, S], F32), "yf": ([S, 256], F32), "x1_loc": ([4096, DM], F32), "x2_loc": ([4096, DM], F32),
    "h2_loc": ([4096, DM], BF16), "h2_all": ([S, DM], BF16), "aff_loc": ([NE, 4096], F32), "aff_all": ([4 * NE, 4096], F32),
    "aff_my": ([NE, 4096], F32), "sel": ([4 * CAP, 3], F32), "ydense": ([S, DM], F32), "ydense_bf": ([S, DM], BF16), "ysum_all": ([S, DM], BF16),
    "edges_loc": ([2, DM], F32), "edges_all": ([8, DM], F32),
}


def moe_allreduce(kb, io, key):
    for c in range(8):
        tc = T(io["ydense_bf"].t[c * 2048:(c + 1) * 2048, :], "ybf%d" % c)
        dc = T(io["ysum_all"].t[c * 2048:(c + 1) * 2048, :], "ysum%d" % c)
        for a in range(2):
            rs = slice((2 * c + a) * 1024, (2 * c + a + 1) * 1024)
            kb.dma("gpsimd", tc, io["ydense_bf"].t[rs, :], io["ydense"], io["ydense"].t[rs, :], "ycast", part=True)
        collective(kb, "AllReduce", ALU.add, tc, dc, key, 1)


def misc_phase(kb, fn):
    with contextlib.ExitStack() as ph:
        kb.ph = ph
        fn()
        kb.P.barrier()
        st = kb.P.emit_phase()
        kb.ph = None
    return st


def build_program(consts, debug=False, upto=99):
    nc = bass.Bass("TRN2", target_bir_lowering=False)
    with contextlib.ExitStack() as st:
        kb = KB(nc, st)
        io = {}
        for n, (shape, dt) in IN_SPECS.items():
            if upto < 3 and n[:2] in ("wg", "wu", "wd"):
                continue
            io[n] = kb.dram(n, shape, dt, "ExternalInput")
        for n in CONST_NAMES:
            io[n] = kb.dram(n, list(consts[n].shape), CONST_DT.get(n, F32), "ExternalInput")
        for n, (shape, dt) in SCRATCH.items():
            io[n] = kb.dram(n, shape, dt)
        io["out"] = kb.dram("out", [4096, DM], F32, "ExternalOutput")
        dbg = {}
        if debug:
            for n, shape in (("dbg_x1", [4096, DM]), ("dbg_x2", [4096, DM]), ("dbg_x3", [4096, DM]), ("dbg_aff0", [NE, 4096]),
                             ("dbg_aff1", [NE, 4096]), ("dbg_sel", [4 * CAP, 3])):
                dbg[n] = kb.dram(n, shape, F32, "ExternalOutput")


        def cp(dst, src, key):
            kb.dma("sync", dst, dst.t, src, src.t, key)

        import os
        if not os.environ.get("K_SKIP1"):
            phase1_attn(kb, io, 0)
            phase1_attn(kb, io, 1)
            phase1_ssd_in(kb, io)
            phase1_ssd_scan(kb, io, 0)
            phase1_ssd_scan(kb, io, 1)

        def ph_a():
            collective(kb, "AllGather", ALU.bypass, io["mixS_loc"], io["mixS_all"], "cc0", 8)
            zero_ydense(kb, io)

        misc_phase(kb, ph_a)
        if upto >= 2:
            phase2(kb, io, int(os.environ.get("K_NT", 32)))

            def ph_b(L):
                def f():
                    if L == 0:
                        kb.dma("sync", io["edges_loc"], io["edges_loc"].t[0:1, :], io["x1_loc"], io["x1_loc"].t[0:1, :], "edg", part=True)
                        kb.dma("sync", io["edges_loc"], io["edges_loc"].t[1:2, :], io["x1_loc"], io["x1_loc"].t[4095:4096, :], "edg", part=True)
                        collective(kb, "AllGather", ALU.bypass, io["edges_loc"], io["edges_all"], "cc1")
                    collective(kb, "AllGather", ALU.bypass, io["aff_loc"], io["aff_all"], "cc3")
                    if debug:
                        cp(dbg["dbg_x1" if L == 0 else "dbg_x3"], io["x1_loc"], "dbgx")
                        cp(dbg["dbg_aff%d" % L], io["aff_loc"], "dbga")
                return f
            misc_phase(kb, ph_b(0))
        if upto >= 3:
            moe_phase(kb, io, 0)

            def ph_c():
                moe_allreduce(kb, io, "cc4")
                if debug:
                    cp(dbg["dbg_sel"], io["sel"], "dbgs")
            misc_phase(kb, ph_c)
        if upto >= 4:
            phase4(kb, io)

            if debug:
                misc_phase(kb, lambda: cp(dbg["dbg_x2"], io["x2_loc"], "dbgx2"))
            misc_phase(kb, ph_b(1))
        if upto >= 5:
            moe_phase(kb, io, 1)
            misc_phase(kb, lambda: moe_allreduce(kb, io, "cc5"))
            final_phase(kb, io)
        print("semaphores:", len(kb.P.dsems) + 5)
    return nc


def core_inputs(inp, c, consts):
    b, r = c // 4, c % 4
    hg = r
    g = hg // 2
    d = {}
    x = inp["x"]
    d["xT"] = np.ascontiguousarray(x[b].T)
    d["x_tok"] = np.ascontiguousarray(x[b, r * 4096:(r + 1) * 4096])
    w = inp["w_in_even"][0]
    hs = slice(hg * 256, (hg + 1) * 256)
    cols = [w[:, 0:1024][:, hs], w[:, 1024:2048][:, hs], w[:, 2048:3072][:, hs], w[:, 4096:5120][:, hs],
            w[:, 5120 + g * 128:5120 + (g + 1) * 128], w[:, 5376 + g * 128:5376 + (g + 1) * 128], w[:, 3072:4096][:, hs],
            w[:, 5632 + hg * 4:5632 + hg * 4 + 4], w[:, 5648 + hg * 4:5648 + hg * 4 + 4]]
    d["w1"] = np.ascontiguousarray(np.concatenate(cols, 1))
    d["g_attn"] = np.ascontiguousarray(inp["attn_norm"][0].reshape(8, 128).T)
    d["gqk"] = np.ascontiguousarray(np.stack([np.tile(inp["q_norm"][0], 2), np.tile(inp["k_norm"][0], 2)], 1))
    cw_full = inp["ssd_conv_w"][0]; cb = inp["ssd_conv_b"][0]
    chans = np.concatenate([np.arange(hg * 256, hg * 256 + 256), 1024 + g * 128 + np.arange(128), 1280 + g * 128 + np.arange(128)])
    cw = np.concatenate([cw_full[:, chans], cb[None, chans]], 0)
    d["convw"] = np.ascontiguousarray(cw.reshape(6, 4, 128).transpose(2, 1, 0)).astype(np.float32)
    h4 = slice(hg * 4, hg * 4 + 4)
    d["dtc"] = np.stack([np.concatenate([inp["ssd_dt_bias_fwd"][0][h4], inp["ssd_dt_bias_bwd"][0][h4]]),
                         np.concatenate([inp["ssd_a_log_fwd"][0][h4], inp["ssd_a_log_bwd"][0][h4]])], 1).astype(np.float32)
    d["Dbc"] = np.ascontiguousarray(np.tile(np.repeat(inp["ssd_d"][0][h4], 64)[None, :], (128, 1))).astype(np.float32)
    wo = inp["w_out_even"][0]
    perm = np.concatenate([np.concatenate([np.arange(q * 256, (q + 1) * 256), 1024 + np.arange(q * 256, (q + 1) * 256)]) for q in range(4)])
    d["wo"] = np.ascontiguousarray(wo[perm])
    gfull = np.concatenate([np.ones(1024, np.float32), inp["ssd_out_norm"][0]])[perm]
    d["g_wo"] = np.ascontiguousarray(gfull.reshape(16, 128).T).astype(np.float32)
    mi = np.zeros((128, 8, 16), np.int32)
    for wdx in range(8):
        for ck in range(16):
            L = (8 * r + wdx) * 256 + (ck % 2) * 128 + np.arange(128)
            mi[:, wdx, ck] = (L // 1024) * 4096 + (ck // 4) * 1024 + (L % 1024)
    d["mixidx"] = mi
    for L in range(2):
        d["wr%d" % L] = np.ascontiguousarray(inp["router_w"][L])
        d["g_ffn_col%d" % L] = np.ascontiguousarray(inp["ffn_norm"][L].reshape(8, 128).T)
        d["g_ffn_bc%d" % L] = np.ascontiguousarray(np.tile(inp["ffn_norm"][L][None, :], (128, 1)))
        es = slice(4 * r, 4 * r + 4)
        d["wg%d" % L] = np.ascontiguousarray(inp["expert_w_gate"][L, es])
        d["wu%d" % L] = np.ascontiguousarray(inp["expert_w_up"][L, es])
        d["wd%d" % L] = np.ascontiguousarray(inp["expert_w_down"][L, es])
    ai = np.zeros((128, 1), np.int32)
    for e in range(4):
        for q in range(4):
            ai[e * 4 + q, 0] = q * 16 + 4 * r + e
    d["affidx"] = ai
    mr = np.zeros((128, 33), np.int32)
    for i in range(32):
        mr[:, i] = r * 4096 + i * 128 + np.arange(128)
    mr[0, 32] = max(r * 4096 - 1, 0)
    mr[1, 32] = min(r * 4096 + 4096, S - 1)
    d["myrows"] = mr
    hi = np.zeros((128, 1), np.int32); hm = np.zeros((128, 1), np.float32)
    if r > 0:
        hi[0, 0] = 2 * (r - 1) + 1; hm[0, 0] = 1.0
    if r < 3:
        hi[1, 0] = 2 * (r + 1); hm[1, 0] = 1.0
    d["haloidx"] = hi; d["halomsk"] = hm
    d["g_conv_bc"] = np.ascontiguousarray(np.tile(inp["conv_norm"][0][None, :], (128, 1)))
    d["w2"] = np.ascontiguousarray(inp["conv_w_in"][0])
    d["cw3"] = np.ascontiguousarray(inp["conv_w"][0].reshape(3, 8, 128).transpose(2, 1, 0))
    d["w3"] = np.ascontiguousarray(inp["conv_w_out"][0])
    for n in CONST_NAMES:
        d[n] = consts[n]
    return d


def kernel(**inputs):
    from concourse.bass_utils import run_bass_kernel_spmd
    inp = {k: np.asarray(v) for k, v in inputs.items()}
    consts = host_constants()
    nc = build_program(consts)
    in_maps = [core_inputs(inp, c, consts) for c in range(NCORE)]
    res = run_bass_kernel_spmd(nc, in_maps, core_ids=list(range(NCORE)))
    out = np.zeros((2, S, DM), np.float32)
    for c in range(NCORE):
        b, r = c // 4, c % 4
        out[b, r * 4096:(r + 1) * 4096] = res.results[c]["out"]
    return out
```
